# Optimizing a Trainium2 kernel written in Bass

```python
import math
import jax, jax.numpy as jnp
from jax import lax
import numpy as np


D_MODEL = 1024
BATCH = 32
SEQ = 2048
DEPTH = 1

NORM_EPS = 1e-6
HG_HEADS = 4
HG_DK = 128
HG_DV = 128
HG_QK = HG_HEADS * HG_DK
HG_WIDTH = HG_HEADS * HG_DV
HG_CHUNK = 32
NSA_HEADS = 8
NSA_KV_HEADS = 2
NSA_GROUP = NSA_HEADS // NSA_KV_HEADS
NSA_HD = 64
NSA_WIDTH = NSA_HEADS * NSA_HD
NSA_KV_WIDTH = NSA_KV_HEADS * NSA_HD
NSA_BRANCHES = 3
CMP_BLOCK = 32
CMP_STRIDE = 16
CMP_HIDDEN = 4 * NSA_HD
SLC_BLOCK = 64
SLC_TOPK = 16
WINDOW = 512
WIN_Q_BLOCK = 128
SLC_Q_BLOCK = 16
ROT_DIM = NSA_HD // 4
ROPE_THETA = 500000.0
MIX_WIDTH = HG_WIDTH + NSA_WIDTH
IN_SEGMENTS = (HG_QK, HG_QK, HG_WIDTH, HG_WIDTH, NSA_WIDTH) + (NSA_KV_WIDTH,) * 6 + (NSA_HEADS * NSA_BRANCHES,)
IN_WIDTH = sum(IN_SEGMENTS)
PEER_HEADS = 8
PEER_NKEYS = 128
PEER_EXPERTS = PEER_NKEYS * PEER_NKEYS
PEER_TOPK = 16
PEER_DK = 256
PEER_CHUNK = 128

kernel_name = 'hybrid_hgrn2_nsa_peer'


def rms_norm(x, w):
    x = x.astype(jnp.float32)
    return x * lax.rsqrt(jnp.mean(x * x, axis=-1, keepdims=True) + NORM_EPS) * w.astype(jnp.float32)


def masked_softmax(s, mask):
    s = jnp.where(mask, s.astype(jnp.float32), -jnp.inf)
    m = jnp.max(s, axis=-1, keepdims=True)
    m = jnp.where(jnp.isfinite(m), m, 0.0)
    e = jnp.where(mask, jnp.exp(s - m), 0.0)
    return e / jnp.maximum(jnp.sum(e, axis=-1, keepdims=True), jnp.finfo(jnp.float32).tiny)


def rope_tables(pos):
    inv = ROPE_THETA ** (-jnp.arange(0, ROT_DIM, 2, dtype=jnp.float32) / ROT_DIM)
    ang = pos.astype(jnp.float32)[..., None] * inv
    return jnp.cos(ang), jnp.sin(ang)


def partial_rope(x, cos, sin):
    half = ROT_DIM // 2
    x1, x2, rest = x[..., :half], x[..., half:ROT_DIM], x[..., ROT_DIM:]
    return jnp.concatenate([x1 * cos - x2 * sin, x2 * cos + x1 * sin, rest], axis=-1)


def hgrn2_mixer(q, f_raw, i_in, g, lb, out_norm_w):
    B, S, _ = q.shape
    H, C = HG_HEADS, HG_CHUNK
    n = S // C
    f = lb + (1.0 - lb) * jax.nn.sigmoid(f_raw)
    log_f = jnp.log(f)
    k = 1.0 - f

    def chunks(t, d):
        return t.reshape(B, n, C, H, d).transpose(1, 0, 3, 2, 4)

    qc, kc, vc = chunks(q, HG_DK), chunks(k, HG_DK), chunks(i_in, HG_DV)
    bc = jnp.cumsum(chunks(log_f, HG_DK), axis=3)
    causal = jnp.tril(jnp.ones((C, C), dtype=bool))[:, :, None]

    def step(state, inp):
        q_, k_, v_, b_ = inp
        o_inter = jnp.einsum('bhtk,bhkv->bhtv', q_ * jnp.exp(b_), state)
        decay = jnp.exp(jnp.where(causal, b_[:, :, :, None, :] - b_[:, :, None, :, :], -jnp.inf))
        scores = jnp.einsum('bhtk,bhsk,bhtsk->bhts', q_, k_, decay)
        o_intra = jnp.einsum('bhts,bhsv->bhtv', scores, v_)
        b_end = b_[:, :, -1:, :]
        state = jnp.exp(b_end[:, :, 0, :, None]) * state + jnp.einsum('bhsk,bhsv->bhkv', k_ * jnp.exp(b_end - b_), v_)
        return state, o_inter + o_intra

    state0 = jnp.zeros((B, H, HG_DK, HG_DV), jnp.float32)
    _, o = lax.scan(step, state0, (qc, kc, vc, bc))
    o = o.transpose(1, 0, 3, 2, 4).reshape(B, S, H, HG_DV)
    gate = jax.nn.silu(g).reshape(B, S, H, HG_DV)
    return (rms_norm(o, out_norm_w) * gate).reshape(B, S, HG_WIDTH)


def slc_importance_map(n_c, n_s):
    c0 = np.arange(n_c)[:, None] * CMP_STRIDE
    j0 = np.arange(n_s)[None, :] * SLC_BLOCK
    ov = np.clip(np.minimum(c0 + CMP_BLOCK, j0 + SLC_BLOCK) - np.maximum(c0, j0), 0, None)
    return jnp.asarray(ov / CMP_BLOCK, dtype=jnp.float32)


def nsa_mixer(q, kc, vc, ks, vs, kw, vw, gate_raw, positions, q_norm_w, k_norm_w,
              pe_k, pe_v, w1_k, w2_k, w1_v, w2_v, out_norm_w):
    B, S, _ = q.shape
    G, R, Hd = NSA_KV_HEADS, NSA_GROUP, NSA_HD
    cos, sin = rope_tables(positions)
    q = rms_norm(q.reshape(B, S, G, R, Hd).transpose(0, 2, 3, 1, 4), q_norm_w)
    q = partial_rope(q, cos[:, None, None], sin[:, None, None]) * (Hd ** -0.5)

    def kv_heads(t):
        return t.reshape(B, S, G, Hd).transpose(0, 2, 1, 3)

    def token_keys(t, w):
        return partial_rope(rms_norm(kv_heads(t), w), cos[:, None], sin[:, None])

    t_idx = jnp.arange(S)

    n_c = (S - CMP_BLOCK) // CMP_STRIDE + 1
    ends = np.arange(n_c) * CMP_STRIDE + CMP_BLOCK - 1
    blk_idx = ends[:, None] - (CMP_BLOCK - 1) + np.arange(CMP_BLOCK)[None, :]

    def compress(t, pe, w1, w2):
        blocks = kv_heads(t)[:, :, blk_idx] + pe
        return jax.nn.gelu(blocks.reshape(B, G, n_c, CMP_BLOCK * Hd) @ w1) @ w2

    cos_c, sin_c = rope_tables(positions[:, ends])
    k_cmp = partial_rope(rms_norm(compress(kc, pe_k, w1_k, w2_k), k_norm_w[0]), cos_c[:, None], sin_c[:, None])
    v_cmp = compress(vc, pe_v, w1_v, w2_v)
    mask_c = jnp.asarray(ends)[None, :] <= t_idx[:, None]
    p_cmp = masked_softmax(jnp.einsum('bgrtd,bgcd->bgrtc', q, k_cmp), mask_c)
    o_cmp = jnp.einsum('bgrtc,bgcd->bgrtd', p_cmp, v_cmp)

    n_s = S // SLC_BLOCK
    n_sel = min(SLC_TOPK, n_s)
    imp = jnp.einsum('bgrtc,cj->bgtj', p_cmp, slc_importance_map(n_c, n_s))
    blk = jnp.arange(n_s)[None, :]
    cur = (t_idx // SLC_BLOCK)[:, None]
    forced = (blk == 0) | (blk == cur) | (blk == cur - 1)
    score = jnp.where(forced, jnp.inf, jnp.where(blk <= cur, imp, -jnp.inf))
    _, sel = lax.top_k(score, n_sel)
    k_blocks = token_keys(ks, k_norm_w[1]).reshape(B, G, n_s, SLC_BLOCK, Hd)
    v_blocks = kv_heads(vs).reshape(B, G, n_s, SLC_BLOCK, Hd)
    b_ix = jnp.arange(B)[:, None, None, None]
    g_ix = jnp.arange(G)[None, :, None, None]

    def slc_block(args):
        qb, ib, t0 = args
        k_sel = k_blocks[b_ix, g_ix, ib]
        v_sel = v_blocks[b_ix, g_ix, ib]
        s = jnp.einsum('bgrqd,bgqnld->bgrqnl', qb, k_sel).reshape(B, G, R, SLC_Q_BLOCK, n_sel * SLC_BLOCK)
        tok = (ib[..., None] * SLC_BLOCK + jnp.arange(SLC_BLOCK)).reshape(B, G, SLC_Q_BLOCK, n_sel * SLC_BLOCK)
        mask = (tok <= (t0 + jnp.arange(SLC_Q_BLOCK))[:, None])[:, :, None]
        p = masked_softmax(s, mask).reshape(B, G, R, SLC_Q_BLOCK, n_sel, SLC_BLOCK)
        return jnp.einsum('bgrqnl,bgqnld->bgrqd', p, v_sel)

    n_qs = S // SLC_Q_BLOCK
    o_slc = lax.map(slc_block, (jnp.moveaxis(q.reshape(B, G, R, n_qs, SLC_Q_BLOCK, Hd), 3, 0),
                                jnp.moveaxis(sel.reshape(B, G, n_qs, SLC_Q_BLOCK, n_sel), 2, 0),
                                jnp.arange(n_qs) * SLC_Q_BLOCK))
    o_slc = jnp.moveaxis(o_slc, 0, 3).reshape(B, G, R, S, Hd)

    pad = ((0, 0), (0, 0), (WINDOW, 0), (0, 0))
    k_win = jnp.pad(token_keys(kw, k_norm_w[2]), pad)
    v_win = jnp.pad(kv_heads(vw), pad)
    span = WINDOW + WIN_Q_BLOCK

    def win_block(args):
        qb, t0 = args
        kb = lax.dynamic_slice_in_dim(k_win, t0, span, axis=2)
        vb = lax.dynamic_slice_in_dim(v_win, t0, span, axis=2)
        tq = (t0 + jnp.arange(WIN_Q_BLOCK))[:, None]
        sk = (t0 - WINDOW + jnp.arange(span))[None, :]
        mask = (sk >= 0) & (sk <= tq) & (tq - sk < WINDOW)
        p = masked_softmax(jnp.einsum('bgrqd,bgkd->bgrqk', qb, kb), mask)
        return jnp.einsum('bgrqk,bgkd->bgrqd', p, vb)

    n_qw = S // WIN_Q_BLOCK
    o_win = lax.map(win_block, (jnp.moveaxis(q.reshape(B, G, R, n_qw, WIN_Q_BLOCK, Hd), 3, 0),
                                jnp.arange(n_qw) * WIN_Q_BLOCK))
    o_win = jnp.moveaxis(o_win, 0, 3).reshape(B, G, R, S, Hd)

    gates = jax.nn.sigmoid(gate_raw).reshape(B, S, G, R, NSA_BRANCHES).transpose(0, 2, 3, 1, 4)[..., None]
    o = gates[..., 0, :] * o_cmp + gates[..., 1, :] * o_slc + gates[..., 2, :] * o_win
    o = rms_norm(o, out_norm_w)
    return o.transpose(0, 3, 1, 2, 4).reshape(B, S, NSA_WIDTH)


def peer_ffn(h, w_q, sub_keys, u_tab, v_tab):
    B, S, D = h.shape
    half = PEER_DK // 2

    def chunk(xc):
        q = (xc @ w_q).reshape(PEER_CHUNK, PEER_HEADS, 2, half)
        s1 = jnp.einsum('thd,nd->thn', q[:, :, 0], sub_keys[0])
        s2 = jnp.einsum('thd,nd->thn', q[:, :, 1], sub_keys[1])
        v1, i1 = lax.top_k(s1, PEER_TOPK)
        v2, i2 = lax.top_k(s2, PEER_TOPK)
        cand_s = (v1[..., :, None] + v2[..., None, :]).reshape(PEER_CHUNK, PEER_HEADS, PEER_TOPK * PEER_TOPK)
        cand_i = (i1[..., :, None] * PEER_NKEYS + i2[..., None, :]).reshape(PEER_CHUNK, PEER_HEADS, PEER_TOPK * PEER_TOPK)
        top_s, pos = lax.top_k(cand_s, PEER_TOPK)
        e_idx = jnp.take_along_axis(cand_i, pos, axis=-1)
        gate = jax.nn.softmax(top_s.astype(jnp.float32), axis=-1)
        act = jax.nn.gelu(jnp.einsum('tpkd,td->tpk', u_tab[e_idx], xc), approximate=False) * gate
        return jnp.einsum('tpk,tpkd->td', act, v_tab[e_idx])

    return lax.map(chunk, h.reshape(-1, PEER_CHUNK, D)).reshape(B, S, D)


def setup_inputs(seed: int = 0) -> dict:
    key = jax.random.key(seed)
    ks = jax.random.split(key, 24)
    nrm = lambda k, shape, scale: jax.random.normal(k, shape, jnp.float32) * scale
    gain = lambda k, shape: 1.0 + 0.05 * jax.random.normal(k, shape, jnp.float32)
    x = jax.random.normal(ks[0], (BATCH, SEQ, D_MODEL), jnp.float32)
    positions = jnp.arange(SEQ, dtype=jnp.int32)[None, :] + jax.random.randint(ks[1], (BATCH, 1), 0, 4096, dtype=jnp.int32)
    return {
        'x': x,
        'positions': positions,
        'norm1_w': gain(ks[2], (DEPTH, D_MODEL)),
        'w_in': nrm(ks[3], (DEPTH, D_MODEL, IN_WIDTH), D_MODEL ** -0.5),
        'hg_lb_logits': nrm(ks[4], (DEPTH + 1, HG_QK), 1.0),
        'hg_out_norm_w': gain(ks[5], (DEPTH, HG_DV)),
        'nsa_q_norm_w': gain(ks[6], (DEPTH, NSA_HD)),
        'nsa_k_norm_w': gain(ks[7], (DEPTH, NSA_BRANCHES, NSA_HD)),
        'cmp_pe_k': nrm(ks[8], (DEPTH, CMP_BLOCK, NSA_HD), 0.1),
        'cmp_pe_v': nrm(ks[9], (DEPTH, CMP_BLOCK, NSA_HD), 0.1),
        'cmp_w1_k': nrm(ks[10], (DEPTH, CMP_BLOCK * NSA_HD, CMP_HIDDEN), (CMP_BLOCK * NSA_HD) ** -0.5),
        'cmp_w2_k': nrm(ks[11], (DEPTH, CMP_HIDDEN, NSA_HD), CMP_HIDDEN ** -0.5),
        'cmp_w1_v': nrm(ks[12], (DEPTH, CMP_BLOCK * NSA_HD, CMP_HIDDEN), (CMP_BLOCK * NSA_HD) ** -0.5),
        'cmp_w2_v': nrm(ks[13], (DEPTH, CMP_HIDDEN, NSA_HD), CMP_HIDDEN ** -0.5),
        'nsa_out_norm_w': gain(ks[14], (DEPTH, NSA_HD)),
        'w_out': nrm(ks[15], (DEPTH, MIX_WIDTH, D_MODEL), MIX_WIDTH ** -0.5),
        'norm2_w': gain(ks[16], (DEPTH, D_MODEL)),
        'peer_w_q': nrm(ks[17], (DEPTH, D_MODEL, PEER_HEADS * PEER_DK), D_MODEL ** -0.5),
        'peer_sub_keys': nrm(ks[18], (DEPTH, 2, PEER_NKEYS, PEER_DK // 2), (PEER_DK // 2) ** -0.5),
        'peer_u': nrm(ks[19], (DEPTH, PEER_EXPERTS, D_MODEL), D_MODEL ** -0.5),
        'peer_v': nrm(ks[20], (DEPTH, PEER_EXPERTS, D_MODEL), PEER_HEADS ** -0.5),
    }


def reference(x, positions, norm1_w, w_in, hg_lb_logits, hg_out_norm_w, nsa_q_norm_w, nsa_k_norm_w,
              cmp_pe_k, cmp_pe_v, cmp_w1_k, cmp_w2_k, cmp_w1_v, cmp_w2_v, nsa_out_norm_w, w_out,
              norm2_w, peer_w_q, peer_sub_keys, peer_u, peer_v):
    lower_bounds = jnp.cumsum(jax.nn.softmax(hg_lb_logits.astype(jnp.float32), axis=0), axis=0)
    split_at = [int(v) for v in np.cumsum(IN_SEGMENTS)[:-1]]
    h = x.astype(jnp.float32)
    for l in range(DEPTH):
        a = rms_norm(h, norm1_w[l])
        (hg_q, hg_f, hg_i, hg_g, n_q, n_kc, n_vc, n_ks, n_vs, n_kw, n_vw, n_gate) = jnp.split(a @ w_in[l], split_at, axis=-1)
        y_hg = hgrn2_mixer(hg_q, hg_f, hg_i, hg_g, lower_bounds[l], hg_out_norm_w[l])
        y_nsa = nsa_mixer(n_q, n_kc, n_vc, n_ks, n_vs, n_kw, n_vw, n_gate, positions,
                          nsa_q_norm_w[l], nsa_k_norm_w[l], cmp_pe_k[l], cmp_pe_v[l],
                          cmp_w1_k[l], cmp_w2_k[l], cmp_w1_v[l], cmp_w2_v[l], nsa_out_norm_w[l])
        h = h + jnp.concatenate([y_hg, y_nsa], axis=-1) @ w_out[l]
        h = h + peer_ffn(rms_norm(h, norm2_w[l]), peer_w_q[l], peer_sub_keys[l], peer_u[l], peer_v[l])
    return h.astype(x.dtype)
```

```python
import contextlib
import numpy as np
import ml_dtypes
import concourse.bass as bass
import concourse.mybir as mybir
from concourse.bass_utils import run_bass_kernel_spmd

F32 = mybir.dt.float32
BF16 = mybir.dt.bfloat16
I32 = mybir.dt.int32
U32 = mybir.dt.uint32
AF = mybir.ActivationFunctionType
ALU = mybir.AluOpType
AX = mybir.AxisListType

ENGS = ("pe", "dve", "act", "pool", "sp")
NEG = -30000.0


class Sched:
    def __init__(self, nc, stack, ndma=12):
        self.nc = nc
        self.q = {e: [] for e in ENGS}
        self.cnt = {e: 0 for e in ENGS}
        self.esem = {e: stack.enter_context(nc.semaphore("es_" + e)) for e in ENGS}
        self.ndma = ndma
        self.dsem = {e: [stack.enter_context(nc.semaphore("ds_%s_%d" % (e, i))) for i in range(ndma)]
                     for e in ("sp", "pool", "act")}
        self.dcnt = {e: 0 for e in ("sp", "pool", "act")}
        self.seen = {e: {} for e in ENGS}
        self.st = {}
        self.ninst = 0

    def _sem(self, key):
        if key[0] == "dma":
            return self.dsem[key[1]][key[2]]
        return self.esem[key[0]]

    def _wait(self, eng, ev):
        key, val = ev
        if self.seen[eng].get(key, 0) >= val:
            return
        self.seen[eng][key] = val
        sem = self._sem(key)
        self.q[eng].append(lambda e, sem=sem, val=val: e.wait_ge(sem, val))
        self.ninst += 1

    def _deps(self, eng, reads, writes):
        deps = []
        for k in reads:
            s = self.st.get(k)
            if s and s[0] is not None:
                deps.append(s[0])
            if s and k[1:3] == "p_":
                deps.extend(ev for ek, ev in s[1].items() if ek != (eng,))
        for k in writes:
            s = self.st.get(k)
            if s:
                if s[0] is not None:
                    deps.append(s[0])
                deps.extend(s[1].values())
        for ev in deps:
            if eng == "pe" and ev[0] == ("pe",):
                continue
            self._wait(eng, ev)

    def _commit(self, ev, evkey, reads, writes):
        for k in reads:
            s = self.st.setdefault(k, [None, {}])
            s[1][evkey] = ev
        for k in writes:
            self.st[k] = [ev, {}]

    def op(self, eng, fn, reads=(), writes=()):
        self._deps(eng, reads, writes)
        self.cnt[eng] += 1
        sem = self.esem[eng]
        self.q[eng].append(lambda e, fn=fn, sem=sem: fn(e).then_inc(sem, 1))
        self.ninst += 1
        ev = ((eng,), self.cnt[eng])
        self._commit(ev, (eng,), reads, writes)

    def dma(self, eng, fn, reads=(), writes=()):
        self._deps(eng, reads, writes)
        k = self.dcnt[eng]
        slot = k % self.ndma
        key = ("dma", eng, slot)
        if k >= self.ndma:
            self._wait(eng, (key, 16 * (k // self.ndma)))
        self.dcnt[eng] += 1
        val = 16 * (k // self.ndma + 1)
        sem = self.dsem[eng][slot]
        self.q[eng].append(lambda e, fn=fn, sem=sem: fn(e).then_inc(sem, 16))
        self.ninst += 1
        ev = (key, val)
        self._commit(ev, key, reads, writes)

    def finish(self):
        for eng in ("sp", "pool", "act"):
            k = self.dcnt[eng]
            for slot in range(min(k, self.ndma)):
                n = (k - 1 - slot) // self.ndma + 1
                self._wait(eng, (("dma", eng, slot), 16 * n))

    def replay(self, block):
        q = self.q

        @block.sync
        def _(e):
            for f in q["sp"]:
                f(e)

        @block.gpsimd
        def _(e):
            for f in q["pool"]:
                f(e)

        @block.tensor
        def _(e):
            for f in q["pe"]:
                f(e)

        @block.scalar
        def _(e):
            for f in q["act"]:
                f(e)

        @block.vector
        def _(e):
            for f in q["dve"]:
                f(e)


D = 1024
S = 2048
PEER_HEADS = 8
PEER_K = 16
EPS = 1e-6


def peer_consts():
    c = np.zeros((128, 64), np.float32)
    c[:, 0:16] = np.arange(16, dtype=np.float32)[None, :] * 16.0
    c[:, 16:32] = np.arange(16, dtype=np.float32)[None, :]
    return c


class Peer:
    def __init__(self, nc, sc, stack, norm2_w, w_q, sub_keys, uv_tab, ident_f, ident_b, pc, wsel_dram, NB=10):
        self.nc, self.sc = nc, sc
        self.uv_tab = uv_tab
        self.NB = NB
        T = lambda name, shape, dt: stack.enter_context(nc.sbuf_tensor(name, shape, dt))
        P = lambda name, shape, dt: stack.enter_context(nc.psum_tensor(name, shape, dt))
        self.ident_f, self.ident_b, self.pc = ident_f, ident_b, pc
        self.w2b = T("pr_w2b", [128, D], F32)
        self.wq = T("pr_wq", [128, 4, 2, 2048], BF16)
        self.skT = T("pr_skT", [128, 2, 128], BF16)
        self.skl = T("pr_skl", [128, 2, 128], F32)
        self.junk = T("pr_junk", [128, D], BF16)
        self.ss = T("pr_ss", [128, 1], F32)
        self.rstd = T("pr_rstd", [128, 1], F32)
        self.xn = T("pr_xn", [128, D], BF16)
        self.xnT = [T("pr_xnT%d" % i, [128, 4, 128, 2], BF16) for i in range(2)]
        self.qT = T("pr_qT", [128, 16, 128], BF16)
        self.Ssb = T("pr_S", [128, 16, 128], F32)
        self.Swk = T("pr_Swk", [128, 128], F32)
        self.v = T("pr_v", [128, 16, 16], F32)
        self.ix = T("pr_ix", [128, 16, 16], U32)
        self.ixf = T("pr_ixf", [128, 16, 16], F32)
        self.cand = T("pr_cand", [128, 8, 256], F32)
        self.cwk = T("pr_cwk", [128, 256], F32)
        self.tv = T("pr_tv", [128, 8, 16], F32)
        self.pos = T("pr_pos", [128, 8, 16], U32)
        self.posa = T("pr_posa", [128, 8, 16], U32)
        self.posb = T("pr_posb", [128, 8, 16], U32)
        self.epsc = T("pr_epsc", [128, 1], F32)
        self.pb = T("pr_pb", [128, 8, 16], F32)
        self.pa = T("pr_pa", [128, 8, 16], F32)
        self.eq = T("pr_eq", [128, 8, 16, 16], F32)
        self.e1 = T("pr_e1", [128, 8, 16], F32)
        self.e2 = T("pr_e2", [128, 8, 16], F32)
        self.gt = T("pr_gt", [128, 8, 16], F32)
        self.gs = T("pr_gs", [128, 8], F32)
        self.eTi = [T("pr_eTi%d" % i, [128, 128], I32) for i in range(2)]
        self.eTf = T("pr_eTf", [128, 128], F32)
        self.gT = [T("pr_gT%d" % i, [128, 128], F32) for i in range(2)]
        self.actb = T("pr_actb", [128, 128], BF16)
        self.oT = T("pr_oT", [128, 8, 128], F32)
        self.osb = T("pr_osb", [128, D], F32)
        self.G = [T("pr_G%d" % i, [128, 2 * D], BF16) for i in range(NB)]
        self.GT = [T("pr_GT%d" % i, [128, 4, 128, 2], BF16) for i in range(2)]
        self.hg = T("pr_hg", [128, 128], F32)
        self.wsel = T("pr_wsel", [128, 256], BF16)
        self.actD = [T("pr_actD%d" % i, [128, 128], BF16) for i in range(3)]
        self.ps_a = P("pp_a", [128, 4, 128], F32)
        self.ps_b = P("pp_b", [128, 4, 128], F32)
        self.ps_t = P("pp_t", [128, 4, 128], F32)
        self.ps_t_b = self.ps_t[:].rearrange("p a b -> p (a b)").bitcast(BF16).rearrange("p (a b) -> p a b", b=128)
        self.ps_g = [P("pp_g%d" % i, [128, 4, 128], F32) for i in range(2)]
        self.ps_hd = P("pp_hd", [128, 512], F32)
        self.ps_o = P("pp_o", [128, 8, 128], F32)
        self.tok = 0

        sc.op("dve", lambda e: e.memset(self.epsc[:], EPS), writes=["pr_epsc"])
        sc.dma("pool", lambda e: e.dma_start(out=self.wsel[:], in_=wsel_dram), writes=["pr_wsel"])
        sc.dma("sp", lambda e: e.dma_start(out=self.w2b[:], in_=norm2_w[0:1, :].partition_broadcast(128)),
               writes=["pr_w2b"])
        sc.dma("pool", lambda e: e.dma_start(out=self.wq[:], in_=w_q.rearrange("(c dp two) n -> dp c two n", dp=128, two=2)),
               writes=["pr_wq"])
        sc.dma("sp", lambda e: e.dma_start(out=self.skl[:], in_=sub_keys.rearrange("j n d -> n j d")),
               writes=["pr_skl"])
        for j in range(2):
            sc.op("pe", lambda e, j=j: e.transpose(out=self.ps_a[:, j, :], in_=self.skl[:, j, :], identity=ident_f[:]),
                  reads=["pr_skl", "ident_f"], writes=["pp_a"])
        sc.op("act", lambda e: e.copy(out=self.skT[:], in_=self.ps_a[:, 0:2, :]), reads=["pp_a"], writes=["pr_skT"])

    def pre_ops(self, i, h_t, hkey):
        sc = self.sc
        p = i % 2
        ops = []
        add = lambda *a, **k: ops.append(lambda: sc.op(*a, **k))
        xnT, eTi, gT = self.xnT[p], self.eTi[p], self.gT[p]
        kxnT, keTi, kgT = "pr_xnT%d" % p, "pr_eTi%d" % p, "pr_gT%d" % p
        add("act", lambda e: e.activation(out=self.junk[:], in_=h_t, func=AF.Square, accum_out=self.ss[:]),
            reads=[hkey], writes=["pr_junk", "pr_ss"])
        add("act", lambda e: e.activation(out=self.rstd[:], in_=self.ss[:], func=AF.Sqrt, scale=1.0 / D, bias=self.epsc[:]),
            reads=["pr_ss"], writes=["pr_rstd"])
        add("dve", lambda e: e.reciprocal(out=self.rstd[:], in_=self.rstd[:]), reads=["pr_rstd"], writes=["pr_rstd"])
        add("dve", lambda e: e.scalar_tensor_tensor(out=self.xn[:], in0=h_t, scalar=self.rstd[:, 0:1],
                                                    in1=self.w2b[:], op0=ALU.mult, op1=ALU.mult),
            reads=[hkey, "pr_rstd", "pr_w2b"], writes=["pr_xn"])
        xnf = self.xn[:].bitcast(F32)
        for c in range(4):
            add("pe", lambda e, c=c: e.transpose(out=self.ps_t[:, c, :], in_=xnf[:, c * 128:(c + 1) * 128],
                                                 identity=self.ident_f[:]),
                reads=["pr_xn", "ident_f"], writes=["pp_t"])
        add("act", lambda e: e.copy(out=xnT[:].rearrange("p c t two -> p (c t two)"),
                                    in_=self.ps_t_b.rearrange("p a b -> p (a b)")), reads=["pp_t"], writes=[kxnT])
        for grp in range(4):
            ps = self.ps_a if grp % 2 == 0 else self.ps_b
            pk = "pp_a" if grp % 2 == 0 else "pp_b"
            for c4 in range(4):
                cq = grp * 4 + c4
                for kc in range(8):
                    add("pe", lambda e, ps=ps, c4=c4, cq=cq, kc=kc: e.matmul(
                        ps[:, c4, :], lhsT=self.wq[:, kc // 2, kc % 2, cq * 128:(cq + 1) * 128], rhs=xnT[:, kc // 2, :, kc % 2],
                        start=(kc == 0), stop=(kc == 7)), reads=["pr_wq", kxnT], writes=[pk])
            add("act", lambda e, ps=ps, grp=grp: e.copy(out=self.qT[:, grp * 4:(grp + 1) * 4, :], in_=ps[:]),
                reads=[pk], writes=["pr_qT%d" % grp])
        for grp in range(4):
            ps = self.ps_a if grp % 2 == 0 else self.ps_b
            pk = "pp_a" if grp % 2 == 0 else "pp_b"
            for c4 in range(4):
                cq = grp * 4 + c4
                add("pe", lambda e, ps=ps, c4=c4, cq=cq: e.matmul(
                    ps[:, c4, :], lhsT=self.qT[:, cq, :], rhs=self.skT[:, cq % 2, :], start=True, stop=True),
                    reads=["pr_qT%d" % grp, "pr_skT"], writes=[pk])
            add("act", lambda e, ps=ps, grp=grp: e.copy(out=self.Ssb[:, grp * 4:(grp + 1) * 4, :], in_=ps[:]),
                reads=[pk], writes=["pr_S%d" % grp])
        for cq in range(16):
            sk = "pr_S%d" % (cq // 4)
            add("dve", lambda e, cq=cq: e.max(out=self.v[:, cq, 0:8], in_=self.Ssb[:, cq, :]), reads=[sk], writes=["pr_v"])
            add("dve", lambda e, cq=cq: e.max_index(out=self.ix[:, cq, 0:8], in_max=self.v[:, cq, 0:8],
                                                    in_values=self.Ssb[:, cq, :]), reads=[sk, "pr_v"], writes=["pr_ix"])
            add("dve", lambda e, cq=cq: e.match_replace(out=self.Swk[:], in_to_replace=self.v[:, cq, 0:8],
                                                        in_values=self.Ssb[:, cq, :], imm_value=-1e30),
                reads=[sk, "pr_v"], writes=["pr_Swk"])
            add("dve", lambda e, cq=cq: e.max(out=self.v[:, cq, 8:16], in_=self.Swk[:]), reads=["pr_Swk"], writes=["pr_v"])
            add("dve", lambda e, cq=cq: e.max_index(out=self.ix[:, cq, 8:16], in_max=self.v[:, cq, 8:16],
                                                    in_values=self.Swk[:]), reads=["pr_Swk", "pr_v"], writes=["pr_ix"])
        add("dve", lambda e: e.tensor_copy(out=self.ixf[:], in_=self.ix[:]), reads=["pr_ix"], writes=["pr_ixf"])
        for h in range(8):
            add("dve", lambda e, h=h: e.tensor_tensor(
                out=self.cand[:, h, :].rearrange("p (a b) -> p a b", b=16),
                in0=self.v[:, 2 * h, :].unsqueeze(2).broadcast_to([128, 16, 16]),
                in1=self.v[:, 2 * h + 1, :].unsqueeze(1).broadcast_to([128, 16, 16]), op=ALU.add),
                reads=["pr_v"], writes=["pr_cand"])
        for h in range(8):
            add("dve", lambda e, h=h: e.max(out=self.tv[:, h, 0:8], in_=self.cand[:, h, :]), reads=["pr_cand"], writes=["pr_tv"])
            add("dve", lambda e, h=h: e.max_index(out=self.pos[:, h, 0:8], in_max=self.tv[:, h, 0:8],
                                                  in_values=self.cand[:, h, :]), reads=["pr_cand", "pr_tv"], writes=["pr_pos"])
            add("dve", lambda e, h=h: e.match_replace(out=self.cwk[:], in_to_replace=self.tv[:, h, 0:8],
                                                      in_values=self.cand[:, h, :], imm_value=-1e30),
                reads=["pr_cand", "pr_tv"], writes=["pr_cwk"])
            add("dve", lambda e, h=h: e.max(out=self.tv[:, h, 8:16], in_=self.cwk[:]), reads=["pr_cwk"], writes=["pr_tv"])
            add("dve", lambda e, h=h: e.max_index(out=self.pos[:, h, 8:16], in_max=self.tv[:, h, 8:16],
                                                  in_values=self.cwk[:]), reads=["pr_cwk", "pr_tv"], writes=["pr_pos"])
        add("dve", lambda e: e.tensor_scalar(out=self.posb[:], in0=self.pos[:], scalar1=15, scalar2=None,
                                             op0=ALU.bitwise_and), reads=["pr_pos"], writes=["pr_posb"])
        add("dve", lambda e: e.tensor_scalar(out=self.posa[:], in0=self.pos[:], scalar1=240, scalar2=None,
                                             op0=ALU.bitwise_and), reads=["pr_pos"], writes=["pr_posa"])
        add("dve", lambda e: e.tensor_copy(out=self.pb[:], in_=self.posb[:]), reads=["pr_posb"], writes=["pr_pb"])
        add("dve", lambda e: e.tensor_copy(out=self.pa[:], in_=self.posa[:]), reads=["pr_posa"], writes=["pr_pa"])
        eq3 = self.eq[:].rearrange("p h k a -> p (h k) a")
        for which, (src, c0, j, dst) in enumerate(((self.pa, 0, 0, self.e1), (self.pb, 16, 1, self.e2))):
            add("dve", lambda e, src=src, c0=c0: e.tensor_tensor(
                out=eq3, in0=src[:].rearrange("p h k -> p (h k)").unsqueeze(2).broadcast_to([128, 128, 16]),
                in1=self.pc[:, c0:c0 + 16].unsqueeze(1).broadcast_to([128, 128, 16]), op=ALU.is_equal),
                reads=["pr_pa", "pr_pb", "pc"], writes=["pr_eq"])
            for h in range(8):
                add("dve", lambda e, j=j, h=h: e.tensor_tensor(
                    out=self.eq[:, h, :, :], in0=self.eq[:, h, :, :],
                    in1=self.ixf[:, 2 * h + j, :].unsqueeze(1).broadcast_to([128, 16, 16]),
                    op=ALU.mult), reads=["pr_eq", "pr_ixf"], writes=["pr_eq"])
            add("dve", lambda e, dst=dst: e.tensor_reduce(out=dst[:].rearrange("p h k -> p (h k)"), in_=eq3,
                                                          axis=AX.X, op=ALU.add),
                reads=["pr_eq"], writes=["pr_e%d" % (which + 1)])
        add("dve", lambda e: e.scalar_tensor_tensor(out=self.e1[:], in0=self.e1[:], scalar=128.0, in1=self.e2[:],
                                                    op0=ALU.mult, op1=ALU.add),
            reads=["pr_e1", "pr_e2"], writes=["pr_e1"])
        add("dve", lambda e: e.tensor_tensor(out=self.gt[:], in0=self.tv[:],
                                             in1=self.tv[:, :, 0:1].broadcast_to([128, 8, 16]), op=ALU.subtract),
            reads=["pr_tv"], writes=["pr_gt"])
        add("act", lambda e: e.activation(out=self.gt[:], in_=self.gt[:], func=AF.Exp), reads=["pr_gt"], writes=["pr_gt"])
        add("dve", lambda e: e.tensor_reduce(out=self.gs[:], in_=self.gt[:], axis=AX.X, op=ALU.add),
            reads=["pr_gt"], writes=["pr_gs"])
        add("dve", lambda e: e.reciprocal(out=self.gs[:], in_=self.gs[:]), reads=["pr_gs"], writes=["pr_gs"])
        add("dve", lambda e: e.tensor_tensor(out=self.gt[:], in0=self.gt[:],
                                             in1=self.gs[:].unsqueeze(2).broadcast_to([128, 8, 16]), op=ALU.mult),
            reads=["pr_gt", "pr_gs"], writes=["pr_gt"])
        add("pe", lambda e: e.transpose(out=self.ps_a[:, 0, :], in_=self.e1[:].rearrange("p h k -> p (h k)"),
                                        identity=self.ident_f[:]), reads=["pr_e1", "ident_f"], writes=["pp_a"])
        add("pe", lambda e: e.transpose(out=self.ps_a[:, 1, :], in_=self.gt[:].rearrange("p h k -> p (h k)"),
                                        identity=self.ident_f[:]), reads=["pr_gt", "ident_f"], writes=["pp_a"])
        add("act", lambda e: e.copy(out=self.eTf[:], in_=self.ps_a[:, 0, :]), reads=["pp_a"], writes=["pr_eTf"])
        add("dve", lambda e: e.tensor_copy(out=eTi[:], in_=self.eTf[:]), reads=["pr_eTf"], writes=[keTi])
        add("act", lambda e: e.copy(out=gT[:], in_=self.ps_a[:, 1, :]), reads=["pp_a"], writes=[kgT])
        return ops

    def loop(self, i, h_t, hkey, out_ap, outkey, pending):
        sc = self.sc
        p = i % 2
        xnT, eTi, gT = self.xnT[p], self.eTi[p], self.gT[p]
        kxnT, keTi, kgT = "pr_xnT%d" % p, "pr_eTi%d" % p, "pr_gT%d" % p
        NB = self.NB
        per = (len(pending) + 119) // 120 if pending else 0
        bufs, gbs = {}, {}

        def e1(t):
            b = self.tok % NB
            g2 = self.tok % 2
            self.tok += 1
            bufs[t], gbs[t] = b, g2
            gk = "pr_G%d" % b
            sc.dma("pool", lambda e, b=b, t=t: e.indirect_dma_start(
                out=self.G[b][:], out_offset=None, in_=self.uv_tab,
                in_offset=bass.IndirectOffsetOnAxis(ap=eTi[:, t:t + 1], axis=0)), reads=[keTi], writes=[gk])
            Gf = self.G[b][:, 0:D].bitcast(F32)
            for c in range(4):
                sc.op("pe", lambda e, Gf=Gf, c=c, g2=g2: e.transpose(
                    out=self.ps_g[g2][:, c, :], in_=Gf[:, c * 128:(c + 1) * 128], identity=self.ident_f[:]),
                    reads=[gk, "ident_f"], writes=["pp_g%d" % g2])
            sc.op("act", lambda e, g2=g2: e.copy(
                out=self.GT[g2][:].rearrange("p c s two -> p (c s two)"),
                in_=self.ps_g[g2][:].rearrange("p a b -> p (a b)").bitcast(BF16)),
                reads=["pp_g%d" % g2], writes=["pr_GT%d" % g2])

        def e2(t):
            g2 = gbs[t]
            for kc in range(8):
                sc.op("pe", lambda e, kc=kc, g2=g2, t=t: e.matmul(
                    self.ps_hd[:, t:t + 1], lhsT=self.GT[g2][:, kc // 2, :, kc % 2], rhs=xnT[:, kc // 2, t:t + 1, kc % 2],
                    start=(kc == 0), stop=(kc == 7)), reads=["pr_GT%d" % g2, kxnT], writes=["pp_hd"])
            sc.op("act", lambda e, t=t: e.activation(out=self.hg[:, t:t + 1], in_=self.ps_hd[:, t:t + 1], func=AF.Gelu),
                  reads=["pp_hd"], writes=["pr_hg%d" % (t % 4)])
            sc.op("dve", lambda e, t=t: e.tensor_scalar(out=self.actD[t % 3][:], in0=self.wsel[:, 127 - t:255 - t],
                                                        scalar1=self.hg[:, t:t + 1], scalar2=gT[:, t:t + 1],
                                                        op0=ALU.mult, op1=ALU.mult),
                  reads=["pr_hg%d" % (t % 4), kgT, "pr_wsel"], writes=["pr_actD%d" % (t % 3)])

        def e3(t):
            b = bufs[t]
            po = self.ps_o[:].rearrange("p a b -> p (a b)")
            for hf in range(2):
                sc.op("pe", lambda e, b=b, t=t, hf=hf, po=po: e.matmul(
                    po[:, hf * 512:(hf + 1) * 512], lhsT=self.actD[t % 3][:], rhs=self.G[b][:, D + hf * 512:D + (hf + 1) * 512],
                    start=(t == 0), stop=(t == 127)), reads=["pr_G%d" % b, "pr_actD%d" % (t % 3)], writes=["pp_o"])

        e1(0)
        for t in range(-1, 129):
            if 0 <= t + 1 < 128 and t + 1 > 0:
                e1(t + 1)
            if 0 <= t < 128:
                e2(t)
            if 0 <= t - 1 < 128:
                e3(t - 1)
            for _ in range(per):
                if pending:
                    pending.pop(0)()
        while pending:
            pending.pop(0)()
        po = self.ps_o[:].rearrange("p a b -> p (a b)")
        for hf in range(2):
            sc.op("dve", lambda e, hf=hf, po=po: e.tensor_tensor(
                out=self.osb[:, hf * 512:(hf + 1) * 512], in0=po[:, hf * 512:(hf + 1) * 512],
                in1=h_t[:, hf * 512:(hf + 1) * 512], op=ALU.add), reads=["pp_o", hkey], writes=["pr_osb"])
        sc.dma("sp", lambda e: e.dma_start(out=out_ap, in_=self.osb[:]), reads=["pr_osb"], writes=[outkey])


FMCH = [0, 1, 2, 3, 4, 5, 6, 7, 16, 17, 18, 19, 20, 21, 22, 24]
INW = 3352
DELTAS = [-512, -384, -256, -128, 0, 128, 256, 384]


def host_consts():
    bf = ml_dtypes.bfloat16
    c = {}
    c["c_identf"] = np.eye(128, dtype=np.float32)
    c["c_pc"] = peer_consts()
    wsel = np.zeros((128, 256), np.float32)
    wsel[:, 127] = 1.0
    c["c_wsel"] = wsel
    s = np.arange(128)[:, None]
    t = np.arange(128)[None, :]
    same = (s // 32) == (t // 32)
    c["c_ublk"] = (same & (s <= t)).astype(np.float32)
    c["c_wrev"] = (same & (s > t)).astype(np.float32)
    c["c_rowm"] = (np.arange(128)[:, None] // 32 == np.arange(4)[None, :]).astype(np.float32)
    c["c_colm"] = np.broadcast_to((np.arange(128)[None, None, :] // 32 == np.arange(4)[None, :, None]), (128, 4, 128)).astype(np.float32).copy()
    inv = (500000.0 ** (-np.arange(0, 16, 2, dtype=np.float32) / 16)).astype(np.float32)
    misc = np.zeros((128, 8), np.float32)
    misc[0:16, 0] = np.concatenate([inv, inv])
    c["c_misc"] = misc
    rm = np.zeros((64, 64), np.float32)
    for d in range(8):
        rm[d + 8, d] = -1.0
        rm[d, d + 8] = 1.0
    c["c_rm"] = rm
    ss = np.arange(128)[:, None]
    tt = np.arange(512)[None, :]
    dm = np.zeros((128, 8, 512), np.float32)
    for i, dl in enumerate(DELTAS):
        ok = (dl + ss <= tt) & (tt - ss - dl < 512)
        dm[:, i, :] = np.where(ok, 0.0, NEG)
    c["c_dmask"] = dm
    cm = np.zeros((128, 4, 512), np.float32)
    cc = np.arange(128)[:, None]
    for qb in range(4):
        ok = (16 * cc + 31) <= (qb * 512 + tt)
        cm[:, qb, :] = np.where(ok, 0.0, NEG)
    c["c_cmask"] = cm
    em = np.zeros((32, 2048), np.float32)
    em[np.arange(2048) // 64, np.arange(2048)] = 1.0
    c["c_emat"] = em
    c0 = np.arange(127)[:, None] * 16
    j0 = np.arange(32)[None, :] * 64
    ov = np.clip(np.minimum(c0 + 32, j0 + 64) - np.maximum(c0, j0), 0, None) / 32.0
    va = np.zeros((128, 33), np.float32)
    va[:, 0] = 1.0
    va[:127, 1:] = ov
    c["c_vaug"] = va
    tok = (np.arange(16)[None, :, None] * 128 + np.arange(128)[:, None, None])
    cur = tok // 64
    j = np.arange(32)[None, None, :]
    forced = (j == 0) | (j == cur) | (j == cur - 1)
    valid = (j <= cur) & ~forced
    c["c_valid"] = valid.astype(np.float32)
    c["c_addc"] = np.where(forced, 1e30, np.where(valid, 0.0, -1e30)).astype(np.float32)
    return c


CONST_SHAPES = {"c_identf": [128, 128], "c_pc": [128, 64], "c_wsel": [128, 256], "c_ublk": [128, 128], "c_wrev": [128, 128],
                "c_misc": [128, 8], "c_rowm": [128, 4], "c_colm": [128, 4, 128], "c_rm": [64, 64], "c_dmask": [128, 8, 512], "c_cmask": [128, 4, 512],
                "c_emat": [32, 2048], "c_vaug": [128, 33], "c_valid": [128, 16, 32], "c_addc": [128, 16, 32]}


def barrier(sc):
    for e in ("sp", "pool", "act"):
        k = sc.dcnt[e]
        for slot in range(min(k, sc.ndma)):
            n = (k - 1 - slot) // sc.ndma + 1
            for eng in ENGS:
                sc._wait(eng, (("dma", e, slot), 16 * n))
    for eng in ENGS:
        for e2 in ("pe", "dve", "act", "pool"):
            if sc.cnt[e2] > 0 and not (eng == "pe" and e2 == "pe"):
                sc._wait(eng, ((e2,), sc.cnt[e2]))
    sc.st = {}


def rms_rstd(sc, src_ap, srckey, junk, ss, rstd, epsc, n, pfx):
    sc.op("act", lambda e: e.activation(out=junk, in_=src_ap, func=AF.Square, accum_out=ss),
          reads=[srckey], writes=[pfx + "junk", pfx + "ss"])
    sc.op("act", lambda e: e.activation(out=rstd, in_=ss, func=AF.Sqrt, scale=1.0 / n, bias=epsc),
          reads=[pfx + "ss"], writes=[pfx + "rstd"])
    sc.op("dve", lambda e: e.reciprocal(out=rstd, in_=rstd), reads=[pfx + "rstd"], writes=[pfx + "rstd"])


TM_RANGES = [(512, 1024), (1024, 1536), (1536, 2048), (2944, 3072), (3200, 3352)]


def stage_proj(nc, sc, NT, x, norm1_w, w_in, P_tm, P_fm, ident_b, epsc, uv=None):
    with contextlib.ExitStack() as st:
        T = lambda name, shape, dt: st.enter_context(nc.sbuf_tensor(name, shape, dt))
        P = lambda name, shape, dt: st.enter_context(nc.psum_tensor(name, shape, dt))
        win = T("a_win", [128, 8, INW], BF16)
        n1w = T("a_n1w", [128, 8], F32)
        xt = [T("a_xt%d" % i, [128, D], F32) for i in range(2)]
        junk = T("a_junk", [128, D], BF16)
        ss = T("a_ss", [128, 1], F32)
        rstd = T("a_rstd", [128, 1], F32)
        xn = T("a_xn", [128, D], BF16)
        xnT = T("a_xnT", [128, 8, 128], BF16)
        ptm = [T("a_ptm%d" % i, [128, INW], F32) for i in range(2)]
        pfm = [T("a_pfm%d" % i, [128, 16, 128], F32) for i in range(2)]
        ps_t = P("ap_t", [128, 8, 128], BF16)
        ps_m = [P("ap_m%d" % i, [128, 512], F32) for i in range(2)]
        ps_f = [P("ap_f%d" % i, [128, 4, 128], F32) for i in range(2)]
        for kc in range(8):
            sc.dma("pool", lambda e, kc=kc: e.dma_start(out=win[:, kc, :], in_=w_in[kc * 128:(kc + 1) * 128, :]),
                   writes=["a_win"])
        uv_ops = []
        if uv is not None:
            u_tab, v_tab, UV = uv
            tmp = [T("uv_tmp%d" % i, [128, 8, D], BF16) for i in range(3)]
            UVv = UV.rearrange("(p r) d -> p r d", p=128)
            k = 0
            for half, tab in enumerate((u_tab, v_tab)):
                tv = tab.rearrange("(p r) d -> p r d", p=128)
                for ci in range(16):
                    bb = k % 3
                    k += 1
                    uv_ops.append(lambda bb=bb, tv=tv, ci=ci: sc.dma(
                        "pool", lambda e: e.dma_start(out=tmp[bb][:], in_=tv[:, ci * 8:(ci + 1) * 8, :]), writes=["uv_tmp%d" % bb]))
                    uv_ops.append(lambda bb=bb, ci=ci, half=half: sc.dma(
                        "sp", lambda e: e.dma_start(out=UVv[:, ci * 8:(ci + 1) * 8, half * D:(half + 1) * D], in_=tmp[bb][:]),
                        reads=["uv_tmp%d" % bb], writes=["UV"]))
        sc.dma("sp", lambda e: e.dma_start(out=n1w[:], in_=norm1_w.rearrange("o (kc p) -> p (o kc)", p=128),
                                           allow_slow_non_contiguous=True), writes=["a_n1w"])
        P_fm_v = P_fm.rearrange("(c p) t -> p c t", p=128)
        ev = 0
        for i in range(NT):
            b = i % 2
            xk = "a_xt%d" % b
            sc.dma("sp", lambda e, b=b, i=i: e.dma_start(out=xt[b][:], in_=x[i * 128:(i + 1) * 128, :]), writes=[xk])
            rms_rstd(sc, xt[b][:], xk, junk[:], ss[:], rstd[:], epsc[:], D, "a_")
            sc.op("dve", lambda e, b=b: e.tensor_scalar(out=xn[:], in0=xt[b][:], scalar1=rstd[:, 0:1], scalar2=None,
                                                        op0=ALU.mult), reads=[xk, "a_rstd"], writes=["a_xn"])
            for kc in range(8):
                sc.op("pe", lambda e, kc=kc: e.transpose(out=ps_t[:, kc, :], in_=xn[:, kc * 128:(kc + 1) * 128],
                                                         identity=ident_b[:]), reads=["a_xn", "ident_b"], writes=["ap_t"])
            sc.op("dve", lambda e: e.tensor_tensor(out=xnT[:], in0=ps_t[:],
                                                   in1=n1w[:].unsqueeze(2).broadcast_to([128, 8, 128]), op=ALU.mult),
                  reads=["ap_t", "a_n1w"], writes=["a_xnT"])
            if uv_ops:
                uv_ops.pop(0)()
            for cg, (c0, c1) in enumerate(TM_RANGES):
                pm = ps_m[cg % 2]
                pk = "ap_m%d" % (cg % 2)
                for kc in range(8):
                    sc.op("pe", lambda e, kc=kc, c0=c0, c1=c1, pm=pm: e.matmul(
                        pm[:, 0:c1 - c0], lhsT=xnT[:, kc, :], rhs=win[:, kc, c0:c1], start=(kc == 0), stop=(kc == 7)),
                        reads=["a_xnT", "a_win"], writes=[pk])
                if cg % 2 == 0:
                    sc.op("act", lambda e, b=b, c0=c0, c1=c1, pm=pm: e.copy(out=ptm[b][:, c0:c1], in_=pm[:, 0:c1 - c0]),
                          reads=[pk], writes=["a_ptm%d" % b])
                else:
                    sc.op("dve", lambda e, b=b, c0=c0, c1=c1, pm=pm: e.tensor_copy(out=ptm[b][:, c0:c1], in_=pm[:, 0:c1 - c0]),
                          reads=[pk], writes=["a_ptm%d" % b])
            sc.dma("sp", lambda e, b=b, i=i: e.dma_start(out=P_tm[i * 128:(i + 1) * 128, 512:2048], in_=ptm[b][:, 512:2048]),
                   reads=["a_ptm%d" % b], writes=["P_tm"])
            sc.dma("sp", lambda e, b=b, i=i: e.dma_start(out=P_tm[i * 128:(i + 1) * 128, 2944:INW], in_=ptm[b][:, 2944:INW]),
                   reads=["a_ptm%d" % b], writes=["P_tm"])
            for fg in range(4):
                pf = ps_f[fg % 2]
                pk = "ap_f%d" % (fg % 2)
                for f4 in range(4):
                    ch = FMCH[fg * 4 + f4]
                    for kc in range(8):
                        sc.op("pe", lambda e, kc=kc, ch=ch, f4=f4, pf=pf: e.matmul(
                            pf[:, f4, :], lhsT=win[:, kc, ch * 128:(ch + 1) * 128], rhs=xnT[:, kc, :],
                            start=(kc == 0), stop=(kc == 7)), reads=["a_xnT", "a_win"], writes=[pk])
                if fg % 2 == 0:
                    sc.op("act", lambda e, b=b, fg=fg, pf=pf: e.copy(out=pfm[b][:, fg * 4:(fg + 1) * 4, :], in_=pf[:]),
                          reads=[pk], writes=["a_pfm%d" % b])
                else:
                    sc.op("dve", lambda e, b=b, fg=fg, pf=pf: e.tensor_copy(out=pfm[b][:, fg * 4:(fg + 1) * 4, :], in_=pf[:]),
                          reads=[pk], writes=["a_pfm%d" % b])
            sc.dma("sp", lambda e, b=b, i=i: e.dma_start(out=P_fm_v[:, :, i * 128:(i + 1) * 128], in_=pfm[b][:]),
                   reads=["a_pfm%d" % b], writes=["P_fm"])
        while uv_ops:
            uv_ops.pop(0)()
        barrier(sc)


def stage_hgrn(nc, sc, NT, P_tm, P_fm, Y_tm, lb_logits, hg_onw, ublk, wrev, epsc, c_rowm, c_colm):
    with contextlib.ExitStack() as st:
        T = lambda name, shape, dt: st.enter_context(nc.sbuf_tensor(name, shape, dt))
        P = lambda name, shape, dt: st.enter_context(nc.psum_tensor(name, shape, dt))
        lbb = T("h_lbb", [128, 2, 512], F32)
        omlb = T("h_omlb", [128, 512], F32)
        lbf = T("h_lbf", [128, 2, 4], F32)
        omlf = T("h_omlf", [128, 4], F32)
        hgw = T("h_hgw", [128, 128], F32)
        ftm = [T("h_ftm%d" % i, [128, 1536], F32) for i in range(2)]
        ffm = [T("h_ffm%d" % i, [128, 8, 128], F32) for i in range(2)]
        sig = T("h_sig", [128, 512], F32)
        logf = T("h_logf", [128, 512], F32)
        ktm = T("h_ktm", [128, 512], F32)
        erev = T("h_erev", [128, 512], F32)
        khat = T("h_khat", [128, 512], F32)
        khat4 = T("h_khat4", [128, 4, 512], BF16)
        rowm = T("h_rowm", [128, 4], F32)
        colm = T("h_colm", [128, 4, 128], F32)
        vbf = T("h_vbf", [128, 512], BF16)
        sgg = T("h_sgg", [128, 512], F32)
        fT = T("h_fT", [128, 4, 128], F32)
        ET = T("h_ET", [128, 4, 128], F32)
        EiT = T("h_EiT", [128, 4, 128], F32)
        qtT = T("h_qtT", [128, 4, 128], BF16)
        ktT = T("h_ktT", [128, 4, 128], BF16)
        qtT4 = [T("h_qtT4%d" % h, [128, 4, 128], BF16) for h in range(4)]
        scm = T("h_scm", [128, 4, 128], BF16)
        state = [T("h_st%d" % h, [128, 128], F32) for h in range(4)]
        stbf = [T("h_sb%d" % h, [128, 128], BF16) for h in range(4)]
        junk = T("h_junk", [128, 128], F32)
        ss = T("h_ss", [128, 4], F32)
        rstd = T("h_rstd", [128, 4], F32)
        yt = [T("h_yt%d" % i, [128, 512], F32) for i in range(2)]
        onec = T("h_onec", [128, 1], F32)
        sc.op("dve", lambda e: e.memset(onec[:], 1.0), writes=["h_onec"])
        ps_m = [P("hp_m%d" % i, [128, 512], F32) for i in range(4)]
        ps_o = [P("hp_o%d" % i, [128, 512], F32) for i in range(4)]
        ps_rev = ps_m[0]
        ps_bT = ps_m[1][:].rearrange("p (h t) -> p h t", h=4)
        ps_sc = ps_m[2][:].rearrange("p (h t) -> p h t", h=4)
        sc.dma("sp", lambda e: e.dma_start(out=rowm[:], in_=c_rowm), writes=["h_rowm"])
        sc.dma("sp", lambda e: e.dma_start(out=colm[:], in_=c_colm), writes=["h_colm"])
        sc.dma("sp", lambda e: e.dma_start(out=lbb[:, 0, :], in_=lb_logits[0:1, :].partition_broadcast(128)), writes=["h_lbb"])
        sc.dma("sp", lambda e: e.dma_start(out=lbb[:, 1, :], in_=lb_logits[1:2, :].partition_broadcast(128)), writes=["h_lbb"])
        sc.dma("sp", lambda e: e.dma_start(out=lbf[:], in_=lb_logits.rearrange("r (h p) -> p r h", p=128),
                                           allow_slow_non_contiguous=True), writes=["h_lbf"])
        sc.dma("sp", lambda e: e.dma_start(out=hgw[:], in_=hg_onw[0:1, :].partition_broadcast(128)), writes=["h_hgw"])

        def sigmoid_to(ap_out, ap_in, keys_in, key_out):
            sc.op("act", lambda e: e.activation(out=ap_out, in_=ap_in, func=AF.Exp, scale=-1.0), reads=keys_in, writes=[key_out])
            sc.op("act", lambda e: e.activation(out=ap_out, in_=ap_out, func=AF.Ln, bias=onec[:]), reads=[key_out], writes=[key_out])
            sc.op("act", lambda e: e.activation(out=ap_out, in_=ap_out, func=AF.Exp, scale=-1.0), reads=[key_out], writes=[key_out])

        sc.op("dve", lambda e: e.tensor_tensor(out=lbb[:, 0, :], in0=lbb[:, 0, :], in1=lbb[:, 1, :], op=ALU.subtract),
              reads=["h_lbb"], writes=["h_lbb"])
        sigmoid_to(lbb[:, 0, :], lbb[:, 0, :], ["h_lbb"], "h_lbb")
        sc.op("dve", lambda e: e.tensor_scalar(out=omlb[:], in0=lbb[:, 0, :], scalar1=-1.0, scalar2=1.0, op0=ALU.mult,
                                               op1=ALU.add), reads=["h_lbb"], writes=["h_omlb"])
        sc.op("dve", lambda e: e.tensor_tensor(out=lbf[:, 0, :], in0=lbf[:, 0, :], in1=lbf[:, 1, :], op=ALU.subtract),
              reads=["h_lbf"], writes=["h_lbf"])
        sigmoid_to(lbf[:, 0, :], lbf[:, 0, :], ["h_lbf"], "h_lbf")
        sc.op("dve", lambda e: e.tensor_scalar(out=omlf[:], in0=lbf[:, 0, :], scalar1=-1.0, scalar2=1.0, op0=ALU.mult,
                                               op1=ALU.add), reads=["h_lbf"], writes=["h_omlf"])
        P_fm_v = P_fm.rearrange("(c p) t -> p c t", p=128)
        for i in range(NT):
            b = i % 2
            fk, mk = "h_ftm%d" % b, "h_ffm%d" % b
            F_, M_ = ftm[b], ffm[b]
            sc.dma("sp", lambda e, b=b, i=i: e.dma_start(out=ftm[b][:], in_=P_tm[i * 128:(i + 1) * 128, 512:2048]),
                   reads=["P_tm"], writes=[fk])
            sc.dma("sp", lambda e, b=b, i=i: e.dma_start(out=ffm[b][:], in_=P_fm_v[:, 0:8, i * 128:(i + 1) * 128]),
                   reads=["P_fm"], writes=[mk])
            if i % 16 == 0:
                for h in range(4):
                    sc.op("dve", lambda e, h=h: e.memset(state[h][:], 0.0), writes=["h_st%d" % h])
                    sc.op("dve", lambda e, h=h: e.memset(stbf[h][:], 0.0), writes=["h_sb%d" % h])
            opsA, opsB = [], []
            OP_A = lambda *a, **k: opsA.append(lambda: sc.op(*a, **k))
            OP_B = lambda *a, **k: opsB.append(lambda: sc.op(*a, **k))
            SIG_A = lambda *a: opsA.append(lambda: sigmoid_to(*a))
            SIG_B = lambda *a: opsB.append(lambda: sigmoid_to(*a))
            SIG_A(sig[:], F_[:, 0:512], [fk], "h_sig")
            OP_A("dve", lambda e: e.tensor_tensor(out=sig[:], in0=sig[:], in1=omlb[:], op=ALU.mult),
                  reads=["h_sig", "h_omlb"], writes=["h_sig"])
            OP_A("dve", lambda e: e.tensor_tensor(out=sig[:], in0=sig[:], in1=lbb[:, 0, :], op=ALU.add),
                  reads=["h_sig", "h_lbb"], writes=["h_sig"])
            OP_A("act", lambda e: e.activation(out=logf[:], in_=sig[:], func=AF.Ln), reads=["h_sig"], writes=["h_logf"])
            OP_A("dve", lambda e: e.tensor_scalar(out=ktm[:], in0=sig[:], scalar1=-1.0, scalar2=1.0, op0=ALU.mult,
                                                   op1=ALU.add), reads=["h_sig"], writes=["h_ktm"])
            OP_A("pe", lambda e: e.matmul(ps_rev[:], lhsT=wrev[:], rhs=logf[:], start=True, stop=True),
                  reads=["wrev", "h_logf"], writes=["hp_m0"])
            OP_A("act", lambda e: e.activation(out=erev[:], in_=ps_rev[:], func=AF.Exp), reads=["hp_m0"], writes=["h_erev"])
            OP_A("dve", lambda e: e.tensor_tensor(out=khat[:], in0=ktm[:], in1=erev[:], op=ALU.mult),
                  reads=["h_ktm", "h_erev"], writes=["h_khat"])
            OP_A("dve", lambda e: e.tensor_tensor(out=khat4[:], in0=khat[:].unsqueeze(1).broadcast_to([128, 4, 512]),
                                                   in1=rowm[:].unsqueeze(2).broadcast_to([128, 4, 512]), op=ALU.mult),
                  reads=["h_khat", "h_rowm"], writes=["h_khat4"])
            OP_A("act", lambda e, F_=F_: e.copy(out=vbf[:], in_=F_[:, 512:1024]), reads=[fk], writes=["h_vbf"])
            SIG_A(sgg[:], F_[:, 1024:1536], [fk], "h_sgg")
            OP_A("dve", lambda e, F_=F_: e.tensor_tensor(out=sgg[:], in0=sgg[:], in1=F_[:, 1024:1536], op=ALU.mult),
                  reads=["h_sgg", fk], writes=["h_sgg"])
            SIG_B(fT[:], M_[:, 4:8, :], [mk], "h_fT")
            for h in range(4):
                OP_B("dve", lambda e, h=h: e.tensor_scalar(out=fT[:, h, :], in0=fT[:, h, :], scalar1=omlf[:, h:h + 1],
                                                            scalar2=lbf[:, 0, h:h + 1], op0=ALU.mult, op1=ALU.add),
                      reads=["h_fT", "h_omlf", "h_lbf"], writes=["h_fT"])
            OP_B("dve", lambda e: e.tensor_scalar(out=fT[:], in0=fT[:], scalar1=-1.0, scalar2=1.0, op0=ALU.mult,
                                                   op1=ALU.add), reads=["h_fT"], writes=["h_fT"])
            for h in range(4):
                OP_B("pe", lambda e, h=h: e.matmul(ps_bT[:, h, :], lhsT=logf[:, h * 128:(h + 1) * 128], rhs=ublk[:],
                                                    start=True, stop=True), reads=["h_logf", "ublk"], writes=["hp_m1"])
            OP_B("act", lambda e: e.activation(out=ET[:], in_=ps_bT, func=AF.Exp), reads=["hp_m1"], writes=["h_ET"])
            OP_B("act", lambda e: e.activation(out=EiT[:], in_=ps_bT, func=AF.Exp, scale=-1.0),
                  reads=["hp_m1"], writes=["h_EiT"])
            OP_B("dve", lambda e, M_=M_: e.tensor_tensor(out=qtT[:], in0=M_[:, 0:4, :], in1=ET[:], op=ALU.mult),
                  reads=[mk, "h_ET"], writes=["h_qtT"])
            OP_B("dve", lambda e: e.tensor_tensor(out=ktT[:], in0=fT[:], in1=EiT[:], op=ALU.mult),
                  reads=["h_fT", "h_EiT"], writes=["h_ktT"])
            for h in range(4):
                OP_B("dve", lambda e, h=h: e.tensor_tensor(out=qtT4[h][:], in0=qtT[:, h, :].unsqueeze(1).broadcast_to([128, 4, 128]),
                                                            in1=colm[:], op=ALU.mult),
                      reads=["h_qtT", "h_colm"], writes=["h_qtT4%d" % h])
                OP_B("pe", lambda e, h=h: e.matmul(ps_sc[:, h, :], lhsT=ktT[:, h, :], rhs=qtT[:, h, :], start=True, stop=True),
                      reads=["h_ktT", "h_qtT"], writes=["hp_m2"])
            OP_B("dve", lambda e: e.tensor_tensor(out=scm[:], in0=ps_sc, in1=ublk[:].unsqueeze(1).broadcast_to([128, 4, 128]),
                                                   op=ALU.mult), reads=["hp_m2", "ublk"], writes=["h_scm"])
            for k_ in range(max(len(opsA), len(opsB))):
                if k_ < len(opsA):
                    opsA[k_]()
                if k_ < len(opsB):
                    opsB[k_]()
            for h in range(4):
                sc.op("pe", lambda e, h=h: e.matmul(ps_o[h][:, 0:128], lhsT=scm[:, h, :], rhs=vbf[:, h * 128:(h + 1) * 128],
                                                    start=True, stop=False), reads=["h_scm", "h_vbf"], writes=["hp_o%d" % h])
            for c4 in range(4):
                for h in range(4):
                    hs = slice(h * 128, (h + 1) * 128)
                    sc.op("pe", lambda e, h=h, c4=c4: e.matmul(ps_o[h][:, 0:128], lhsT=qtT4[h][:, c4, :], rhs=stbf[h][:],
                                                               start=False, stop=(c4 == 3)),
                          reads=["h_qtT4%d" % h, "h_sb%d" % h], writes=["hp_o%d" % h])
                    sc.op("pe", lambda e, c4=c4, hs=hs, h=h: e.matmul(ps_m[h][:, 0:128], lhsT=khat4[:, c4, hs], rhs=vbf[:, hs],
                                                                      start=True, stop=True),
                          reads=["h_khat4", "h_vbf"], writes=["hp_m%d" % h])
                    sc.op("dve", lambda e, h=h, c4=c4: e.scalar_tensor_tensor(
                        out=state[h][:], in0=state[h][:], scalar=ET[:, h, c4 * 32 + 31:c4 * 32 + 32], in1=ps_m[h][:, 0:128],
                        op0=ALU.mult, op1=ALU.add), reads=["h_st%d" % h, "h_ET", "hp_m%d" % h], writes=["h_st%d" % h])
                    sc.op("act", lambda e, h=h: e.copy(out=stbf[h][:], in_=state[h][:]),
                          reads=["h_st%d" % h], writes=["h_sb%d" % h])
            for h in range(4):
                sc.op("act", lambda e, h=h: e.activation(out=junk[:], in_=ps_o[h][:, 0:128], func=AF.Square, accum_out=ss[:, h:h + 1]),
                      reads=["hp_o%d" % h], writes=["h_junk", "h_ss"])
            sc.op("act", lambda e: e.activation(out=rstd[:], in_=ss[:], func=AF.Ln, scale=1.0 / 128, bias=epsc[:]),
                  reads=["h_ss"], writes=["h_rstd"])
            sc.op("act", lambda e: e.activation(out=rstd[:], in_=rstd[:], func=AF.Exp, scale=-0.5),
                  reads=["h_rstd"], writes=["h_rstd"])
            for h in range(4):
                hs = slice(h * 128, (h + 1) * 128)
                sc.op("dve", lambda e, h=h, b=b, hs=hs: e.scalar_tensor_tensor(
                    out=yt[b][:, hs], in0=ps_o[h][:, 0:128], scalar=rstd[:, h:h + 1], in1=hgw[:], op0=ALU.mult, op1=ALU.mult),
                    reads=["hp_o%d" % h, "h_rstd", "h_hgw"], writes=["h_yt%d" % b])
            sc.op("dve", lambda e, b=b: e.tensor_tensor(out=yt[b][:], in0=yt[b][:], in1=sgg[:], op=ALU.mult),
                  reads=["h_yt%d" % b, "h_sgg"], writes=["h_yt%d" % b])
            sc.dma("sp", lambda e, b=b, i=i: e.dma_start(out=Y_tm[i * 128:(i + 1) * 128, 0:512], in_=yt[b][:]),
                   reads=["h_yt%d" % b], writes=["Y_tm"])
        barrier(sc)


NSA_WARM = 1


def stage_nsa(nc, sc, NSEQ, P_tm, P_fm, Y_tm, positions, qnw, knw, pe_k, pe_v, w1k, w2k, w1v, w2v, onw, cst,
              ident_b, epsc, dbg=None):
    TINY = 1e-30
    with contextlib.ExitStack() as st:
        T = lambda name, shape, dt: st.enter_context(nc.sbuf_tensor(name, shape, dt))
        P = lambda name, shape, dt: st.enter_context(nc.psum_tensor(name, shape, dt))
        dmask = T("n_dmask", [128, 8, 512], BF16)
        cmask = T("n_cmask", [128, 4, 512], BF16)
        rm = T("n_rm", [64, 64], BF16)
        ones64 = T("n_ones", [64, 64], BF16)
        misc = T("n_misc", [128, 8], F32)
        valid = T("n_valid", [128, 16, 32], F32)
        addc = T("n_addc", [128, 16, 32], F32)
        wcol = T("n_wcol", [64, 4], F32)
        onwb = T("n_onwb", [128, 64], F32)
        w1 = [T("n_w1%d" % i, [64, 32, 256], BF16) for i in range(2)]
        w2 = [T("n_w2%d" % i, [128, 2, 64], BF16) for i in range(2)]
        peT = T("n_peT", [64, 2, 32], BF16)
        peTf = T("n_peTf", [64, 2, 32], F32)
        cbias = T("n_cbias", [128, 2, 2], F32)
        for name, t_, src in (("n_dmask", dmask, cst["c_dmask"]), ("n_cmask", cmask, cst["c_cmask"]),
                              ("n_rm", rm, cst["c_rm"])):
            sc.dma("pool", lambda e, t_=t_, src=src: e.dma_start(out=t_[:], in_=src), writes=[name])
        sc.dma("sp", lambda e: e.dma_start(out=misc[:], in_=cst["c_misc"]), writes=["n_misc"])
        sc.dma("sp", lambda e: e.dma_start(out=valid[:], in_=cst["c_valid"]), writes=["n_valid"])
        sc.dma("sp", lambda e: e.dma_start(out=addc[:], in_=cst["c_addc"]), writes=["n_addc"])
        sc.op("dve", lambda e: e.memset(ones64[:], 1.0), writes=["n_ones"])
        sc.dma("pool", lambda e: e.dma_start(out=ksT[64:96, :], in_=cst["c_emat"]), writes=["n_ksT"])
        sc.op("dve", lambda e: e.tensor_scalar(out=dm01[:], in0=dmask[:], scalar1=-1.0, scalar2=None, op0=ALU.is_ge),
              reads=["n_dmask"], writes=["n_dm01"])
        sc.op("dve", lambda e: e.memset(selb[:], 0.0), writes=["n_selb"])
        sc.op("dve", lambda e: e.memset(kwT[64:96, :], 0.0), writes=["n_kwT"])
        sc.op("dve", lambda e: e.memset(kcmpT[64:96, :], 0.0), writes=["n_kcmpT"])
        for r_ in range(4):
            sc.op("dve", lambda e, r_=r_: e.memset(qT[r_][64:96, :], 0.0), writes=["n_qT%d" % r_])
        sc.dma("sp", lambda e: e.dma_start(out=wcol[:, 0:1], in_=qnw.rearrange("o d -> d o"), allow_slow_non_contiguous=True),
               writes=["n_wcol"])
        sc.dma("sp", lambda e: e.dma_start(out=wcol[:, 1:4], in_=knw.rearrange("b d -> d b"), allow_slow_non_contiguous=True),
               writes=["n_wcol"])
        sc.op("dve", lambda e: e.tensor_scalar(out=wcol[:, 0:1], in0=wcol[:, 0:1], scalar1=0.125, scalar2=None, op0=ALU.mult),
              reads=["n_wcol"], writes=["n_wcol"])
        sc.dma("sp", lambda e: e.dma_start(out=onwb[:], in_=onw[0:1, :].partition_broadcast(128)), writes=["n_onwb"])
        for i, (w1_, w2_) in enumerate(((w1k, w2k), (w1v, w2v))):
            sc.dma("pool", lambda e, i=i, w1_=w1_: e.dma_start(out=w1[i][:], in_=w1_.rearrange("(l d) n -> d l n", d=64)),
                   writes=["n_w1%d" % i])
            sc.dma("pool", lambda e, i=i, w2_=w2_: e.dma_start(out=w2[i][:], in_=w2_.rearrange("(a p) n -> p a n", p=128)),
                   writes=["n_w2%d" % i])
        sc.dma("sp", lambda e: e.dma_start(out=peTf[:, 0, :], in_=pe_k.rearrange("l d -> d l"), allow_slow_non_contiguous=True),
               writes=["n_peTf"])
        sc.dma("sp", lambda e: e.dma_start(out=peTf[:, 1, :], in_=pe_v.rearrange("l d -> d l"), allow_slow_non_contiguous=True),
               writes=["n_peTf"])
        sc.op("dve", lambda e: e.tensor_copy(out=peT[:], in_=peTf[:]), reads=["n_peTf"], writes=["n_peT"])
        ki = T("n_ki", [16, 2048], I32)
        posi = ki
        kf = T("n_kf", [16, 2048], F32)
        CT = T("n_CT", [64, 2048], F32)
        ST = T("n_ST", [64, 2048], F32)
        src = T("n_src", [64, 2048], F32)
        sq = [T("n_sq%d" % i, [64, 512], BF16) for i in range(2)]
        rinv = [T("n_rinv%d" % i, [64, 512], F32) for i in range(2)]
        xnm = [T("n_xnm%d" % i, [64, 512], F32) for i in range(2)]
        xb = [T("n_xb%d" % i, [64, 512], BF16) for i in range(2)]
        t1 = [T("n_t1%d" % i, [64, 512], F32) for i in range(2)]
        t2 = [T("n_t2%d" % i, [64, 512], F32) for i in range(2)]
        qT = [T("n_qT%d" % r, [96, 2048], BF16) for r in range(4)]
        ksT = T("n_ksT", [96, 2048], BF16)
        dm01 = dmask
        kwT = T("n_kwT", [96, 2048], BF16)
        kcb = T("n_kcb", [64, 2048], BF16)
        kcmpT = T("n_kcmpT", [96, 128], BF16)
        hx = T("n_hx", [128, 127], F32)
        hx2 = T("n_hx2", [128, 127], F32)
        hT = T("n_hT", [128, 2, 127], BF16)
        vcaug = T("n_vcaug", [128, 97], BF16)
        vaugf = T("n_vaugf", [128, 33], F32)
        vsa = T("n_vsa", [128, 16, 65], BF16)
        vwa = T("n_vwa", [128, 16, 65], BF16)
        vld = T("n_vld", [128, 16, 64], F32)
        gsig = T("n_gsig", [128, 16, 24], F32)
        PT = [T("n_PT%d" % i, [128, 512], BF16) for i in range(5)]
        oacc = T("n_oacc", [128, 16, 4, 64], F32)
        imp = T("n_imp", [128, 16, 32], F32)
        score = T("n_score", [128, 16, 32], F32)
        swk = T("n_swk", [128, 4, 32], F32)
        m8a = T("n_m8a", [128, 4, 8], F32)
        m8b = T("n_m8b", [128, 4, 8], F32)
        sel = T("n_sel", [128, 4, 32], F32)
        selb = T("n_selb", [128, 4, 96], BF16)
        nselT = T("n_nselT", [96, 2048], BF16)
        r1 = T("n_r1", [128, 1], F32)
        coef = T("n_coef", [128, 1], F32)
        ssq = T("n_ssq", [128, 64, 64], F32)
        ang = ssq[0:16, 0:32, :].rearrange("p a b -> p (a b)")
        rr = ssq[0:16, 32:64, :].rearrange("p a b -> p (a b)")
        ss64 = T("n_ss64", [128, 64], F32)
        ps_s = [P("np_s%d" % i, [128, 512], F32) for i in range(5)]
        ps_a = [P("np_a%d" % i, [128, 512], F32) for i in range(2)]
        ps_warm = P("np_w", [128, 512], F32)
        ps_x = [ps_s[2], ps_s[3]]
        sc.dma("sp", lambda e: e.dma_start(out=vaugf[:], in_=cst["c_vaug"]), writes=["n_vaugf"])
        sc.op("dve", lambda e: e.tensor_copy(out=vcaug[:, 64:97], in_=vaugf[:]), reads=["n_vaugf"], writes=["n_vcaug"])
        sc.op("dve", lambda e: e.memset(vcaug[:, 0:64], 0.0), writes=["n_vcaug"])
        sc.op("dve", lambda e: e.memset(vsa[:], 1.0), writes=["n_vsa"])
        sc.op("dve", lambda e: e.memset(vwa[:], 1.0), writes=["n_vwa"])
        sc.op("dve", lambda e: e.memset(CT[:], 1.0), writes=["n_CT"])
        sc.op("dve", lambda e: e.memset(ST[:], 0.0), writes=["n_ST"])
        for kv in range(2):
            for hh in range(2):
                for l in range(32):
                    sc.op("pe", lambda e, kv=kv, hh=hh, l=l: e.matmul(
                        ps_x[0][:, 0:1], lhsT=w1[kv][:, l, hh * 128:(hh + 1) * 128], rhs=peT[:, kv, l:l + 1],
                        start=(l == 0), stop=(l == 31)), reads=["n_w1%d" % kv, "n_peT"], writes=["np_s2"])
                sc.op("act", lambda e, kv=kv, hh=hh: e.copy(out=cbias[:, kv, hh:hh + 1], in_=ps_x[0][:, 0:1]),
                      reads=["np_s2"], writes=["n_cbias"])

        def norm_rope(calls):
            S = list(enumerate(calls))
            pa = [(ps_s[2], "np_s2", ps_s[3], "np_s3"), (ps_s[0], "np_s0", ps_s[1], "np_s1")]
            for k, (srcap, srckey, n, wc, Cap, Sap, outap, outkey) in S:
                sc.op("act", lambda e, k=k, n=n, srcap=srcap: e.activation(out=sq[k][:, 0:n], in_=srcap, func=AF.Square),
                      reads=[srckey], writes=["n_sq%d" % k])
            for k, (srcap, srckey, n, wc, Cap, Sap, outap, outkey) in S:
                sc.op("pe", lambda e, k=k, n=n: e.matmul(pa[k][0][0:64, 0:n], lhsT=ones64[:], rhs=sq[k][:, 0:n], start=True, stop=True),
                      reads=["n_sq%d" % k, "n_ones"], writes=[pa[k][1]])
            for k, (srcap, srckey, n, wc, Cap, Sap, outap, outkey) in S:
                sc.op("act", lambda e, k=k, n=n: e.activation(out=rinv[k][:, 0:n], in_=pa[k][0][0:64, 0:n], func=AF.Sqrt,
                                                              scale=1.0 / 64, bias=epsc[0:64, :]),
                      reads=[pa[k][1]], writes=["n_rinv%d" % k])
            for k, (srcap, srckey, n, wc, Cap, Sap, outap, outkey) in S:
                sc.op("dve", lambda e, k=k, n=n: e.reciprocal(out=rinv[k][:, 0:n], in_=rinv[k][:, 0:n]),
                      reads=["n_rinv%d" % k], writes=["n_rinv%d" % k])
            for k, (srcap, srckey, n, wc, Cap, Sap, outap, outkey) in S:
                sc.op("dve", lambda e, k=k, n=n, srcap=srcap, wc=wc: e.scalar_tensor_tensor(
                    out=xnm[k][:, 0:n], in0=srcap, scalar=wcol[:, wc:wc + 1], in1=rinv[k][:, 0:n], op0=ALU.mult, op1=ALU.mult),
                    reads=[srckey, "n_wcol", "n_rinv%d" % k], writes=["n_xnm%d" % k])
            for k, (srcap, srckey, n, wc, Cap, Sap, outap, outkey) in S:
                sc.op("act", lambda e, k=k, n=n: e.copy(out=xb[k][:, 0:n], in_=xnm[k][:, 0:n]),
                      reads=["n_xnm%d" % k], writes=["n_xb%d" % k])
            for k, (srcap, srckey, n, wc, Cap, Sap, outap, outkey) in S:
                sc.op("pe", lambda e, k=k, n=n: e.matmul(pa[k][2][0:64, 0:n], lhsT=rm[:], rhs=xb[k][:, 0:n], start=True, stop=True),
                      reads=["n_xb%d" % k, "n_rm"], writes=[pa[k][3]])
            for k, (srcap, srckey, n, wc, Cap, Sap, outap, outkey) in S:
                sc.op("dve", lambda e, k=k, n=n, Cap=Cap: e.tensor_tensor(out=t1[k][:, 0:n], in0=xnm[k][:, 0:n], in1=Cap, op=ALU.mult),
                      reads=["n_xnm%d" % k, "n_CT"], writes=["n_t1%d" % k])
            for k, (srcap, srckey, n, wc, Cap, Sap, outap, outkey) in S:
                sc.op("dve", lambda e, k=k, n=n, Sap=Sap: e.tensor_tensor(out=t2[k][:, 0:n], in0=pa[k][2][0:64, 0:n], in1=Sap, op=ALU.mult),
                      reads=[pa[k][3], "n_ST"], writes=["n_t2%d" % k])
            for k, (srcap, srckey, n, wc, Cap, Sap, outap, outkey) in S:
                sc.op("dve", lambda e, k=k, n=n, outap=outap: e.tensor_tensor(out=outap, in0=t1[k][:, 0:n], in1=t2[k][:, 0:n], op=ALU.add),
                      reads=["n_t1%d" % k, "n_t2%d" % k], writes=[outkey])

        P_fm_v = P_fm
        PTc = [0]
        ACc = [0, 0]
        accs = [T("n_accs%d" % i, [128, 4, 97], F32) for i in range(3)]
        r4 = T("n_r4", [128, 4], F32)
        c4t = T("n_c4", [128, 4], F32)
        o4 = T("n_o4", [128, 4, 64], F32)
        i4 = T("n_i4", [128, 4, 32], F32)

        def attn(kT, kkey, vfn, vkey, W, kts_fn, mask_fn, nk_fn, epi, krows=64, post_fn=None):
            SK = 4
            for r in range(4):
                units = [(qb, idx, kt, len(kts_fn(qb))) for qb in range(4) for idx, kt in enumerate(kts_fn(qb))]
                slot = {}

                def emit_qk(u):
                    qb, idx, kt, nkt = units[u]
                    nk = nk_fn(kt)
                    pb = PTc[0] % 5
                    PTc[0] += 1
                    slot[u] = pb
                    ps, psk = ps_s[pb], "np_s%d" % pb
                    mms = [(kT[0:krows, kt * 128:kt * 128 + nk], qT[r][0:krows, qb * 512:(qb + 1) * 512], [kkey, "n_qT%d" % r])]
                    mms += mask_fn(kt, qb)
                    for mi, (l_, r_, rk) in enumerate(mms):
                        sc.op("pe", lambda e, l_=l_, r_=r_, ps=ps, nk=nk, mi=mi, last=(mi == len(mms) - 1): e.matmul(
                            ps[0:nk, :], lhsT=l_, rhs=r_, start=(mi == 0), stop=last), reads=rk, writes=[psk])
                    sc.op("act", lambda e, ps=ps, nk=nk, pb=pb: e.activation(out=PT[pb][0:nk, :], in_=ps[0:nk, :], func=AF.Exp),
                          reads=[psk], writes=["n_PT%d" % pb])
                    for _w in range(NSA_WARM):
                        sc.op("pe", lambda e: e.matmul(ps_warm[:], lhsT=ident_b[:], rhs=dm01[:, 4, :], start=True, stop=True),
                              reads=["ident_b", "n_dm01"], writes=["np_w"])
                    m01 = post_fn(kt, qb) if post_fn else None
                    if m01 is not None:
                        sc.op("pool", lambda e, pb=pb, m01=m01: e.tensor_tensor(out=PT[pb][:], in0=PT[pb][:], in1=m01, op=ALU.mult),
                              reads=["n_PT%d" % pb, "n_dm01"], writes=["n_PT%d" % pb])

                def emit_pv(u):
                    qb, idx, kt, nkt = units[u]
                    nk = nk_fn(kt)
                    pb = slot[u]
                    ab = ACc[0] % 2
                    for ts in range(4):
                        sc.op("pe", lambda e, pb=pb, nk=nk, ts=ts, kt=kt, idx=idx, ab=ab, last=(idx == nkt - 1): e.matmul(
                            ps_a[ab][:, ts * W:(ts + 1) * W], lhsT=PT[pb][0:nk, ts * 128:(ts + 1) * 128], rhs=vfn(kt)[0:nk, :],
                            start=(idx == 0 and ts == 0), stop=last, skip_group_check=True),
                            reads=["n_PT%d" % pb, vkey], writes=["np_a%d" % ab])
                    if idx == nkt - 1:
                        ACc[0] += 1
                        sb3 = ACc[1] % 3
                        ACc[1] += 1
                        sc.op("act", lambda e, ab=ab, sb3=sb3: e.copy(
                            out=accs[sb3][:, :, 0:W], in_=ps_a[ab][:, 0:4 * W].rearrange("p (a b) -> p a b", b=W)),
                            reads=["np_a%d" % ab], writes=["n_accs%d" % sb3])
                        epi(r, qb, accs[sb3], "n_accs%d" % sb3)

                for j in range(len(units) + SK):
                    if j < len(units):
                        emit_qk(j)
                    if j - SK >= 0:
                        emit_pv(j - SK)

        def mk_epi(g, branch, first):
            def epi(r, qb, acc, acck):
                tsl = slice(qb * 4, qb * 4 + 4)
                gc = (g * 4 + r) * 3 + branch
                sc.op("dve", lambda e: e.tensor_scalar(out=r4[:], in0=acc[:, :, 64], scalar1=TINY, scalar2=None, op0=ALU.max),
                      reads=[acck], writes=["n_r4"])
                sc.op("dve", lambda e: e.reciprocal(out=r4[:], in_=r4[:]), reads=["n_r4"], writes=["n_r4"])
                sc.op("dve", lambda e: e.tensor_tensor(out=c4t[:], in0=r4[:], in1=gsig[:, tsl, gc], op=ALU.mult),
                      reads=["n_r4", "n_gsig"], writes=["n_c4"])
                if first:
                    sc.op("dve", lambda e: e.tensor_tensor(out=oacc[:, tsl, r, :], in0=acc[:, :, 0:64],
                                                           in1=c4t[:].unsqueeze(2).broadcast_to([128, 4, 64]), op=ALU.mult),
                          reads=[acck, "n_c4"], writes=["n_oacc"])
                    if r == 0:
                        sc.op("dve", lambda e: e.tensor_tensor(out=imp[:, tsl, :], in0=acc[:, :, 65:97],
                                                               in1=r4[:].unsqueeze(2).broadcast_to([128, 4, 32]), op=ALU.mult),
                              reads=[acck, "n_r4"], writes=["n_imp"])
                    else:
                        sc.op("dve", lambda e: e.tensor_tensor(out=i4[:], in0=acc[:, :, 65:97],
                                                               in1=r4[:].unsqueeze(2).broadcast_to([128, 4, 32]), op=ALU.mult),
                              reads=[acck, "n_r4"], writes=["n_i4"])
                        sc.op("dve", lambda e: e.tensor_tensor(out=imp[:, tsl, :], in0=imp[:, tsl, :], in1=i4[:], op=ALU.add),
                              reads=["n_i4", "n_imp"], writes=["n_imp"])
                else:
                    sc.op("dve", lambda e: e.tensor_tensor(out=o4[:], in0=acc[:, :, 0:64],
                                                           in1=c4t[:].unsqueeze(2).broadcast_to([128, 4, 64]), op=ALU.mult),
                          reads=[acck, "n_c4"], writes=["n_o4"])
                    sc.op("dve", lambda e: e.tensor_tensor(out=oacc[:, tsl, r, :], in0=oacc[:, tsl, r, :], in1=o4[:], op=ALU.add),
                          reads=["n_o4", "n_oacc"], writes=["n_oacc"])
            return epi

        TWO_PI = 6.283185307179586
        for s in range(NSEQ):
            sb = s * 2048
            sc.dma("sp", lambda e, s=s: e.dma_start(out=posi[:], in_=positions[s:s + 1, :].partition_broadcast(16)),
                   writes=["n_ki"])
            sc.op("dve", lambda e: e.tensor_copy(out=ang, in_=posi[:]), reads=["n_ki"], writes=["n_ssq"])
            sc.op("dve", lambda e: e.tensor_scalar(out=ang, in0=ang, scalar1=misc[0:16, 0:1], scalar2=None, op0=ALU.mult),
                  reads=["n_ssq", "n_misc"], writes=["n_ssq"])
            for tab, tkey, shift in ((ST, "n_ST", 0.0), (CT, "n_CT", 1.5707963267948966)):
                sc.op("dve", lambda e, shift=shift: e.tensor_scalar(out=rr, in0=ang, scalar1=shift, scalar2=None, op0=ALU.add),
                      reads=["n_ssq"], writes=["n_ssq"])
                sc.op("dve", lambda e: e.tensor_scalar(out=kf[:], in0=rr, scalar1=1.0 / TWO_PI, scalar2=None, op0=ALU.mult),
                      reads=["n_ssq"], writes=["n_kf"])
                sc.op("dve", lambda e: e.tensor_copy(out=ki[:], in_=kf[:]), reads=["n_kf"], writes=["n_ki"])
                sc.op("dve", lambda e: e.tensor_copy(out=kf[:], in_=ki[:]), reads=["n_ki"], writes=["n_kf"])
                sc.op("dve", lambda e: e.scalar_tensor_tensor(out=rr, in0=kf[:], scalar=-TWO_PI, in1=rr, op0=ALU.mult,
                                                              op1=ALU.add), reads=["n_kf", "n_ssq"], writes=["n_ssq"])
                sc.op("dve", lambda e: e.tensor_scalar(out=kf[:], in0=rr, scalar1=3.141592653589793, scalar2=-TWO_PI,
                                                       op0=ALU.is_gt, op1=ALU.mult), reads=["n_ssq"], writes=["n_kf"])
                sc.op("dve", lambda e: e.tensor_tensor(out=rr, in0=rr, in1=kf[:], op=ALU.add), reads=["n_ssq", "n_kf"], writes=["n_ssq"])
                sc.op("dve", lambda e: e.tensor_scalar(out=kf[:], in0=rr, scalar1=-3.141592653589793, scalar2=TWO_PI,
                                                       op0=ALU.is_lt, op1=ALU.mult), reads=["n_ssq"], writes=["n_kf"])
                sc.op("dve", lambda e: e.tensor_tensor(out=rr, in0=rr, in1=kf[:], op=ALU.add), reads=["n_ssq", "n_kf"], writes=["n_ssq"])
                sc.op("dve", lambda e: e.tensor_scalar(out=rr, in0=rr, scalar1=3.1415925, scalar2=-3.1415925,
                                                       op0=ALU.min, op1=ALU.max), reads=["n_ssq"], writes=["n_ssq"])
                sc.op("act", lambda e, tab=tab: e.activation(out=tab[0:16, :], in_=rr, func=AF.Sin), reads=["n_ssq"], writes=[tkey])
            sc.dma("sp", lambda e, sb=sb: e.dma_start(out=gsig[:], in_=P_tm[sb:sb + 2048, 3328:3352].rearrange("(tt p) c -> p tt c", p=128)),
                   reads=["P_tm"], writes=["n_gsig"])
            sc.op("act", lambda e: e.activation(out=gsig[:], in_=gsig[:], func=AF.Sigmoid), reads=["n_gsig"], writes=["n_gsig"])
            for g in range(2):
                def load_fm(fmidx, half, dstap, dkey, eng="sp", sb=sb):
                    row0 = fmidx * 128 + half * 64
                    sc.dma(eng, lambda e, row0=row0, dstap=dstap, sb=sb: e.dma_start(out=dstap, in_=P_fm[row0:row0 + 64, sb:sb + 2048]),
                           reads=["P_fm"], writes=[dkey])
                for r in range(4):
                    hh = g * 4 + r
                    load_fm(8 + hh // 2, hh % 2, src[:], "n_src")
                    for q2 in range(2):
                        css = [slice(qb * 512, (qb + 1) * 512) for qb in (2 * q2, 2 * q2 + 1)]
                        norm_rope([(src[:, cs], "n_src", 512, 0, CT[:, cs], ST[:, cs], qT[r][0:64, cs], "n_qT%d" % r) for cs in css])
                for fmidx, wc, dst, dkey in ((14, 2, ksT, "n_ksT"), (15, 3, kwT, "n_kwT")):
                    load_fm(fmidx, g, src[:], "n_src")
                    for q2 in range(2):
                        css = [slice(qb * 512, (qb + 1) * 512) for qb in (2 * q2, 2 * q2 + 1)]
                        norm_rope([(src[:, cs], "n_src", 512, wc, CT[:, cs], ST[:, cs], dst[0:64, cs], dkey) for cs in css])
                for col0, va, vk in ((2944, vsa, "n_vsa"), (3200, vwa, "n_vwa")):
                    sc.dma("sp", lambda e, col0=col0, g=g, sb=sb: e.dma_start(
                        out=vld[:], in_=P_tm[sb:sb + 2048, col0 + g * 64:col0 + g * 64 + 64].rearrange("(tt p) c -> p tt c", p=128)),
                        reads=["P_tm"], writes=["n_vld"])
                    sc.op("act", lambda e, va=va: e.copy(out=va[:, :, 0:64], in_=vld[:]), reads=["n_vld"], writes=[vk])
                for kv in range(2):
                    load_fm(12 + kv, g, kcb[:], "n_kcb", eng="pool")
                    for hh in range(2):
                        for l in range(32):
                            sc.op("pe", lambda e, kv=kv, hh=hh, l=l: e.matmul(
                                ps_x[0][:, 0:127], lhsT=w1[kv][:, l, hh * 128:(hh + 1) * 128], rhs=kcb[:, l:l + 2017:16],
                                start=(l == 0), stop=(l == 31)), reads=["n_w1%d" % kv, "n_kcb"], writes=["np_s2"])
                        sc.op("act", lambda e, kv=kv, hh=hh: e.activation(out=hx[:], in_=ps_x[0][:, 0:127], func=AF.Identity,
                                                                          bias=cbias[:, kv, hh:hh + 1]),
                              reads=["np_s2", "n_cbias"], writes=["n_hx"])
                        sc.op("act", lambda e: e.activation(out=hx2[:], in_=hx[:], func=AF.Square), reads=["n_hx"], writes=["n_hx2"])
                        sc.op("dve", lambda e: e.tensor_scalar(out=hx2[:], in0=hx2[:], scalar1=0.044715, scalar2=1.0, op0=ALU.mult,
                                                               op1=ALU.add), reads=["n_hx2"], writes=["n_hx2"])
                        sc.op("dve", lambda e: e.tensor_tensor(out=hx2[:], in0=hx2[:], in1=hx[:], op=ALU.mult),
                              reads=["n_hx2", "n_hx"], writes=["n_hx2"])
                        sc.op("act", lambda e: e.activation(out=hx2[:], in_=hx2[:], func=AF.Sigmoid, scale=1.5957691216057308),
                              reads=["n_hx2"], writes=["n_hx2"])
                        sc.op("dve", lambda e, hh=hh: e.tensor_tensor(out=hT[:, hh, :], in0=hx[:], in1=hx2[:], op=ALU.mult),
                              reads=["n_hx", "n_hx2"], writes=["n_hT"])
                    if kv == 0:
                        for hh in range(2):
                            sc.op("pe", lambda e, hh=hh: e.matmul(ps_x[1][0:64, 0:127], lhsT=w2[0][:, hh, :], rhs=hT[:, hh, :],
                                                                  start=(hh == 0), stop=(hh == 1)),
                                  reads=["n_w20", "n_hT"], writes=["np_s3"])
                        sc.op("act", lambda e: e.copy(out=src[:, 0:127], in_=ps_x[1][0:64, 0:127]), reads=["np_s3"], writes=["n_src"])
                        norm_rope([(src[:, 0:127], "n_src", 127, 1, CT[:, 31:2048:16], ST[:, 31:2048:16], kcmpT[0:64, 0:127], "n_kcmpT")])
                    else:
                        for hh in range(2):
                            sc.op("pe", lambda e, hh=hh: e.matmul(ps_x[1][0:127, 0:64], lhsT=hT[:, hh, :], rhs=w2[1][:, hh, :],
                                                                  start=(hh == 0), stop=(hh == 1)),
                                  reads=["n_w21", "n_hT"], writes=["np_s3"])
                        sc.op("act", lambda e: e.copy(out=vcaug[0:127, 0:64], in_=ps_x[1][0:127, 0:64]), reads=["np_s3"], writes=["n_vcaug"])
                attn(kcmpT, "n_kcmpT", lambda kt: vcaug, "n_vcaug", 97, lambda qb: [0],
                     lambda kt, qb: [(ident_b[0:127, 0:127], cmask[0:127, qb, :], ["ident_b", "n_cmask"])],
                     lambda kt: 127, mk_epi(g, 0, True), krows=96)
                sc.op("dve", lambda e: e.tensor_tensor(out=score[:], in0=imp[:], in1=valid[:], op=ALU.mult),
                      reads=["n_imp", "n_valid"], writes=["n_score"])
                sc.op("dve", lambda e: e.tensor_tensor(out=score[:], in0=score[:], in1=addc[:], op=ALU.add),
                      reads=["n_score", "n_addc"], writes=["n_score"])
                for g4 in range(4):
                    tts = [g4 * 4 + k for k in range(4)]
                    for k, tt in enumerate(tts):
                        sc.op("dve", lambda e, tt=tt, k=k: e.max(out=m8a[:, k, :], in_=score[:, tt, :]),
                              reads=["n_score"], writes=["n_m8a%d" % k])
                    for k, tt in enumerate(tts):
                        sc.op("dve", lambda e, tt=tt, k=k: e.match_replace(out=swk[:, k, :], in_to_replace=m8a[:, k, :],
                                                                           in_values=score[:, tt, :], imm_value=-3e38),
                              reads=["n_score", "n_m8a%d" % k], writes=["n_swk%d" % k])
                    for k, tt in enumerate(tts):
                        sc.op("dve", lambda e, k=k: e.max(out=m8b[:, k, :], in_=swk[:, k, :]),
                              reads=["n_swk%d" % k], writes=["n_m8b%d" % k])
                    for k, tt in enumerate(tts):
                        sc.op("dve", lambda e, tt=tt, k=k: e.tensor_scalar(out=sel[:, k, :], in0=score[:, tt, :], scalar1=m8b[:, k, 7:8],
                                                                           scalar2=None, op0=ALU.is_ge),
                              reads=["n_score", "n_m8b%d" % k], writes=["n_sel%d" % k])
                    sc.op("dve", lambda e: e.tensor_scalar(out=selb[:, :, 64:96], in0=sel[:], scalar1=-NEG, scalar2=NEG, op0=ALU.mult,
                                                           op1=ALU.add), reads=["n_sel%d" % k for k in range(4)], writes=["n_selb"])
                    pst = ps_x[0][:, 0:256].bitcast(BF16).rearrange("p (a b) -> p a b", b=128)
                    for k in range(4):
                        sc.op("pe", lambda e, pst=pst, k=k: e.transpose(out=pst[0:96, k, :], in_=selb[:, k, :], identity=ident_b[:]),
                              reads=["n_selb", "ident_b"], writes=["np_s2"])
                    sc.op("act", lambda e, g4=g4, pst=pst: e.copy(
                        out=nselT[64:96, g4 * 512:(g4 + 1) * 512].rearrange("p (a b) -> p a b", b=128), in_=pst[64:96, :, :]),
                        reads=["np_s2"], writes=["n_nselT"])
                for r in range(4):
                    sc.op("act" if r % 2 == 0 else "dve",
                          (lambda e, r=r: e.copy(out=qT[r][64:96, :], in_=nselT[64:96, :])) if r % 2 == 0 else
                          (lambda e, r=r: e.tensor_copy(out=qT[r][64:96, :], in_=nselT[64:96, :])),
                          reads=["n_nselT"], writes=["n_qT%d" % r])
                attn(ksT, "n_ksT", lambda kt: vsa[:, kt, :], "n_vsa", 65, lambda qb: list(range(0, 4 * qb + 4)),
                     lambda kt, qb: [], lambda kt: 128, mk_epi(g, 1, False), krows=96,
                     post_fn=lambda kt, qb: dm01[:, 4 + kt - 4 * qb, :] if kt >= 4 * qb else None)
                attn(kwT, "n_kwT", lambda kt: vwa[:, kt, :], "n_vwa", 65, lambda qb: list(range(max(0, 4 * qb - 4), 4 * qb + 4)),
                     lambda kt, qb: [], lambda kt: 128, mk_epi(g, 2, False), krows=96,
                     post_fn=lambda kt, qb: dm01[:, 4 + kt - 4 * qb, :])
                o3 = oacc[:].rearrange("p t r d -> p (t r) d")
                sc.op("dve", lambda e: e.tensor_tensor(out=ssq[:], in0=o3, in1=o3, op=ALU.mult), reads=["n_oacc"], writes=["n_ssq"])
                sc.op("dve", lambda e: e.tensor_reduce(out=ss64[:], in_=ssq[:], axis=AX.X, op=ALU.add), reads=["n_ssq"], writes=["n_ss64"])
                sc.op("act", lambda e: e.activation(out=ss64[:], in_=ss64[:], func=AF.Sqrt, scale=1.0 / 64, bias=epsc[:]),
                      reads=["n_ss64"], writes=["n_ss64"])
                sc.op("dve", lambda e: e.reciprocal(out=ss64[:], in_=ss64[:]), reads=["n_ss64"], writes=["n_ss64"])
                sc.op("dve", lambda e: e.tensor_tensor(out=ssq[:], in0=o3, in1=ss64[:].unsqueeze(2).broadcast_to([128, 64, 64]),
                                                       op=ALU.mult), reads=["n_oacc", "n_ss64"], writes=["n_ssq"])
                sc.op("dve", lambda e: e.tensor_tensor(out=ssq[:], in0=ssq[:], in1=onwb[:].unsqueeze(1).broadcast_to([128, 64, 64]),
                                                       op=ALU.mult), reads=["n_ssq", "n_onwb"], writes=["n_ssq"])
                for tt in range(16):
                    sc.dma("sp", lambda e, tt=tt, g=g, sb=sb: e.dma_start(
                        out=Y_tm[sb + tt * 128:sb + (tt + 1) * 128, 512 + g * 256:512 + (g + 1) * 256],
                        in_=ssq[:, tt * 4:(tt + 1) * 4, :].rearrange("p r d -> p (r d)")), reads=["n_ssq"], writes=["Y_tm"])
        barrier(sc)


def stage_uv(nc, sc, u_tab, v_tab, UV):
    with contextlib.ExitStack() as st:
        tmp = [st.enter_context(nc.sbuf_tensor("uv_tmp%d" % i, [128, 8, D], BF16)) for i in range(3)]
        UVv = UV.rearrange("(p r) d -> p r d", p=128)
        k = 0
        for half, tab in enumerate((u_tab, v_tab)):
            tv = tab.rearrange("(p r) d -> p r d", p=128)
            for ci in range(16):
                b = k % 3
                k += 1
                sc.dma("pool", lambda e, b=b, tv=tv, ci=ci: e.dma_start(out=tmp[b][:], in_=tv[:, ci * 8:(ci + 1) * 8, :]),
                       writes=["uv_tmp%d" % b])
                sc.dma("sp", lambda e, b=b, ci=ci, half=half: e.dma_start(
                    out=UVv[:, ci * 8:(ci + 1) * 8, half * D:(half + 1) * D], in_=tmp[b][:]),
                    reads=["uv_tmp%d" % b], writes=["UV"])
        barrier(sc)


def stage_out(nc, sc, stack, NT, x, Y_tm, w_out, out, peer):
    T = lambda name, shape, dt: stack.enter_context(nc.sbuf_tensor(name, shape, dt))
    wout = T("o_wout", [128, 8, D], BF16)
    yt = [T("o_yt%d" % i, [128, D], F32) for i in range(2)]
    xt = [T("o_xt%d" % i, [128, D], F32) for i in range(2)]
    ht = [T("o_ht%d" % i, [128, D], F32) for i in range(3)]
    ybf = T("o_ybf", [128, D], BF16)
    yT = T("o_yT", [128, 8, 128], BF16)
    sc.dma("pool", lambda e: e.dma_start(out=wout[:], in_=w_out.rearrange("(kc p) n -> p kc n", p=128)), writes=["o_wout"])

    def tile_ops(i):
        b = i % 2
        hb = i % 3
        ops = []
        add = lambda *a, **k: ops.append(lambda: sc.op(*a, **k))
        ops.append(lambda: sc.dma("sp", lambda e: e.dma_start(out=yt[b][:], in_=Y_tm[i * 128:(i + 1) * 128, :]),
                                  reads=["Y_tm"], writes=["o_yt%d" % b]))
        ops.append(lambda: sc.dma("sp", lambda e: e.dma_start(out=xt[b][:], in_=x[i * 128:(i + 1) * 128, :]),
                                  writes=["o_xt%d" % b]))
        add("act", lambda e: e.copy(out=ybf[:], in_=yt[b][:]), reads=["o_yt%d" % b], writes=["o_ybf"])
        for kc in range(8):
            add("pe", lambda e, kc=kc: e.transpose(out=peer.ps_t_b[:, kc, :], in_=ybf[:, kc * 128:(kc + 1) * 128],
                                                   identity=peer.ident_b[:]), reads=["o_ybf", "ident_b"], writes=["pp_t"])
        add("act", lambda e: e.copy(out=yT[:], in_=peer.ps_t_b), reads=["pp_t"], writes=["o_yT"])
        for hf in range(2):
            ps = peer.ps_a if hf == 0 else peer.ps_b
            pk = "pp_a" if hf == 0 else "pp_b"
            for kc in range(8):
                add("pe", lambda e, hf=hf, kc=kc, ps=ps: e.matmul(ps[:].rearrange("p a b -> p (a b)"), lhsT=yT[:, kc, :],
                                                                  rhs=wout[:, kc, hf * 512:(hf + 1) * 512],
                                                                  start=(kc == 0), stop=(kc == 7)),
                    reads=["o_yT", "o_wout"], writes=[pk])
            add("dve", lambda e, hf=hf, ps=ps: e.tensor_tensor(
                out=ht[hb][:, hf * 512:(hf + 1) * 512], in0=ps[:].rearrange("p a b -> p (a b)"),
                in1=xt[b][:, hf * 512:(hf + 1) * 512], op=ALU.add), reads=[pk, "o_xt%d" % b], writes=["o_ht%d" % hb])
        return ops + peer.pre_ops(i, ht[hb][:], "o_ht%d" % hb)

    for f in tile_ops(0):
        f()
    for i in range(NT):
        pending = tile_ops(i + 1) if i + 1 < NT else []
        peer.loop(i, ht[i % 3][:], "o_ht%d" % (i % 3), out[i * 128:(i + 1) * 128, :], "out", pending)


WNAMES = ["norm1_w", "w_in", "hg_lb_logits", "hg_out_norm_w", "nsa_q_norm_w", "nsa_k_norm_w", "cmp_pe_k", "cmp_pe_v",
          "cmp_w1_k", "cmp_w2_k", "cmp_w1_v", "cmp_w2_v", "nsa_out_norm_w", "w_out", "norm2_w", "peer_w_q",
          "peer_sub_keys", "peer_u", "peer_v"]
WSHAPES = {"norm1_w": [1, D], "w_in": [D, INW], "hg_lb_logits": [2, 512], "hg_out_norm_w": [1, 128],
           "nsa_q_norm_w": [1, 64], "nsa_k_norm_w": [3, 64], "cmp_pe_k": [32, 64], "cmp_pe_v": [32, 64],
           "cmp_w1_k": [2048, 256], "cmp_w2_k": [256, 64], "cmp_w1_v": [2048, 256], "cmp_w2_v": [256, 64],
           "nsa_out_norm_w": [1, 64], "w_out": [D, D], "norm2_w": [1, D], "peer_w_q": [D, 2048],
           "peer_sub_keys": [2, 128, 128], "peer_u": [16384, D], "peer_v": [16384, D]}


def build_program(NSEQ):
    NT = NSEQ * 16
    nc = bass.Bass("TRN2", target_bir_lowering=False)
    din = lambda name, shape, dt=F32: nc.dram_tensor(name, shape, dt, kind="ExternalInput").ap()
    x = din("x", [NT * 128, D])
    positions = din("positions", [NSEQ, 2048], I32)
    w = {k: din(k, v) for k, v in WSHAPES.items()}
    cst = {k: din(k, v) for k, v in CONST_SHAPES.items()}
    out = nc.dram_tensor("out", [NT * 128, D], F32, kind="ExternalOutput").ap()
    P_tm = nc.dram_tensor("P_tm", [NT * 128, INW], F32, kind="Internal").ap()
    P_fm = nc.dram_tensor("P_fm", [16 * 128, NT * 128], F32, kind="Internal").ap()
    Y_tm = nc.dram_tensor("Y_tm", [NT * 128, D], F32, kind="Internal").ap()
    UV = nc.dram_tensor("UV", [16384, 2 * D], BF16, kind="Internal").ap()
    with contextlib.ExitStack() as stack:
        sc = Sched(nc, stack)
        T = lambda name, shape, dt: stack.enter_context(nc.sbuf_tensor(name, shape, dt))
        ident_f = T("ident_f", [128, 128], F32)
        ident_b = T("ident_b", [128, 128], BF16)
        ublk = T("ublk", [128, 128], F32)
        wrev = T("wrev", [128, 128], F32)
        epsc = T("epsc", [128, 1], F32)
        pc = T("pc", [128, 64], F32)
        sc.dma("sp", lambda e: e.dma_start(out=ident_f[:], in_=cst["c_identf"]), writes=["ident_f"])
        sc.dma("pool", lambda e: e.dma_start(out=ident_b[:], in_=cst["c_identf"]), writes=["ident_b"])
        sc.dma("sp", lambda e: e.dma_start(out=ublk[:], in_=cst["c_ublk"]), writes=["ublk"])
        sc.dma("sp", lambda e: e.dma_start(out=wrev[:], in_=cst["c_wrev"]), writes=["wrev"])
        sc.dma("sp", lambda e: e.dma_start(out=pc[:], in_=cst["c_pc"]), writes=["pc"])
        sc.op("dve", lambda e: e.memset(epsc[:], EPS), writes=["epsc"])
        barrier(sc)
        stage_proj(nc, sc, NT, x, w["norm1_w"], w["w_in"], P_tm, P_fm, ident_b, epsc,
                   uv=(w["peer_u"], w["peer_v"], UV))
        stage_hgrn(nc, sc, NT, P_tm, P_fm, Y_tm, w["hg_lb_logits"], w["hg_out_norm_w"], ublk, wrev, epsc,
                   cst["c_rowm"], cst["c_colm"])
        stage_nsa(nc, sc, NSEQ, P_tm, P_fm, Y_tm, positions, w["nsa_q_norm_w"], w["nsa_k_norm_w"], w["cmp_pe_k"],
                  w["cmp_pe_v"], w["cmp_w1_k"], w["cmp_w2_k"], w["cmp_w1_v"], w["cmp_w2_v"], w["nsa_out_norm_w"],
                  cst, ident_b, epsc)
        with contextlib.ExitStack() as st2:
            peer = Peer(nc, sc, st2, w["norm2_w"], w["peer_w_q"], w["peer_sub_keys"], UV, ident_f, ident_b, pc, cst["c_wsel"])
            stage_out(nc, sc, st2, NT, x, Y_tm, w["w_out"], out, peer)
            sc.finish()
            with nc.Block() as block:
                sc.replay(block)
    return nc


def make_inputs(inputs, c0, c1):
    m = {"x": np.ascontiguousarray(inputs["x"][c0:c1]).reshape(-1, D).astype(np.float32, copy=False),
         "positions": np.ascontiguousarray(inputs["positions"][c0:c1]).astype(np.int32, copy=False)}
    for k in WNAMES:
        a = np.asarray(inputs[k])
        if k != "hg_lb_logits":
            a = a[0]
        m[k] = np.ascontiguousarray(a, dtype=np.float32).reshape(WSHAPES[k])
    return m


def kernel(**inputs):
    ncores = 8
    B = inputs["x"].shape[0]
    per = B // ncores
    nc = build_program(per)
    consts = host_consts()
    in_maps = []
    for c in range(ncores):
        m = make_inputs(inputs, c * per, (c + 1) * per)
        m.update(consts)
        in_maps.append(m)
    res = run_bass_kernel_spmd(nc, in_maps, core_ids=list(range(ncores)))
    outs = [np.asarray(r["out"]).reshape(per, S, D) for r in res.results]
    return np.concatenate(outs, axis=0).astype(np.float32, copy=False)
```

```python
import contextlib
import numpy as np
import ml_dtypes
import concourse.bass as bass
import concourse.mybir as mybir
from concourse.bass_utils import run_bass_kernel_spmd

F32 = mybir.dt.float32
BF16 = mybir.dt.bfloat16
I32 = mybir.dt.int32
U32 = mybir.dt.uint32
AF = mybir.ActivationFunctionType
ALU = mybir.AluOpType
AX = mybir.AxisListType

ENGS = ("pe", "dve", "act", "pool", "sp")
NEG = -30000.0


class Sched:
    def __init__(self, nc, stack, ndma=12):
        self.nc = nc
        self.q = {e: [] for e in ENGS}
        self.cnt = {e: 0 for e in ENGS}
        self.esem = {e: stack.enter_context(nc.semaphore("es_" + e)) for e in ENGS}
        self.ndma = ndma
        self.dsem = {e: [stack.enter_context(nc.semaphore("ds_%s_%d" % (e, i))) for i in range(ndma)]
                     for e in ("sp", "pool", "act")}
        self.dcnt = {e: 0 for e in ("sp", "pool", "act")}
        self.seen = {e: {} for e in ENGS}
        self.st = {}
        self.ninst = 0

    def _sem(self, key):
        if key[0] == "dma":
            return self.dsem[key[1]][key[2]]
        return self.esem[key[0]]

    def _wait(self, eng, ev):
        key, val = ev
        if self.seen[eng].get(key, 0) >= val:
            return
        self.seen[eng][key] = val
        sem = self._sem(key)
        self.q[eng].append(lambda e, sem=sem, val=val: e.wait_ge(sem, val))
        self.ninst += 1

    def _deps(self, eng, reads, writes):
        deps = []
        for k in reads:
            s = self.st.get(k)
            if s and s[0] is not None:
                deps.append(s[0])
            if s and k[1:3] == "p_":
                deps.extend(ev for ek, ev in s[1].items() if ek != (eng,))
        for k in writes:
            s = self.st.get(k)
            if s:
                if s[0] is not None:
                    deps.append(s[0])
                deps.extend(s[1].values())
        for ev in deps:
            if eng == "pe" and ev[0] == ("pe",):
                continue
            self._wait(eng, ev)

    def _commit(self, ev, evkey, reads, writes):
        for k in reads:
            s = self.st.setdefault(k, [None, {}])
            s[1][evkey] = ev
        for k in writes:
            self.st[k] = [ev, {}]

    def op(self, eng, fn, reads=(), writes=()):
        self._deps(eng, reads, writes)
        self.cnt[eng] += 1
        sem = self.esem[eng]
        self.q[eng].append(lambda e, fn=fn, sem=sem: fn(e).then_inc(sem, 1))
        self.ninst += 1
        ev = ((eng,), self.cnt[eng])
        self._commit(ev, (eng,), reads, writes)

    def dma(self, eng, fn, reads=(), writes=()):
        self._deps(eng, reads, writes)
        k = self.dcnt[eng]
        slot = k % self.ndma
        key = ("dma", eng, slot)
        if k >= self.ndma:
            self._wait(eng, (key, 16 * (k // self.ndma)))
        self.dcnt[eng] += 1
        val = 16 * (k // self.ndma + 1)
        sem = self.dsem[eng][slot]
        self.q[eng].append(lambda e, fn=fn, sem=sem: fn(e).then_inc(sem, 16))
        self.ninst += 1
        ev = (key, val)
        self._commit(ev, key, reads, writes)

    def finish(self):
        for eng in ("sp", "pool", "act"):
            k = self.dcnt[eng]
            for slot in range(min(k, self.ndma)):
                n = (k - 1 - slot) // self.ndma + 1
                self._wait(eng, (("dma", eng, slot), 16 * n))

    def replay(self, block):
        q = self.q

        @block.sync
        def _(e):
            for f in q["sp"]:
                f(e)

        @block.gpsimd
        def _(e):
            for f in q["pool"]:
                f(e)

        @block.tensor
        def _(e):
            for f in q["pe"]:
                f(e)

        @block.scalar
        def _(e):
            for f in q["act"]:
                f(e)

        @block.vector
        def _(e):
            for f in q["dve"]:
                f(e)


D = 1024
S = 2048
PEER_HEADS = 8
PEER_K = 16
EPS = 1e-6


def peer_consts():
    c = np.zeros((128, 64), np.float32)
    c[:, 0:16] = np.arange(16, dtype=np.float32)[None, :] * 16.0
    c[:, 16:32] = np.arange(16, dtype=np.float32)[None, :]
    return c


class Peer:
    def __init__(self, nc, sc, stack, norm2_w, w_q, sub_keys, uv_tab, ident_f, ident_b, pc, wsel_dram, NB=10):
        self.nc, self.sc = nc, sc
        self.uv_tab = uv_tab
        self.NB = NB
        T = lambda name, shape, dt: stack.enter_context(nc.sbuf_tensor(name, shape, dt))
        P = lambda name, shape, dt: stack.enter_context(nc.psum_tensor(name, shape, dt))
        self.ident_f, self.ident_b, self.pc = ident_f, ident_b, pc
        self.w2b = T("pr_w2b", [128, D], F32)
        self.wq = T("pr_wq", [128, 4, 2, 2048], BF16)
        self.skT = T("pr_skT", [128, 2, 128], BF16)
        self.skl = T("pr_skl", [128, 2, 128], F32)
        self.junk = T("pr_junk", [128, D], BF16)
        self.ss = T("pr_ss", [128, 1], F32)
        self.rstd = T("pr_rstd", [128, 1], F32)
        self.xn = T("pr_xn", [128, D], BF16)
        self.xnT = [T("pr_xnT%d" % i, [128, 4, 128, 2], BF16) for i in range(2)]
        self.qT = T("pr_qT", [128, 16, 128], BF16)
        self.Ssb = T("pr_S", [128, 16, 128], F32)
        self.Swk = T("pr_Swk", [128, 128], F32)
        self.v = T("pr_v", [128, 16, 16], F32)
        self.ix = T("pr_ix", [128, 16, 16], U32)
        self.ixf = T("pr_ixf", [128, 16, 16], F32)
        self.cand = T("pr_cand", [128, 8, 256], F32)
        self.cwk = T("pr_cwk", [128, 256], F32)
        self.tv = T("pr_tv", [128, 8, 16], F32)
        self.pos = T("pr_pos", [128, 8, 16], U32)
        self.posa = T("pr_posa", [128, 8, 16], U32)
        self.posb = T("pr_posb", [128, 8, 16], U32)
        self.epsc = T("pr_epsc", [128, 1], F32)
        self.pb = T("pr_pb", [128, 8, 16], F32)
        self.pa = T("pr_pa", [128, 8, 16], F32)
        self.eq = T("pr_eq", [128, 8, 16, 16], F32)
        self.e1 = T("pr_e1", [128, 8, 16], F32)
        self.e2 = T("pr_e2", [128, 8, 16], F32)
        self.gt = T("pr_gt", [128, 8, 16], F32)
        self.gs = T("pr_gs", [128, 8], F32)
        self.eTi = [T("pr_eTi%d" % i, [128, 128], I32) for i in range(2)]
        self.eTf = T("pr_eTf", [128, 128], F32)
        self.gT = [T("pr_gT%d" % i, [128, 128], F32) for i in range(2)]
        self.actb = T("pr_actb", [128, 128], BF16)
        self.oT = T("pr_oT", [128, 8, 128], F32)
        self.osb = T("pr_osb", [128, D], F32)
        self.G = [T("pr_G%d" % i, [128, 2 * D], BF16) for i in range(NB)]
        self.GT = [T("pr_GT%d" % i, [128, 4, 128, 2], BF16) for i in range(2)]
        self.hg = T("pr_hg", [128, 128], F32)
        self.wsel = T("pr_wsel", [128, 256], BF16)
        self.actD = [T("pr_actD%d" % i, [128, 128], BF16) for i in range(3)]
        self.ps_a = P("pp_a", [128, 4, 128], F32)
        self.ps_b = P("pp_b", [128, 4, 128], F32)
        self.ps_t = P("pp_t", [128, 4, 128], F32)
        self.ps_t_b = self.ps_t[:].rearrange("p a b -> p (a b)").bitcast(BF16).rearrange("p (a b) -> p a b", b=128)
        self.ps_g = [P("pp_g%d" % i, [128, 4, 128], F32) for i in range(2)]
        self.ps_hd = P("pp_hd", [128, 512], F32)
        self.ps_o = P("pp_o", [128, 8, 128], F32)
        self.tok = 0

        sc.op("dve", lambda e: e.memset(self.epsc[:], EPS), writes=["pr_epsc"])
        sc.dma("pool", lambda e: e.dma_start(out=self.wsel[:], in_=wsel_dram), writes=["pr_wsel"])
        sc.dma("sp", lambda e: e.dma_start(out=self.w2b[:], in_=norm2_w[0:1, :].partition_broadcast(128)),
               writes=["pr_w2b"])
        sc.dma("pool", lambda e: e.dma_start(out=self.wq[:], in_=w_q.rearrange("(c dp two) n -> dp c two n", dp=128, two=2)),
               writes=["pr_wq"])
        sc.dma("sp", lambda e: e.dma_start(out=self.skl[:], in_=sub_keys.rearrange("j n d -> n j d")),
               writes=["pr_skl"])
        for j in range(2):
            sc.op("pe", lambda e, j=j: e.transpose(out=self.ps_a[:, j, :], in_=self.skl[:, j, :], identity=ident_f[:]),
                  reads=["pr_skl", "ident_f"], writes=["pp_a"])
        sc.op("act", lambda e: e.copy(out=self.skT[:], in_=self.ps_a[:, 0:2, :]), reads=["pp_a"], writes=["pr_skT"])

    def pre_ops(self, i, h_t, hkey):
        sc = self.sc
        p = i % 2
        ops = []
        add = lambda *a, **k: ops.append(lambda: sc.op(*a, **k))
        xnT, eTi, gT = self.xnT[p], self.eTi[p], self.gT[p]
        kxnT, keTi, kgT = "pr_xnT%d" % p, "pr_eTi%d" % p, "pr_gT%d" % p
        add("act", lambda e: e.activation(out=self.junk[:], in_=h_t, func=AF.Square, accum_out=self.ss[:]),
            reads=[hkey], writes=["pr_junk", "pr_ss"])
        add("act", lambda e: e.activation(out=self.rstd[:], in_=self.ss[:], func=AF.Sqrt, scale=1.0 / D, bias=self.epsc[:]),
            reads=["pr_ss"], writes=["pr_rstd"])
        add("dve", lambda e: e.reciprocal(out=self.rstd[:], in_=self.rstd[:]), reads=["pr_rstd"], writes=["pr_rstd"])
        add("dve", lambda e: e.scalar_tensor_tensor(out=self.xn[:], in0=h_t, scalar=self.rstd[:, 0:1],
                                                    in1=self.w2b[:], op0=ALU.mult, op1=ALU.mult),
            reads=[hkey, "pr_rstd", "pr_w2b"], writes=["pr_xn"])
        xnf = self.xn[:].bitcast(F32)
        for c in range(4):
            add("pe", lambda e, c=c: e.transpose(out=self.ps_t[:, c, :], in_=xnf[:, c * 128:(c + 1) * 128],
                                                 identity=self.ident_f[:]),
                reads=["pr_xn", "ident_f"], writes=["pp_t"])
        add("act", lambda e: e.copy(out=xnT[:].rearrange("p c t two -> p (c t two)"),
                                    in_=self.ps_t_b.rearrange("p a b -> p (a b)")), reads=["pp_t"], writes=[kxnT])
        for grp in range(4):
            ps = self.ps_a if grp % 2 == 0 else self.ps_b
            pk = "pp_a" if grp % 2 == 0 else "pp_b"
            for c4 in range(4):
                cq = grp * 4 + c4
                for kc in range(8):
                    add("pe", lambda e, ps=ps, c4=c4, cq=cq, kc=kc: e.matmul(
                        ps[:, c4, :], lhsT=self.wq[:, kc // 2, kc % 2, cq * 128:(cq + 1) * 128], rhs=xnT[:, kc // 2, :, kc % 2],
                        start=(kc == 0), stop=(kc == 7)), reads=["pr_wq", kxnT], writes=[pk])
            add("act", lambda e, ps=ps, grp=grp: e.copy(out=self.qT[:, grp * 4:(grp + 1) * 4, :], in_=ps[:]),
                reads=[pk], writes=["pr_qT%d" % grp])
        for grp in range(4):
            ps = self.ps_a if grp % 2 == 0 else self.ps_b
            pk = "pp_a" if grp % 2 == 0 else "pp_b"
            for c4 in range(4):
                cq = grp * 4 + c4
                add("pe", lambda e, ps=ps, c4=c4, cq=cq: e.matmul(
                    ps[:, c4, :], lhsT=self.qT[:, cq, :], rhs=self.skT[:, cq % 2, :], start=True, stop=True),
                    reads=["pr_qT%d" % grp, "pr_skT"], writes=[pk])
            add("act", lambda e, ps=ps, grp=grp: e.copy(out=self.Ssb[:, grp * 4:(grp + 1) * 4, :], in_=ps[:]),
                reads=[pk], writes=["pr_S%d" % grp])
        for cq in range(16):
            sk = "pr_S%d" % (cq // 4)
            add("dve", lambda e, cq=cq: e.max(out=self.v[:, cq, 0:8], in_=self.Ssb[:, cq, :]), reads=[sk], writes=["pr_v"])
            add("dve", lambda e, cq=cq: e.max_index(out=self.ix[:, cq, 0:8], in_max=self.v[:, cq, 0:8],
                                                    in_values=self.Ssb[:, cq, :]), reads=[sk, "pr_v"], writes=["pr_ix"])
            add("dve", lambda e, cq=cq: e.match_replace(out=self.Swk[:], in_to_replace=self.v[:, cq, 0:8],
                                                        in_values=self.Ssb[:, cq, :], imm_value=-1e30),
                reads=[sk, "pr_v"], writes=["pr_Swk"])
            add("dve", lambda e, cq=cq: e.max(out=self.v[:, cq, 8:16], in_=self.Swk[:]), reads=["pr_Swk"], writes=["pr_v"])
            add("dve", lambda e, cq=cq: e.max_index(out=self.ix[:, cq, 8:16], in_max=self.v[:, cq, 8:16],
                                                    in_values=self.Swk[:]), reads=["pr_Swk", "pr_v"], writes=["pr_ix"])
        add("dve", lambda e: e.tensor_copy(out=self.ixf[:], in_=self.ix[:]), reads=["pr_ix"], writes=["pr_ixf"])
        for h in range(8):
            add("dve", lambda e, h=h: e.tensor_tensor(
                out=self.cand[:, h, :].rearrange("p (a b) -> p a b", b=16),
                in0=self.v[:, 2 * h, :].unsqueeze(2).broadcast_to([128, 16, 16]),
                in1=self.v[:, 2 * h + 1, :].unsqueeze(1).broadcast_to([128, 16, 16]), op=ALU.add),
                reads=["pr_v"], writes=["pr_cand"])
        for h in range(8):
            add("dve", lambda e, h=h: e.max(out=self.tv[:, h, 0:8], in_=self.cand[:, h, :]), reads=["pr_cand"], writes=["pr_tv"])
            add("dve", lambda e, h=h: e.max_index(out=self.pos[:, h, 0:8], in_max=self.tv[:, h, 0:8],
                                                  in_values=self.cand[:, h, :]), reads=["pr_cand", "pr_tv"], writes=["pr_pos"])
            add("dve", lambda e, h=h: e.match_replace(out=self.cwk[:], in_to_replace=self.tv[:, h, 0:8],
                                                      in_values=self.cand[:, h, :], imm_value=-1e30),
                reads=["pr_cand", "pr_tv"], writes=["pr_cwk"])
            add("dve", lambda e, h=h: e.max(out=self.tv[:, h, 8:16], in_=self.cwk[:]), reads=["pr_cwk"], writes=["pr_tv"])
            add("dve", lambda e, h=h: e.max_index(out=self.pos[:, h, 8:16], in_max=self.tv[:, h, 8:16],
                                                  in_values=self.cwk[:]), reads=["pr_cwk", "pr_tv"], writes=["pr_pos"])
        add("dve", lambda e: e.tensor_scalar(out=self.posb[:], in0=self.pos[:], scalar1=15, scalar2=None,
                                             op0=ALU.bitwise_and), reads=["pr_pos"], writes=["pr_posb"])
        add("dve", lambda e: e.tensor_scalar(out=self.posa[:], in0=self.pos[:], scalar1=240, scalar2=None,
                                             op0=ALU.bitwise_and), reads=["pr_pos"], writes=["pr_posa"])
        add("dve", lambda e: e.tensor_copy(out=self.pb[:], in_=self.posb[:]), reads=["pr_posb"], writes=["pr_pb"])
        add("dve", lambda e: e.tensor_copy(out=self.pa[:], in_=self.posa[:]), reads=["pr_posa"], writes=["pr_pa"])
        eq3 = self.eq[:].rearrange("p h k a -> p (h k) a")
        for which, (src, c0, j, dst) in enumerate(((self.pa, 0, 0, self.e1), (self.pb, 16, 1, self.e2))):
            add("dve", lambda e, src=src, c0=c0: e.tensor_tensor(
                out=eq3, in0=src[:].rearrange("p h k -> p (h k)").unsqueeze(2).broadcast_to([128, 128, 16]),
                in1=self.pc[:, c0:c0 + 16].unsqueeze(1).broadcast_to([128, 128, 16]), op=ALU.is_equal),
                reads=["pr_pa", "pr_pb", "pc"], writes=["pr_eq"])
            for h in range(8):
                add("dve", lambda e, j=j, h=h: e.tensor_tensor(
                    out=self.eq[:, h, :, :], in0=self.eq[:, h, :, :],
                    in1=self.ixf[:, 2 * h + j, :].unsqueeze(1).broadcast_to([128, 16, 16]),
                    op=ALU.mult), reads=["pr_eq", "pr_ixf"], writes=["pr_eq"])
            add("dve", lambda e, dst=dst: e.tensor_reduce(out=dst[:].rearrange("p h k -> p (h k)"), in_=eq3,
                                                          axis=AX.X, op=ALU.add),
                reads=["pr_eq"], writes=["pr_e%d" % (which + 1)])
        add("dve", lambda e: e.scalar_tensor_tensor(out=self.e1[:], in0=self.e1[:], scalar=128.0, in1=self.e2[:],
                                                    op0=ALU.mult, op1=ALU.add),
            reads=["pr_e1", "pr_e2"], writes=["pr_e1"])
        add("dve", lambda e: e.tensor_tensor(out=self.gt[:], in0=self.tv[:],
                                             in1=self.tv[:, :, 0:1].broadcast_to([128, 8, 16]), op=ALU.subtract),
            reads=["pr_tv"], writes=["pr_gt"])
        add("act", lambda e: e.activation(out=self.gt[:], in_=self.gt[:], func=AF.Exp), reads=["pr_gt"], writes=["pr_gt"])
        add("dve", lambda e: e.tensor_reduce(out=self.gs[:], in_=self.gt[:], axis=AX.X, op=ALU.add),
            reads=["pr_gt"], writes=["pr_gs"])
        add("dve", lambda e: e.reciprocal(out=self.gs[:], in_=self.gs[:]), reads=["pr_gs"], writes=["pr_gs"])
        add("dve", lambda e: e.tensor_tensor(out=self.gt[:], in0=self.gt[:],
                                             in1=self.gs[:].unsqueeze(2).broadcast_to([128, 8, 16]), op=ALU.mult),
            reads=["pr_gt", "pr_gs"], writes=["pr_gt"])
        add("pe", lambda e: e.transpose(out=self.ps_a[:, 0, :], in_=self.e1[:].rearrange("p h k -> p (h k)"),
                                        identity=self.ident_f[:]), reads=["pr_e1", "ident_f"], writes=["pp_a"])
        add("pe", lambda e: e.transpose(out=self.ps_a[:, 1, :], in_=self.gt[:].rearrange("p h k -> p (h k)"),
                                        identity=self.ident_f[:]), reads=["pr_gt", "ident_f"], writes=["pp_a"])
        add("act", lambda e: e.copy(out=self.eTf[:], in_=self.ps_a[:, 0, :]), reads=["pp_a"], writes=["pr_eTf"])
        add("dve", lambda e: e.tensor_copy(out=eTi[:], in_=self.eTf[:]), reads=["pr_eTf"], writes=[keTi])
        add("act", lambda e: e.copy(out=gT[:], in_=self.ps_a[:, 1, :]), reads=["pp_a"], writes=[kgT])
        return ops

    def loop(self, i, h_t, hkey, out_ap, outkey, pending):
        sc = self.sc
        p = i % 2
        xnT, eTi, gT = self.xnT[p], self.eTi[p], self.gT[p]
        kxnT, keTi, kgT = "pr_xnT%d" % p, "pr_eTi%d" % p, "pr_gT%d" % p
        NB = self.NB
        per = (len(pending) + 119) // 120 if pending else 0
        bufs, gbs = {}, {}

        def e1(t):
            b = self.tok % NB
            g2 = self.tok % 2
            self.tok += 1
            bufs[t], gbs[t] = b, g2
            gk = "pr_G%d" % b
            sc.dma("pool", lambda e, b=b, t=t: e.indirect_dma_start(
                out=self.G[b][:], out_offset=None, in_=self.uv_tab,
                in_offset=bass.IndirectOffsetOnAxis(ap=eTi[:, t:t + 1], axis=0)), reads=[keTi], writes=[gk])
            Gf = self.G[b][:, 0:D].bitcast(F32)
            for c in range(4):
                sc.op("pe", lambda e, Gf=Gf, c=c, g2=g2: e.transpose(
                    out=self.ps_g[g2][:, c, :], in_=Gf[:, c * 128:(c + 1) * 128], identity=self.ident_f[:]),
                    reads=[gk, "ident_f"], writes=["pp_g%d" % g2])
            sc.op("act", lambda e, g2=g2: e.copy(
                out=self.GT[g2][:].rearrange("p c s two -> p (c s two)"),
                in_=self.ps_g[g2][:].rearrange("p a b -> p (a b)").bitcast(BF16)),
                reads=["pp_g%d" % g2], writes=["pr_GT%d" % g2])

        def e2(t):
            g2 = gbs[t]
            for kc in range(8):
                sc.op("pe", lambda e, kc=kc, g2=g2, t=t: e.matmul(
                    self.ps_hd[:, t:t + 1], lhsT=self.GT[g2][:, kc // 2, :, kc % 2], rhs=xnT[:, kc // 2, t:t + 1, kc % 2],
                    start=(kc == 0), stop=(kc == 7)), reads=["pr_GT%d" % g2, kxnT], writes=["pp_hd"])
            sc.op("act", lambda e, t=t: e.activation(out=self.hg[:, t:t + 1], in_=self.ps_hd[:, t:t + 1], func=AF.Gelu),
                  reads=["pp_hd"], writes=["pr_hg%d" % (t % 4)])
            sc.op("dve", lambda e, t=t: e.tensor_scalar(out=self.actD[t % 3][:], in0=self.wsel[:, 127 - t:255 - t],
                                                        scalar1=self.hg[:, t:t + 1], scalar2=gT[:, t:t + 1],
                                                        op0=ALU.mult, op1=ALU.mult),
                  reads=["pr_hg%d" % (t % 4), kgT, "pr_wsel"], writes=["pr_actD%d" % (t % 3)])

        def e3(t):
            b = bufs[t]
            po = self.ps_o[:].rearrange("p a b -> p (a b)")
            for hf in range(2):
                sc.op("pe", lambda e, b=b, t=t, hf=hf, po=po: e.matmul(
                    po[:, hf * 512:(hf + 1) * 512], lhsT=self.actD[t % 3][:], rhs=self.G[b][:, D + hf * 512:D + (hf + 1) * 512],
                    start=(t == 0), stop=(t == 127)), reads=["pr_G%d" % b, "pr_actD%d" % (t % 3)], writes=["pp_o"])

        e1(0)
        for t in range(-1, 129):
            if 0 <= t + 1 < 128 and t + 1 > 0:
                e1(t + 1)
            if 0 <= t < 128:
                e2(t)
            if 0 <= t - 1 < 128:
                e3(t - 1)
            for _ in range(per):
                if pending:
                    pending.pop(0)()
        while pending:
            pending.pop(0)()
        po = self.ps_o[:].rearrange("p a b -> p (a b)")
        for hf in range(2):
            sc.op("dve", lambda e, hf=hf, po=po: e.tensor_tensor(
                out=self.osb[:, hf * 512:(hf + 1) * 512], in0=po[:, hf * 512:(hf + 1) * 512],
                in1=h_t[:, hf * 512:(hf + 1) * 512], op=ALU.add), reads=["pp_o", hkey], writes=["pr_osb"])
        sc.dma("sp", lambda e: e.dma_start(out=out_ap, in_=self.osb[:]), reads=["pr_osb"], writes=[outkey])


FMCH = [0, 1, 2, 3, 4, 5, 6, 7, 16, 17, 18, 19, 20, 21, 22, 24]
INW = 3352
DELTAS = [-512, -384, -256, -128, 0, 128, 256, 384]


def host_consts():
    bf = ml_dtypes.bfloat16
    c = {}
    c["c_identf"] = np.eye(128, dtype=np.float32)
    c["c_pc"] = peer_consts()
    wsel = np.zeros((128, 256), np.float32)
    wsel[:, 127] = 1.0
    c["c_wsel"] = wsel
    s = np.arange(128)[:, None]
    t = np.arange(128)[None, :]
    same = (s // 32) == (t // 32)
    c["c_ublk"] = (same & (s <= t)).astype(np.float32)
    c["c_wrev"] = (same & (s > t)).astype(np.float32)
    c["c_rowm"] = (np.arange(128)[:, None] // 32 == np.arange(4)[None, :]).astype(np.float32)
    c["c_colm"] = np.broadcast_to((np.arange(128)[None, None, :] // 32 == np.arange(4)[None, :, None]), (128, 4, 128)).astype(np.float32).copy()
    inv = (500000.0 ** (-np.arange(0, 16, 2, dtype=np.float32) / 16)).astype(np.float32)
    misc = np.zeros((128, 8), np.float32)
    misc[0:16, 0] = np.concatenate([inv, inv])
    misc[:, 1] = np.tile(np.concatenate([inv, inv]), 8)
    c["c_misc"] = misc
    rm = np.zeros((64, 64), np.float32)
    for d in range(8):
        rm[d + 8, d] = -1.0
        rm[d, d + 8] = 1.0
    c["c_rm"] = rm
    ss = np.arange(128)[:, None]
    tt = np.arange(512)[None, :]
    dm = np.zeros((128, 8, 512), np.float32)
    for i, dl in enumerate(DELTAS):
        ok = (dl + ss <= tt) & (tt - ss - dl < 512)
        dm[:, i, :] = np.where(ok, 0.0, NEG)
    c["c_dmask"] = dm
    cm = np.zeros((128, 4, 512), np.float32)
    cc = np.arange(128)[:, None]
    for qb in range(4):
        ok = (16 * cc + 31) <= (qb * 512 + tt)
        cm[:, qb, :] = np.where(ok, 0.0, NEG)
    c["c_cmask"] = cm
    em = np.zeros((32, 2048), np.float32)
    em[np.arange(2048) // 64, np.arange(2048)] = 1.0
    c["c_emat"] = em
    c0 = np.arange(127)[:, None] * 16
    j0 = np.arange(32)[None, :] * 64
    ov = np.clip(np.minimum(c0 + 32, j0 + 64) - np.maximum(c0, j0), 0, None) / 32.0
    va = np.zeros((128, 33), np.float32)
    va[:, 0] = 1.0
    va[:127, 1:] = ov
    c["c_vaug"] = va
    tok = (np.arange(16)[None, :, None] * 128 + np.arange(128)[:, None, None])
    cur = tok // 64
    j = np.arange(32)[None, None, :]
    forced = (j == 0) | (j == cur) | (j == cur - 1)
    valid = (j <= cur) & ~forced
    c["c_valid"] = valid.astype(np.float32)
    c["c_addc"] = np.where(forced, 1e30, np.where(valid, 0.0, -1e30)).astype(np.float32)
    return c


CONST_SHAPES = {"c_identf": [128, 128], "c_pc": [128, 64], "c_wsel": [128, 256], "c_ublk": [128, 128], "c_wrev": [128, 128],
                "c_misc": [128, 8], "c_rowm": [128, 4], "c_colm": [128, 4, 128], "c_rm": [64, 64], "c_dmask": [128, 8, 512], "c_cmask": [128, 4, 512],
                "c_emat": [32, 2048], "c_vaug": [128, 33], "c_valid": [128, 16, 32], "c_addc": [128, 16, 32]}


def barrier(sc):
    for e in ("sp", "pool", "act"):
        k = sc.dcnt[e]
        for slot in range(min(k, sc.ndma)):
            n = (k - 1 - slot) // sc.ndma + 1
            for eng in ENGS:
                sc._wait(eng, (("dma", e, slot), 16 * n))
    for eng in ENGS:
        for e2 in ("pe", "dve", "act", "pool"):
            if sc.cnt[e2] > 0 and not (eng == "pe" and e2 == "pe"):
                sc._wait(eng, ((e2,), sc.cnt[e2]))
    sc.st = {}


def rms_rstd(sc, src_ap, srckey, junk, ss, rstd, epsc, n, pfx):
    sc.op("act", lambda e: e.activation(out=junk, in_=src_ap, func=AF.Square, accum_out=ss),
          reads=[srckey], writes=[pfx + "junk", pfx + "ss"])
    sc.op("act", lambda e: e.activation(out=rstd, in_=ss, func=AF.Sqrt, scale=1.0 / n, bias=epsc),
          reads=[pfx + "ss"], writes=[pfx + "rstd"])
    sc.op("dve", lambda e: e.reciprocal(out=rstd, in_=rstd), reads=[pfx + "rstd"], writes=[pfx + "rstd"])


TM_RANGES = [(512, 1024), (1024, 1536), (1536, 2048), (2944, 3072), (3200, 3352)]


def stage_proj(nc, sc, NT, x, norm1_w, w_in, P_tm, P_fm, ident_b, epsc, uv=None):
    with contextlib.ExitStack() as st:
        T = lambda name, shape, dt: st.enter_context(nc.sbuf_tensor(name, shape, dt))
        P = lambda name, shape, dt: st.enter_context(nc.psum_tensor(name, shape, dt))
        win = T("a_win", [128, 8, INW], BF16)
        n1w = T("a_n1w", [128, 8], F32)
        xt = [T("a_xt%d" % i, [128, D], F32) for i in range(2)]
        junk = T("a_junk", [128, D], BF16)
        ss = T("a_ss", [128, 1], F32)
        rstd = T("a_rstd", [128, 1], F32)
        xn = T("a_xn", [128, D], BF16)
        xnT = T("a_xnT", [128, 8, 128], BF16)
        ptm = [T("a_ptm%d" % i, [128, INW], F32) for i in range(2)]
        pfm = [T("a_pfm%d" % i, [128, 16, 128], F32) for i in range(2)]
        ps_t = P("ap_t", [128, 8, 128], BF16)
        ps_m = [P("ap_m%d" % i, [128, 512], F32) for i in range(2)]
        ps_f = [P("ap_f%d" % i, [128, 4, 128], F32) for i in range(2)]
        for kc in range(8):
            sc.dma("pool", lambda e, kc=kc: e.dma_start(out=win[:, kc, :], in_=w_in[kc * 128:(kc + 1) * 128, :]),
                   writes=["a_win"])
        uv_ops = []
        if uv is not None:
            u_tab, v_tab, UV = uv
            tmp = [T("uv_tmp%d" % i, [128, 8, D], BF16) for i in range(3)]
            UVv = UV.rearrange("(p r) d -> p r d", p=128)
            k = 0
            for half, tab in enumerate((u_tab, v_tab)):
                tv = tab.rearrange("(p r) d -> p r d", p=128)
                for ci in range(16):
                    bb = k % 3
                    k += 1
                    uv_ops.append(lambda bb=bb, tv=tv, ci=ci: sc.dma(
                        "pool", lambda e: e.dma_start(out=tmp[bb][:], in_=tv[:, ci * 8:(ci + 1) * 8, :]), writes=["uv_tmp%d" % bb]))
                    uv_ops.append(lambda bb=bb, ci=ci, half=half: sc.dma(
                        "sp", lambda e: e.dma_start(out=UVv[:, ci * 8:(ci + 1) * 8, half * D:(half + 1) * D], in_=tmp[bb][:]),
                        reads=["uv_tmp%d" % bb], writes=["UV"]))
        sc.dma("sp", lambda e: e.dma_start(out=n1w[:], in_=norm1_w.rearrange("o (kc p) -> p (o kc)", p=128),
                                           allow_slow_non_contiguous=True), writes=["a_n1w"])
        P_fm_v = P_fm.rearrange("(c p) t -> p c t", p=128)
        ev = 0
        for i in range(NT):
            b = i % 2
            xk = "a_xt%d" % b
            sc.dma("sp", lambda e, b=b, i=i: e.dma_start(out=xt[b][:], in_=x[i * 128:(i + 1) * 128, :]), writes=[xk])
            rms_rstd(sc, xt[b][:], xk, junk[:], ss[:], rstd[:], epsc[:], D, "a_")
            sc.op("dve", lambda e, b=b: e.tensor_scalar(out=xn[:], in0=xt[b][:], scalar1=rstd[:, 0:1], scalar2=None,
                                                        op0=ALU.mult), reads=[xk, "a_rstd"], writes=["a_xn"])
            for kc in range(8):
                sc.op("pe", lambda e, kc=kc: e.transpose(out=ps_t[:, kc, :], in_=xn[:, kc * 128:(kc + 1) * 128],
                                                         identity=ident_b[:]), reads=["a_xn", "ident_b"], writes=["ap_t"])
            sc.op("dve", lambda e: e.tensor_tensor(out=xnT[:], in0=ps_t[:],
                                                   in1=n1w[:].unsqueeze(2).broadcast_to([128, 8, 128]), op=ALU.mult),
                  reads=["ap_t", "a_n1w"], writes=["a_xnT"])
            if uv_ops:
                uv_ops.pop(0)()
            for cg, (c0, c1) in enumerate(TM_RANGES):
                pm = ps_m[cg % 2]
                pk = "ap_m%d" % (cg % 2)
                for kc in range(8):
                    sc.op("pe", lambda e, kc=kc, c0=c0, c1=c1, pm=pm: e.matmul(
                        pm[:, 0:c1 - c0], lhsT=xnT[:, kc, :], rhs=win[:, kc, c0:c1], start=(kc == 0), stop=(kc == 7)),
                        reads=["a_xnT", "a_win"], writes=[pk])
                if cg % 2 == 0:
                    sc.op("act", lambda e, b=b, c0=c0, c1=c1, pm=pm: e.copy(out=ptm[b][:, c0:c1], in_=pm[:, 0:c1 - c0]),
                          reads=[pk], writes=["a_ptm%d" % b])
                else:
                    sc.op("dve", lambda e, b=b, c0=c0, c1=c1, pm=pm: e.tensor_copy(out=ptm[b][:, c0:c1], in_=pm[:, 0:c1 - c0]),
                          reads=[pk], writes=["a_ptm%d" % b])
            sc.dma("sp", lambda e, b=b, i=i: e.dma_start(out=P_tm[i * 128:(i + 1) * 128, 512:2048], in_=ptm[b][:, 512:2048]),
                   reads=["a_ptm%d" % b], writes=["P_tm"])
            sc.dma("sp", lambda e, b=b, i=i: e.dma_start(out=P_tm[i * 128:(i + 1) * 128, 2944:INW], in_=ptm[b][:, 2944:INW]),
                   reads=["a_ptm%d" % b], writes=["P_tm"])
            for fg in range(4):
                pf = ps_f[fg % 2]
                pk = "ap_f%d" % (fg % 2)
                for f4 in range(4):
                    ch = FMCH[fg * 4 + f4]
                    for kc in range(8):
                        sc.op("pe", lambda e, kc=kc, ch=ch, f4=f4, pf=pf: e.matmul(
                            pf[:, f4, :], lhsT=win[:, kc, ch * 128:(ch + 1) * 128], rhs=xnT[:, kc, :],
                            start=(kc == 0), stop=(kc == 7)), reads=["a_xnT", "a_win"], writes=[pk])
                if fg % 2 == 0:
                    sc.op("act", lambda e, b=b, fg=fg, pf=pf: e.copy(out=pfm[b][:, fg * 4:(fg + 1) * 4, :], in_=pf[:]),
                          reads=[pk], writes=["a_pfm%d" % b])
                else:
                    sc.op("dve", lambda e, b=b, fg=fg, pf=pf: e.tensor_copy(out=pfm[b][:, fg * 4:(fg + 1) * 4, :], in_=pf[:]),
                          reads=[pk], writes=["a_pfm%d" % b])
            sc.dma("sp", lambda e, b=b, i=i: e.dma_start(out=P_fm_v[:, :, i * 128:(i + 1) * 128], in_=pfm[b][:]),
                   reads=["a_pfm%d" % b], writes=["P_fm"])
        while uv_ops:
            uv_ops.pop(0)()
        barrier(sc)


def stage_hgrn(nc, sc, NT, P_tm, P_fm, Y_tm, lb_logits, hg_onw, ublk, wrev, epsc, c_rowm, c_colm):
    with contextlib.ExitStack() as st:
        T = lambda name, shape, dt: st.enter_context(nc.sbuf_tensor(name, shape, dt))
        P = lambda name, shape, dt: st.enter_context(nc.psum_tensor(name, shape, dt))
        lbb = T("h_lbb", [128, 2, 512], F32)
        omlb = T("h_omlb", [128, 512], F32)
        lbf = T("h_lbf", [128, 2, 4], F32)
        omlf = T("h_omlf", [128, 4], F32)
        hgw = T("h_hgw", [128, 128], F32)
        ftm = [T("h_ftm%d" % i, [128, 1536], F32) for i in range(2)]
        ffm = [T("h_ffm%d" % i, [128, 8, 128], F32) for i in range(2)]
        sig = T("h_sig", [128, 512], F32)
        logf = T("h_logf", [128, 512], F32)
        ktm = T("h_ktm", [128, 512], F32)
        erev = T("h_erev", [128, 512], F32)
        khat = T("h_khat", [128, 512], F32)
        khat4 = T("h_khat4", [128, 4, 512], BF16)
        rowm = T("h_rowm", [128, 4], F32)
        colm = T("h_colm", [128, 4, 128], F32)
        vbf = T("h_vbf", [128, 512], BF16)
        sgg = T("h_sgg", [128, 512], F32)
        fT = T("h_fT", [128, 4, 128], F32)
        ET = T("h_ET", [128, 4, 128], F32)
        EiT = T("h_EiT", [128, 4, 128], F32)
        qtT = T("h_qtT", [128, 4, 128], BF16)
        ktT = T("h_ktT", [128, 4, 128], BF16)
        qtT4 = [T("h_qtT4%d" % h, [128, 4, 128], BF16) for h in range(4)]
        scm = T("h_scm", [128, 4, 128], BF16)
        state = [T("h_st%d" % h, [128, 128], F32) for h in range(4)]
        stbf = [T("h_sb%d" % h, [128, 128], BF16) for h in range(4)]
        junk = T("h_junk", [128, 128], F32)
        ss = T("h_ss", [128, 4], F32)
        rstd = T("h_rstd", [128, 4], F32)
        yt = [T("h_yt%d" % i, [128, 512], F32) for i in range(2)]
        onec = T("h_onec", [128, 1], F32)
        sc.op("dve", lambda e: e.memset(onec[:], 1.0), writes=["h_onec"])
        ps_m = [P("hp_m%d" % i, [128, 512], F32) for i in range(4)]
        ps_o = [P("hp_o%d" % i, [128, 512], F32) for i in range(4)]
        ps_rev = ps_m[0]
        ps_bT = ps_m[1][:].rearrange("p (h t) -> p h t", h=4)
        ps_sc = ps_m[2][:].rearrange("p (h t) -> p h t", h=4)
        sc.dma("sp", lambda e: e.dma_start(out=rowm[:], in_=c_rowm), writes=["h_rowm"])
        sc.dma("sp", lambda e: e.dma_start(out=colm[:], in_=c_colm), writes=["h_colm"])
        sc.dma("sp", lambda e: e.dma_start(out=lbb[:, 0, :], in_=lb_logits[0:1, :].partition_broadcast(128)), writes=["h_lbb"])
        sc.dma("sp", lambda e: e.dma_start(out=lbb[:, 1, :], in_=lb_logits[1:2, :].partition_broadcast(128)), writes=["h_lbb"])
        sc.dma("sp", lambda e: e.dma_start(out=lbf[:], in_=lb_logits.rearrange("r (h p) -> p r h", p=128),
                                           allow_slow_non_contiguous=True), writes=["h_lbf"])
        sc.dma("sp", lambda e: e.dma_start(out=hgw[:], in_=hg_onw[0:1, :].partition_broadcast(128)), writes=["h_hgw"])

        def sigmoid_to(ap_out, ap_in, keys_in, key_out):
            sc.op("act", lambda e: e.activation(out=ap_out, in_=ap_in, func=AF.Exp, scale=-1.0), reads=keys_in, writes=[key_out])
            sc.op("act", lambda e: e.activation(out=ap_out, in_=ap_out, func=AF.Ln, bias=onec[:]), reads=[key_out], writes=[key_out])
            sc.op("act", lambda e: e.activation(out=ap_out, in_=ap_out, func=AF.Exp, scale=-1.0), reads=[key_out], writes=[key_out])

        sc.op("dve", lambda e: e.tensor_tensor(out=lbb[:, 0, :], in0=lbb[:, 0, :], in1=lbb[:, 1, :], op=ALU.subtract),
              reads=["h_lbb"], writes=["h_lbb"])
        sigmoid_to(lbb[:, 0, :], lbb[:, 0, :], ["h_lbb"], "h_lbb")
        sc.op("dve", lambda e: e.tensor_scalar(out=omlb[:], in0=lbb[:, 0, :], scalar1=-1.0, scalar2=1.0, op0=ALU.mult,
                                               op1=ALU.add), reads=["h_lbb"], writes=["h_omlb"])
        sc.op("dve", lambda e: e.tensor_tensor(out=lbf[:, 0, :], in0=lbf[:, 0, :], in1=lbf[:, 1, :], op=ALU.subtract),
              reads=["h_lbf"], writes=["h_lbf"])
        sigmoid_to(lbf[:, 0, :], lbf[:, 0, :], ["h_lbf"], "h_lbf")
        sc.op("dve", lambda e: e.tensor_scalar(out=omlf[:], in0=lbf[:, 0, :], scalar1=-1.0, scalar2=1.0, op0=ALU.mult,
                                               op1=ALU.add), reads=["h_lbf"], writes=["h_omlf"])
        P_fm_v = P_fm.rearrange("(c p) t -> p c t", p=128)
        for i in range(NT):
            b = i % 2
            fk, mk = "h_ftm%d" % b, "h_ffm%d" % b
            F_, M_ = ftm[b], ffm[b]
            sc.dma("sp", lambda e, b=b, i=i: e.dma_start(out=ftm[b][:], in_=P_tm[i * 128:(i + 1) * 128, 512:2048]),
                   reads=["P_tm"], writes=[fk])
            sc.dma("sp", lambda e, b=b, i=i: e.dma_start(out=ffm[b][:], in_=P_fm_v[:, 0:8, i * 128:(i + 1) * 128]),
                   reads=["P_fm"], writes=[mk])
            if i % 16 == 0:
                for h in range(4):
                    sc.op("dve", lambda e, h=h: e.memset(state[h][:], 0.0), writes=["h_st%d" % h])
                    sc.op("dve", lambda e, h=h: e.memset(stbf[h][:], 0.0), writes=["h_sb%d" % h])
            opsA, opsB = [], []
            OP_A = lambda *a, **k: opsA.append(lambda: sc.op(*a, **k))
            OP_B = lambda *a, **k: opsB.append(lambda: sc.op(*a, **k))
            SIG_A = lambda *a: opsA.append(lambda: sigmoid_to(*a))
            SIG_B = lambda *a: opsB.append(lambda: sigmoid_to(*a))
            SIG_A(sig[:], F_[:, 0:512], [fk], "h_sig")
            OP_A("dve", lambda e: e.tensor_tensor(out=sig[:], in0=sig[:], in1=omlb[:], op=ALU.mult),
                  reads=["h_sig", "h_omlb"], writes=["h_sig"])
            OP_A("dve", lambda e: e.tensor_tensor(out=sig[:], in0=sig[:], in1=lbb[:, 0, :], op=ALU.add),
                  reads=["h_sig", "h_lbb"], writes=["h_sig"])
            OP_A("act", lambda e: e.activation(out=logf[:], in_=sig[:], func=AF.Ln), reads=["h_sig"], writes=["h_logf"])
            OP_A("dve", lambda e: e.tensor_scalar(out=ktm[:], in0=sig[:], scalar1=-1.0, scalar2=1.0, op0=ALU.mult,
                                                   op1=ALU.add), reads=["h_sig"], writes=["h_ktm"])
            OP_A("pe", lambda e: e.matmul(ps_rev[:], lhsT=wrev[:], rhs=logf[:], start=True, stop=True),
                  reads=["wrev", "h_logf"], writes=["hp_m0"])
            OP_A("act", lambda e: e.activation(out=erev[:], in_=ps_rev[:], func=AF.Exp), reads=["hp_m0"], writes=["h_erev"])
            OP_A("dve", lambda e: e.tensor_tensor(out=khat[:], in0=ktm[:], in1=erev[:], op=ALU.mult),
                  reads=["h_ktm", "h_erev"], writes=["h_khat"])
            OP_A("dve", lambda e: e.tensor_tensor(out=khat4[:], in0=khat[:].unsqueeze(1).broadcast_to([128, 4, 512]),
                                                   in1=rowm[:].unsqueeze(2).broadcast_to([128, 4, 512]), op=ALU.mult),
                  reads=["h_khat", "h_rowm"], writes=["h_khat4"])
            OP_A("act", lambda e, F_=F_: e.copy(out=vbf[:], in_=F_[:, 512:1024]), reads=[fk], writes=["h_vbf"])
            SIG_A(sgg[:], F_[:, 1024:1536], [fk], "h_sgg")
            OP_A("dve", lambda e, F_=F_: e.tensor_tensor(out=sgg[:], in0=sgg[:], in1=F_[:, 1024:1536], op=ALU.mult),
                  reads=["h_sgg", fk], writes=["h_sgg"])
            SIG_B(fT[:], M_[:, 4:8, :], [mk], "h_fT")
            for h in range(4):
                OP_B("dve", lambda e, h=h: e.tensor_scalar(out=fT[:, h, :], in0=fT[:, h, :], scalar1=omlf[:, h:h + 1],
                                                            scalar2=lbf[:, 0, h:h + 1], op0=ALU.mult, op1=ALU.add),
                      reads=["h_fT", "h_omlf", "h_lbf"], writes=["h_fT"])
            OP_B("dve", lambda e: e.tensor_scalar(out=fT[:], in0=fT[:], scalar1=-1.0, scalar2=1.0, op0=ALU.mult,
                                                   op1=ALU.add), reads=["h_fT"], writes=["h_fT"])
            for h in range(4):
                OP_B("pe", lambda e, h=h: e.matmul(ps_bT[:, h, :], lhsT=logf[:, h * 128:(h + 1) * 128], rhs=ublk[:],
                                                    start=True, stop=True), reads=["h_logf", "ublk"], writes=["hp_m1"])
            OP_B("act", lambda e: e.activation(out=ET[:], in_=ps_bT, func=AF.Exp), reads=["hp_m1"], writes=["h_ET"])
            OP_B("act", lambda e: e.activation(out=EiT[:], in_=ps_bT, func=AF.Exp, scale=-1.0),
                  reads=["hp_m1"], writes=["h_EiT"])
            OP_B("dve", lambda e, M_=M_: e.tensor_tensor(out=qtT[:], in0=M_[:, 0:4, :], in1=ET[:], op=ALU.mult),
                  reads=[mk, "h_ET"], writes=["h_qtT"])
            OP_B("dve", lambda e: e.tensor_tensor(out=ktT[:], in0=fT[:], in1=EiT[:], op=ALU.mult),
                  reads=["h_fT", "h_EiT"], writes=["h_ktT"])
            for h in range(4):
                OP_B("dve", lambda e, h=h: e.tensor_tensor(out=qtT4[h][:], in0=qtT[:, h, :].unsqueeze(1).broadcast_to([128, 4, 128]),
                                                            in1=colm[:], op=ALU.mult),
                      reads=["h_qtT", "h_colm"], writes=["h_qtT4%d" % h])
                OP_B("pe", lambda e, h=h: e.matmul(ps_sc[:, h, :], lhsT=ktT[:, h, :], rhs=qtT[:, h, :], start=True, stop=True),
                      reads=["h_ktT", "h_qtT"], writes=["hp_m2"])
            OP_B("dve", lambda e: e.tensor_tensor(out=scm[:], in0=ps_sc, in1=ublk[:].unsqueeze(1).broadcast_to([128, 4, 128]),
                                                   op=ALU.mult), reads=["hp_m2", "ublk"], writes=["h_scm"])
            for k_ in range(max(len(opsA), len(opsB))):
                if k_ < len(opsA):
                    opsA[k_]()
                if k_ < len(opsB):
                    opsB[k_]()
            for h in range(4):
                sc.op("pe", lambda e, h=h: e.matmul(ps_o[h][:, 0:128], lhsT=scm[:, h, :], rhs=vbf[:, h * 128:(h + 1) * 128],
                                                    start=True, stop=False), reads=["h_scm", "h_vbf"], writes=["hp_o%d" % h])
            for c4 in range(4):
                for h in range(4):
                    hs = slice(h * 128, (h + 1) * 128)
                    sc.op("pe", lambda e, h=h, c4=c4: e.matmul(ps_o[h][:, 0:128], lhsT=qtT4[h][:, c4, :], rhs=stbf[h][:],
                                                               start=False, stop=(c4 == 3)),
                          reads=["h_qtT4%d" % h, "h_sb%d" % h], writes=["hp_o%d" % h])
                    sc.op("pe", lambda e, c4=c4, hs=hs, h=h: e.matmul(ps_m[h][:, 0:128], lhsT=khat4[:, c4, hs], rhs=vbf[:, hs],
                                                                      start=True, stop=True),
                          reads=["h_khat4", "h_vbf"], writes=["hp_m%d" % h])
                    sc.op("dve", lambda e, h=h, c4=c4: e.scalar_tensor_tensor(
                        out=state[h][:], in0=state[h][:], scalar=ET[:, h, c4 * 32 + 31:c4 * 32 + 32], in1=ps_m[h][:, 0:128],
                        op0=ALU.mult, op1=ALU.add), reads=["h_st%d" % h, "h_ET", "hp_m%d" % h], writes=["h_st%d" % h])
                    sc.op("act", lambda e, h=h: e.copy(out=stbf[h][:], in_=state[h][:]),
                          reads=["h_st%d" % h], writes=["h_sb%d" % h])
            for h in range(4):
                sc.op("act", lambda e, h=h: e.activation(out=junk[:], in_=ps_o[h][:, 0:128], func=AF.Square, accum_out=ss[:, h:h + 1]),
                      reads=["hp_o%d" % h], writes=["h_junk", "h_ss"])
            sc.op("act", lambda e: e.activation(out=rstd[:], in_=ss[:], func=AF.Ln, scale=1.0 / 128, bias=epsc[:]),
                  reads=["h_ss"], writes=["h_rstd"])
            sc.op("act", lambda e: e.activation(out=rstd[:], in_=rstd[:], func=AF.Exp, scale=-0.5),
                  reads=["h_rstd"], writes=["h_rstd"])
            for h in range(4):
                hs = slice(h * 128, (h + 1) * 128)
                sc.op("dve", lambda e, h=h, b=b, hs=hs: e.scalar_tensor_tensor(
                    out=yt[b][:, hs], in0=ps_o[h][:, 0:128], scalar=rstd[:, h:h + 1], in1=hgw[:], op0=ALU.mult, op1=ALU.mult),
                    reads=["hp_o%d" % h, "h_rstd", "h_hgw"], writes=["h_yt%d" % b])
            sc.op("dve", lambda e, b=b: e.tensor_tensor(out=yt[b][:], in0=yt[b][:], in1=sgg[:], op=ALU.mult),
                  reads=["h_yt%d" % b, "h_sgg"], writes=["h_yt%d" % b])
            sc.dma("sp", lambda e, b=b, i=i: e.dma_start(out=Y_tm[i * 128:(i + 1) * 128, 0:512], in_=yt[b][:]),
                   reads=["h_yt%d" % b], writes=["Y_tm"])
        barrier(sc)


NSA_WARM = 1


def stage_nsa(nc, sc, NSEQ, P_tm, P_fm, Y_tm, positions, qnw, knw, pe_k, pe_v, w1k, w2k, w1v, w2v, onw, cst,
              ident_b, epsc, dbg=None):
    TINY = 1e-30
    with contextlib.ExitStack() as st:
        T = lambda name, shape, dt: st.enter_context(nc.sbuf_tensor(name, shape, dt))
        P = lambda name, shape, dt: st.enter_context(nc.psum_tensor(name, shape, dt))
        dmask = T("n_dmask", [128, 8, 512], BF16)
        cmask = T("n_cmask", [128, 4, 512], BF16)
        rm = T("n_rm", [64, 64], BF16)
        ones64 = T("n_ones", [64, 64], BF16)
        misc = T("n_misc", [128, 8], F32)
        valid = T("n_valid", [128, 16, 32], F32)
        addc = T("n_addc", [128, 16, 32], F32)
        wcol = T("n_wcol", [64, 4], F32)
        onwb = T("n_onwb", [128, 64], F32)
        w1 = [T("n_w1%d" % i, [64, 32, 256], BF16) for i in range(2)]
        w2 = [T("n_w2%d" % i, [128, 2, 64], BF16) for i in range(2)]
        peT = T("n_peT", [64, 2, 32], BF16)
        peTf = T("n_peTf", [64, 2, 32], F32)
        cbias = T("n_cbias", [128, 2, 2], F32)
        for name, t_, src in (("n_dmask", dmask, cst["c_dmask"]), ("n_cmask", cmask, cst["c_cmask"]),
                              ("n_rm", rm, cst["c_rm"])):
            sc.dma("pool", lambda e, t_=t_, src=src: e.dma_start(out=t_[:], in_=src), writes=[name])
        sc.dma("sp", lambda e: e.dma_start(out=misc[:], in_=cst["c_misc"]), writes=["n_misc"])
        sc.dma("sp", lambda e: e.dma_start(out=valid[:], in_=cst["c_valid"]), writes=["n_valid"])
        sc.dma("sp", lambda e: e.dma_start(out=addc[:], in_=cst["c_addc"]), writes=["n_addc"])
        sc.op("dve", lambda e: e.memset(ones64[:], 1.0), writes=["n_ones"])
        sc.dma("pool", lambda e: e.dma_start(out=ksT[64:96, :], in_=cst["c_emat"]), writes=["n_ksT"])
        sc.op("dve", lambda e: e.tensor_scalar(out=dm01[:], in0=dmask[:], scalar1=-1.0, scalar2=None, op0=ALU.is_ge),
              reads=["n_dmask"], writes=["n_dm01"])
        sc.op("dve", lambda e: e.memset(selb[:], 0.0), writes=["n_selb"])
        sc.op("dve", lambda e: e.memset(kwT[64:96, :], 0.0), writes=["n_kwT"])
        sc.op("dve", lambda e: e.memset(kcmpT[64:96, :], 0.0), writes=["n_kcmpT"])
        for r_ in range(4):
            sc.op("dve", lambda e, r_=r_: e.memset(qT[r_][64:96, :], 0.0), writes=["n_qT%d" % r_])
        sc.dma("sp", lambda e: e.dma_start(out=wcol[:, 0:1], in_=qnw.rearrange("o d -> d o"), allow_slow_non_contiguous=True),
               writes=["n_wcol"])
        sc.dma("sp", lambda e: e.dma_start(out=wcol[:, 1:4], in_=knw.rearrange("b d -> d b"), allow_slow_non_contiguous=True),
               writes=["n_wcol"])
        sc.op("dve", lambda e: e.tensor_scalar(out=wcol[:, 0:1], in0=wcol[:, 0:1], scalar1=0.125, scalar2=None, op0=ALU.mult),
              reads=["n_wcol"], writes=["n_wcol"])
        sc.dma("sp", lambda e: e.dma_start(out=onwb[:], in_=onw[0:1, :].partition_broadcast(128)), writes=["n_onwb"])
        for i, (w1_, w2_) in enumerate(((w1k, w2k), (w1v, w2v))):
            sc.dma("pool", lambda e, i=i, w1_=w1_: e.dma_start(out=w1[i][:], in_=w1_.rearrange("(l d) n -> d l n", d=64)),
                   writes=["n_w1%d" % i])
            sc.dma("pool", lambda e, i=i, w2_=w2_: e.dma_start(out=w2[i][:], in_=w2_.rearrange("(a p) n -> p a n", p=128)),
                   writes=["n_w2%d" % i])
        sc.dma("sp", lambda e: e.dma_start(out=peTf[:, 0, :], in_=pe_k.rearrange("l d -> d l"), allow_slow_non_contiguous=True),
               writes=["n_peTf"])
        sc.dma("sp", lambda e: e.dma_start(out=peTf[:, 1, :], in_=pe_v.rearrange("l d -> d l"), allow_slow_non_contiguous=True),
               writes=["n_peTf"])
        sc.op("dve", lambda e: e.tensor_copy(out=peT[:], in_=peTf[:]), reads=["n_peTf"], writes=["n_peT"])
        ki = T("n_ki", [128, 256], I32)
        ang = T("n_ang", [128, 256], F32)
        rr = T("n_rr", [128, 256], F32)
        tab2 = T("n_tab2", [128, 256], F32)
        kf = T("n_kf", [128, 256], F32)
        CT = T("n_CT", [64, 2048], F32)
        ST = T("n_ST", [64, 2048], F32)
        src = T("n_src", [64, 2048], F32)
        sq = [T("n_sq%d" % i, [64, 512], BF16) for i in range(2)]
        rinv = [T("n_rinv%d" % i, [64, 512], F32) for i in range(2)]
        xnm = [T("n_xnm%d" % i, [64, 512], F32) for i in range(2)]
        xb = [T("n_xb%d" % i, [64, 512], BF16) for i in range(2)]
        t1 = [T("n_t1%d" % i, [64, 512], F32) for i in range(2)]
        t2 = [T("n_t2%d" % i, [64, 512], F32) for i in range(2)]
        qT = [T("n_qT%d" % r, [96, 2048], BF16) for r in range(4)]
        ksT = T("n_ksT", [96, 2048], BF16)
        dm01 = dmask
        kwT = T("n_kwT", [96, 2048], BF16)
        kcb = T("n_kcb", [64, 2048], BF16)
        kcmpT = T("n_kcmpT", [96, 128], BF16)
        hx = T("n_hx", [128, 127], F32)
        hx2 = T("n_hx2", [128, 127], F32)
        hT = T("n_hT", [128, 2, 127], BF16)
        vcaug = T("n_vcaug", [128, 97], BF16)
        vaugf = T("n_vaugf", [128, 33], F32)
        vsa = T("n_vsa", [128, 16, 65], BF16)
        vwa = T("n_vwa", [128, 16, 65], BF16)
        vld = T("n_vld", [128, 16, 64], F32)
        gsig = T("n_gsig", [128, 16, 24], F32)
        PT = [T("n_PT%d" % i, [128, 512], BF16) for i in range(5)]
        oacc = T("n_oacc", [128, 16, 4, 64], F32)
        imp = T("n_imp", [128, 16, 32], F32)
        score = T("n_score", [128, 16, 32], F32)
        swk = T("n_swk", [128, 4, 32], F32)
        m8a = T("n_m8a", [128, 4, 8], F32)
        m8b = T("n_m8b", [128, 4, 8], F32)
        sel = T("n_sel", [128, 4, 32], F32)
        selb = T("n_selb", [128, 4, 96], BF16)
        nselT = T("n_nselT", [96, 2048], BF16)
        r1 = T("n_r1", [128, 1], F32)
        coef = T("n_coef", [128, 1], F32)
        ssq = T("n_ssq", [128, 64, 64], F32)
        ss64 = T("n_ss64", [128, 64], F32)
        ps_s = [P("np_s%d" % i, [128, 512], F32) for i in range(5)]
        ps_a = [P("np_a%d" % i, [128, 512], F32) for i in range(2)]
        ps_warm = P("np_w", [128, 512], F32)
        ps_x = [ps_s[2], ps_s[3]]
        sc.dma("sp", lambda e: e.dma_start(out=vaugf[:], in_=cst["c_vaug"]), writes=["n_vaugf"])
        sc.op("dve", lambda e: e.tensor_copy(out=vcaug[:, 64:97], in_=vaugf[:]), reads=["n_vaugf"], writes=["n_vcaug"])
        sc.op("dve", lambda e: e.memset(vcaug[:, 0:64], 0.0), writes=["n_vcaug"])
        sc.op("dve", lambda e: e.memset(vsa[:], 1.0), writes=["n_vsa"])
        sc.op("dve", lambda e: e.memset(vwa[:], 1.0), writes=["n_vwa"])
        sc.op("dve", lambda e: e.memset(CT[:], 1.0), writes=["n_CT"])
        sc.op("dve", lambda e: e.memset(ST[:], 0.0), writes=["n_ST"])
        for kv in range(2):
            for hh in range(2):
                for l in range(32):
                    sc.op("pe", lambda e, kv=kv, hh=hh, l=l: e.matmul(
                        ps_x[0][:, 0:1], lhsT=w1[kv][:, l, hh * 128:(hh + 1) * 128], rhs=peT[:, kv, l:l + 1],
                        start=(l == 0), stop=(l == 31)), reads=["n_w1%d" % kv, "n_peT"], writes=["np_s2"])
                sc.op("act", lambda e, kv=kv, hh=hh: e.copy(out=cbias[:, kv, hh:hh + 1], in_=ps_x[0][:, 0:1]),
                      reads=["np_s2"], writes=["n_cbias"])

        def norm_rope(calls):
            S = list(enumerate(calls))
            pa = [(ps_s[2], "np_s2", ps_s[3], "np_s3"), (ps_s[0], "np_s0", ps_s[1], "np_s1")]
            for k, (srcap, srckey, n, wc, Cap, Sap, outap, outkey) in S:
                sc.op("act", lambda e, k=k, n=n, srcap=srcap: e.activation(out=sq[k][:, 0:n], in_=srcap, func=AF.Square),
                      reads=[srckey], writes=["n_sq%d" % k])
            for k, (srcap, srckey, n, wc, Cap, Sap, outap, outkey) in S:
                sc.op("pe", lambda e, k=k, n=n: e.matmul(pa[k][0][0:64, 0:n], lhsT=ones64[:], rhs=sq[k][:, 0:n], start=True, stop=True),
                      reads=["n_sq%d" % k, "n_ones"], writes=[pa[k][1]])
            for k, (srcap, srckey, n, wc, Cap, Sap, outap, outkey) in S:
                sc.op("act", lambda e, k=k, n=n: e.activation(out=rinv[k][:, 0:n], in_=pa[k][0][0:64, 0:n], func=AF.Ln,
                                                              scale=1.0 / 64, bias=epsc[0:64, :]),
                      reads=[pa[k][1]], writes=["n_rinv%d" % k])
            for k, (srcap, srckey, n, wc, Cap, Sap, outap, outkey) in S:
                sc.op("act", lambda e, k=k, n=n: e.activation(out=rinv[k][:, 0:n], in_=rinv[k][:, 0:n], func=AF.Exp, scale=-0.5),
                      reads=["n_rinv%d" % k], writes=["n_rinv%d" % k])
            for k, (srcap, srckey, n, wc, Cap, Sap, outap, outkey) in S:
                sc.op("dve", lambda e, k=k, n=n, srcap=srcap, wc=wc: e.scalar_tensor_tensor(
                    out=xnm[k][:, 0:n], in0=srcap, scalar=wcol[:, wc:wc + 1], in1=rinv[k][:, 0:n], op0=ALU.mult, op1=ALU.mult),
                    reads=[srckey, "n_wcol", "n_rinv%d" % k], writes=["n_xnm%d" % k])
            for k, (srcap, srckey, n, wc, Cap, Sap, outap, outkey) in S:
                sc.op("act", lambda e, k=k, n=n: e.copy(out=xb[k][:, 0:n], in_=xnm[k][:, 0:n]),
                      reads=["n_xnm%d" % k], writes=["n_xb%d" % k])
            for k, (srcap, srckey, n, wc, Cap, Sap, outap, outkey) in S:
                sc.op("pe", lambda e, k=k, n=n: e.matmul(pa[k][2][0:64, 0:n], lhsT=rm[:], rhs=xb[k][:, 0:n], start=True, stop=True),
                      reads=["n_xb%d" % k, "n_rm"], writes=[pa[k][3]])
            for k, (srcap, srckey, n, wc, Cap, Sap, outap, outkey) in S:
                sc.op("dve", lambda e, k=k, n=n, Cap=Cap: e.tensor_tensor(out=t1[k][:, 0:n], in0=xnm[k][:, 0:n], in1=Cap, op=ALU.mult),
                      reads=["n_xnm%d" % k, "n_CT"], writes=["n_t1%d" % k])
            for k, (srcap, srckey, n, wc, Cap, Sap, outap, outkey) in S:
                sc.op("dve", lambda e, k=k, n=n, Sap=Sap: e.tensor_tensor(out=t2[k][:, 0:n], in0=pa[k][2][0:64, 0:n], in1=Sap, op=ALU.mult),
                      reads=[pa[k][3], "n_ST"], writes=["n_t2%d" % k])
            for k, (srcap, srckey, n, wc, Cap, Sap, outap, outkey) in S:
                sc.op("dve", lambda e, k=k, n=n, outap=outap: e.tensor_tensor(out=outap, in0=t1[k][:, 0:n], in1=t2[k][:, 0:n], op=ALU.add),
                      reads=["n_t1%d" % k, "n_t2%d" % k], writes=[outkey])

        P_fm_v = P_fm
        PTc = [0]
        ACc = [0, 0]
        accs = [T("n_accs%d" % i, [128, 4, 97], F32) for i in range(3)]
        r4 = T("n_r4", [128, 4], F32)
        c4t = T("n_c4", [128, 4], F32)
        o4 = T("n_o4", [128, 4, 64], F32)
        i4 = T("n_i4", [128, 4, 32], F32)

        def attn(kT, kkey, vfn, vkey, W, kts_fn, mask_fn, nk_fn, epi, krows=64, post_fn=None):
            SK = 4
            for r in range(4):
                units = [(qb, idx, kt, len(kts_fn(qb))) for qb in range(4) for idx, kt in enumerate(kts_fn(qb))]
                slot = {}

                def emit_qk(u):
                    qb, idx, kt, nkt = units[u]
                    nk = nk_fn(kt)
                    pb = PTc[0] % 5
                    PTc[0] += 1
                    slot[u] = pb
                    ps, psk = ps_s[pb], "np_s%d" % pb
                    mms = [(kT[0:krows, kt * 128:kt * 128 + nk], qT[r][0:krows, qb * 512:(qb + 1) * 512], [kkey, "n_qT%d" % r])]
                    mms += mask_fn(kt, qb)
                    for mi, (l_, r_, rk) in enumerate(mms):
                        sc.op("pe", lambda e, l_=l_, r_=r_, ps=ps, nk=nk, mi=mi, last=(mi == len(mms) - 1): e.matmul(
                            ps[0:nk, :], lhsT=l_, rhs=r_, start=(mi == 0), stop=last), reads=rk, writes=[psk])
                    sc.op("act", lambda e, ps=ps, nk=nk, pb=pb: e.activation(out=PT[pb][0:nk, :], in_=ps[0:nk, :], func=AF.Exp),
                          reads=[psk], writes=["n_PT%d" % pb])
                    for _w in range(NSA_WARM):
                        sc.op("pe", lambda e: e.matmul(ps_warm[:], lhsT=ident_b[:], rhs=dm01[:, 4, :], start=True, stop=True),
                              reads=["ident_b", "n_dm01"], writes=["np_w"])
                    m01 = post_fn(kt, qb) if post_fn else None
                    if m01 is not None:
                        sc.op("pool", lambda e, pb=pb, m01=m01: e.tensor_tensor(out=PT[pb][:], in0=PT[pb][:], in1=m01, op=ALU.mult),
                              reads=["n_PT%d" % pb, "n_dm01"], writes=["n_PT%d" % pb])

                def emit_pv(u):
                    qb, idx, kt, nkt = units[u]
                    nk = nk_fn(kt)
                    pb = slot[u]
                    ab = ACc[0] % 2
                    for ts in range(4):
                        sc.op("pe", lambda e, pb=pb, nk=nk, ts=ts, kt=kt, idx=idx, ab=ab, last=(idx == nkt - 1): e.matmul(
                            ps_a[ab][:, ts * W:(ts + 1) * W], lhsT=PT[pb][0:nk, ts * 128:(ts + 1) * 128], rhs=vfn(kt)[0:nk, :],
                            start=(idx == 0 and ts == 0), stop=last, skip_group_check=True),
                            reads=["n_PT%d" % pb, vkey], writes=["np_a%d" % ab])
                    if idx == nkt - 1:
                        ACc[0] += 1
                        sb3 = ACc[1] % 3
                        ACc[1] += 1
                        sc.op("act", lambda e, ab=ab, sb3=sb3: e.copy(
                            out=accs[sb3][:, :, 0:W], in_=ps_a[ab][:, 0:4 * W].rearrange("p (a b) -> p a b", b=W)),
                            reads=["np_a%d" % ab], writes=["n_accs%d" % sb3])
                        epi(r, qb, accs[sb3], "n_accs%d" % sb3)

                for j in range(len(units) + SK):
                    if j < len(units):
                        emit_qk(j)
                    if j - SK >= 0:
                        emit_pv(j - SK)

        def mk_epi(g, branch, first):
            def epi(r, qb, acc, acck):
                tsl = slice(qb * 4, qb * 4 + 4)
                gc = (g * 4 + r) * 3 + branch
                sc.op("dve", lambda e: e.tensor_scalar(out=r4[:], in0=acc[:, :, 64], scalar1=TINY, scalar2=None, op0=ALU.max),
                      reads=[acck], writes=["n_r4"])
                sc.op("dve", lambda e: e.reciprocal(out=r4[:], in_=r4[:]), reads=["n_r4"], writes=["n_r4"])
                sc.op("dve", lambda e: e.tensor_tensor(out=c4t[:], in0=r4[:], in1=gsig[:, tsl, gc], op=ALU.mult),
                      reads=["n_r4", "n_gsig"], writes=["n_c4"])
                if first:
                    sc.op("dve", lambda e: e.tensor_tensor(out=oacc[:, tsl, r, :], in0=acc[:, :, 0:64],
                                                           in1=c4t[:].unsqueeze(2).broadcast_to([128, 4, 64]), op=ALU.mult),
                          reads=[acck, "n_c4"], writes=["n_oacc"])
                    if r == 0:
                        sc.op("dve", lambda e: e.tensor_tensor(out=imp[:, tsl, :], in0=acc[:, :, 65:97],
                                                               in1=r4[:].unsqueeze(2).broadcast_to([128, 4, 32]), op=ALU.mult),
                              reads=[acck, "n_r4"], writes=["n_imp"])
                    else:
                        sc.op("dve", lambda e: e.tensor_tensor(out=i4[:], in0=acc[:, :, 65:97],
                                                               in1=r4[:].unsqueeze(2).broadcast_to([128, 4, 32]), op=ALU.mult),
                              reads=[acck, "n_r4"], writes=["n_i4"])
                        sc.op("dve", lambda e: e.tensor_tensor(out=imp[:, tsl, :], in0=imp[:, tsl, :], in1=i4[:], op=ALU.add),
                              reads=["n_i4", "n_imp"], writes=["n_imp"])
                else:
                    sc.op("dve", lambda e: e.tensor_tensor(out=o4[:], in0=acc[:, :, 0:64],
                                                           in1=c4t[:].unsqueeze(2).broadcast_to([128, 4, 64]), op=ALU.mult),
                          reads=[acck, "n_c4"], writes=["n_o4"])
                    sc.op("dve", lambda e: e.tensor_tensor(out=oacc[:, tsl, r, :], in0=oacc[:, tsl, r, :], in1=o4[:], op=ALU.add),
                          reads=["n_o4", "n_oacc"], writes=["n_oacc"])
            return epi

        TWO_PI = 6.283185307179586
        for s in range(NSEQ):
            sb = s * 2048
            for c in range(8):
                sc.dma("sp", lambda e, s=s, c=c: e.dma_start(
                    out=ki[c * 16:(c + 1) * 16, :], in_=positions[s:s + 1, c * 256:(c + 1) * 256].partition_broadcast(16)),
                    writes=["n_ki"])
            sc.op("dve", lambda e: e.tensor_copy(out=ang[:], in_=ki[:]), reads=["n_ki"], writes=["n_ang"])
            sc.op("dve", lambda e: e.tensor_scalar(out=ang[:], in0=ang[:], scalar1=misc[:, 1:2], scalar2=None, op0=ALU.mult),
                  reads=["n_ang", "n_misc"], writes=["n_ang"])
            for tab, tkey, shift in ((ST, "n_ST", 0.0), (CT, "n_CT", 1.5707963267948966)):
                sc.op("dve", lambda e, shift=shift: e.tensor_scalar(out=rr[:], in0=ang[:], scalar1=shift, scalar2=None, op0=ALU.add),
                      reads=["n_ang"], writes=["n_rr"])
                sc.op("dve", lambda e: e.tensor_scalar(out=kf[:], in0=rr[:], scalar1=1.0 / TWO_PI, scalar2=None, op0=ALU.mult),
                      reads=["n_rr"], writes=["n_kf"])
                sc.op("dve", lambda e: e.tensor_copy(out=ki[:], in_=kf[:]), reads=["n_kf"], writes=["n_ki"])
                sc.op("dve", lambda e: e.tensor_copy(out=kf[:], in_=ki[:]), reads=["n_ki"], writes=["n_kf"])
                sc.op("dve", lambda e: e.scalar_tensor_tensor(out=rr[:], in0=kf[:], scalar=-TWO_PI, in1=rr[:], op0=ALU.mult,
                                                              op1=ALU.add), reads=["n_kf", "n_rr"], writes=["n_rr"])
                sc.op("dve", lambda e: e.tensor_scalar(out=kf[:], in0=rr[:], scalar1=3.141592653589793, scalar2=-TWO_PI,
                                                       op0=ALU.is_gt, op1=ALU.mult), reads=["n_rr"], writes=["n_kf"])
                sc.op("dve", lambda e: e.tensor_tensor(out=rr[:], in0=rr[:], in1=kf[:], op=ALU.add), reads=["n_rr", "n_kf"], writes=["n_rr"])
                sc.op("dve", lambda e: e.tensor_scalar(out=kf[:], in0=rr[:], scalar1=-3.141592653589793, scalar2=TWO_PI,
                                                       op0=ALU.is_lt, op1=ALU.mult), reads=["n_rr"], writes=["n_kf"])
                sc.op("dve", lambda e: e.tensor_tensor(out=rr[:], in0=rr[:], in1=kf[:], op=ALU.add), reads=["n_rr", "n_kf"], writes=["n_rr"])
                sc.op("dve", lambda e: e.tensor_scalar(out=rr[:], in0=rr[:], scalar1=3.1415925, scalar2=-3.1415925,
                                                       op0=ALU.min, op1=ALU.max), reads=["n_rr"], writes=["n_rr"])
                sc.op("act", lambda e: e.activation(out=tab2[:], in_=rr[:], func=AF.Sin), reads=["n_rr"], writes=["n_tab2"])
                for c in range(8):
                    sc.dma("sp", lambda e, tab=tab, c=c: e.dma_start(out=tab[0:16, c * 256:(c + 1) * 256],
                                                                     in_=tab2[c * 16:(c + 1) * 16, :]),
                           reads=["n_tab2"], writes=[tkey])
            sc.dma("sp", lambda e, sb=sb: e.dma_start(out=gsig[:], in_=P_tm[sb:sb + 2048, 3328:3352].rearrange("(tt p) c -> p tt c", p=128)),
                   reads=["P_tm"], writes=["n_gsig"])
            sc.op("act", lambda e: e.activation(out=gsig[:], in_=gsig[:], func=AF.Sigmoid), reads=["n_gsig"], writes=["n_gsig"])
            for g in range(2):
                def load_fm(fmidx, half, dstap, dkey, eng="sp", sb=sb):
                    row0 = fmidx * 128 + half * 64
                    sc.dma(eng, lambda e, row0=row0, dstap=dstap, sb=sb: e.dma_start(out=dstap, in_=P_fm[row0:row0 + 64, sb:sb + 2048]),
                           reads=["P_fm"], writes=[dkey])
                for r in range(4):
                    hh = g * 4 + r
                    load_fm(8 + hh // 2, hh % 2, src[:], "n_src")
                    for q2 in range(2):
                        css = [slice(qb * 512, (qb + 1) * 512) for qb in (2 * q2, 2 * q2 + 1)]
                        norm_rope([(src[:, cs], "n_src", 512, 0, CT[:, cs], ST[:, cs], qT[r][0:64, cs], "n_qT%d" % r) for cs in css])
                for fmidx, wc, dst, dkey in ((14, 2, ksT, "n_ksT"), (15, 3, kwT, "n_kwT")):
                    load_fm(fmidx, g, src[:], "n_src")
                    for q2 in range(2):
                        css = [slice(qb * 512, (qb + 1) * 512) for qb in (2 * q2, 2 * q2 + 1)]
                        norm_rope([(src[:, cs], "n_src", 512, wc, CT[:, cs], ST[:, cs], dst[0:64, cs], dkey) for cs in css])
                for col0, va, vk in ((2944, vsa, "n_vsa"), (3200, vwa, "n_vwa")):
                    sc.dma("sp", lambda e, col0=col0, g=g, sb=sb: e.dma_start(
                        out=vld[:], in_=P_tm[sb:sb + 2048, col0 + g * 64:col0 + g * 64 + 64].rearrange("(tt p) c -> p tt c", p=128)),
                        reads=["P_tm"], writes=["n_vld"])
                    sc.op("act", lambda e, va=va: e.copy(out=va[:, :, 0:64], in_=vld[:]), reads=["n_vld"], writes=[vk])
                for kv in range(2):
                    load_fm(12 + kv, g, kcb[:], "n_kcb", eng="pool")
                    for hh in range(2):
                        for l in range(32):
                            sc.op("pe", lambda e, kv=kv, hh=hh, l=l: e.matmul(
                                ps_x[0][:, 0:127], lhsT=w1[kv][:, l, hh * 128:(hh + 1) * 128], rhs=kcb[:, l:l + 2017:16],
                                start=(l == 0), stop=(l == 31)), reads=["n_w1%d" % kv, "n_kcb"], writes=["np_s2"])
                        sc.op("act", lambda e, kv=kv, hh=hh: e.activation(out=hx[:], in_=ps_x[0][:, 0:127], func=AF.Identity,
                                                                          bias=cbias[:, kv, hh:hh + 1]),
                              reads=["np_s2", "n_cbias"], writes=["n_hx"])
                        sc.op("act", lambda e: e.activation(out=hx2[:], in_=hx[:], func=AF.Square), reads=["n_hx"], writes=["n_hx2"])
                        sc.op("dve", lambda e: e.tensor_scalar(out=hx2[:], in0=hx2[:], scalar1=0.044715, scalar2=1.0, op0=ALU.mult,
                                                               op1=ALU.add), reads=["n_hx2"], writes=["n_hx2"])
                        sc.op("dve", lambda e: e.tensor_tensor(out=hx2[:], in0=hx2[:], in1=hx[:], op=ALU.mult),
                              reads=["n_hx2", "n_hx"], writes=["n_hx2"])
                        sc.op("act", lambda e: e.activation(out=hx2[:], in_=hx2[:], func=AF.Sigmoid, scale=1.5957691216057308),
                              reads=["n_hx2"], writes=["n_hx2"])
                        sc.op("dve", lambda e, hh=hh: e.tensor_tensor(out=hT[:, hh, :], in0=hx[:], in1=hx2[:], op=ALU.mult),
                              reads=["n_hx", "n_hx2"], writes=["n_hT"])
                    if kv == 0:
                        for hh in range(2):
                            sc.op("pe", lambda e, hh=hh: e.matmul(ps_x[1][0:64, 0:127], lhsT=w2[0][:, hh, :], rhs=hT[:, hh, :],
                                                                  start=(hh == 0), stop=(hh == 1)),
                                  reads=["n_w20", "n_hT"], writes=["np_s3"])
                        sc.op("act", lambda e: e.copy(out=src[:, 0:127], in_=ps_x[1][0:64, 0:127]), reads=["np_s3"], writes=["n_src"])
                        norm_rope([(src[:, 0:127], "n_src", 127, 1, CT[:, 31:2048:16], ST[:, 31:2048:16], kcmpT[0:64, 0:127], "n_kcmpT")])
                    else:
                        for hh in range(2):
                            sc.op("pe", lambda e, hh=hh: e.matmul(ps_x[1][0:127, 0:64], lhsT=hT[:, hh, :], rhs=w2[1][:, hh, :],
                                                                  start=(hh == 0), stop=(hh == 1)),
                                  reads=["n_w21", "n_hT"], writes=["np_s3"])
                        sc.op("act", lambda e: e.copy(out=vcaug[0:127, 0:64], in_=ps_x[1][0:127, 0:64]), reads=["np_s3"], writes=["n_vcaug"])
                attn(kcmpT, "n_kcmpT", lambda kt: vcaug, "n_vcaug", 97, lambda qb: [0],
                     lambda kt, qb: [(ident_b[0:127, 0:127], cmask[0:127, qb, :], ["ident_b", "n_cmask"])],
                     lambda kt: 127, mk_epi(g, 0, True), krows=96)
                sc.op("dve", lambda e: e.tensor_tensor(out=score[:], in0=imp[:], in1=valid[:], op=ALU.mult),
                      reads=["n_imp", "n_valid"], writes=["n_score"])
                sc.op("dve", lambda e: e.tensor_tensor(out=score[:], in0=score[:], in1=addc[:], op=ALU.add),
                      reads=["n_score", "n_addc"], writes=["n_score"])
                for g4 in range(4):
                    tts = [g4 * 4 + k for k in range(4)]
                    for k, tt in enumerate(tts):
                        sc.op("dve", lambda e, tt=tt, k=k: e.max(out=m8a[:, k, :], in_=score[:, tt, :]),
                              reads=["n_score"], writes=["n_m8a%d" % k])
                    for k, tt in enumerate(tts):
                        sc.op("dve", lambda e, tt=tt, k=k: e.match_replace(out=swk[:, k, :], in_to_replace=m8a[:, k, :],
                                                                           in_values=score[:, tt, :], imm_value=-3e38),
                              reads=["n_score", "n_m8a%d" % k], writes=["n_swk%d" % k])
                    for k, tt in enumerate(tts):
                        sc.op("dve", lambda e, k=k: e.max(out=m8b[:, k, :], in_=swk[:, k, :]),
                              reads=["n_swk%d" % k], writes=["n_m8b%d" % k])
                    for k, tt in enumerate(tts):
                        sc.op("dve", lambda e, tt=tt, k=k: e.tensor_scalar(out=sel[:, k, :], in0=score[:, tt, :], scalar1=m8b[:, k, 7:8],
                                                                           scalar2=None, op0=ALU.is_ge),
                              reads=["n_score", "n_m8b%d" % k], writes=["n_sel%d" % k])
                    sc.op("dve", lambda e: e.tensor_scalar(out=selb[:, :, 64:96], in0=sel[:], scalar1=-NEG, scalar2=NEG, op0=ALU.mult,
                                                           op1=ALU.add), reads=["n_sel%d" % k for k in range(4)], writes=["n_selb"])
                    pst = ps_x[0][:, 0:256].bitcast(BF16).rearrange("p (a b) -> p a b", b=128)
                    for k in range(4):
                        sc.op("pe", lambda e, pst=pst, k=k: e.transpose(out=pst[0:96, k, :], in_=selb[:, k, :], identity=ident_b[:]),
                              reads=["n_selb", "ident_b"], writes=["np_s2"])
                    sc.op("act", lambda e, g4=g4, pst=pst: e.copy(
                        out=nselT[64:96, g4 * 512:(g4 + 1) * 512].rearrange("p (a b) -> p a b", b=128), in_=pst[64:96, :, :]),
                        reads=["np_s2"], writes=["n_nselT"])
                for r in range(4):
                    sc.op("act" if r % 2 == 0 else "dve",
                          (lambda e, r=r: e.copy(out=qT[r][64:96, :], in_=nselT[64:96, :])) if r % 2 == 0 else
                          (lambda e, r=r: e.tensor_copy(out=qT[r][64:96, :], in_=nselT[64:96, :])),
                          reads=["n_nselT"], writes=["n_qT%d" % r])
                attn(ksT, "n_ksT", lambda kt: vsa[:, kt, :], "n_vsa", 65, lambda qb: list(range(0, 4 * qb + 4)),
                     lambda kt, qb: [], lambda kt: 128, mk_epi(g, 1, False), krows=96,
                     post_fn=lambda kt, qb: dm01[:, 4 + kt - 4 * qb, :] if kt >= 4 * qb else None)
                attn(kwT, "n_kwT", lambda kt: vwa[:, kt, :], "n_vwa", 65, lambda qb: list(range(max(0, 4 * qb - 4), 4 * qb + 4)),
                     lambda kt, qb: [], lambda kt: 128, mk_epi(g, 2, False), krows=96,
                     post_fn=lambda kt, qb: dm01[:, 4 + kt - 4 * qb, :])
                o3 = oacc[:].rearrange("p t r d -> p (t r) d")
                sc.op("dve", lambda e: e.tensor_tensor(out=ssq[:], in0=o3, in1=o3, op=ALU.mult), reads=["n_oacc"], writes=["n_ssq"])
                sc.op("dve", lambda e: e.tensor_reduce(out=ss64[:], in_=ssq[:], axis=AX.X, op=ALU.add), reads=["n_ssq"], writes=["n_ss64"])
                sc.op("act", lambda e: e.activation(out=ss64[:], in_=ss64[:], func=AF.Sqrt, scale=1.0 / 64, bias=epsc[:]),
                      reads=["n_ss64"], writes=["n_ss64"])
                sc.op("dve", lambda e: e.reciprocal(out=ss64[:], in_=ss64[:]), reads=["n_ss64"], writes=["n_ss64"])
                sc.op("dve", lambda e: e.tensor_tensor(out=ssq[:], in0=o3, in1=ss64[:].unsqueeze(2).broadcast_to([128, 64, 64]),
                                                       op=ALU.mult), reads=["n_oacc", "n_ss64"], writes=["n_ssq"])
                sc.op("dve", lambda e: e.tensor_tensor(out=ssq[:], in0=ssq[:], in1=onwb[:].unsqueeze(1).broadcast_to([128, 64, 64]),
                                                       op=ALU.mult), reads=["n_ssq", "n_onwb"], writes=["n_ssq"])
                for tt in range(16):
                    sc.dma("sp", lambda e, tt=tt, g=g, sb=sb: e.dma_start(
                        out=Y_tm[sb + tt * 128:sb + (tt + 1) * 128, 512 + g * 256:512 + (g + 1) * 256],
                        in_=ssq[:, tt * 4:(tt + 1) * 4, :].rearrange("p r d -> p (r d)")), reads=["n_ssq"], writes=["Y_tm"])
        barrier(sc)


def stage_uv(nc, sc, u_tab, v_tab, UV):
    with contextlib.ExitStack() as st:
        tmp = [st.enter_context(nc.sbuf_tensor("uv_tmp%d" % i, [128, 8, D], BF16)) for i in range(3)]
        UVv = UV.rearrange("(p r) d -> p r d", p=128)
        k = 0
        for half, tab in enumerate((u_tab, v_tab)):
            tv = tab.rearrange("(p r) d -> p r d", p=128)
            for ci in range(16):
                b = k % 3
                k += 1
                sc.dma("pool", lambda e, b=b, tv=tv, ci=ci: e.dma_start(out=tmp[b][:], in_=tv[:, ci * 8:(ci + 1) * 8, :]),
                       writes=["uv_tmp%d" % b])
                sc.dma("sp", lambda e, b=b, ci=ci, half=half: e.dma_start(
                    out=UVv[:, ci * 8:(ci + 1) * 8, half * D:(half + 1) * D], in_=tmp[b][:]),
                    reads=["uv_tmp%d" % b], writes=["UV"])
        barrier(sc)


def stage_out(nc, sc, stack, NT, x, Y_tm, w_out, out, peer):
    T = lambda name, shape, dt: stack.enter_context(nc.sbuf_tensor(name, shape, dt))
    wout = T("o_wout", [128, 8, D], BF16)
    yt = [T("o_yt%d" % i, [128, D], F32) for i in range(2)]
    xt = [T("o_xt%d" % i, [128, D], F32) for i in range(2)]
    ht = [T("o_ht%d" % i, [128, D], F32) for i in range(3)]
    ybf = T("o_ybf", [128, D], BF16)
    yT = T("o_yT", [128, 8, 128], BF16)
    sc.dma("pool", lambda e: e.dma_start(out=wout[:], in_=w_out.rearrange("(kc p) n -> p kc n", p=128)), writes=["o_wout"])

    def tile_ops(i):
        b = i % 2
        hb = i % 3
        ops = []
        add = lambda *a, **k: ops.append(lambda: sc.op(*a, **k))
        ops.append(lambda: sc.dma("sp", lambda e: e.dma_start(out=yt[b][:], in_=Y_tm[i * 128:(i + 1) * 128, :]),
                                  reads=["Y_tm"], writes=["o_yt%d" % b]))
        ops.append(lambda: sc.dma("sp", lambda e: e.dma_start(out=xt[b][:], in_=x[i * 128:(i + 1) * 128, :]),
                                  writes=["o_xt%d" % b]))
        add("act", lambda e: e.copy(out=ybf[:], in_=yt[b][:]), reads=["o_yt%d" % b], writes=["o_ybf"])
        for kc in range(8):
            add("pe", lambda e, kc=kc: e.transpose(out=peer.ps_t_b[:, kc, :], in_=ybf[:, kc * 128:(kc + 1) * 128],
                                                   identity=peer.ident_b[:]), reads=["o_ybf", "ident_b"], writes=["pp_t"])
        add("act", lambda e: e.copy(out=yT[:], in_=peer.ps_t_b), reads=["pp_t"], writes=["o_yT"])
        for hf in range(2):
            ps = peer.ps_a if hf == 0 else peer.ps_b
            pk = "pp_a" if hf == 0 else "pp_b"
            for kc in range(8):
                add("pe", lambda e, hf=hf, kc=kc, ps=ps: e.matmul(ps[:].rearrange("p a b -> p (a b)"), lhsT=yT[:, kc, :],
                                                                  rhs=wout[:, kc, hf * 512:(hf + 1) * 512],
                                                                  start=(kc == 0), stop=(kc == 7)),
                    reads=["o_yT", "o_wout"], writes=[pk])
            add("dve", lambda e, hf=hf, ps=ps: e.tensor_tensor(
                out=ht[hb][:, hf * 512:(hf + 1) * 512], in0=ps[:].rearrange("p a b -> p (a b)"),
                in1=xt[b][:, hf * 512:(hf + 1) * 512], op=ALU.add), reads=[pk, "o_xt%d" % b], writes=["o_ht%d" % hb])
        return ops + peer.pre_ops(i, ht[hb][:], "o_ht%d" % hb)

    for f in tile_ops(0):
        f()
    for i in range(NT):
        pending = tile_ops(i + 1) if i + 1 < NT else []
        peer.loop(i, ht[i % 3][:], "o_ht%d" % (i % 3), out[i * 128:(i + 1) * 128, :], "out", pending)


WNAMES = ["norm1_w", "w_in", "hg_lb_logits", "hg_out_norm_w", "nsa_q_norm_w", "nsa_k_norm_w", "cmp_pe_k", "cmp_pe_v",
          "cmp_w1_k", "cmp_w2_k", "cmp_w1_v", "cmp_w2_v", "nsa_out_norm_w", "w_out", "norm2_w", "peer_w_q",
          "peer_sub_keys", "peer_u", "peer_v"]
WSHAPES = {"norm1_w": [1, D], "w_in": [D, INW], "hg_lb_logits": [2, 512], "hg_out_norm_w": [1, 128],
           "nsa_q_norm_w": [1, 64], "nsa_k_norm_w": [3, 64], "cmp_pe_k": [32, 64], "cmp_pe_v": [32, 64],
           "cmp_w1_k": [2048, 256], "cmp_w2_k": [256, 64], "cmp_w1_v": [2048, 256], "cmp_w2_v": [256, 64],
           "nsa_out_norm_w": [1, 64], "w_out": [D, D], "norm2_w": [1, D], "peer_w_q": [D, 2048],
           "peer_sub_keys": [2, 128, 128], "peer_u": [16384, D], "peer_v": [16384, D]}


def build_program(NSEQ):
    NT = NSEQ * 16
    nc = bass.Bass("TRN2", target_bir_lowering=False)
    din = lambda name, shape, dt=F32: nc.dram_tensor(name, shape, dt, kind="ExternalInput").ap()
    x = din("x", [NT * 128, D])
    positions = din("positions", [NSEQ, 2048], I32)
    w = {k: din(k, v) for k, v in WSHAPES.items()}
    cst = {k: din(k, v) for k, v in CONST_SHAPES.items()}
    out = nc.dram_tensor("out", [NT * 128, D], F32, kind="ExternalOutput").ap()
    P_tm = nc.dram_tensor("P_tm", [NT * 128, INW], F32, kind="Internal").ap()
    P_fm = nc.dram_tensor("P_fm", [16 * 128, NT * 128], F32, kind="Internal").ap()
    Y_tm = nc.dram_tensor("Y_tm", [NT * 128, D], F32, kind="Internal").ap()
    UV = nc.dram_tensor("UV", [16384, 2 * D], BF16, kind="Internal").ap()
    with contextlib.ExitStack() as stack:
        sc = Sched(nc, stack)
        T = lambda name, shape, dt: stack.enter_context(nc.sbuf_tensor(name, shape, dt))
        ident_f = T("ident_f", [128, 128], F32)
        ident_b = T("ident_b", [128, 128], BF16)
        ublk = T("ublk", [128, 128], F32)
        wrev = T("wrev", [128, 128], F32)
        epsc = T("epsc", [128, 1], F32)
        pc = T("pc", [128, 64], F32)
        sc.dma("sp", lambda e: e.dma_start(out=ident_f[:], in_=cst["c_identf"]), writes=["ident_f"])
        sc.dma("pool", lambda e: e.dma_start(out=ident_b[:], in_=cst["c_identf"]), writes=["ident_b"])
        sc.dma("sp", lambda e: e.dma_start(out=ublk[:], in_=cst["c_ublk"]), writes=["ublk"])
        sc.dma("sp", lambda e: e.dma_start(out=wrev[:], in_=cst["c_wrev"]), writes=["wrev"])
        sc.dma("sp", lambda e: e.dma_start(out=pc[:], in_=cst["c_pc"]), writes=["pc"])
        sc.op("dve", lambda e: e.memset(epsc[:], EPS), writes=["epsc"])
        barrier(sc)
        stage_proj(nc, sc, NT, x, w["norm1_w"], w["w_in"], P_tm, P_fm, ident_b, epsc,
                   uv=(w["peer_u"], w["peer_v"], UV))
        stage_hgrn(nc, sc, NT, P_tm, P_fm, Y_tm, w["hg_lb_logits"], w["hg_out_norm_w"], ublk, wrev, epsc,
                   cst["c_rowm"], cst["c_colm"])
        stage_nsa(nc, sc, NSEQ, P_tm, P_fm, Y_tm, positions, w["nsa_q_norm_w"], w["nsa_k_norm_w"], w["cmp_pe_k"],
                  w["cmp_pe_v"], w["cmp_w1_k"], w["cmp_w2_k"], w["cmp_w1_v"], w["cmp_w2_v"], w["nsa_out_norm_w"],
                  cst, ident_b, epsc)
        with contextlib.ExitStack() as st2:
            peer = Peer(nc, sc, st2, w["norm2_w"], w["peer_w_q"], w["peer_sub_keys"], UV, ident_f, ident_b, pc, cst["c_wsel"])
            stage_out(nc, sc, st2, NT, x, Y_tm, w["w_out"], out, peer)
            sc.finish()
            with nc.Block() as block:
                sc.replay(block)
    return nc


def make_inputs(inputs, c0, c1):
    m = {"x": np.ascontiguousarray(inputs["x"][c0:c1]).reshape(-1, D).astype(np.float32, copy=False),
         "positions": np.ascontiguousarray(inputs["positions"][c0:c1]).astype(np.int32, copy=False)}
    for k in WNAMES:
        a = np.asarray(inputs[k])
        if k != "hg_lb_logits":
            a = a[0]
        m[k] = np.ascontiguousarray(a, dtype=np.float32).reshape(WSHAPES[k])
    return m


def kernel(**inputs):
    ncores = 8
    B = inputs["x"].shape[0]
    per = B // ncores
    nc = build_program(per)
    consts = host_consts()
    in_maps = []
    for c in range(ncores):
        m = make_inputs(inputs, c * per, (c + 1) * per)
        m.update(consts)
        in_maps.append(m)
    res = run_bass_kernel_spmd(nc, in_maps, core_ids=list(range(ncores)))
    outs = [np.asarray(r["out"]).reshape(per, S, D) for r in res.results]
    return np.concatenate(outs, axis=0).astype(np.float32, copy=False)
```

```python
import contextlib
import numpy as np
import ml_dtypes
import concourse.bass as bass
import concourse.mybir as mybir
from concourse.bass_utils import run_bass_kernel_spmd

F32 = mybir.dt.float32
BF16 = mybir.dt.bfloat16
I32 = mybir.dt.int32
U32 = mybir.dt.uint32
AF = mybir.ActivationFunctionType
ALU = mybir.AluOpType
AX = mybir.AxisListType

ENGS = ("pe", "dve", "act", "pool", "sp")
NEG = -30000.0


class Sched:
    def __init__(self, nc, stack, ndma=12):
        self.nc = nc
        self.q = {e: [] for e in ENGS}
        self.cnt = {e: 0 for e in ENGS}
        self.esem = {e: stack.enter_context(nc.semaphore("es_" + e)) for e in ENGS}
        self.ndma = ndma
        self.dsem = {e: [stack.enter_context(nc.semaphore("ds_%s_%d" % (e, i))) for i in range(ndma)]
                     for e in ("sp", "pool", "act")}
        self.dcnt = {e: 0 for e in ("sp", "pool", "act")}
        self.seen = {e: {} for e in ENGS}
        self.st = {}
        self.ninst = 0

    def _sem(self, key):
        if key[0] == "dma":
            return self.dsem[key[1]][key[2]]
        return self.esem[key[0]]

    def _wait(self, eng, ev):
        key, val = ev
        if self.seen[eng].get(key, 0) >= val:
            return
        self.seen[eng][key] = val
        sem = self._sem(key)
        self.q[eng].append(lambda e, sem=sem, val=val: e.wait_ge(sem, val))
        self.ninst += 1

    def _deps(self, eng, reads, writes):
        deps = []
        for k in reads:
            s = self.st.get(k)
            if s and s[0] is not None:
                deps.append(s[0])
            if s and k[1:3] == "p_":
                deps.extend(ev for ek, ev in s[1].items() if ek != (eng,))
        for k in writes:
            s = self.st.get(k)
            if s:
                if s[0] is not None:
                    deps.append(s[0])
                deps.extend(s[1].values())
        for ev in deps:
            if eng == "pe" and ev[0] == ("pe",):
                continue
            self._wait(eng, ev)

    def _commit(self, ev, evkey, reads, writes):
        for k in reads:
            s = self.st.setdefault(k, [None, {}])
            s[1][evkey] = ev
        for k in writes:
            self.st[k] = [ev, {}]

    def op(self, eng, fn, reads=(), writes=()):
        self._deps(eng, reads, writes)
        self.cnt[eng] += 1
        sem = self.esem[eng]
        self.q[eng].append(lambda e, fn=fn, sem=sem: fn(e).then_inc(sem, 1))
        self.ninst += 1
        ev = ((eng,), self.cnt[eng])
        self._commit(ev, (eng,), reads, writes)

    def dma(self, eng, fn, reads=(), writes=()):
        self._deps(eng, reads, writes)
        k = self.dcnt[eng]
        slot = k % self.ndma
        key = ("dma", eng, slot)
        if k >= self.ndma:
            self._wait(eng, (key, 16 * (k // self.ndma)))
        self.dcnt[eng] += 1
        val = 16 * (k // self.ndma + 1)
        sem = self.dsem[eng][slot]
        self.q[eng].append(lambda e, fn=fn, sem=sem: fn(e).then_inc(sem, 16))
        self.ninst += 1
        ev = (key, val)
        self._commit(ev, key, reads, writes)

    def finish(self):
        for eng in ("sp", "pool", "act"):
            k = self.dcnt[eng]
            for slot in range(min(k, self.ndma)):
                n = (k - 1 - slot) // self.ndma + 1
                self._wait(eng, (("dma", eng, slot), 16 * n))

    def replay(self, block):
        q = self.q

        @block.sync
        def _(e):
            for f in q["sp"]:
                f(e)

        @block.gpsimd
        def _(e):
            for f in q["pool"]:
                f(e)

        @block.tensor
        def _(e):
            for f in q["pe"]:
                f(e)

        @block.scalar
        def _(e):
            for f in q["act"]:
                f(e)

        @block.vector
        def _(e):
            for f in q["dve"]:
                f(e)


D = 1024
S = 2048
PEER_HEADS = 8
PEER_K = 16
EPS = 1e-6


def peer_consts():
    c = np.zeros((128, 64), np.float32)
    c[:, 0:16] = np.arange(16, dtype=np.float32)[None, :] * 16.0
    c[:, 16:32] = np.arange(16, dtype=np.float32)[None, :]
    return c


class Peer:
    def __init__(self, nc, sc, stack, norm2_w, w_q, sub_keys, uv_tab, ident_f, ident_b, pc, wsel_dram, NB=10):
        self.nc, self.sc = nc, sc
        self.uv_tab = uv_tab
        self.NB = NB
        T = lambda name, shape, dt: stack.enter_context(nc.sbuf_tensor(name, shape, dt))
        P = lambda name, shape, dt: stack.enter_context(nc.psum_tensor(name, shape, dt))
        self.ident_f, self.ident_b, self.pc = ident_f, ident_b, pc
        self.w2b = T("pr_w2b", [128, D], F32)
        self.wq = T("pr_wq", [128, 4, 2, 2048], BF16)
        self.skT = T("pr_skT", [128, 2, 128], BF16)
        self.skl = T("pr_skl", [128, 2, 128], F32)
        self.junk = T("pr_junk", [128, D], BF16)
        self.ss = T("pr_ss", [128, 1], F32)
        self.rstd = T("pr_rstd", [128, 1], F32)
        self.xn = T("pr_xn", [128, D], BF16)
        self.xnT = [T("pr_xnT%d" % i, [128, 4, 128, 2], BF16) for i in range(2)]
        self.qT = T("pr_qT", [128, 16, 128], BF16)
        self.Ssb = T("pr_S", [128, 16, 128], F32)
        self.Swk = T("pr_Swk", [128, 128], F32)
        self.v = T("pr_v", [128, 16, 16], F32)
        self.ix = T("pr_ix", [128, 16, 16], U32)
        self.ixf = T("pr_ixf", [128, 16, 16], F32)
        self.cand = T("pr_cand", [128, 8, 256], F32)
        self.cwk = T("pr_cwk", [128, 256], F32)
        self.tv = T("pr_tv", [128, 8, 16], F32)
        self.pos = T("pr_pos", [128, 8, 16], U32)
        self.posa = T("pr_posa", [128, 8, 16], U32)
        self.posb = T("pr_posb", [128, 8, 16], U32)
        self.epsc = T("pr_epsc", [128, 1], F32)
        self.pb = T("pr_pb", [128, 8, 16], F32)
        self.pa = T("pr_pa", [128, 8, 16], F32)
        self.eq = T("pr_eq", [128, 8, 16, 16], F32)
        self.e1 = T("pr_e1", [128, 8, 16], F32)
        self.e2 = T("pr_e2", [128, 8, 16], F32)
        self.gt = T("pr_gt", [128, 8, 16], F32)
        self.gs = T("pr_gs", [128, 8], F32)
        self.eTi = [T("pr_eTi%d" % i, [128, 128], I32) for i in range(2)]
        self.eTf = T("pr_eTf", [128, 128], F32)
        self.gT = [T("pr_gT%d" % i, [128, 128], F32) for i in range(2)]
        self.actb = T("pr_actb", [128, 128], BF16)
        self.oT = T("pr_oT", [128, 8, 128], F32)
        self.osb = T("pr_osb", [128, D], F32)
        self.G = [T("pr_G%d" % i, [128, 2 * D], BF16) for i in range(NB)]
        self.GT = [T("pr_GT%d" % i, [128, 4, 128, 2], BF16) for i in range(2)]
        self.hg = T("pr_hg", [128, 128], F32)
        self.wsel = T("pr_wsel", [128, 256], BF16)
        self.actD = [T("pr_actD%d" % i, [128, 128], BF16) for i in range(3)]
        self.ps_a = P("pp_a", [128, 4, 128], F32)
        self.ps_b = P("pp_b", [128, 4, 128], F32)
        self.ps_t = P("pp_t", [128, 4, 128], F32)
        self.ps_t_b = self.ps_t[:].rearrange("p a b -> p (a b)").bitcast(BF16).rearrange("p (a b) -> p a b", b=128)
        self.ps_g = [P("pp_g%d" % i, [128, 4, 128], F32) for i in range(2)]
        self.ps_hd = P("pp_hd", [128, 512], F32)
        self.ps_o = P("pp_o", [128, 8, 128], F32)
        self.tok = 0

        sc.op("dve", lambda e: e.memset(self.epsc[:], EPS), writes=["pr_epsc"])
        sc.dma("pool", lambda e: e.dma_start(out=self.wsel[:], in_=wsel_dram), writes=["pr_wsel"])
        sc.dma("sp", lambda e: e.dma_start(out=self.w2b[:], in_=norm2_w[0:1, :].partition_broadcast(128)),
               writes=["pr_w2b"])
        sc.dma("pool", lambda e: e.dma_start(out=self.wq[:], in_=w_q.rearrange("(c dp two) n -> dp c two n", dp=128, two=2)),
               writes=["pr_wq"])
        sc.dma("sp", lambda e: e.dma_start(out=self.skl[:], in_=sub_keys.rearrange("j n d -> n j d")),
               writes=["pr_skl"])
        for j in range(2):
            sc.op("pe", lambda e, j=j: e.transpose(out=self.ps_a[:, j, :], in_=self.skl[:, j, :], identity=ident_f[:]),
                  reads=["pr_skl", "ident_f"], writes=["pp_a"])
        sc.op("act", lambda e: e.copy(out=self.skT[:], in_=self.ps_a[:, 0:2, :]), reads=["pp_a"], writes=["pr_skT"])

    def pre_ops(self, i, h_t, hkey):
        sc = self.sc
        p = i % 2
        ops = []
        add = lambda *a, **k: ops.append(lambda: sc.op(*a, **k))
        xnT, eTi, gT = self.xnT[p], self.eTi[p], self.gT[p]
        kxnT, keTi, kgT = "pr_xnT%d" % p, "pr_eTi%d" % p, "pr_gT%d" % p
        add("act", lambda e: e.activation(out=self.junk[:], in_=h_t, func=AF.Square, accum_out=self.ss[:]),
            reads=[hkey], writes=["pr_junk", "pr_ss"])
        add("act", lambda e: e.activation(out=self.rstd[:], in_=self.ss[:], func=AF.Sqrt, scale=1.0 / D, bias=self.epsc[:]),
            reads=["pr_ss"], writes=["pr_rstd"])
        add("dve", lambda e: e.reciprocal(out=self.rstd[:], in_=self.rstd[:]), reads=["pr_rstd"], writes=["pr_rstd"])
        add("dve", lambda e: e.scalar_tensor_tensor(out=self.xn[:], in0=h_t, scalar=self.rstd[:, 0:1],
                                                    in1=self.w2b[:], op0=ALU.mult, op1=ALU.mult),
            reads=[hkey, "pr_rstd", "pr_w2b"], writes=["pr_xn"])
        xnf = self.xn[:].bitcast(F32)
        for c in range(4):
            add("pe", lambda e, c=c: e.transpose(out=self.ps_t[:, c, :], in_=xnf[:, c * 128:(c + 1) * 128],
                                                 identity=self.ident_f[:]),
                reads=["pr_xn", "ident_f"], writes=["pp_t"])
        add("act", lambda e: e.copy(out=xnT[:].rearrange("p c t two -> p (c t two)"),
                                    in_=self.ps_t_b.rearrange("p a b -> p (a b)")), reads=["pp_t"], writes=[kxnT])
        for grp in range(4):
            ps = self.ps_a if grp % 2 == 0 else self.ps_b
            pk = "pp_a" if grp % 2 == 0 else "pp_b"
            for c4 in range(4):
                cq = grp * 4 + c4
                for kc in range(8):
                    add("pe", lambda e, ps=ps, c4=c4, cq=cq, kc=kc: e.matmul(
                        ps[:, c4, :], lhsT=self.wq[:, kc // 2, kc % 2, cq * 128:(cq + 1) * 128], rhs=xnT[:, kc // 2, :, kc % 2],
                        start=(kc == 0), stop=(kc == 7)), reads=["pr_wq", kxnT], writes=[pk])
            add("act", lambda e, ps=ps, grp=grp: e.copy(out=self.qT[:, grp * 4:(grp + 1) * 4, :], in_=ps[:]),
                reads=[pk], writes=["pr_qT%d" % grp])
        for grp in range(4):
            ps = self.ps_a if grp % 2 == 0 else self.ps_b
            pk = "pp_a" if grp % 2 == 0 else "pp_b"
            for c4 in range(4):
                cq = grp * 4 + c4
                add("pe", lambda e, ps=ps, c4=c4, cq=cq: e.matmul(
                    ps[:, c4, :], lhsT=self.qT[:, cq, :], rhs=self.skT[:, cq % 2, :], start=True, stop=True),
                    reads=["pr_qT%d" % grp, "pr_skT"], writes=[pk])
            add("act", lambda e, ps=ps, grp=grp: e.copy(out=self.Ssb[:, grp * 4:(grp + 1) * 4, :], in_=ps[:]),
                reads=[pk], writes=["pr_S%d" % grp])
        for cq in range(16):
            sk = "pr_S%d" % (cq // 4)
            add("dve", lambda e, cq=cq: e.max(out=self.v[:, cq, 0:8], in_=self.Ssb[:, cq, :]), reads=[sk], writes=["pr_v"])
            add("dve", lambda e, cq=cq: e.max_index(out=self.ix[:, cq, 0:8], in_max=self.v[:, cq, 0:8],
                                                    in_values=self.Ssb[:, cq, :]), reads=[sk, "pr_v"], writes=["pr_ix"])
            add("dve", lambda e, cq=cq: e.match_replace(out=self.Swk[:], in_to_replace=self.v[:, cq, 0:8],
                                                        in_values=self.Ssb[:, cq, :], imm_value=-1e30),
                reads=[sk, "pr_v"], writes=["pr_Swk"])
            add("dve", lambda e, cq=cq: e.max(out=self.v[:, cq, 8:16], in_=self.Swk[:]), reads=["pr_Swk"], writes=["pr_v"])
            add("dve", lambda e, cq=cq: e.max_index(out=self.ix[:, cq, 8:16], in_max=self.v[:, cq, 8:16],
                                                    in_values=self.Swk[:]), reads=["pr_Swk", "pr_v"], writes=["pr_ix"])
        add("dve", lambda e: e.tensor_copy(out=self.ixf[:], in_=self.ix[:]), reads=["pr_ix"], writes=["pr_ixf"])
        for h in range(8):
            add("dve", lambda e, h=h: e.tensor_tensor(
                out=self.cand[:, h, :].rearrange("p (a b) -> p a b", b=16),
                in0=self.v[:, 2 * h, :].unsqueeze(2).broadcast_to([128, 16, 16]),
                in1=self.v[:, 2 * h + 1, :].unsqueeze(1).broadcast_to([128, 16, 16]), op=ALU.add),
                reads=["pr_v"], writes=["pr_cand"])
        for h in range(8):
            add("dve", lambda e, h=h: e.max(out=self.tv[:, h, 0:8], in_=self.cand[:, h, :]), reads=["pr_cand"], writes=["pr_tv"])
            add("dve", lambda e, h=h: e.max_index(out=self.pos[:, h, 0:8], in_max=self.tv[:, h, 0:8],
                                                  in_values=self.cand[:, h, :]), reads=["pr_cand", "pr_tv"], writes=["pr_pos"])
            add("dve", lambda e, h=h: e.match_replace(out=self.cwk[:], in_to_replace=self.tv[:, h, 0:8],
                                                      in_values=self.cand[:, h, :], imm_value=-1e30),
                reads=["pr_cand", "pr_tv"], writes=["pr_cwk"])
            add("dve", lambda e, h=h: e.max(out=self.tv[:, h, 8:16], in_=self.cwk[:]), reads=["pr_cwk"], writes=["pr_tv"])
            add("dve", lambda e, h=h: e.max_index(out=self.pos[:, h, 8:16], in_max=self.tv[:, h, 8:16],
                                                  in_values=self.cwk[:]), reads=["pr_cwk", "pr_tv"], writes=["pr_pos"])
        add("dve", lambda e: e.tensor_scalar(out=self.posb[:], in0=self.pos[:], scalar1=15, scalar2=None,
                                             op0=ALU.bitwise_and), reads=["pr_pos"], writes=["pr_posb"])
        add("dve", lambda e: e.tensor_scalar(out=self.posa[:], in0=self.pos[:], scalar1=240, scalar2=None,
                                             op0=ALU.bitwise_and), reads=["pr_pos"], writes=["pr_posa"])
        add("dve", lambda e: e.tensor_copy(out=self.pb[:], in_=self.posb[:]), reads=["pr_posb"], writes=["pr_pb"])
        add("dve", lambda e: e.tensor_copy(out=self.pa[:], in_=self.posa[:]), reads=["pr_posa"], writes=["pr_pa"])
        eq3 = self.eq[:].rearrange("p h k a -> p (h k) a")
        for which, (src, c0, j, dst) in enumerate(((self.pa, 0, 0, self.e1), (self.pb, 16, 1, self.e2))):
            add("dve", lambda e, src=src, c0=c0: e.tensor_tensor(
                out=eq3, in0=src[:].rearrange("p h k -> p (h k)").unsqueeze(2).broadcast_to([128, 128, 16]),
                in1=self.pc[:, c0:c0 + 16].unsqueeze(1).broadcast_to([128, 128, 16]), op=ALU.is_equal),
                reads=["pr_pa", "pr_pb", "pc"], writes=["pr_eq"])
            for h in range(8):
                add("dve", lambda e, j=j, h=h: e.tensor_tensor(
                    out=self.eq[:, h, :, :], in0=self.eq[:, h, :, :],
                    in1=self.ixf[:, 2 * h + j, :].unsqueeze(1).broadcast_to([128, 16, 16]),
                    op=ALU.mult), reads=["pr_eq", "pr_ixf"], writes=["pr_eq"])
            add("dve", lambda e, dst=dst: e.tensor_reduce(out=dst[:].rearrange("p h k -> p (h k)"), in_=eq3,
                                                          axis=AX.X, op=ALU.add),
                reads=["pr_eq"], writes=["pr_e%d" % (which + 1)])
        add("dve", lambda e: e.scalar_tensor_tensor(out=self.e1[:], in0=self.e1[:], scalar=128.0, in1=self.e2[:],
                                                    op0=ALU.mult, op1=ALU.add),
            reads=["pr_e1", "pr_e2"], writes=["pr_e1"])
        add("dve", lambda e: e.tensor_tensor(out=self.gt[:], in0=self.tv[:],
                                             in1=self.tv[:, :, 0:1].broadcast_to([128, 8, 16]), op=ALU.subtract),
            reads=["pr_tv"], writes=["pr_gt"])
        add("act", lambda e: e.activation(out=self.gt[:], in_=self.gt[:], func=AF.Exp), reads=["pr_gt"], writes=["pr_gt"])
        add("dve", lambda e: e.tensor_reduce(out=self.gs[:], in_=self.gt[:], axis=AX.X, op=ALU.add),
            reads=["pr_gt"], writes=["pr_gs"])
        add("dve", lambda e: e.reciprocal(out=self.gs[:], in_=self.gs[:]), reads=["pr_gs"], writes=["pr_gs"])
        add("dve", lambda e: e.tensor_tensor(out=self.gt[:], in0=self.gt[:],
                                             in1=self.gs[:].unsqueeze(2).broadcast_to([128, 8, 16]), op=ALU.mult),
            reads=["pr_gt", "pr_gs"], writes=["pr_gt"])
        add("pe", lambda e: e.transpose(out=self.ps_a[:, 0, :], in_=self.e1[:].rearrange("p h k -> p (h k)"),
                                        identity=self.ident_f[:]), reads=["pr_e1", "ident_f"], writes=["pp_a"])
        add("pe", lambda e: e.transpose(out=self.ps_a[:, 1, :], in_=self.gt[:].rearrange("p h k -> p (h k)"),
                                        identity=self.ident_f[:]), reads=["pr_gt", "ident_f"], writes=["pp_a"])
        add("act", lambda e: e.copy(out=self.eTf[:], in_=self.ps_a[:, 0, :]), reads=["pp_a"], writes=["pr_eTf"])
        add("dve", lambda e: e.tensor_copy(out=eTi[:], in_=self.eTf[:]), reads=["pr_eTf"], writes=[keTi])
        add("act", lambda e: e.copy(out=gT[:], in_=self.ps_a[:, 1, :]), reads=["pp_a"], writes=[kgT])
        return ops

    def loop(self, i, h_t, hkey, out_ap, outkey, pending):
        sc = self.sc
        p = i % 2
        xnT, eTi, gT = self.xnT[p], self.eTi[p], self.gT[p]
        kxnT, keTi, kgT = "pr_xnT%d" % p, "pr_eTi%d" % p, "pr_gT%d" % p
        NB = self.NB
        per = (len(pending) + 119) // 120 if pending else 0
        bufs, gbs = {}, {}

        def e1(t):
            b = self.tok % NB
            g2 = self.tok % 2
            self.tok += 1
            bufs[t], gbs[t] = b, g2
            gk = "pr_G%d" % b
            sc.dma("pool", lambda e, b=b, t=t: e.indirect_dma_start(
                out=self.G[b][:], out_offset=None, in_=self.uv_tab,
                in_offset=bass.IndirectOffsetOnAxis(ap=eTi[:, t:t + 1], axis=0)), reads=[keTi], writes=[gk])
            Gf = self.G[b][:, 0:D].bitcast(F32)
            for c in range(4):
                sc.op("pe", lambda e, Gf=Gf, c=c, g2=g2: e.transpose(
                    out=self.ps_g[g2][:, c, :], in_=Gf[:, c * 128:(c + 1) * 128], identity=self.ident_f[:]),
                    reads=[gk, "ident_f"], writes=["pp_g%d" % g2])
            sc.op("act", lambda e, g2=g2: e.copy(
                out=self.GT[g2][:].rearrange("p c s two -> p (c s two)"),
                in_=self.ps_g[g2][:].rearrange("p a b -> p (a b)").bitcast(BF16)),
                reads=["pp_g%d" % g2], writes=["pr_GT%d" % g2])

        def e2(t):
            g2 = gbs[t]
            for kc in range(8):
                sc.op("pe", lambda e, kc=kc, g2=g2, t=t: e.matmul(
                    self.ps_hd[:, t:t + 1], lhsT=self.GT[g2][:, kc // 2, :, kc % 2], rhs=xnT[:, kc // 2, t:t + 1, kc % 2],
                    start=(kc == 0), stop=(kc == 7)), reads=["pr_GT%d" % g2, kxnT], writes=["pp_hd"])
            sc.op("act", lambda e, t=t: e.activation(out=self.hg[:, t:t + 1], in_=self.ps_hd[:, t:t + 1], func=AF.Gelu),
                  reads=["pp_hd"], writes=["pr_hg%d" % (t % 4)])
            sc.op("dve", lambda e, t=t: e.tensor_scalar(out=self.actD[t % 3][:], in0=self.wsel[:, 127 - t:255 - t],
                                                        scalar1=self.hg[:, t:t + 1], scalar2=gT[:, t:t + 1],
                                                        op0=ALU.mult, op1=ALU.mult),
                  reads=["pr_hg%d" % (t % 4), kgT, "pr_wsel"], writes=["pr_actD%d" % (t % 3)])

        def e3(t):
            b = bufs[t]
            po = self.ps_o[:].rearrange("p a b -> p (a b)")
            for hf in range(2):
                sc.op("pe", lambda e, b=b, t=t, hf=hf, po=po: e.matmul(
                    po[:, hf * 512:(hf + 1) * 512], lhsT=self.actD[t % 3][:], rhs=self.G[b][:, D + hf * 512:D + (hf + 1) * 512],
                    start=(t == 0), stop=(t == 127)), reads=["pr_G%d" % b, "pr_actD%d" % (t % 3)], writes=["pp_o"])

        e1(0)
        for t in range(-1, 129):
            if 0 <= t + 1 < 128 and t + 1 > 0:
                e1(t + 1)
            if 0 <= t < 128:
                e2(t)
            if 0 <= t - 1 < 128:
                e3(t - 1)
            for _ in range(per):
                if pending:
                    pending.pop(0)()
        while pending:
            pending.pop(0)()
        po = self.ps_o[:].rearrange("p a b -> p (a b)")
        for hf in range(2):
            sc.op("dve", lambda e, hf=hf, po=po: e.tensor_tensor(
                out=self.osb[:, hf * 512:(hf + 1) * 512], in0=po[:, hf * 512:(hf + 1) * 512],
                in1=h_t[:, hf * 512:(hf + 1) * 512], op=ALU.add), reads=["pp_o", hkey], writes=["pr_osb"])
        sc.dma("sp", lambda e: e.dma_start(out=out_ap, in_=self.osb[:]), reads=["pr_osb"], writes=[outkey])


FMCH = [0, 1, 2, 3, 4, 5, 6, 7, 16, 17, 18, 19, 20, 21, 22, 24]
INW = 3352
DELTAS = [-512, -384, -256, -128, 0, 128, 256, 384]


def host_consts():
    bf = ml_dtypes.bfloat16
    c = {}
    c["c_identf"] = np.eye(128, dtype=np.float32)
    c["c_pc"] = peer_consts()
    wsel = np.zeros((128, 256), np.float32)
    wsel[:, 127] = 1.0
    c["c_wsel"] = wsel
    s = np.arange(128)[:, None]
    t = np.arange(128)[None, :]
    same = (s // 32) == (t // 32)
    c["c_ublk"] = (same & (s <= t)).astype(np.float32)
    c["c_wrev"] = (same & (s > t)).astype(np.float32)
    c["c_rowm"] = (np.arange(128)[:, None] // 32 == np.arange(4)[None, :]).astype(np.float32)
    c["c_colm"] = np.broadcast_to((np.arange(128)[None, None, :] // 32 == np.arange(4)[None, :, None]), (128, 4, 128)).astype(np.float32).copy()
    inv = (500000.0 ** (-np.arange(0, 16, 2, dtype=np.float32) / 16)).astype(np.float32)
    misc = np.zeros((128, 8), np.float32)
    misc[0:16, 0] = np.concatenate([inv, inv])
    misc[:, 1] = np.tile(np.concatenate([inv, inv]), 8)
    c["c_misc"] = misc
    rm = np.zeros((64, 64), np.float32)
    for d in range(8):
        rm[d + 8, d] = -1.0
        rm[d, d + 8] = 1.0
    c["c_rm"] = rm
    ss = np.arange(128)[:, None]
    tt = np.arange(512)[None, :]
    dm = np.zeros((128, 8, 512), np.float32)
    for i, dl in enumerate(DELTAS):
        ok = (dl + ss <= tt) & (tt - ss - dl < 512)
        dm[:, i, :] = np.where(ok, 0.0, NEG)
    c["c_dmask"] = dm
    cm = np.zeros((128, 4, 512), np.float32)
    cc = np.arange(128)[:, None]
    for qb in range(4):
        ok = (16 * cc + 31) <= (qb * 512 + tt)
        cm[:, qb, :] = np.where(ok, 0.0, NEG)
    c["c_cmask"] = cm
    em = np.zeros((32, 2048), np.float32)
    em[np.arange(2048) // 64, np.arange(2048)] = 1.0
    c["c_emat"] = em
    c0 = np.arange(127)[:, None] * 16
    j0 = np.arange(32)[None, :] * 64
    ov = np.clip(np.minimum(c0 + 32, j0 + 64) - np.maximum(c0, j0), 0, None) / 32.0
    va = np.zeros((128, 33), np.float32)
    va[:, 0] = 1.0
    va[:127, 1:] = ov
    c["c_vaug"] = va
    tok = (np.arange(16)[None, :, None] * 128 + np.arange(128)[:, None, None])
    cur = tok // 64
    j = np.arange(32)[None, None, :]
    forced = (j == 0) | (j == cur) | (j == cur - 1)
    valid = (j <= cur) & ~forced
    c["c_valid"] = valid.astype(np.float32)
    c["c_addc"] = np.where(forced, 1e30, np.where(valid, 0.0, -1e30)).astype(np.float32)
    return c


CONST_SHAPES = {"c_identf": [128, 128], "c_pc": [128, 64], "c_wsel": [128, 256], "c_ublk": [128, 128], "c_wrev": [128, 128],
                "c_misc": [128, 8], "c_rowm": [128, 4], "c_colm": [128, 4, 128], "c_rm": [64, 64], "c_dmask": [128, 8, 512], "c_cmask": [128, 4, 512],
                "c_emat": [32, 2048], "c_vaug": [128, 33], "c_valid": [128, 16, 32], "c_addc": [128, 16, 32]}


def barrier(sc):
    for e in ("sp", "pool", "act"):
        k = sc.dcnt[e]
        for slot in range(min(k, sc.ndma)):
            n = (k - 1 - slot) // sc.ndma + 1
            for eng in ENGS:
                sc._wait(eng, (("dma", e, slot), 16 * n))
    for eng in ENGS:
        for e2 in ("pe", "dve", "act", "pool"):
            if sc.cnt[e2] > 0 and not (eng == "pe" and e2 == "pe"):
                sc._wait(eng, ((e2,), sc.cnt[e2]))
    sc.st = {}


def rms_rstd(sc, src_ap, srckey, junk, ss, rstd, epsc, n, pfx):
    sc.op("act", lambda e: e.activation(out=junk, in_=src_ap, func=AF.Square, accum_out=ss),
          reads=[srckey], writes=[pfx + "junk", pfx + "ss"])
    sc.op("act", lambda e: e.activation(out=rstd, in_=ss, func=AF.Sqrt, scale=1.0 / n, bias=epsc),
          reads=[pfx + "ss"], writes=[pfx + "rstd"])
    sc.op("dve", lambda e: e.reciprocal(out=rstd, in_=rstd), reads=[pfx + "rstd"], writes=[pfx + "rstd"])


TM_RANGES = [(512, 1024), (1024, 1536), (1536, 2048), (2944, 3072), (3200, 3352)]


def stage_proj(nc, sc, NT, x, norm1_w, w_in, P_tm, P_fm, ident_b, epsc, uv=None):
    with contextlib.ExitStack() as st:
        T = lambda name, shape, dt: st.enter_context(nc.sbuf_tensor(name, shape, dt))
        P = lambda name, shape, dt: st.enter_context(nc.psum_tensor(name, shape, dt))
        win = T("a_win", [128, 8, INW], BF16)
        n1w = T("a_n1w", [128, 8], F32)
        xt = [T("a_xt%d" % i, [128, D], F32) for i in range(2)]
        junk = T("a_junk", [128, D], BF16)
        ss = T("a_ss", [128, 1], F32)
        rstd = T("a_rstd", [128, 1], F32)
        xn = T("a_xn", [128, D], BF16)
        xnT = T("a_xnT", [128, 8, 128], BF16)
        ptm = [T("a_ptm%d" % i, [128, INW], F32) for i in range(2)]
        pfm = [T("a_pfm%d" % i, [128, 16, 128], F32) for i in range(2)]
        ps_t = P("ap_t", [128, 8, 128], BF16)
        ps_m = [P("ap_m%d" % i, [128, 512], F32) for i in range(2)]
        ps_f = [P("ap_f%d" % i, [128, 4, 128], F32) for i in range(2)]
        for kc in range(8):
            sc.dma("pool", lambda e, kc=kc: e.dma_start(out=win[:, kc, :], in_=w_in[kc * 128:(kc + 1) * 128, :]),
                   writes=["a_win"])
        uv_ops = []
        if uv is not None:
            u_tab, v_tab, UV = uv
            tmp = [T("uv_tmp%d" % i, [128, 8, D], BF16) for i in range(3)]
            UVv = UV.rearrange("(p r) d -> p r d", p=128)
            k = 0
            for half, tab in enumerate((u_tab, v_tab)):
                tv = tab.rearrange("(p r) d -> p r d", p=128)
                for ci in range(16):
                    bb = k % 3
                    k += 1
                    uv_ops.append(lambda bb=bb, tv=tv, ci=ci: sc.dma(
                        "pool", lambda e: e.dma_start(out=tmp[bb][:], in_=tv[:, ci * 8:(ci + 1) * 8, :]), writes=["uv_tmp%d" % bb]))
                    uv_ops.append(lambda bb=bb, ci=ci, half=half: sc.dma(
                        "sp", lambda e: e.dma_start(out=UVv[:, ci * 8:(ci + 1) * 8, half * D:(half + 1) * D], in_=tmp[bb][:]),
                        reads=["uv_tmp%d" % bb], writes=["UV"]))
        sc.dma("sp", lambda e: e.dma_start(out=n1w[:], in_=norm1_w.rearrange("o (kc p) -> p (o kc)", p=128),
                                           allow_slow_non_contiguous=True), writes=["a_n1w"])
        P_fm_v = P_fm.rearrange("(c p) t -> p c t", p=128)
        ev = 0
        for i in range(NT):
            b = i % 2
            xk = "a_xt%d" % b
            sc.dma("sp", lambda e, b=b, i=i: e.dma_start(out=xt[b][:], in_=x[i * 128:(i + 1) * 128, :]), writes=[xk])
            rms_rstd(sc, xt[b][:], xk, junk[:], ss[:], rstd[:], epsc[:], D, "a_")
            sc.op("dve", lambda e, b=b: e.tensor_scalar(out=xn[:], in0=xt[b][:], scalar1=rstd[:, 0:1], scalar2=None,
                                                        op0=ALU.mult), reads=[xk, "a_rstd"], writes=["a_xn"])
            for kc in range(8):
                sc.op("pe", lambda e, kc=kc: e.transpose(out=ps_t[:, kc, :], in_=xn[:, kc * 128:(kc + 1) * 128],
                                                         identity=ident_b[:]), reads=["a_xn", "ident_b"], writes=["ap_t"])
            sc.op("dve", lambda e: e.tensor_tensor(out=xnT[:], in0=ps_t[:],
                                                   in1=n1w[:].unsqueeze(2).broadcast_to([128, 8, 128]), op=ALU.mult),
                  reads=["ap_t", "a_n1w"], writes=["a_xnT"])
            if uv_ops:
                uv_ops.pop(0)()
            for cg, (c0, c1) in enumerate(TM_RANGES):
                pm = ps_m[cg % 2]
                pk = "ap_m%d" % (cg % 2)
                for kc in range(8):
                    sc.op("pe", lambda e, kc=kc, c0=c0, c1=c1, pm=pm: e.matmul(
                        pm[:, 0:c1 - c0], lhsT=xnT[:, kc, :], rhs=win[:, kc, c0:c1], start=(kc == 0), stop=(kc == 7)),
                        reads=["a_xnT", "a_win"], writes=[pk])
                if cg % 2 == 0:
                    sc.op("act", lambda e, b=b, c0=c0, c1=c1, pm=pm: e.copy(out=ptm[b][:, c0:c1], in_=pm[:, 0:c1 - c0]),
                          reads=[pk], writes=["a_ptm%d" % b])
                else:
                    sc.op("dve", lambda e, b=b, c0=c0, c1=c1, pm=pm: e.tensor_copy(out=ptm[b][:, c0:c1], in_=pm[:, 0:c1 - c0]),
                          reads=[pk], writes=["a_ptm%d" % b])
            sc.dma("sp", lambda e, b=b, i=i: e.dma_start(out=P_tm[i * 128:(i + 1) * 128, 512:2048], in_=ptm[b][:, 512:2048]),
                   reads=["a_ptm%d" % b], writes=["P_tm"])
            sc.dma("sp", lambda e, b=b, i=i: e.dma_start(out=P_tm[i * 128:(i + 1) * 128, 2944:INW], in_=ptm[b][:, 2944:INW]),
                   reads=["a_ptm%d" % b], writes=["P_tm"])
            for fg in range(4):
                pf = ps_f[fg % 2]
                pk = "ap_f%d" % (fg % 2)
                for f4 in range(4):
                    ch = FMCH[fg * 4 + f4]
                    for kc in range(8):
                        sc.op("pe", lambda e, kc=kc, ch=ch, f4=f4, pf=pf: e.matmul(
                            pf[:, f4, :], lhsT=win[:, kc, ch * 128:(ch + 1) * 128], rhs=xnT[:, kc, :],
                            start=(kc == 0), stop=(kc == 7)), reads=["a_xnT", "a_win"], writes=[pk])
                if fg % 2 == 0:
                    sc.op("act", lambda e, b=b, fg=fg, pf=pf: e.copy(out=pfm[b][:, fg * 4:(fg + 1) * 4, :], in_=pf[:]),
                          reads=[pk], writes=["a_pfm%d" % b])
                else:
                    sc.op("dve", lambda e, b=b, fg=fg, pf=pf: e.tensor_copy(out=pfm[b][:, fg * 4:(fg + 1) * 4, :], in_=pf[:]),
                          reads=[pk], writes=["a_pfm%d" % b])
            sc.dma("sp", lambda e, b=b, i=i: e.dma_start(out=P_fm_v[:, :, i * 128:(i + 1) * 128], in_=pfm[b][:]),
                   reads=["a_pfm%d" % b], writes=["P_fm"])
        while uv_ops:
            uv_ops.pop(0)()
        barrier(sc)


def stage_hgrn(nc, sc, NT, P_tm, P_fm, Y_tm, lb_logits, hg_onw, ublk, wrev, epsc, c_rowm, c_colm):
    with contextlib.ExitStack() as st:
        T = lambda name, shape, dt: st.enter_context(nc.sbuf_tensor(name, shape, dt))
        P = lambda name, shape, dt: st.enter_context(nc.psum_tensor(name, shape, dt))
        lbb = T("h_lbb", [128, 2, 512], F32)
        omlb = T("h_omlb", [128, 512], F32)
        lbf = T("h_lbf", [128, 2, 4], F32)
        omlf = T("h_omlf", [128, 4], F32)
        hgw = T("h_hgw", [128, 128], F32)
        ftm = [T("h_ftm%d" % i, [128, 1536], F32) for i in range(2)]
        ffm = [T("h_ffm%d" % i, [128, 8, 128], F32) for i in range(2)]
        sig = T("h_sig", [128, 512], F32)
        logf = T("h_logf", [128, 512], F32)
        ktm = T("h_ktm", [128, 512], F32)
        erev = T("h_erev", [128, 512], F32)
        khat = T("h_khat", [128, 512], F32)
        khat4 = T("h_khat4", [128, 4, 512], BF16)
        rowm = T("h_rowm", [128, 4], F32)
        colm = T("h_colm", [128, 4, 128], F32)
        vbf = T("h_vbf", [128, 512], BF16)
        sgg = T("h_sgg", [128, 512], F32)
        fT = T("h_fT", [128, 4, 128], F32)
        ET = T("h_ET", [128, 4, 128], F32)
        EiT = T("h_EiT", [128, 4, 128], F32)
        qtT = T("h_qtT", [128, 4, 128], BF16)
        ktT = T("h_ktT", [128, 4, 128], BF16)
        qtT4 = [T("h_qtT4%d" % h, [128, 4, 128], BF16) for h in range(4)]
        scm = T("h_scm", [128, 4, 128], BF16)
        state = [T("h_st%d" % h, [128, 128], F32) for h in range(4)]
        stbf = [T("h_sb%d" % h, [128, 128], BF16) for h in range(4)]
        junk = T("h_junk", [128, 128], F32)
        ss = T("h_ss", [128, 4], F32)
        rstd = T("h_rstd", [128, 4], F32)
        yt = [T("h_yt%d" % i, [128, 512], F32) for i in range(2)]
        onec = T("h_onec", [128, 1], F32)
        sc.op("dve", lambda e: e.memset(onec[:], 1.0), writes=["h_onec"])
        ps_m = [P("hp_m%d" % i, [128, 512], F32) for i in range(4)]
        ps_o = [P("hp_o%d" % i, [128, 512], F32) for i in range(4)]
        ps_rev = ps_m[0]
        ps_bT = ps_m[1][:].rearrange("p (h t) -> p h t", h=4)
        ps_sc = ps_m[2][:].rearrange("p (h t) -> p h t", h=4)
        sc.dma("sp", lambda e: e.dma_start(out=rowm[:], in_=c_rowm), writes=["h_rowm"])
        sc.dma("sp", lambda e: e.dma_start(out=colm[:], in_=c_colm), writes=["h_colm"])
        sc.dma("sp", lambda e: e.dma_start(out=lbb[:, 0, :], in_=lb_logits[0:1, :].partition_broadcast(128)), writes=["h_lbb"])
        sc.dma("sp", lambda e: e.dma_start(out=lbb[:, 1, :], in_=lb_logits[1:2, :].partition_broadcast(128)), writes=["h_lbb"])
        sc.dma("sp", lambda e: e.dma_start(out=lbf[:], in_=lb_logits.rearrange("r (h p) -> p r h", p=128),
                                           allow_slow_non_contiguous=True), writes=["h_lbf"])
        sc.dma("sp", lambda e: e.dma_start(out=hgw[:], in_=hg_onw[0:1, :].partition_broadcast(128)), writes=["h_hgw"])

        def sigmoid_to(ap_out, ap_in, keys_in, key_out):
            sc.op("act", lambda e: e.activation(out=ap_out, in_=ap_in, func=AF.Exp, scale=-1.0), reads=keys_in, writes=[key_out])
            sc.op("act", lambda e: e.activation(out=ap_out, in_=ap_out, func=AF.Ln, bias=onec[:]), reads=[key_out], writes=[key_out])
            sc.op("act", lambda e: e.activation(out=ap_out, in_=ap_out, func=AF.Exp, scale=-1.0), reads=[key_out], writes=[key_out])

        sc.op("dve", lambda e: e.tensor_tensor(out=lbb[:, 0, :], in0=lbb[:, 0, :], in1=lbb[:, 1, :], op=ALU.subtract),
              reads=["h_lbb"], writes=["h_lbb"])
        sigmoid_to(lbb[:, 0, :], lbb[:, 0, :], ["h_lbb"], "h_lbb")
        sc.op("dve", lambda e: e.tensor_scalar(out=omlb[:], in0=lbb[:, 0, :], scalar1=-1.0, scalar2=1.0, op0=ALU.mult,
                                               op1=ALU.add), reads=["h_lbb"], writes=["h_omlb"])
        sc.op("dve", lambda e: e.tensor_tensor(out=lbf[:, 0, :], in0=lbf[:, 0, :], in1=lbf[:, 1, :], op=ALU.subtract),
              reads=["h_lbf"], writes=["h_lbf"])
        sigmoid_to(lbf[:, 0, :], lbf[:, 0, :], ["h_lbf"], "h_lbf")
        sc.op("dve", lambda e: e.tensor_scalar(out=omlf[:], in0=lbf[:, 0, :], scalar1=-1.0, scalar2=1.0, op0=ALU.mult,
                                               op1=ALU.add), reads=["h_lbf"], writes=["h_omlf"])
        P_fm_v = P_fm.rearrange("(c p) t -> p c t", p=128)
        for i in range(NT):
            b = i % 2
            fk, mk = "h_ftm%d" % b, "h_ffm%d" % b
            F_, M_ = ftm[b], ffm[b]
            sc.dma("sp", lambda e, b=b, i=i: e.dma_start(out=ftm[b][:], in_=P_tm[i * 128:(i + 1) * 128, 512:2048]),
                   reads=["P_tm"], writes=[fk])
            sc.dma("sp", lambda e, b=b, i=i: e.dma_start(out=ffm[b][:], in_=P_fm_v[:, 0:8, i * 128:(i + 1) * 128]),
                   reads=["P_fm"], writes=[mk])
            if i % 16 == 0:
                for h in range(4):
                    sc.op("dve", lambda e, h=h: e.memset(state[h][:], 0.0), writes=["h_st%d" % h])
                    sc.op("dve", lambda e, h=h: e.memset(stbf[h][:], 0.0), writes=["h_sb%d" % h])
            opsA, opsB = [], []
            OP_A = lambda *a, **k: opsA.append(lambda: sc.op(*a, **k))
            OP_B = lambda *a, **k: opsB.append(lambda: sc.op(*a, **k))
            SIG_A = lambda *a: opsA.append(lambda: sigmoid_to(*a))
            SIG_B = lambda *a: opsB.append(lambda: sigmoid_to(*a))
            SIG_A(sig[:], F_[:, 0:512], [fk], "h_sig")
            OP_A("dve", lambda e: e.tensor_tensor(out=sig[:], in0=sig[:], in1=omlb[:], op=ALU.mult),
                  reads=["h_sig", "h_omlb"], writes=["h_sig"])
            OP_A("dve", lambda e: e.tensor_tensor(out=sig[:], in0=sig[:], in1=lbb[:, 0, :], op=ALU.add),
                  reads=["h_sig", "h_lbb"], writes=["h_sig"])
            OP_A("act", lambda e: e.activation(out=logf[:], in_=sig[:], func=AF.Ln), reads=["h_sig"], writes=["h_logf"])
            OP_A("dve", lambda e: e.tensor_scalar(out=ktm[:], in0=sig[:], scalar1=-1.0, scalar2=1.0, op0=ALU.mult,
                                                   op1=ALU.add), reads=["h_sig"], writes=["h_ktm"])
            OP_A("pe", lambda e: e.matmul(ps_rev[:], lhsT=wrev[:], rhs=logf[:], start=True, stop=True),
                  reads=["wrev", "h_logf"], writes=["hp_m0"])
            OP_A("act", lambda e: e.activation(out=erev[:], in_=ps_rev[:], func=AF.Exp), reads=["hp_m0"], writes=["h_erev"])
            OP_A("dve", lambda e: e.tensor_tensor(out=khat[:], in0=ktm[:], in1=erev[:], op=ALU.mult),
                  reads=["h_ktm", "h_erev"], writes=["h_khat"])
            OP_A("dve", lambda e: e.tensor_tensor(out=khat4[:], in0=khat[:].unsqueeze(1).broadcast_to([128, 4, 512]),
                                                   in1=rowm[:].unsqueeze(2).broadcast_to([128, 4, 512]), op=ALU.mult),
                  reads=["h_khat", "h_rowm"], writes=["h_khat4"])
            OP_A("act", lambda e, F_=F_: e.copy(out=vbf[:], in_=F_[:, 512:1024]), reads=[fk], writes=["h_vbf"])
            SIG_A(sgg[:], F_[:, 1024:1536], [fk], "h_sgg")
            OP_A("dve", lambda e, F_=F_: e.tensor_tensor(out=sgg[:], in0=sgg[:], in1=F_[:, 1024:1536], op=ALU.mult),
                  reads=["h_sgg", fk], writes=["h_sgg"])
            SIG_B(fT[:], M_[:, 4:8, :], [mk], "h_fT")
            for h in range(4):
                OP_B("dve", lambda e, h=h: e.tensor_scalar(out=fT[:, h, :], in0=fT[:, h, :], scalar1=omlf[:, h:h + 1],
                                                            scalar2=lbf[:, 0, h:h + 1], op0=ALU.mult, op1=ALU.add),
                      reads=["h_fT", "h_omlf", "h_lbf"], writes=["h_fT"])
            OP_B("dve", lambda e: e.tensor_scalar(out=fT[:], in0=fT[:], scalar1=-1.0, scalar2=1.0, op0=ALU.mult,
                                                   op1=ALU.add), reads=["h_fT"], writes=["h_fT"])
            for h in range(4):
                OP_B("pe", lambda e, h=h: e.matmul(ps_bT[:, h, :], lhsT=logf[:, h * 128:(h + 1) * 128], rhs=ublk[:],
                                                    start=True, stop=True), reads=["h_logf", "ublk"], writes=["hp_m1"])
            OP_B("act", lambda e: e.activation(out=ET[:], in_=ps_bT, func=AF.Exp), reads=["hp_m1"], writes=["h_ET"])
            OP_B("act", lambda e: e.activation(out=EiT[:], in_=ps_bT, func=AF.Exp, scale=-1.0),
                  reads=["hp_m1"], writes=["h_EiT"])
            OP_B("dve", lambda e, M_=M_: e.tensor_tensor(out=qtT[:], in0=M_[:, 0:4, :], in1=ET[:], op=ALU.mult),
                  reads=[mk, "h_ET"], writes=["h_qtT"])
            OP_B("dve", lambda e: e.tensor_tensor(out=ktT[:], in0=fT[:], in1=EiT[:], op=ALU.mult),
                  reads=["h_fT", "h_EiT"], writes=["h_ktT"])
            for h in range(4):
                OP_B("dve", lambda e, h=h: e.tensor_tensor(out=qtT4[h][:], in0=qtT[:, h, :].unsqueeze(1).broadcast_to([128, 4, 128]),
                                                            in1=colm[:], op=ALU.mult),
                      reads=["h_qtT", "h_colm"], writes=["h_qtT4%d" % h])
                OP_B("pe", lambda e, h=h: e.matmul(ps_sc[:, h, :], lhsT=ktT[:, h, :], rhs=qtT[:, h, :], start=True, stop=True),
                      reads=["h_ktT", "h_qtT"], writes=["hp_m2"])
            OP_B("dve", lambda e: e.tensor_tensor(out=scm[:], in0=ps_sc, in1=ublk[:].unsqueeze(1).broadcast_to([128, 4, 128]),
                                                   op=ALU.mult), reads=["hp_m2", "ublk"], writes=["h_scm"])
            for k_ in range(max(len(opsA), len(opsB))):
                if k_ < len(opsA):
                    opsA[k_]()
                if k_ < len(opsB):
                    opsB[k_]()
            for h in range(4):
                sc.op("pe", lambda e, h=h: e.matmul(ps_o[h][:, 0:128], lhsT=scm[:, h, :], rhs=vbf[:, h * 128:(h + 1) * 128],
                                                    start=True, stop=False), reads=["h_scm", "h_vbf"], writes=["hp_o%d" % h])
            for c4 in range(4):
                for h in range(4):
                    hs = slice(h * 128, (h + 1) * 128)
                    sc.op("pe", lambda e, h=h, c4=c4: e.matmul(ps_o[h][:, 0:128], lhsT=qtT4[h][:, c4, :], rhs=stbf[h][:],
                                                               start=False, stop=(c4 == 3)),
                          reads=["h_qtT4%d" % h, "h_sb%d" % h], writes=["hp_o%d" % h])
                    sc.op("pe", lambda e, c4=c4, hs=hs, h=h: e.matmul(ps_m[h][:, 0:128], lhsT=khat4[:, c4, hs], rhs=vbf[:, hs],
                                                                      start=True, stop=True),
                          reads=["h_khat4", "h_vbf"], writes=["hp_m%d" % h])
                    sc.op("dve", lambda e, h=h, c4=c4: e.scalar_tensor_tensor(
                        out=state[h][:], in0=state[h][:], scalar=ET[:, h, c4 * 32 + 31:c4 * 32 + 32], in1=ps_m[h][:, 0:128],
                        op0=ALU.mult, op1=ALU.add), reads=["h_st%d" % h, "h_ET", "hp_m%d" % h], writes=["h_st%d" % h])
                    sc.op("act", lambda e, h=h: e.copy(out=stbf[h][:], in_=state[h][:]),
                          reads=["h_st%d" % h], writes=["h_sb%d" % h])
            for h in range(4):
                sc.op("act", lambda e, h=h: e.activation(out=junk[:], in_=ps_o[h][:, 0:128], func=AF.Square, accum_out=ss[:, h:h + 1]),
                      reads=["hp_o%d" % h], writes=["h_junk", "h_ss"])
            sc.op("act", lambda e: e.activation(out=rstd[:], in_=ss[:], func=AF.Ln, scale=1.0 / 128, bias=epsc[:]),
                  reads=["h_ss"], writes=["h_rstd"])
            sc.op("act", lambda e: e.activation(out=rstd[:], in_=rstd[:], func=AF.Exp, scale=-0.5),
                  reads=["h_rstd"], writes=["h_rstd"])
            for h in range(4):
                hs = slice(h * 128, (h + 1) * 128)
                sc.op("dve", lambda e, h=h, b=b, hs=hs: e.scalar_tensor_tensor(
                    out=yt[b][:, hs], in0=ps_o[h][:, 0:128], scalar=rstd[:, h:h + 1], in1=hgw[:], op0=ALU.mult, op1=ALU.mult),
                    reads=["hp_o%d" % h, "h_rstd", "h_hgw"], writes=["h_yt%d" % b])
            sc.op("dve", lambda e, b=b: e.tensor_tensor(out=yt[b][:], in0=yt[b][:], in1=sgg[:], op=ALU.mult),
                  reads=["h_yt%d" % b, "h_sgg"], writes=["h_yt%d" % b])
            sc.dma("sp", lambda e, b=b, i=i: e.dma_start(out=Y_tm[i * 128:(i + 1) * 128, 0:512], in_=yt[b][:]),
                   reads=["h_yt%d" % b], writes=["Y_tm"])
        barrier(sc)


NSA_WARM = 1


def stage_nsa(nc, sc, NSEQ, P_tm, P_fm, Y_tm, positions, qnw, knw, pe_k, pe_v, w1k, w2k, w1v, w2v, onw, cst,
              ident_b, epsc, dbg=None):
    TINY = 1e-30
    with contextlib.ExitStack() as st:
        T = lambda name, shape, dt: st.enter_context(nc.sbuf_tensor(name, shape, dt))
        P = lambda name, shape, dt: st.enter_context(nc.psum_tensor(name, shape, dt))
        dmask = T("n_dmask", [128, 8, 512], BF16)
        cmask = T("n_cmask", [128, 4, 512], BF16)
        rm = T("n_rm", [64, 64], BF16)
        ones64 = T("n_ones", [64, 64], BF16)
        misc = T("n_misc", [128, 8], F32)
        valid = T("n_valid", [128, 16, 32], F32)
        addc = T("n_addc", [128, 16, 32], F32)
        wcol = T("n_wcol", [64, 4], F32)
        onwb = T("n_onwb", [128, 64], F32)
        w1 = [T("n_w1%d" % i, [64, 32, 256], BF16) for i in range(2)]
        w2 = [T("n_w2%d" % i, [128, 2, 64], BF16) for i in range(2)]
        peT = T("n_peT", [64, 2, 32], BF16)
        peTf = T("n_peTf", [64, 2, 32], F32)
        cbias = T("n_cbias", [128, 2, 2], F32)
        for name, t_, src in (("n_dmask", dmask, cst["c_dmask"]), ("n_cmask", cmask, cst["c_cmask"]),
                              ("n_rm", rm, cst["c_rm"])):
            sc.dma("pool", lambda e, t_=t_, src=src: e.dma_start(out=t_[:], in_=src), writes=[name])
        sc.dma("sp", lambda e: e.dma_start(out=misc[:], in_=cst["c_misc"]), writes=["n_misc"])
        sc.dma("sp", lambda e: e.dma_start(out=valid[:], in_=cst["c_valid"]), writes=["n_valid"])
        sc.dma("sp", lambda e: e.dma_start(out=addc[:], in_=cst["c_addc"]), writes=["n_addc"])
        sc.op("dve", lambda e: e.memset(ones64[:], 1.0), writes=["n_ones"])
        sc.dma("pool", lambda e: e.dma_start(out=ksT[64:96, :], in_=cst["c_emat"]), writes=["n_ksT"])
        sc.op("dve", lambda e: e.tensor_scalar(out=dm01[:], in0=dmask[:], scalar1=-1.0, scalar2=None, op0=ALU.is_ge),
              reads=["n_dmask"], writes=["n_dm01"])
        sc.op("dve", lambda e: e.memset(selb[:], 0.0), writes=["n_selb"])
        sc.op("dve", lambda e: e.memset(kwT[64:96, :], 0.0), writes=["n_kwT"])
        sc.op("dve", lambda e: e.memset(kcmpT[64:96, :], 0.0), writes=["n_kcmpT"])
        for r_ in range(4):
            sc.op("dve", lambda e, r_=r_: e.memset(qT[r_][64:96, :], 0.0), writes=["n_qT%d" % r_])
        sc.dma("sp", lambda e: e.dma_start(out=wcol[:, 0:1], in_=qnw.rearrange("o d -> d o"), allow_slow_non_contiguous=True),
               writes=["n_wcol"])
        sc.dma("sp", lambda e: e.dma_start(out=wcol[:, 1:4], in_=knw.rearrange("b d -> d b"), allow_slow_non_contiguous=True),
               writes=["n_wcol"])
        sc.op("dve", lambda e: e.tensor_scalar(out=wcol[:, 0:1], in0=wcol[:, 0:1], scalar1=0.125, scalar2=None, op0=ALU.mult),
              reads=["n_wcol"], writes=["n_wcol"])
        sc.dma("sp", lambda e: e.dma_start(out=onwb[:], in_=onw[0:1, :].partition_broadcast(128)), writes=["n_onwb"])
        for i, (w1_, w2_) in enumerate(((w1k, w2k), (w1v, w2v))):
            sc.dma("pool", lambda e, i=i, w1_=w1_: e.dma_start(out=w1[i][:], in_=w1_.rearrange("(l d) n -> d l n", d=64)),
                   writes=["n_w1%d" % i])
            sc.dma("pool", lambda e, i=i, w2_=w2_: e.dma_start(out=w2[i][:], in_=w2_.rearrange("(a p) n -> p a n", p=128)),
                   writes=["n_w2%d" % i])
        sc.dma("sp", lambda e: e.dma_start(out=peTf[:, 0, :], in_=pe_k.rearrange("l d -> d l"), allow_slow_non_contiguous=True),
               writes=["n_peTf"])
        sc.dma("sp", lambda e: e.dma_start(out=peTf[:, 1, :], in_=pe_v.rearrange("l d -> d l"), allow_slow_non_contiguous=True),
               writes=["n_peTf"])
        sc.op("dve", lambda e: e.tensor_copy(out=peT[:], in_=peTf[:]), reads=["n_peTf"], writes=["n_peT"])
        ki = T("n_ki", [128, 256], I32)
        ang = T("n_ang", [128, 256], F32)
        rr = T("n_rr", [128, 256], F32)
        tab2 = T("n_tab2", [128, 256], F32)
        kf = T("n_kf", [128, 256], F32)
        CT = T("n_CT", [64, 2048], F32)
        ST = T("n_ST", [64, 2048], F32)
        srcs = [T("n_src%d" % i, [64, 2048], F32) for i in range(2)]
        SRCc = [0]

        def nxt_src():
            i_ = SRCc[0] % 2
            SRCc[0] += 1
            return srcs[i_], "n_src%d" % i_
        sq = [T("n_sq%d" % i, [64, 512], BF16) for i in range(2)]
        rinv = [T("n_rinv%d" % i, [64, 512], F32) for i in range(2)]
        xnm = [T("n_xnm%d" % i, [64, 512], F32) for i in range(2)]
        xb = [T("n_xb%d" % i, [64, 512], BF16) for i in range(2)]
        t1 = [T("n_t1%d" % i, [64, 512], F32) for i in range(2)]
        t2 = [T("n_t2%d" % i, [64, 512], F32) for i in range(2)]
        qT = [T("n_qT%d" % r, [96, 2048], BF16) for r in range(4)]
        ksT = T("n_ksT", [96, 2048], BF16)
        dm01 = dmask
        kwT = T("n_kwT", [96, 2048], BF16)
        kcb = T("n_kcb", [64, 2048], BF16)
        kcmpT = T("n_kcmpT", [96, 128], BF16)
        hx = T("n_hx", [128, 127], F32)
        hx2 = T("n_hx2", [128, 127], F32)
        hT = T("n_hT", [128, 2, 127], BF16)
        vcaug = T("n_vcaug", [128, 97], BF16)
        vaugf = T("n_vaugf", [128, 33], F32)
        vsa = T("n_vsa", [128, 16, 65], BF16)
        vwa = T("n_vwa", [128, 16, 65], BF16)
        vld = T("n_vld", [128, 16, 64], F32)
        gsig = T("n_gsig", [128, 16, 24], F32)
        PT = [T("n_PT%d" % i, [128, 512], BF16) for i in range(5)]
        oacc = T("n_oacc", [128, 16, 4, 64], F32)
        imp = T("n_imp", [128, 16, 32], F32)
        score = T("n_score", [128, 16, 32], F32)
        swk = T("n_swk", [128, 4, 32], F32)
        m8a = T("n_m8a", [128, 4, 8], F32)
        m8b = T("n_m8b", [128, 4, 8], F32)
        sel = T("n_sel", [128, 4, 32], F32)
        selb = T("n_selb", [128, 4, 96], BF16)
        nselT = T("n_nselT", [96, 2048], BF16)
        r1 = T("n_r1", [128, 1], F32)
        coef = T("n_coef", [128, 1], F32)
        ssq = T("n_ssq", [128, 64, 64], F32)
        ss64 = T("n_ss64", [128, 64], F32)
        ps_s = [P("np_s%d" % i, [128, 512], F32) for i in range(5)]
        ps_a = [P("np_a%d" % i, [128, 512], F32) for i in range(2)]
        ps_warm = P("np_w", [128, 512], F32)
        ps_x = [ps_s[2], ps_s[3]]
        sc.dma("sp", lambda e: e.dma_start(out=vaugf[:], in_=cst["c_vaug"]), writes=["n_vaugf"])
        sc.op("dve", lambda e: e.tensor_copy(out=vcaug[:, 64:97], in_=vaugf[:]), reads=["n_vaugf"], writes=["n_vcaug"])
        sc.op("dve", lambda e: e.memset(vcaug[:, 0:64], 0.0), writes=["n_vcaug"])
        sc.op("dve", lambda e: e.memset(vsa[:], 1.0), writes=["n_vsa"])
        sc.op("dve", lambda e: e.memset(vwa[:], 1.0), writes=["n_vwa"])
        sc.op("dve", lambda e: e.memset(CT[:], 1.0), writes=["n_CT"])
        sc.op("dve", lambda e: e.memset(ST[:], 0.0), writes=["n_ST"])
        for kv in range(2):
            for hh in range(2):
                for l in range(32):
                    sc.op("pe", lambda e, kv=kv, hh=hh, l=l: e.matmul(
                        ps_x[0][:, 0:1], lhsT=w1[kv][:, l, hh * 128:(hh + 1) * 128], rhs=peT[:, kv, l:l + 1],
                        start=(l == 0), stop=(l == 31)), reads=["n_w1%d" % kv, "n_peT"], writes=["np_s2"])
                sc.op("act", lambda e, kv=kv, hh=hh: e.copy(out=cbias[:, kv, hh:hh + 1], in_=ps_x[0][:, 0:1]),
                      reads=["np_s2"], writes=["n_cbias"])

        def norm_rope(calls):
            S = list(enumerate(calls))
            pa = [(ps_s[2], "np_s2", ps_s[3], "np_s3"), (ps_s[0], "np_s0", ps_s[1], "np_s1")]
            for k, (srcap, srckey, n, wc, Cap, Sap, outap, outkey) in S:
                sc.op("act", lambda e, k=k, n=n, srcap=srcap: e.activation(out=sq[k][:, 0:n], in_=srcap, func=AF.Square),
                      reads=[srckey], writes=["n_sq%d" % k])
            for k, (srcap, srckey, n, wc, Cap, Sap, outap, outkey) in S:
                sc.op("pe", lambda e, k=k, n=n: e.matmul(pa[k][0][0:64, 0:n], lhsT=ones64[:], rhs=sq[k][:, 0:n], start=True, stop=True),
                      reads=["n_sq%d" % k, "n_ones"], writes=[pa[k][1]])
            for k, (srcap, srckey, n, wc, Cap, Sap, outap, outkey) in S:
                sc.op("act", lambda e, k=k, n=n: e.activation(out=rinv[k][:, 0:n], in_=pa[k][0][0:64, 0:n], func=AF.Ln,
                                                              scale=1.0 / 64, bias=epsc[0:64, :]),
                      reads=[pa[k][1]], writes=["n_rinv%d" % k])
            for k, (srcap, srckey, n, wc, Cap, Sap, outap, outkey) in S:
                sc.op("act", lambda e, k=k, n=n: e.activation(out=rinv[k][:, 0:n], in_=rinv[k][:, 0:n], func=AF.Exp, scale=-0.5),
                      reads=["n_rinv%d" % k], writes=["n_rinv%d" % k])
            for k, (srcap, srckey, n, wc, Cap, Sap, outap, outkey) in S:
                sc.op("dve", lambda e, k=k, n=n, srcap=srcap, wc=wc: e.scalar_tensor_tensor(
                    out=xnm[k][:, 0:n], in0=srcap, scalar=wcol[:, wc:wc + 1], in1=rinv[k][:, 0:n], op0=ALU.mult, op1=ALU.mult),
                    reads=[srckey, "n_wcol", "n_rinv%d" % k], writes=["n_xnm%d" % k])
            for k, (srcap, srckey, n, wc, Cap, Sap, outap, outkey) in S:
                sc.op("act", lambda e, k=k, n=n: e.copy(out=xb[k][:, 0:n], in_=xnm[k][:, 0:n]),
                      reads=["n_xnm%d" % k], writes=["n_xb%d" % k])
            for k, (srcap, srckey, n, wc, Cap, Sap, outap, outkey) in S:
                sc.op("pe", lambda e, k=k, n=n: e.matmul(pa[k][2][0:64, 0:n], lhsT=rm[:], rhs=xb[k][:, 0:n], start=True, stop=True),
                      reads=["n_xb%d" % k, "n_rm"], writes=[pa[k][3]])
            for k, (srcap, srckey, n, wc, Cap, Sap, outap, outkey) in S:
                sc.op("dve", lambda e, k=k, n=n, Cap=Cap: e.tensor_tensor(out=t1[k][:, 0:n], in0=xnm[k][:, 0:n], in1=Cap, op=ALU.mult),
                      reads=["n_xnm%d" % k, "n_CT"], writes=["n_t1%d" % k])
            for k, (srcap, srckey, n, wc, Cap, Sap, outap, outkey) in S:
                sc.op("dve", lambda e, k=k, n=n, Sap=Sap: e.tensor_tensor(out=t2[k][:, 0:n], in0=pa[k][2][0:64, 0:n], in1=Sap, op=ALU.mult),
                      reads=[pa[k][3], "n_ST"], writes=["n_t2%d" % k])
            for k, (srcap, srckey, n, wc, Cap, Sap, outap, outkey) in S:
                sc.op("dve", lambda e, k=k, n=n, outap=outap: e.tensor_tensor(out=outap, in0=t1[k][:, 0:n], in1=t2[k][:, 0:n], op=ALU.add),
                      reads=["n_t1%d" % k, "n_t2%d" % k], writes=[outkey])

        P_fm_v = P_fm
        PTc = [0]
        ACc = [0, 0]
        accs = [T("n_accs%d" % i, [128, 4, 97], F32) for i in range(3)]
        r4 = T("n_r4", [128, 4], F32)
        c4t = T("n_c4", [128, 4], F32)
        o4 = T("n_o4", [128, 4, 64], F32)
        i4 = T("n_i4", [128, 4, 32], F32)

        def attn(kT, kkey, vfn, vkey, W, kts_fn, mask_fn, nk_fn, epi, krows=64, post_fn=None):
            SK = 4
            for r in range(4):
                units = [(qb, idx, kt, len(kts_fn(qb))) for qb in range(4) for idx, kt in enumerate(kts_fn(qb))]
                slot = {}

                def emit_qk(u):
                    qb, idx, kt, nkt = units[u]
                    nk = nk_fn(kt)
                    pb = PTc[0] % 5
                    PTc[0] += 1
                    slot[u] = pb
                    ps, psk = ps_s[pb], "np_s%d" % pb
                    mms = [(kT[0:krows, kt * 128:kt * 128 + nk], qT[r][0:krows, qb * 512:(qb + 1) * 512], [kkey, "n_qT%d" % r])]
                    mms += mask_fn(kt, qb)
                    for mi, (l_, r_, rk) in enumerate(mms):
                        sc.op("pe", lambda e, l_=l_, r_=r_, ps=ps, nk=nk, mi=mi, last=(mi == len(mms) - 1): e.matmul(
                            ps[0:nk, :], lhsT=l_, rhs=r_, start=(mi == 0), stop=last), reads=rk, writes=[psk])
                    sc.op("act", lambda e, ps=ps, nk=nk, pb=pb: e.activation(out=PT[pb][0:nk, :], in_=ps[0:nk, :], func=AF.Exp),
                          reads=[psk], writes=["n_PT%d" % pb])
                    for _w in range(NSA_WARM):
                        sc.op("pe", lambda e: e.matmul(ps_warm[:], lhsT=ident_b[:], rhs=dm01[:, 4, :], start=True, stop=True),
                              reads=["ident_b", "n_dm01"], writes=["np_w"])
                    m01 = post_fn(kt, qb) if post_fn else None
                    if m01 is not None:
                        sc.op("pool", lambda e, pb=pb, m01=m01: e.tensor_tensor(out=PT[pb][:], in0=PT[pb][:], in1=m01, op=ALU.mult),
                              reads=["n_PT%d" % pb, "n_dm01"], writes=["n_PT%d" % pb])

                def emit_pv(u):
                    qb, idx, kt, nkt = units[u]
                    nk = nk_fn(kt)
                    pb = slot[u]
                    ab = ACc[0] % 2
                    for ts in range(4):
                        sc.op("pe", lambda e, pb=pb, nk=nk, ts=ts, kt=kt, idx=idx, ab=ab, last=(idx == nkt - 1): e.matmul(
                            ps_a[ab][:, ts * W:(ts + 1) * W], lhsT=PT[pb][0:nk, ts * 128:(ts + 1) * 128], rhs=vfn(kt)[0:nk, :],
                            start=(idx == 0 and ts == 0), stop=last, skip_group_check=True),
                            reads=["n_PT%d" % pb, vkey], writes=["np_a%d" % ab])
                    if idx == nkt - 1:
                        ACc[0] += 1
                        sb3 = ACc[1] % 3
                        ACc[1] += 1
                        sc.op("act", lambda e, ab=ab, sb3=sb3: e.copy(
                            out=accs[sb3][:, :, 0:W], in_=ps_a[ab][:, 0:4 * W].rearrange("p (a b) -> p a b", b=W)),
                            reads=["np_a%d" % ab], writes=["n_accs%d" % sb3])
                        epi(r, qb, accs[sb3], "n_accs%d" % sb3)

                for j in range(len(units) + SK):
                    if j < len(units):
                        emit_qk(j)
                    if j - SK >= 0:
                        emit_pv(j - SK)

        def mk_epi(g, branch, first):
            def epi(r, qb, acc, acck):
                tsl = slice(qb * 4, qb * 4 + 4)
                gc = (g * 4 + r) * 3 + branch
                sc.op("dve", lambda e: e.tensor_scalar(out=r4[:], in0=acc[:, :, 64], scalar1=TINY, scalar2=None, op0=ALU.max),
                      reads=[acck], writes=["n_r4"])
                sc.op("dve", lambda e: e.reciprocal(out=r4[:], in_=r4[:]), reads=["n_r4"], writes=["n_r4"])
                sc.op("dve", lambda e: e.tensor_tensor(out=c4t[:], in0=r4[:], in1=gsig[:, tsl, gc], op=ALU.mult),
                      reads=["n_r4", "n_gsig"], writes=["n_c4"])
                if first:
                    sc.op("dve", lambda e: e.tensor_tensor(out=oacc[:, tsl, r, :], in0=acc[:, :, 0:64],
                                                           in1=c4t[:].unsqueeze(2).broadcast_to([128, 4, 64]), op=ALU.mult),
                          reads=[acck, "n_c4"], writes=["n_oacc"])
                    if r == 0:
                        sc.op("dve", lambda e: e.tensor_tensor(out=imp[:, tsl, :], in0=acc[:, :, 65:97],
                                                               in1=r4[:].unsqueeze(2).broadcast_to([128, 4, 32]), op=ALU.mult),
                              reads=[acck, "n_r4"], writes=["n_imp"])
                    else:
                        sc.op("dve", lambda e: e.tensor_tensor(out=i4[:], in0=acc[:, :, 65:97],
                                                               in1=r4[:].unsqueeze(2).broadcast_to([128, 4, 32]), op=ALU.mult),
                              reads=[acck, "n_r4"], writes=["n_i4"])
                        sc.op("dve", lambda e: e.tensor_tensor(out=imp[:, tsl, :], in0=imp[:, tsl, :], in1=i4[:], op=ALU.add),
                              reads=["n_i4", "n_imp"], writes=["n_imp"])
                else:
                    sc.op("dve", lambda e: e.tensor_tensor(out=o4[:], in0=acc[:, :, 0:64],
                                                           in1=c4t[:].unsqueeze(2).broadcast_to([128, 4, 64]), op=ALU.mult),
                          reads=[acck, "n_c4"], writes=["n_o4"])
                    sc.op("dve", lambda e: e.tensor_tensor(out=oacc[:, tsl, r, :], in0=oacc[:, tsl, r, :], in1=o4[:], op=ALU.add),
                          reads=["n_o4", "n_oacc"], writes=["n_oacc"])
            return epi

        TWO_PI = 6.283185307179586
        for s in range(NSEQ):
            sb = s * 2048
            for c in range(8):
                sc.dma("sp", lambda e, s=s, c=c: e.dma_start(
                    out=ki[c * 16:(c + 1) * 16, :], in_=positions[s:s + 1, c * 256:(c + 1) * 256].partition_broadcast(16)),
                    writes=["n_ki"])
            sc.op("dve", lambda e: e.tensor_copy(out=ang[:], in_=ki[:]), reads=["n_ki"], writes=["n_ang"])
            sc.op("dve", lambda e: e.tensor_scalar(out=ang[:], in0=ang[:], scalar1=misc[:, 1:2], scalar2=None, op0=ALU.mult),
                  reads=["n_ang", "n_misc"], writes=["n_ang"])
            for tab, tkey, shift in ((ST, "n_ST", 0.0), (CT, "n_CT", 1.5707963267948966)):
                sc.op("dve", lambda e, shift=shift: e.tensor_scalar(out=rr[:], in0=ang[:], scalar1=shift, scalar2=None, op0=ALU.add),
                      reads=["n_ang"], writes=["n_rr"])
                sc.op("dve", lambda e: e.tensor_scalar(out=kf[:], in0=rr[:], scalar1=1.0 / TWO_PI, scalar2=None, op0=ALU.mult),
                      reads=["n_rr"], writes=["n_kf"])
                sc.op("dve", lambda e: e.tensor_copy(out=ki[:], in_=kf[:]), reads=["n_kf"], writes=["n_ki"])
                sc.op("dve", lambda e: e.tensor_copy(out=kf[:], in_=ki[:]), reads=["n_ki"], writes=["n_kf"])
                sc.op("dve", lambda e: e.scalar_tensor_tensor(out=rr[:], in0=kf[:], scalar=-TWO_PI, in1=rr[:], op0=ALU.mult,
                                                              op1=ALU.add), reads=["n_kf", "n_rr"], writes=["n_rr"])
                sc.op("dve", lambda e: e.tensor_scalar(out=kf[:], in0=rr[:], scalar1=3.141592653589793, scalar2=-TWO_PI,
                                                       op0=ALU.is_gt, op1=ALU.mult), reads=["n_rr"], writes=["n_kf"])
                sc.op("dve", lambda e: e.tensor_tensor(out=rr[:], in0=rr[:], in1=kf[:], op=ALU.add), reads=["n_rr", "n_kf"], writes=["n_rr"])
                sc.op("dve", lambda e: e.tensor_scalar(out=kf[:], in0=rr[:], scalar1=-3.141592653589793, scalar2=TWO_PI,
                                                       op0=ALU.is_lt, op1=ALU.mult), reads=["n_rr"], writes=["n_kf"])
                sc.op("dve", lambda e: e.tensor_tensor(out=rr[:], in0=rr[:], in1=kf[:], op=ALU.add), reads=["n_rr", "n_kf"], writes=["n_rr"])
                sc.op("dve", lambda e: e.tensor_scalar(out=rr[:], in0=rr[:], scalar1=3.1415925, scalar2=-3.1415925,
                                                       op0=ALU.min, op1=ALU.max), reads=["n_rr"], writes=["n_rr"])
                sc.op("act", lambda e: e.activation(out=tab2[:], in_=rr[:], func=AF.Sin), reads=["n_rr"], writes=["n_tab2"])
                for c in range(8):
                    sc.dma("sp", lambda e, tab=tab, c=c: e.dma_start(out=tab[0:16, c * 256:(c + 1) * 256],
                                                                     in_=tab2[c * 16:(c + 1) * 16, :]),
                           reads=["n_tab2"], writes=[tkey])
            sc.dma("sp", lambda e, sb=sb: e.dma_start(out=gsig[:], in_=P_tm[sb:sb + 2048, 3328:3352].rearrange("(tt p) c -> p tt c", p=128)),
                   reads=["P_tm"], writes=["n_gsig"])
            sc.op("act", lambda e: e.activation(out=gsig[:], in_=gsig[:], func=AF.Sigmoid), reads=["n_gsig"], writes=["n_gsig"])
            for g in range(2):
                def load_fm(fmidx, half, dstap, dkey, eng="sp", sb=sb):
                    row0 = fmidx * 128 + half * 64
                    sc.dma(eng, lambda e, row0=row0, dstap=dstap, sb=sb: e.dma_start(out=dstap, in_=P_fm[row0:row0 + 64, sb:sb + 2048]),
                           reads=["P_fm"], writes=[dkey])
                for r in range(4):
                    hh = g * 4 + r
                    src, skey = nxt_src()
                    load_fm(8 + hh // 2, hh % 2, src[:], skey)
                    for q2 in range(2):
                        css = [slice(qb * 512, (qb + 1) * 512) for qb in (2 * q2, 2 * q2 + 1)]
                        norm_rope([(src[:, cs], skey, 512, 0, CT[:, cs], ST[:, cs], qT[r][0:64, cs], "n_qT%d" % r) for cs in css])
                for fmidx, wc, dst, dkey in ((14, 2, ksT, "n_ksT"), (15, 3, kwT, "n_kwT")):
                    src, skey = nxt_src()
                    load_fm(fmidx, g, src[:], skey)
                    for q2 in range(2):
                        css = [slice(qb * 512, (qb + 1) * 512) for qb in (2 * q2, 2 * q2 + 1)]
                        norm_rope([(src[:, cs], skey, 512, wc, CT[:, cs], ST[:, cs], dst[0:64, cs], dkey) for cs in css])
                for col0, va, vk in ((2944, vsa, "n_vsa"), (3200, vwa, "n_vwa")):
                    sc.dma("sp", lambda e, col0=col0, g=g, sb=sb: e.dma_start(
                        out=vld[:], in_=P_tm[sb:sb + 2048, col0 + g * 64:col0 + g * 64 + 64].rearrange("(tt p) c -> p tt c", p=128)),
                        reads=["P_tm"], writes=["n_vld"])
                    sc.op("act", lambda e, va=va: e.copy(out=va[:, :, 0:64], in_=vld[:]), reads=["n_vld"], writes=[vk])
                for kv in range(2):
                    load_fm(12 + kv, g, kcb[:], "n_kcb", eng="pool")
                    for hh in range(2):
                        for l in range(32):
                            sc.op("pe", lambda e, kv=kv, hh=hh, l=l: e.matmul(
                                ps_x[0][:, 0:127], lhsT=w1[kv][:, l, hh * 128:(hh + 1) * 128], rhs=kcb[:, l:l + 2017:16],
                                start=(l == 0), stop=(l == 31)), reads=["n_w1%d" % kv, "n_kcb"], writes=["np_s2"])
                        sc.op("act", lambda e, kv=kv, hh=hh: e.activation(out=hx[:], in_=ps_x[0][:, 0:127], func=AF.Identity,
                                                                          bias=cbias[:, kv, hh:hh + 1]),
                              reads=["np_s2", "n_cbias"], writes=["n_hx"])
                        sc.op("act", lambda e: e.activation(out=hx2[:], in_=hx[:], func=AF.Square), reads=["n_hx"], writes=["n_hx2"])
                        sc.op("dve", lambda e: e.tensor_scalar(out=hx2[:], in0=hx2[:], scalar1=0.044715, scalar2=1.0, op0=ALU.mult,
                                                               op1=ALU.add), reads=["n_hx2"], writes=["n_hx2"])
                        sc.op("dve", lambda e: e.tensor_tensor(out=hx2[:], in0=hx2[:], in1=hx[:], op=ALU.mult),
                              reads=["n_hx2", "n_hx"], writes=["n_hx2"])
                        sc.op("act", lambda e: e.activation(out=hx2[:], in_=hx2[:], func=AF.Sigmoid, scale=1.5957691216057308),
                              reads=["n_hx2"], writes=["n_hx2"])
                        sc.op("dve", lambda e, hh=hh: e.tensor_tensor(out=hT[:, hh, :], in0=hx[:], in1=hx2[:], op=ALU.mult),
                              reads=["n_hx", "n_hx2"], writes=["n_hT"])
                    if kv == 0:
                        for hh in range(2):
                            sc.op("pe", lambda e, hh=hh: e.matmul(ps_x[1][0:64, 0:127], lhsT=w2[0][:, hh, :], rhs=hT[:, hh, :],
                                                                  start=(hh == 0), stop=(hh == 1)),
                                  reads=["n_w20", "n_hT"], writes=["np_s3"])
                        src, skey = nxt_src()
                        sc.op("act", lambda e, src=src: e.copy(out=src[:, 0:127], in_=ps_x[1][0:64, 0:127]), reads=["np_s3"], writes=[skey])
                        norm_rope([(src[:, 0:127], skey, 127, 1, CT[:, 31:2048:16], ST[:, 31:2048:16], kcmpT[0:64, 0:127], "n_kcmpT")])
                    else:
                        for hh in range(2):
                            sc.op("pe", lambda e, hh=hh: e.matmul(ps_x[1][0:127, 0:64], lhsT=hT[:, hh, :], rhs=w2[1][:, hh, :],
                                                                  start=(hh == 0), stop=(hh == 1)),
                                  reads=["n_w21", "n_hT"], writes=["np_s3"])
                        sc.op("act", lambda e: e.copy(out=vcaug[0:127, 0:64], in_=ps_x[1][0:127, 0:64]), reads=["np_s3"], writes=["n_vcaug"])
                attn(kcmpT, "n_kcmpT", lambda kt: vcaug, "n_vcaug", 97, lambda qb: [0],
                     lambda kt, qb: [(ident_b[0:127, 0:127], cmask[0:127, qb, :], ["ident_b", "n_cmask"])],
                     lambda kt: 127, mk_epi(g, 0, True), krows=96)
                sc.op("dve", lambda e: e.tensor_tensor(out=score[:], in0=imp[:], in1=valid[:], op=ALU.mult),
                      reads=["n_imp", "n_valid"], writes=["n_score"])
                sc.op("dve", lambda e: e.tensor_tensor(out=score[:], in0=score[:], in1=addc[:], op=ALU.add),
                      reads=["n_score", "n_addc"], writes=["n_score"])
                for g4 in range(4):
                    tts = [g4 * 4 + k for k in range(4)]
                    for k, tt in enumerate(tts):
                        sc.op("dve", lambda e, tt=tt, k=k: e.max(out=m8a[:, k, :], in_=score[:, tt, :]),
                              reads=["n_score"], writes=["n_m8a%d" % k])
                    for k, tt in enumerate(tts):
                        sc.op("dve", lambda e, tt=tt, k=k: e.match_replace(out=swk[:, k, :], in_to_replace=m8a[:, k, :],
                                                                           in_values=score[:, tt, :], imm_value=-3e38),
                              reads=["n_score", "n_m8a%d" % k], writes=["n_swk%d" % k])
                    for k, tt in enumerate(tts):
                        sc.op("dve", lambda e, k=k: e.max(out=m8b[:, k, :], in_=swk[:, k, :]),
                              reads=["n_swk%d" % k], writes=["n_m8b%d" % k])
                    for k, tt in enumerate(tts):
                        sc.op("dve", lambda e, tt=tt, k=k: e.tensor_scalar(out=sel[:, k, :], in0=score[:, tt, :], scalar1=m8b[:, k, 7:8],
                                                                           scalar2=None, op0=ALU.is_ge),
                              reads=["n_score", "n_m8b%d" % k], writes=["n_sel%d" % k])
                    sc.op("dve", lambda e: e.tensor_scalar(out=selb[:, :, 64:96], in0=sel[:], scalar1=-NEG, scalar2=NEG, op0=ALU.mult,
                                                           op1=ALU.add), reads=["n_sel%d" % k for k in range(4)], writes=["n_selb"])
                    pst = ps_x[0][:, 0:256].bitcast(BF16).rearrange("p (a b) -> p a b", b=128)
                    for k in range(4):
                        sc.op("pe", lambda e, pst=pst, k=k: e.transpose(out=pst[0:96, k, :], in_=selb[:, k, :], identity=ident_b[:]),
                              reads=["n_selb", "ident_b"], writes=["np_s2"])
                    sc.op("act", lambda e, g4=g4, pst=pst: e.copy(
                        out=nselT[64:96, g4 * 512:(g4 + 1) * 512].rearrange("p (a b) -> p a b", b=128), in_=pst[64:96, :, :]),
                        reads=["np_s2"], writes=["n_nselT"])
                for r in range(4):
                    sc.op("act" if r % 2 == 0 else "dve",
                          (lambda e, r=r: e.copy(out=qT[r][64:96, :], in_=nselT[64:96, :])) if r % 2 == 0 else
                          (lambda e, r=r: e.tensor_copy(out=qT[r][64:96, :], in_=nselT[64:96, :])),
                          reads=["n_nselT"], writes=["n_qT%d" % r])
                attn(ksT, "n_ksT", lambda kt: vsa[:, kt, :], "n_vsa", 65, lambda qb: list(range(0, 4 * qb + 4)),
                     lambda kt, qb: [], lambda kt: 128, mk_epi(g, 1, False), krows=96,
                     post_fn=lambda kt, qb: dm01[:, 4 + kt - 4 * qb, :] if kt >= 4 * qb else None)
                attn(kwT, "n_kwT", lambda kt: vwa[:, kt, :], "n_vwa", 65, lambda qb: list(range(max(0, 4 * qb - 4), 4 * qb + 4)),
                     lambda kt, qb: [], lambda kt: 128, mk_epi(g, 2, False), krows=96,
                     post_fn=lambda kt, qb: dm01[:, 4 + kt - 4 * qb, :])
                o3 = oacc[:].rearrange("p t r d -> p (t r) d")
                sc.op("dve", lambda e: e.tensor_tensor(out=ssq[:], in0=o3, in1=o3, op=ALU.mult), reads=["n_oacc"], writes=["n_ssq"])
                sc.op("dve", lambda e: e.tensor_reduce(out=ss64[:], in_=ssq[:], axis=AX.X, op=ALU.add), reads=["n_ssq"], writes=["n_ss64"])
                sc.op("act", lambda e: e.activation(out=ss64[:], in_=ss64[:], func=AF.Sqrt, scale=1.0 / 64, bias=epsc[:]),
                      reads=["n_ss64"], writes=["n_ss64"])
                sc.op("dve", lambda e: e.reciprocal(out=ss64[:], in_=ss64[:]), reads=["n_ss64"], writes=["n_ss64"])
                sc.op("dve", lambda e: e.tensor_tensor(out=ssq[:], in0=o3, in1=ss64[:].unsqueeze(2).broadcast_to([128, 64, 64]),
                                                       op=ALU.mult), reads=["n_oacc", "n_ss64"], writes=["n_ssq"])
                sc.op("dve", lambda e: e.tensor_tensor(out=ssq[:], in0=ssq[:], in1=onwb[:].unsqueeze(1).broadcast_to([128, 64, 64]),
                                                       op=ALU.mult), reads=["n_ssq", "n_onwb"], writes=["n_ssq"])
                for tt in range(16):
                    sc.dma("sp", lambda e, tt=tt, g=g, sb=sb: e.dma_start(
                        out=Y_tm[sb + tt * 128:sb + (tt + 1) * 128, 512 + g * 256:512 + (g + 1) * 256],
                        in_=ssq[:, tt * 4:(tt + 1) * 4, :].rearrange("p r d -> p (r d)")), reads=["n_ssq"], writes=["Y_tm"])
        barrier(sc)


def stage_uv(nc, sc, u_tab, v_tab, UV):
    with contextlib.ExitStack() as st:
        tmp = [st.enter_context(nc.sbuf_tensor("uv_tmp%d" % i, [128, 8, D], BF16)) for i in range(3)]
        UVv = UV.rearrange("(p r) d -> p r d", p=128)
        k = 0
        for half, tab in enumerate((u_tab, v_tab)):
            tv = tab.rearrange("(p r) d -> p r d", p=128)
            for ci in range(16):
                b = k % 3
                k += 1
                sc.dma("pool", lambda e, b=b, tv=tv, ci=ci: e.dma_start(out=tmp[b][:], in_=tv[:, ci * 8:(ci + 1) * 8, :]),
                       writes=["uv_tmp%d" % b])
                sc.dma("sp", lambda e, b=b, ci=ci, half=half: e.dma_start(
                    out=UVv[:, ci * 8:(ci + 1) * 8, half * D:(half + 1) * D], in_=tmp[b][:]),
                    reads=["uv_tmp%d" % b], writes=["UV"])
        barrier(sc)


def stage_out(nc, sc, stack, NT, x, Y_tm, w_out, out, peer):
    T = lambda name, shape, dt: stack.enter_context(nc.sbuf_tensor(name, shape, dt))
    wout = T("o_wout", [128, 8, D], BF16)
    yt = [T("o_yt%d" % i, [128, D], F32) for i in range(2)]
    xt = [T("o_xt%d" % i, [128, D], F32) for i in range(2)]
    ht = [T("o_ht%d" % i, [128, D], F32) for i in range(3)]
    ybf = T("o_ybf", [128, D], BF16)
    yT = T("o_yT", [128, 8, 128], BF16)
    sc.dma("pool", lambda e: e.dma_start(out=wout[:], in_=w_out.rearrange("(kc p) n -> p kc n", p=128)), writes=["o_wout"])

    def tile_ops(i):
        b = i % 2
        hb = i % 3
        ops = []
        add = lambda *a, **k: ops.append(lambda: sc.op(*a, **k))
        ops.append(lambda: sc.dma("sp", lambda e: e.dma_start(out=yt[b][:], in_=Y_tm[i * 128:(i + 1) * 128, :]),
                                  reads=["Y_tm"], writes=["o_yt%d" % b]))
        ops.append(lambda: sc.dma("sp", lambda e: e.dma_start(out=xt[b][:], in_=x[i * 128:(i + 1) * 128, :]),
                                  writes=["o_xt%d" % b]))
        add("act", lambda e: e.copy(out=ybf[:], in_=yt[b][:]), reads=["o_yt%d" % b], writes=["o_ybf"])
        for kc in range(8):
            add("pe", lambda e, kc=kc: e.transpose(out=peer.ps_t_b[:, kc, :], in_=ybf[:, kc * 128:(kc + 1) * 128],
                                                   identity=peer.ident_b[:]), reads=["o_ybf", "ident_b"], writes=["pp_t"])
        add("act", lambda e: e.copy(out=yT[:], in_=peer.ps_t_b), reads=["pp_t"], writes=["o_yT"])
        for hf in range(2):
            ps = peer.ps_a if hf == 0 else peer.ps_b
            pk = "pp_a" if hf == 0 else "pp_b"
            for kc in range(8):
                add("pe", lambda e, hf=hf, kc=kc, ps=ps: e.matmul(ps[:].rearrange("p a b -> p (a b)"), lhsT=yT[:, kc, :],
                                                                  rhs=wout[:, kc, hf * 512:(hf + 1) * 512],
                                                                  start=(kc == 0), stop=(kc == 7)),
                    reads=["o_yT", "o_wout"], writes=[pk])
            add("dve", lambda e, hf=hf, ps=ps: e.tensor_tensor(
                out=ht[hb][:, hf * 512:(hf + 1) * 512], in0=ps[:].rearrange("p a b -> p (a b)"),
                in1=xt[b][:, hf * 512:(hf + 1) * 512], op=ALU.add), reads=[pk, "o_xt%d" % b], writes=["o_ht%d" % hb])
        return ops + peer.pre_ops(i, ht[hb][:], "o_ht%d" % hb)

    for f in tile_ops(0):
        f()
    for i in range(NT):
        pending = tile_ops(i + 1) if i + 1 < NT else []
        peer.loop(i, ht[i % 3][:], "o_ht%d" % (i % 3), out[i * 128:(i + 1) * 128, :], "out", pending)


WNAMES = ["norm1_w", "w_in", "hg_lb_logits", "hg_out_norm_w", "nsa_q_norm_w", "nsa_k_norm_w", "cmp_pe_k", "cmp_pe_v",
          "cmp_w1_k", "cmp_w2_k", "cmp_w1_v", "cmp_w2_v", "nsa_out_norm_w", "w_out", "norm2_w", "peer_w_q",
          "peer_sub_keys", "peer_u", "peer_v"]
WSHAPES = {"norm1_w": [1, D], "w_in": [D, INW], "hg_lb_logits": [2, 512], "hg_out_norm_w": [1, 128],
           "nsa_q_norm_w": [1, 64], "nsa_k_norm_w": [3, 64], "cmp_pe_k": [32, 64], "cmp_pe_v": [32, 64],
           "cmp_w1_k": [2048, 256], "cmp_w2_k": [256, 64], "cmp_w1_v": [2048, 256], "cmp_w2_v": [256, 64],
           "nsa_out_norm_w": [1, 64], "w_out": [D, D], "norm2_w": [1, D], "peer_w_q": [D, 2048],
           "peer_sub_keys": [2, 128, 128], "peer_u": [16384, D], "peer_v": [16384, D]}


def build_program(NSEQ):
    NT = NSEQ * 16
    nc = bass.Bass("TRN2", target_bir_lowering=False)
    din = lambda name, shape, dt=F32: nc.dram_tensor(name, shape, dt, kind="ExternalInput").ap()
    x = din("x", [NT * 128, D])
    positions = din("positions", [NSEQ, 2048], I32)
    w = {k: din(k, v) for k, v in WSHAPES.items()}
    cst = {k: din(k, v) for k, v in CONST_SHAPES.items()}
    out = nc.dram_tensor("out", [NT * 128, D], F32, kind="ExternalOutput").ap()
    P_tm = nc.dram_tensor("P_tm", [NT * 128, INW], F32, kind="Internal").ap()
    P_fm = nc.dram_tensor("P_fm", [16 * 128, NT * 128], F32, kind="Internal").ap()
    Y_tm = nc.dram_tensor("Y_tm", [NT * 128, D], F32, kind="Internal").ap()
    UV = nc.dram_tensor("UV", [16384, 2 * D], BF16, kind="Internal").ap()
    with contextlib.ExitStack() as stack:
        sc = Sched(nc, stack)
        T = lambda name, shape, dt: stack.enter_context(nc.sbuf_tensor(name, shape, dt))
        ident_f = T("ident_f", [128, 128], F32)
        ident_b = T("ident_b", [128, 128], BF16)
        ublk = T("ublk", [128, 128], F32)
        wrev = T("wrev", [128, 128], F32)
        epsc = T("epsc", [128, 1], F32)
        pc = T("pc", [128, 64], F32)
        sc.dma("sp", lambda e: e.dma_start(out=ident_f[:], in_=cst["c_identf"]), writes=["ident_f"])
        sc.dma("pool", lambda e: e.dma_start(out=ident_b[:], in_=cst["c_identf"]), writes=["ident_b"])
        sc.dma("sp", lambda e: e.dma_start(out=ublk[:], in_=cst["c_ublk"]), writes=["ublk"])
        sc.dma("sp", lambda e: e.dma_start(out=wrev[:], in_=cst["c_wrev"]), writes=["wrev"])
        sc.dma("sp", lambda e: e.dma_start(out=pc[:], in_=cst["c_pc"]), writes=["pc"])
        sc.op("dve", lambda e: e.memset(epsc[:], EPS), writes=["epsc"])
        barrier(sc)
        stage_proj(nc, sc, NT, x, w["norm1_w"], w["w_in"], P_tm, P_fm, ident_b, epsc,
                   uv=(w["peer_u"], w["peer_v"], UV))
        stage_hgrn(nc, sc, NT, P_tm, P_fm, Y_tm, w["hg_lb_logits"], w["hg_out_norm_w"], ublk, wrev, epsc,
                   cst["c_rowm"], cst["c_colm"])
        stage_nsa(nc, sc, NSEQ, P_tm, P_fm, Y_tm, positions, w["nsa_q_norm_w"], w["nsa_k_norm_w"], w["cmp_pe_k"],
                  w["cmp_pe_v"], w["cmp_w1_k"], w["cmp_w2_k"], w["cmp_w1_v"], w["cmp_w2_v"], w["nsa_out_norm_w"],
                  cst, ident_b, epsc)
        with contextlib.ExitStack() as st2:
            peer = Peer(nc, sc, st2, w["norm2_w"], w["peer_w_q"], w["peer_sub_keys"], UV, ident_f, ident_b, pc, cst["c_wsel"])
            stage_out(nc, sc, st2, NT, x, Y_tm, w["w_out"], out, peer)
            sc.finish()
            with nc.Block() as block:
                sc.replay(block)
    return nc


def make_inputs(inputs, c0, c1):
    m = {"x": np.ascontiguousarray(inputs["x"][c0:c1]).reshape(-1, D).astype(np.float32, copy=False),
         "positions": np.ascontiguousarray(inputs["positions"][c0:c1]).astype(np.int32, copy=False)}
    for k in WNAMES:
        a = np.asarray(inputs[k])
        if k != "hg_lb_logits":
            a = a[0]
        m[k] = np.ascontiguousarray(a, dtype=np.float32).reshape(WSHAPES[k])
    return m


def kernel(**inputs):
    ncores = 8
    B = inputs["x"].shape[0]
    per = B // ncores
    nc = build_program(per)
    consts = host_consts()
    in_maps = []
    for c in range(ncores):
        m = make_inputs(inputs, c * per, (c + 1) * per)
        m.update(consts)
        in_maps.append(m)
    res = run_bass_kernel_spmd(nc, in_maps, core_ids=list(range(ncores)))
    outs = [np.asarray(r["out"]).reshape(per, S, D) for r in res.results]
    return np.concatenate(outs, axis=0).astype(np.float32, copy=False)
```

```python
import contextlib
import numpy as np
import ml_dtypes
import concourse.bass as bass
import concourse.mybir as mybir
from concourse.bass_utils import run_bass_kernel_spmd

F32 = mybir.dt.float32
BF16 = mybir.dt.bfloat16
I32 = mybir.dt.int32
U32 = mybir.dt.uint32
AF = mybir.ActivationFunctionType
ALU = mybir.AluOpType
AX = mybir.AxisListType

ENGS = ("pe", "dve", "act", "pool", "sp")
NEG = -30000.0


class Sched:
    def __init__(self, nc, stack, ndma=12):
        self.nc = nc
        self.q = {e: [] for e in ENGS}
        self.cnt = {e: 0 for e in ENGS}
        self.esem = {e: stack.enter_context(nc.semaphore("es_" + e)) for e in ENGS}
        self.ndma = ndma
        self.dsem = {e: [stack.enter_context(nc.semaphore("ds_%s_%d" % (e, i))) for i in range(ndma)]
                     for e in ("sp", "pool", "act")}
        self.dcnt = {e: 0 for e in ("sp", "pool", "act")}
        self.seen = {e: {} for e in ENGS}
        self.st = {}
        self.ninst = 0

    def _sem(self, key):
        if key[0] == "dma":
            return self.dsem[key[1]][key[2]]
        return self.esem[key[0]]

    def _wait(self, eng, ev):
        key, val = ev
        if self.seen[eng].get(key, 0) >= val:
            return
        self.seen[eng][key] = val
        sem = self._sem(key)
        self.q[eng].append(lambda e, sem=sem, val=val: e.wait_ge(sem, val))
        self.ninst += 1

    def _deps(self, eng, reads, writes):
        deps = []
        for k in reads:
            s = self.st.get(k)
            if s and s[0] is not None:
                deps.append(s[0])
            if s and k[1:3] == "p_":
                deps.extend(ev for ek, ev in s[1].items() if ek != (eng,))
        for k in writes:
            s = self.st.get(k)
            if s:
                if s[0] is not None:
                    deps.append(s[0])
                deps.extend(s[1].values())
        for ev in deps:
            if eng == "pe" and ev[0] == ("pe",):
                continue
            self._wait(eng, ev)

    def _commit(self, ev, evkey, reads, writes):
        for k in reads:
            s = self.st.setdefault(k, [None, {}])
            s[1][evkey] = ev
        for k in writes:
            self.st[k] = [ev, {}]

    def op(self, eng, fn, reads=(), writes=()):
        self._deps(eng, reads, writes)
        self.cnt[eng] += 1
        sem = self.esem[eng]
        self.q[eng].append(lambda e, fn=fn, sem=sem: fn(e).then_inc(sem, 1))
        self.ninst += 1
        ev = ((eng,), self.cnt[eng])
        self._commit(ev, (eng,), reads, writes)

    def dma(self, eng, fn, reads=(), writes=()):
        self._deps(eng, reads, writes)
        k = self.dcnt[eng]
        slot = k % self.ndma
        key = ("dma", eng, slot)
        if k >= self.ndma:
            self._wait(eng, (key, 16 * (k // self.ndma)))
        self.dcnt[eng] += 1
        val = 16 * (k // self.ndma + 1)
        sem = self.dsem[eng][slot]
        self.q[eng].append(lambda e, fn=fn, sem=sem: fn(e).then_inc(sem, 16))
        self.ninst += 1
        ev = (key, val)
        self._commit(ev, key, reads, writes)

    def finish(self):
        for eng in ("sp", "pool", "act"):
            k = self.dcnt[eng]
            for slot in range(min(k, self.ndma)):
                n = (k - 1 - slot) // self.ndma + 1
                self._wait(eng, (("dma", eng, slot), 16 * n))

    def replay(self, block):
        q = self.q

        @block.sync
        def _(e):
            for f in q["sp"]:
                f(e)

        @block.gpsimd
        def _(e):
            for f in q["pool"]:
                f(e)

        @block.tensor
        def _(e):
            for f in q["pe"]:
                f(e)

        @block.scalar
        def _(e):
            for f in q["act"]:
                f(e)

        @block.vector
        def _(e):
            for f in q["dve"]:
                f(e)


D = 1024
S = 2048
PEER_HEADS = 8
PEER_K = 16
EPS = 1e-6


def peer_consts():
    c = np.zeros((128, 64), np.float32)
    c[:, 0:16] = np.arange(16, dtype=np.float32)[None, :] * 16.0
    c[:, 16:32] = np.arange(16, dtype=np.float32)[None, :]
    return c


class Peer:
    def __init__(self, nc, sc, stack, norm2_w, w_q, sub_keys, uv_tab, ident_f, ident_b, pc, wsel_dram, NB=10):
        self.nc, self.sc = nc, sc
        self.uv_tab = uv_tab
        self.NB = NB
        T = lambda name, shape, dt: stack.enter_context(nc.sbuf_tensor(name, shape, dt))
        P = lambda name, shape, dt: stack.enter_context(nc.psum_tensor(name, shape, dt))
        self.ident_f, self.ident_b, self.pc = ident_f, ident_b, pc
        self.w2b = T("pr_w2b", [128, D], F32)
        self.wq = T("pr_wq", [128, 4, 2, 2048], BF16)
        self.skT = T("pr_skT", [128, 2, 128], BF16)
        self.skl = T("pr_skl", [128, 2, 128], F32)
        self.junk = T("pr_junk", [128, D], BF16)
        self.ss = T("pr_ss", [128, 1], F32)
        self.rstd = T("pr_rstd", [128, 1], F32)
        self.xn = T("pr_xn", [128, D], BF16)
        self.xnT = [T("pr_xnT%d" % i, [128, 4, 128, 2], BF16) for i in range(2)]
        self.qT = T("pr_qT", [128, 16, 128], BF16)
        self.Ssb = T("pr_S", [128, 16, 128], F32)
        self.Swk = T("pr_Swk", [128, 128], F32)
        self.v = T("pr_v", [128, 16, 16], F32)
        self.ix = T("pr_ix", [128, 16, 16], U32)
        self.ixf = T("pr_ixf", [128, 16, 16], F32)
        self.cand = T("pr_cand", [128, 8, 256], F32)
        self.cwk = T("pr_cwk", [128, 256], F32)
        self.tv = T("pr_tv", [128, 8, 16], F32)
        self.pos = T("pr_pos", [128, 8, 16], U32)
        self.posa = T("pr_posa", [128, 8, 16], U32)
        self.posb = T("pr_posb", [128, 8, 16], U32)
        self.epsc = T("pr_epsc", [128, 1], F32)
        self.pb = T("pr_pb", [128, 8, 16], F32)
        self.pa = T("pr_pa", [128, 8, 16], F32)
        self.eq = T("pr_eq", [128, 8, 16, 16], F32)
        self.e1 = T("pr_e1", [128, 8, 16], F32)
        self.e2 = T("pr_e2", [128, 8, 16], F32)
        self.gt = T("pr_gt", [128, 8, 16], F32)
        self.gs = T("pr_gs", [128, 8], F32)
        self.eTi = [T("pr_eTi%d" % i, [128, 128], I32) for i in range(2)]
        self.eTf = T("pr_eTf", [128, 128], F32)
        self.gT = [T("pr_gT%d" % i, [128, 128], F32) for i in range(2)]
        self.actb = T("pr_actb", [128, 128], BF16)
        self.oT = T("pr_oT", [128, 8, 128], F32)
        self.osb = T("pr_osb", [128, D], F32)
        self.G = [T("pr_G%d" % i, [128, 2 * D], BF16) for i in range(NB)]
        self.GT = [T("pr_GT%d" % i, [128, 4, 128, 2], BF16) for i in range(2)]
        self.hg = T("pr_hg", [128, 128], F32)
        self.wsel = T("pr_wsel", [128, 256], BF16)
        self.actD = [T("pr_actD%d" % i, [128, 128], BF16) for i in range(3)]
        self.ps_a = P("pp_a", [128, 4, 128], F32)
        self.ps_b = P("pp_b", [128, 4, 128], F32)
        self.ps_t = P("pp_t", [128, 4, 128], F32)
        self.ps_t_b = self.ps_t[:].rearrange("p a b -> p (a b)").bitcast(BF16).rearrange("p (a b) -> p a b", b=128)
        self.ps_g = [P("pp_g%d" % i, [128, 4, 128], F32) for i in range(2)]
        self.ps_hd = P("pp_hd", [128, 512], F32)
        self.ps_o = P("pp_o", [128, 8, 128], F32)
        self.tok = 0

        sc.op("dve", lambda e: e.memset(self.epsc[:], EPS), writes=["pr_epsc"])
        sc.dma("pool", lambda e: e.dma_start(out=self.wsel[:], in_=wsel_dram), writes=["pr_wsel"])
        sc.dma("sp", lambda e: e.dma_start(out=self.w2b[:], in_=norm2_w[0:1, :].partition_broadcast(128)),
               writes=["pr_w2b"])
        sc.dma("pool", lambda e: e.dma_start(out=self.wq[:], in_=w_q.rearrange("(c dp two) n -> dp c two n", dp=128, two=2)),
               writes=["pr_wq"])
        sc.dma("sp", lambda e: e.dma_start(out=self.skl[:], in_=sub_keys.rearrange("j n d -> n j d")),
               writes=["pr_skl"])
        for j in range(2):
            sc.op("pe", lambda e, j=j: e.transpose(out=self.ps_a[:, j, :], in_=self.skl[:, j, :], identity=ident_f[:]),
                  reads=["pr_skl", "ident_f"], writes=["pp_a"])
        sc.op("act", lambda e: e.copy(out=self.skT[:], in_=self.ps_a[:, 0:2, :]), reads=["pp_a"], writes=["pr_skT"])

    def pre_ops(self, i, h_t, hkey):
        sc = self.sc
        p = i % 2
        ops = []
        add = lambda *a, **k: ops.append(lambda: sc.op(*a, **k))
        xnT, eTi, gT = self.xnT[p], self.eTi[p], self.gT[p]
        kxnT, keTi, kgT = "pr_xnT%d" % p, "pr_eTi%d" % p, "pr_gT%d" % p
        add("act", lambda e: e.activation(out=self.junk[:], in_=h_t, func=AF.Square, accum_out=self.ss[:]),
            reads=[hkey], writes=["pr_junk", "pr_ss"])
        add("act", lambda e: e.activation(out=self.rstd[:], in_=self.ss[:], func=AF.Sqrt, scale=1.0 / D, bias=self.epsc[:]),
            reads=["pr_ss"], writes=["pr_rstd"])
        add("dve", lambda e: e.reciprocal(out=self.rstd[:], in_=self.rstd[:]), reads=["pr_rstd"], writes=["pr_rstd"])
        add("dve", lambda e: e.scalar_tensor_tensor(out=self.xn[:], in0=h_t, scalar=self.rstd[:, 0:1],
                                                    in1=self.w2b[:], op0=ALU.mult, op1=ALU.mult),
            reads=[hkey, "pr_rstd", "pr_w2b"], writes=["pr_xn"])
        xnf = self.xn[:].bitcast(F32)
        for c in range(4):
            add("pe", lambda e, c=c: e.transpose(out=self.ps_t[:, c, :], in_=xnf[:, c * 128:(c + 1) * 128],
                                                 identity=self.ident_f[:]),
                reads=["pr_xn", "ident_f"], writes=["pp_t"])
        add("act", lambda e: e.copy(out=xnT[:].rearrange("p c t two -> p (c t two)"),
                                    in_=self.ps_t_b.rearrange("p a b -> p (a b)")), reads=["pp_t"], writes=[kxnT])
        for grp in range(4):
            ps = self.ps_a if grp % 2 == 0 else self.ps_b
            pk = "pp_a" if grp % 2 == 0 else "pp_b"
            for c4 in range(4):
                cq = grp * 4 + c4
                for kc in range(8):
                    add("pe", lambda e, ps=ps, c4=c4, cq=cq, kc=kc: e.matmul(
                        ps[:, c4, :], lhsT=self.wq[:, kc // 2, kc % 2, cq * 128:(cq + 1) * 128], rhs=xnT[:, kc // 2, :, kc % 2],
                        start=(kc == 0), stop=(kc == 7)), reads=["pr_wq", kxnT], writes=[pk])
            add("act", lambda e, ps=ps, grp=grp: e.copy(out=self.qT[:, grp * 4:(grp + 1) * 4, :], in_=ps[:]),
                reads=[pk], writes=["pr_qT%d" % grp])
        for grp in range(4):
            ps = self.ps_a if grp % 2 == 0 else self.ps_b
            pk = "pp_a" if grp % 2 == 0 else "pp_b"
            for c4 in range(4):
                cq = grp * 4 + c4
                add("pe", lambda e, ps=ps, c4=c4, cq=cq: e.matmul(
                    ps[:, c4, :], lhsT=self.qT[:, cq, :], rhs=self.skT[:, cq % 2, :], start=True, stop=True),
                    reads=["pr_qT%d" % grp, "pr_skT"], writes=[pk])
            add("act", lambda e, ps=ps, grp=grp: e.copy(out=self.Ssb[:, grp * 4:(grp + 1) * 4, :], in_=ps[:]),
                reads=[pk], writes=["pr_S%d" % grp])
        for cq in range(16):
            sk = "pr_S%d" % (cq // 4)
            add("dve", lambda e, cq=cq: e.max(out=self.v[:, cq, 0:8], in_=self.Ssb[:, cq, :]), reads=[sk], writes=["pr_v"])
            add("dve", lambda e, cq=cq: e.max_index(out=self.ix[:, cq, 0:8], in_max=self.v[:, cq, 0:8],
                                                    in_values=self.Ssb[:, cq, :]), reads=[sk, "pr_v"], writes=["pr_ix"])
            add("dve", lambda e, cq=cq: e.match_replace(out=self.Swk[:], in_to_replace=self.v[:, cq, 0:8],
                                                        in_values=self.Ssb[:, cq, :], imm_value=-1e30),
                reads=[sk, "pr_v"], writes=["pr_Swk"])
            add("dve", lambda e, cq=cq: e.max(out=self.v[:, cq, 8:16], in_=self.Swk[:]), reads=["pr_Swk"], writes=["pr_v"])
            add("dve", lambda e, cq=cq: e.max_index(out=self.ix[:, cq, 8:16], in_max=self.v[:, cq, 8:16],
                                                    in_values=self.Swk[:]), reads=["pr_Swk", "pr_v"], writes=["pr_ix"])
        add("dve", lambda e: e.tensor_copy(out=self.ixf[:], in_=self.ix[:]), reads=["pr_ix"], writes=["pr_ixf"])
        for h in range(8):
            add("dve", lambda e, h=h: e.tensor_tensor(
                out=self.cand[:, h, :].rearrange("p (a b) -> p a b", b=16),
                in0=self.v[:, 2 * h, :].unsqueeze(2).broadcast_to([128, 16, 16]),
                in1=self.v[:, 2 * h + 1, :].unsqueeze(1).broadcast_to([128, 16, 16]), op=ALU.add),
                reads=["pr_v"], writes=["pr_cand"])
        for h in range(8):
            add("dve", lambda e, h=h: e.max(out=self.tv[:, h, 0:8], in_=self.cand[:, h, :]), reads=["pr_cand"], writes=["pr_tv"])
            add("dve", lambda e, h=h: e.max_index(out=self.pos[:, h, 0:8], in_max=self.tv[:, h, 0:8],
                                                  in_values=self.cand[:, h, :]), reads=["pr_cand", "pr_tv"], writes=["pr_pos"])
            add("dve", lambda e, h=h: e.match_replace(out=self.cwk[:], in_to_replace=self.tv[:, h, 0:8],
                                                      in_values=self.cand[:, h, :], imm_value=-1e30),
                reads=["pr_cand", "pr_tv"], writes=["pr_cwk"])
            add("dve", lambda e, h=h: e.max(out=self.tv[:, h, 8:16], in_=self.cwk[:]), reads=["pr_cwk"], writes=["pr_tv"])
            add("dve", lambda e, h=h: e.max_index(out=self.pos[:, h, 8:16], in_max=self.tv[:, h, 8:16],
                                                  in_values=self.cwk[:]), reads=["pr_cwk", "pr_tv"], writes=["pr_pos"])
        add("dve", lambda e: e.tensor_scalar(out=self.posb[:], in0=self.pos[:], scalar1=15, scalar2=None,
                                             op0=ALU.bitwise_and), reads=["pr_pos"], writes=["pr_posb"])
        add("dve", lambda e: e.tensor_scalar(out=self.posa[:], in0=self.pos[:], scalar1=240, scalar2=None,
                                             op0=ALU.bitwise_and), reads=["pr_pos"], writes=["pr_posa"])
        add("dve", lambda e: e.tensor_copy(out=self.pb[:], in_=self.posb[:]), reads=["pr_posb"], writes=["pr_pb"])
        add("dve", lambda e: e.tensor_copy(out=self.pa[:], in_=self.posa[:]), reads=["pr_posa"], writes=["pr_pa"])
        eq3 = self.eq[:].rearrange("p h k a -> p (h k) a")
        for which, (src, c0, j, dst) in enumerate(((self.pa, 0, 0, self.e1), (self.pb, 16, 1, self.e2))):
            add("dve", lambda e, src=src, c0=c0: e.tensor_tensor(
                out=eq3, in0=src[:].rearrange("p h k -> p (h k)").unsqueeze(2).broadcast_to([128, 128, 16]),
                in1=self.pc[:, c0:c0 + 16].unsqueeze(1).broadcast_to([128, 128, 16]), op=ALU.is_equal),
                reads=["pr_pa", "pr_pb", "pc"], writes=["pr_eq"])
            for h in range(8):
                add("dve", lambda e, j=j, h=h: e.tensor_tensor(
                    out=self.eq[:, h, :, :], in0=self.eq[:, h, :, :],
                    in1=self.ixf[:, 2 * h + j, :].unsqueeze(1).broadcast_to([128, 16, 16]),
                    op=ALU.mult), reads=["pr_eq", "pr_ixf"], writes=["pr_eq"])
            add("dve", lambda e, dst=dst: e.tensor_reduce(out=dst[:].rearrange("p h k -> p (h k)"), in_=eq3,
                                                          axis=AX.X, op=ALU.add),
                reads=["pr_eq"], writes=["pr_e%d" % (which + 1)])
        add("dve", lambda e: e.scalar_tensor_tensor(out=self.e1[:], in0=self.e1[:], scalar=128.0, in1=self.e2[:],
                                                    op0=ALU.mult, op1=ALU.add),
            reads=["pr_e1", "pr_e2"], writes=["pr_e1"])
        add("dve", lambda e: e.tensor_tensor(out=self.gt[:], in0=self.tv[:],
                                             in1=self.tv[:, :, 0:1].broadcast_to([128, 8, 16]), op=ALU.subtract),
            reads=["pr_tv"], writes=["pr_gt"])
        add("act", lambda e: e.activation(out=self.gt[:], in_=self.gt[:], func=AF.Exp), reads=["pr_gt"], writes=["pr_gt"])
        add("dve", lambda e: e.tensor_reduce(out=self.gs[:], in_=self.gt[:], axis=AX.X, op=ALU.add),
            reads=["pr_gt"], writes=["pr_gs"])
        add("dve", lambda e: e.reciprocal(out=self.gs[:], in_=self.gs[:]), reads=["pr_gs"], writes=["pr_gs"])
        add("dve", lambda e: e.tensor_tensor(out=self.gt[:], in0=self.gt[:],
                                             in1=self.gs[:].unsqueeze(2).broadcast_to([128, 8, 16]), op=ALU.mult),
            reads=["pr_gt", "pr_gs"], writes=["pr_gt"])
        add("pe", lambda e: e.transpose(out=self.ps_a[:, 0, :], in_=self.e1[:].rearrange("p h k -> p (h k)"),
                                        identity=self.ident_f[:]), reads=["pr_e1", "ident_f"], writes=["pp_a"])
        add("pe", lambda e: e.transpose(out=self.ps_a[:, 1, :], in_=self.gt[:].rearrange("p h k -> p (h k)"),
                                        identity=self.ident_f[:]), reads=["pr_gt", "ident_f"], writes=["pp_a"])
        add("act", lambda e: e.copy(out=self.eTf[:], in_=self.ps_a[:, 0, :]), reads=["pp_a"], writes=["pr_eTf"])
        add("dve", lambda e: e.tensor_copy(out=eTi[:], in_=self.eTf[:]), reads=["pr_eTf"], writes=[keTi])
        add("act", lambda e: e.copy(out=gT[:], in_=self.ps_a[:, 1, :]), reads=["pp_a"], writes=[kgT])
        return ops

    def loop(self, i, h_t, hkey, out_ap, outkey, pending):
        sc = self.sc
        p = i % 2
        xnT, eTi, gT = self.xnT[p], self.eTi[p], self.gT[p]
        kxnT, keTi, kgT = "pr_xnT%d" % p, "pr_eTi%d" % p, "pr_gT%d" % p
        NB = self.NB
        per = (len(pending) + 119) // 120 if pending else 0
        bufs, gbs = {}, {}

        def e1(t):
            b = self.tok % NB
            g2 = self.tok % 2
            self.tok += 1
            bufs[t], gbs[t] = b, g2
            gk = "pr_G%d" % b
            sc.dma("pool", lambda e, b=b, t=t: e.indirect_dma_start(
                out=self.G[b][:], out_offset=None, in_=self.uv_tab,
                in_offset=bass.IndirectOffsetOnAxis(ap=eTi[:, t:t + 1], axis=0)), reads=[keTi], writes=[gk])
            Gf = self.G[b][:, 0:D].bitcast(F32)
            for c in range(4):
                sc.op("pe", lambda e, Gf=Gf, c=c, g2=g2: e.transpose(
                    out=self.ps_g[g2][:, c, :], in_=Gf[:, c * 128:(c + 1) * 128], identity=self.ident_f[:]),
                    reads=[gk, "ident_f"], writes=["pp_g%d" % g2])
            sc.op("act", lambda e, g2=g2: e.copy(
                out=self.GT[g2][:].rearrange("p c s two -> p (c s two)"),
                in_=self.ps_g[g2][:].rearrange("p a b -> p (a b)").bitcast(BF16)),
                reads=["pp_g%d" % g2], writes=["pr_GT%d" % g2])

        def e2(t):
            g2 = gbs[t]
            for kc in range(8):
                sc.op("pe", lambda e, kc=kc, g2=g2, t=t: e.matmul(
                    self.ps_hd[:, t:t + 1], lhsT=self.GT[g2][:, kc // 2, :, kc % 2], rhs=xnT[:, kc // 2, t:t + 1, kc % 2],
                    start=(kc == 0), stop=(kc == 7)), reads=["pr_GT%d" % g2, kxnT], writes=["pp_hd"])
            sc.op("act", lambda e, t=t: e.activation(out=self.hg[:, t:t + 1], in_=self.ps_hd[:, t:t + 1], func=AF.Gelu),
                  reads=["pp_hd"], writes=["pr_hg%d" % (t % 4)])
            sc.op("dve", lambda e, t=t: e.tensor_scalar(out=self.actD[t % 3][:], in0=self.wsel[:, 127 - t:255 - t],
                                                        scalar1=self.hg[:, t:t + 1], scalar2=gT[:, t:t + 1],
                                                        op0=ALU.mult, op1=ALU.mult),
                  reads=["pr_hg%d" % (t % 4), kgT, "pr_wsel"], writes=["pr_actD%d" % (t % 3)])

        def e3(t):
            b = bufs[t]
            po = self.ps_o[:].rearrange("p a b -> p (a b)")
            for hf in range(2):
                sc.op("pe", lambda e, b=b, t=t, hf=hf, po=po: e.matmul(
                    po[:, hf * 512:(hf + 1) * 512], lhsT=self.actD[t % 3][:], rhs=self.G[b][:, D + hf * 512:D + (hf + 1) * 512],
                    start=(t == 0), stop=(t == 127)), reads=["pr_G%d" % b, "pr_actD%d" % (t % 3)], writes=["pp_o"])

        e1(0)
        for t in range(-1, 129):
            if 0 <= t + 1 < 128 and t + 1 > 0:
                e1(t + 1)
            if 0 <= t < 128:
                e2(t)
            if 0 <= t - 1 < 128:
                e3(t - 1)
            for _ in range(per):
                if pending:
                    pending.pop(0)()
        while pending:
            pending.pop(0)()
        po = self.ps_o[:].rearrange("p a b -> p (a b)")
        for hf in range(2):
            sc.op("dve", lambda e, hf=hf, po=po: e.tensor_tensor(
                out=self.osb[:, hf * 512:(hf + 1) * 512], in0=po[:, hf * 512:(hf + 1) * 512],
                in1=h_t[:, hf * 512:(hf + 1) * 512], op=ALU.add), reads=["pp_o", hkey], writes=["pr_osb"])
        sc.dma("sp", lambda e: e.dma_start(out=out_ap, in_=self.osb[:]), reads=["pr_osb"], writes=[outkey])


FMCH = [0, 1, 2, 3, 4, 5, 6, 7, 16, 17, 18, 19, 20, 21, 22, 24]
INW = 3352
DELTAS = [-512, -384, -256, -128, 0, 128, 256, 384]


def host_consts():
    bf = ml_dtypes.bfloat16
    c = {}
    c["c_identf"] = np.eye(128, dtype=np.float32)
    c["c_pc"] = peer_consts()
    wsel = np.zeros((128, 256), np.float32)
    wsel[:, 127] = 1.0
    c["c_wsel"] = wsel
    s = np.arange(128)[:, None]
    t = np.arange(128)[None, :]
    same = (s // 32) == (t // 32)
    c["c_ublk"] = (same & (s <= t)).astype(np.float32)
    c["c_wrev"] = (same & (s > t)).astype(np.float32)
    c["c_rowm"] = (np.arange(128)[:, None] // 32 == np.arange(4)[None, :]).astype(np.float32)
    c["c_colm"] = np.broadcast_to((np.arange(128)[None, None, :] // 32 == np.arange(4)[None, :, None]), (128, 4, 128)).astype(np.float32).copy()
    inv = (500000.0 ** (-np.arange(0, 16, 2, dtype=np.float32) / 16)).astype(np.float32)
    misc = np.zeros((128, 8), np.float32)
    misc[0:16, 0] = np.concatenate([inv, inv])
    misc[:, 1] = np.tile(np.concatenate([inv, inv]), 8)
    c["c_misc"] = misc
    rm = np.zeros((64, 64), np.float32)
    for d in range(8):
        rm[d + 8, d] = -1.0
        rm[d, d + 8] = 1.0
    c["c_rm"] = rm
    ss = np.arange(128)[:, None]
    tt = np.arange(512)[None, :]
    dm = np.zeros((128, 8, 512), np.float32)
    for i, dl in enumerate(DELTAS):
        ok = (dl + ss <= tt) & (tt - ss - dl < 512)
        dm[:, i, :] = np.where(ok, 0.0, NEG)
    c["c_dmask"] = dm
    cm = np.zeros((128, 4, 512), np.float32)
    cc = np.arange(128)[:, None]
    for qb in range(4):
        ok = (16 * cc + 31) <= (qb * 512 + tt)
        cm[:, qb, :] = np.where(ok, 0.0, NEG)
    c["c_cmask"] = cm
    em = np.zeros((32, 2048), np.float32)
    em[np.arange(2048) // 64, np.arange(2048)] = 1.0
    c["c_emat"] = em
    c0 = np.arange(127)[:, None] * 16
    j0 = np.arange(32)[None, :] * 64
    ov = np.clip(np.minimum(c0 + 32, j0 + 64) - np.maximum(c0, j0), 0, None) / 32.0
    va = np.zeros((128, 33), np.float32)
    va[:, 0] = 1.0
    va[:127, 1:] = ov
    c["c_vaug"] = va
    tok = (np.arange(16)[None, :, None] * 128 + np.arange(128)[:, None, None])
    cur = tok // 64
    j = np.arange(32)[None, None, :]
    forced = (j == 0) | (j == cur) | (j == cur - 1)
    valid = (j <= cur) & ~forced
    c["c_valid"] = valid.astype(np.float32)
    c["c_addc"] = np.where(forced, 1e30, np.where(valid, 0.0, -1e30)).astype(np.float32)
    return c


CONST_SHAPES = {"c_identf": [128, 128], "c_pc": [128, 64], "c_wsel": [128, 256], "c_ublk": [128, 128], "c_wrev": [128, 128],
                "c_misc": [128, 8], "c_rowm": [128, 4], "c_colm": [128, 4, 128], "c_rm": [64, 64], "c_dmask": [128, 8, 512], "c_cmask": [128, 4, 512],
                "c_emat": [32, 2048], "c_vaug": [128, 33], "c_valid": [128, 16, 32], "c_addc": [128, 16, 32]}


def barrier(sc):
    for e in ("sp", "pool", "act"):
        k = sc.dcnt[e]
        for slot in range(min(k, sc.ndma)):
            n = (k - 1 - slot) // sc.ndma + 1
            for eng in ENGS:
                sc._wait(eng, (("dma", e, slot), 16 * n))
    for eng in ENGS:
        for e2 in ("pe", "dve", "act", "pool"):
            if sc.cnt[e2] > 0 and not (eng == "pe" and e2 == "pe"):
                sc._wait(eng, ((e2,), sc.cnt[e2]))
    sc.st = {}


def rms_rstd(sc, src_ap, srckey, junk, ss, rstd, epsc, n, pfx):
    sc.op("act", lambda e: e.activation(out=junk, in_=src_ap, func=AF.Square, accum_out=ss),
          reads=[srckey], writes=[pfx + "junk", pfx + "ss"])
    sc.op("act", lambda e: e.activation(out=rstd, in_=ss, func=AF.Sqrt, scale=1.0 / n, bias=epsc),
          reads=[pfx + "ss"], writes=[pfx + "rstd"])
    sc.op("dve", lambda e: e.reciprocal(out=rstd, in_=rstd), reads=[pfx + "rstd"], writes=[pfx + "rstd"])


TM_RANGES = [(512, 1024), (1024, 1536), (1536, 2048), (2944, 3072), (3200, 3352)]


def stage_proj(nc, sc, NT, x, norm1_w, w_in, P_tm, P_fm, ident_b, epsc, uv=None):
    with contextlib.ExitStack() as st:
        T = lambda name, shape, dt: st.enter_context(nc.sbuf_tensor(name, shape, dt))
        P = lambda name, shape, dt: st.enter_context(nc.psum_tensor(name, shape, dt))
        win = T("a_win", [128, 8, INW], BF16)
        n1w = T("a_n1w", [128, 8], F32)
        xt = [T("a_xt%d" % i, [128, D], F32) for i in range(2)]
        junk = T("a_junk", [128, D], BF16)
        ss = T("a_ss", [128, 1], F32)
        rstd = T("a_rstd", [128, 1], F32)
        xn = T("a_xn", [128, D], BF16)
        xnT = T("a_xnT", [128, 8, 128], BF16)
        ptm = [T("a_ptm%d" % i, [128, INW], F32) for i in range(2)]
        pfm = [T("a_pfm%d" % i, [128, 16, 128], F32) for i in range(2)]
        ps_t = P("ap_t", [128, 8, 128], BF16)
        ps_m = [P("ap_m%d" % i, [128, 512], F32) for i in range(2)]
        ps_f = [P("ap_f%d" % i, [128, 4, 128], F32) for i in range(2)]
        for kc in range(8):
            sc.dma("pool", lambda e, kc=kc: e.dma_start(out=win[:, kc, :], in_=w_in[kc * 128:(kc + 1) * 128, :]),
                   writes=["a_win"])
        uv_ops = []
        if uv is not None:
            u_tab, v_tab, UV = uv
            tmp = [T("uv_tmp%d" % i, [128, 8, D], BF16) for i in range(3)]
            UVv = UV.rearrange("(p r) d -> p r d", p=128)
            k = 0
            for half, tab in enumerate((u_tab, v_tab)):
                tv = tab.rearrange("(p r) d -> p r d", p=128)
                for ci in range(16):
                    bb = k % 3
                    k += 1
                    uv_ops.append(lambda bb=bb, tv=tv, ci=ci: sc.dma(
                        "pool", lambda e: e.dma_start(out=tmp[bb][:], in_=tv[:, ci * 8:(ci + 1) * 8, :]), writes=["uv_tmp%d" % bb]))
                    uv_ops.append(lambda bb=bb, ci=ci, half=half: sc.dma(
                        "sp", lambda e: e.dma_start(out=UVv[:, ci * 8:(ci + 1) * 8, half * D:(half + 1) * D], in_=tmp[bb][:]),
                        reads=["uv_tmp%d" % bb], writes=["UV"]))
        sc.dma("sp", lambda e: e.dma_start(out=n1w[:], in_=norm1_w.rearrange("o (kc p) -> p (o kc)", p=128),
                                           allow_slow_non_contiguous=True), writes=["a_n1w"])
        P_fm_v = P_fm.rearrange("(c p) t -> p c t", p=128)
        ev = 0
        for i in range(NT):
            b = i % 2
            xk = "a_xt%d" % b
            sc.dma("sp", lambda e, b=b, i=i: e.dma_start(out=xt[b][:], in_=x[i * 128:(i + 1) * 128, :]), writes=[xk])
            rms_rstd(sc, xt[b][:], xk, junk[:], ss[:], rstd[:], epsc[:], D, "a_")
            sc.op("dve", lambda e, b=b: e.tensor_scalar(out=xn[:], in0=xt[b][:], scalar1=rstd[:, 0:1], scalar2=None,
                                                        op0=ALU.mult), reads=[xk, "a_rstd"], writes=["a_xn"])
            for kc in range(8):
                sc.op("pe", lambda e, kc=kc: e.transpose(out=ps_t[:, kc, :], in_=xn[:, kc * 128:(kc + 1) * 128],
                                                         identity=ident_b[:]), reads=["a_xn", "ident_b"], writes=["ap_t"])
            sc.op("dve", lambda e: e.tensor_tensor(out=xnT[:], in0=ps_t[:],
                                                   in1=n1w[:].unsqueeze(2).broadcast_to([128, 8, 128]), op=ALU.mult),
                  reads=["ap_t", "a_n1w"], writes=["a_xnT"])
            if uv_ops:
                uv_ops.pop(0)()
            for cg, (c0, c1) in enumerate(TM_RANGES):
                pm = ps_m[cg % 2]
                pk = "ap_m%d" % (cg % 2)
                for kc in range(8):
                    sc.op("pe", lambda e, kc=kc, c0=c0, c1=c1, pm=pm: e.matmul(
                        pm[:, 0:c1 - c0], lhsT=xnT[:, kc, :], rhs=win[:, kc, c0:c1], start=(kc == 0), stop=(kc == 7)),
                        reads=["a_xnT", "a_win"], writes=[pk])
                if cg % 2 == 0:
                    sc.op("act", lambda e, b=b, c0=c0, c1=c1, pm=pm: e.copy(out=ptm[b][:, c0:c1], in_=pm[:, 0:c1 - c0]),
                          reads=[pk], writes=["a_ptm%d" % b])
                else:
                    sc.op("dve", lambda e, b=b, c0=c0, c1=c1, pm=pm: e.tensor_copy(out=ptm[b][:, c0:c1], in_=pm[:, 0:c1 - c0]),
                          reads=[pk], writes=["a_ptm%d" % b])
            sc.dma("sp", lambda e, b=b, i=i: e.dma_start(out=P_tm[i * 128:(i + 1) * 128, 512:2048], in_=ptm[b][:, 512:2048]),
                   reads=["a_ptm%d" % b], writes=["P_tm"])
            sc.dma("sp", lambda e, b=b, i=i: e.dma_start(out=P_tm[i * 128:(i + 1) * 128, 2944:INW], in_=ptm[b][:, 2944:INW]),
                   reads=["a_ptm%d" % b], writes=["P_tm"])
            for fg in range(4):
                pf = ps_f[fg % 2]
                pk = "ap_f%d" % (fg % 2)
                for f4 in range(4):
                    ch = FMCH[fg * 4 + f4]
                    for kc in range(8):
                        sc.op("pe", lambda e, kc=kc, ch=ch, f4=f4, pf=pf: e.matmul(
                            pf[:, f4, :], lhsT=win[:, kc, ch * 128:(ch + 1) * 128], rhs=xnT[:, kc, :],
                            start=(kc == 0), stop=(kc == 7)), reads=["a_xnT", "a_win"], writes=[pk])
                if fg % 2 == 0:
                    sc.op("act", lambda e, b=b, fg=fg, pf=pf: e.copy(out=pfm[b][:, fg * 4:(fg + 1) * 4, :], in_=pf[:]),
                          reads=[pk], writes=["a_pfm%d" % b])
                else:
                    sc.op("dve", lambda e, b=b, fg=fg, pf=pf: e.tensor_copy(out=pfm[b][:, fg * 4:(fg + 1) * 4, :], in_=pf[:]),
                          reads=[pk], writes=["a_pfm%d" % b])
            sc.dma("sp", lambda e, b=b, i=i: e.dma_start(out=P_fm_v[:, :, i * 128:(i + 1) * 128], in_=pfm[b][:]),
                   reads=["a_pfm%d" % b], writes=["P_fm"])
        while uv_ops:
            uv_ops.pop(0)()
        barrier(sc)


def stage_hgrn(nc, sc, NT, P_tm, P_fm, Y_tm, lb_logits, hg_onw, ublk, wrev, epsc, c_rowm, c_colm):
    with contextlib.ExitStack() as st:
        T = lambda name, shape, dt: st.enter_context(nc.sbuf_tensor(name, shape, dt))
        P = lambda name, shape, dt: st.enter_context(nc.psum_tensor(name, shape, dt))
        lbb = T("h_lbb", [128, 2, 512], F32)
        omlb = T("h_omlb", [128, 512], F32)
        lbf = T("h_lbf", [128, 2, 4], F32)
        omlf = T("h_omlf", [128, 4], F32)
        hgw = T("h_hgw", [128, 128], F32)
        ftm = [T("h_ftm%d" % i, [128, 1536], F32) for i in range(2)]
        ffm = [T("h_ffm%d" % i, [128, 8, 128], F32) for i in range(2)]
        sig = T("h_sig", [128, 512], F32)
        logf = T("h_logf", [128, 512], F32)
        ktm = T("h_ktm", [128, 512], F32)
        erev = T("h_erev", [128, 512], F32)
        khat = T("h_khat", [128, 512], F32)
        khat4 = T("h_khat4", [128, 4, 512], BF16)
        rowm = T("h_rowm", [128, 4], F32)
        colm = T("h_colm", [128, 4, 128], F32)
        vbf = T("h_vbf", [128, 512], BF16)
        sgg = T("h_sgg", [128, 512], F32)
        fT = T("h_fT", [128, 4, 128], F32)
        ET = T("h_ET", [128, 4, 128], F32)
        EiT = T("h_EiT", [128, 4, 128], F32)
        qtT = T("h_qtT", [128, 4, 128], BF16)
        ktT = T("h_ktT", [128, 4, 128], BF16)
        qtT4 = [T("h_qtT4%d" % h, [128, 4, 128], BF16) for h in range(4)]
        scm = T("h_scm", [128, 4, 128], BF16)
        state = [T("h_st%d" % h, [128, 128], F32) for h in range(4)]
        stbf = [T("h_sb%d" % h, [128, 128], BF16) for h in range(4)]
        junk = T("h_junk", [128, 128], F32)
        ss = T("h_ss", [128, 4], F32)
        rstd = T("h_rstd", [128, 4], F32)
        yt = [T("h_yt%d" % i, [128, 512], F32) for i in range(2)]
        onec = T("h_onec", [128, 1], F32)
        sc.op("dve", lambda e: e.memset(onec[:], 1.0), writes=["h_onec"])
        ps_m = [P("hp_m%d" % i, [128, 512], F32) for i in range(4)]
        ps_o = [P("hp_o%d" % i, [128, 512], F32) for i in range(4)]
        ps_rev = ps_m[0]
        ps_bT = ps_m[1][:].rearrange("p (h t) -> p h t", h=4)
        ps_sc = ps_m[2][:].rearrange("p (h t) -> p h t", h=4)
        sc.dma("sp", lambda e: e.dma_start(out=rowm[:], in_=c_rowm), writes=["h_rowm"])
        sc.dma("sp", lambda e: e.dma_start(out=colm[:], in_=c_colm), writes=["h_colm"])
        sc.dma("sp", lambda e: e.dma_start(out=lbb[:, 0, :], in_=lb_logits[0:1, :].partition_broadcast(128)), writes=["h_lbb"])
        sc.dma("sp", lambda e: e.dma_start(out=lbb[:, 1, :], in_=lb_logits[1:2, :].partition_broadcast(128)), writes=["h_lbb"])
        sc.dma("sp", lambda e: e.dma_start(out=lbf[:], in_=lb_logits.rearrange("r (h p) -> p r h", p=128),
                                           allow_slow_non_contiguous=True), writes=["h_lbf"])
        sc.dma("sp", lambda e: e.dma_start(out=hgw[:], in_=hg_onw[0:1, :].partition_broadcast(128)), writes=["h_hgw"])

        def sigmoid_to(ap_out, ap_in, keys_in, key_out):
            sc.op("act", lambda e: e.activation(out=ap_out, in_=ap_in, func=AF.Exp, scale=-1.0), reads=keys_in, writes=[key_out])
            sc.op("act", lambda e: e.activation(out=ap_out, in_=ap_out, func=AF.Ln, bias=onec[:]), reads=[key_out], writes=[key_out])
            sc.op("act", lambda e: e.activation(out=ap_out, in_=ap_out, func=AF.Exp, scale=-1.0), reads=[key_out], writes=[key_out])

        sc.op("dve", lambda e: e.tensor_tensor(out=lbb[:, 0, :], in0=lbb[:, 0, :], in1=lbb[:, 1, :], op=ALU.subtract),
              reads=["h_lbb"], writes=["h_lbb"])
        sigmoid_to(lbb[:, 0, :], lbb[:, 0, :], ["h_lbb"], "h_lbb")
        sc.op("dve", lambda e: e.tensor_scalar(out=omlb[:], in0=lbb[:, 0, :], scalar1=-1.0, scalar2=1.0, op0=ALU.mult,
                                               op1=ALU.add), reads=["h_lbb"], writes=["h_omlb"])
        sc.op("dve", lambda e: e.tensor_tensor(out=lbf[:, 0, :], in0=lbf[:, 0, :], in1=lbf[:, 1, :], op=ALU.subtract),
              reads=["h_lbf"], writes=["h_lbf"])
        sigmoid_to(lbf[:, 0, :], lbf[:, 0, :], ["h_lbf"], "h_lbf")
        sc.op("dve", lambda e: e.tensor_scalar(out=omlf[:], in0=lbf[:, 0, :], scalar1=-1.0, scalar2=1.0, op0=ALU.mult,
                                               op1=ALU.add), reads=["h_lbf"], writes=["h_omlf"])
        P_fm_v = P_fm.rearrange("(c p) t -> p c t", p=128)
        for i in range(NT):
            b = i % 2
            fk, mk = "h_ftm%d" % b, "h_ffm%d" % b
            F_, M_ = ftm[b], ffm[b]
            sc.dma("sp", lambda e, b=b, i=i: e.dma_start(out=ftm[b][:], in_=P_tm[i * 128:(i + 1) * 128, 512:2048]),
                   reads=["P_tm"], writes=[fk])
            sc.dma("sp", lambda e, b=b, i=i: e.dma_start(out=ffm[b][:], in_=P_fm_v[:, 0:8, i * 128:(i + 1) * 128]),
                   reads=["P_fm"], writes=[mk])
            if i % 16 == 0:
                for h in range(4):
                    sc.op("dve", lambda e, h=h: e.memset(state[h][:], 0.0), writes=["h_st%d" % h])
                    sc.op("dve", lambda e, h=h: e.memset(stbf[h][:], 0.0), writes=["h_sb%d" % h])
            opsA, opsB = [], []
            OP_A = lambda *a, **k: opsA.append(lambda: sc.op(*a, **k))
            OP_B = lambda *a, **k: opsB.append(lambda: sc.op(*a, **k))
            SIG_A = lambda *a: opsA.append(lambda: sigmoid_to(*a))
            SIG_B = lambda *a: opsB.append(lambda: sigmoid_to(*a))
            SIG_A(sig[:], F_[:, 0:512], [fk], "h_sig")
            OP_A("dve", lambda e: e.tensor_tensor(out=sig[:], in0=sig[:], in1=omlb[:], op=ALU.mult),
                  reads=["h_sig", "h_omlb"], writes=["h_sig"])
            OP_A("dve", lambda e: e.tensor_tensor(out=sig[:], in0=sig[:], in1=lbb[:, 0, :], op=ALU.add),
                  reads=["h_sig", "h_lbb"], writes=["h_sig"])
            OP_A("act", lambda e: e.activation(out=logf[:], in_=sig[:], func=AF.Ln), reads=["h_sig"], writes=["h_logf"])
            OP_A("dve", lambda e: e.tensor_scalar(out=ktm[:], in0=sig[:], scalar1=-1.0, scalar2=1.0, op0=ALU.mult,
                                                   op1=ALU.add), reads=["h_sig"], writes=["h_ktm"])
            OP_A("pe", lambda e: e.matmul(ps_rev[:], lhsT=wrev[:], rhs=logf[:], start=True, stop=True),
                  reads=["wrev", "h_logf"], writes=["hp_m0"])
            OP_A("act", lambda e: e.activation(out=erev[:], in_=ps_rev[:], func=AF.Exp), reads=["hp_m0"], writes=["h_erev"])
            OP_A("dve", lambda e: e.tensor_tensor(out=khat[:], in0=ktm[:], in1=erev[:], op=ALU.mult),
                  reads=["h_ktm", "h_erev"], writes=["h_khat"])
            OP_A("dve", lambda e: e.tensor_tensor(out=khat4[:], in0=khat[:].unsqueeze(1).broadcast_to([128, 4, 512]),
                                                   in1=rowm[:].unsqueeze(2).broadcast_to([128, 4, 512]), op=ALU.mult),
                  reads=["h_khat", "h_rowm"], writes=["h_khat4"])
            OP_A("act", lambda e, F_=F_: e.copy(out=vbf[:], in_=F_[:, 512:1024]), reads=[fk], writes=["h_vbf"])
            SIG_A(sgg[:], F_[:, 1024:1536], [fk], "h_sgg")
            OP_A("dve", lambda e, F_=F_: e.tensor_tensor(out=sgg[:], in0=sgg[:], in1=F_[:, 1024:1536], op=ALU.mult),
                  reads=["h_sgg", fk], writes=["h_sgg"])
            SIG_B(fT[:], M_[:, 4:8, :], [mk], "h_fT")
            for h in range(4):
                OP_B("dve", lambda e, h=h: e.tensor_scalar(out=fT[:, h, :], in0=fT[:, h, :], scalar1=omlf[:, h:h + 1],
                                                            scalar2=lbf[:, 0, h:h + 1], op0=ALU.mult, op1=ALU.add),
                      reads=["h_fT", "h_omlf", "h_lbf"], writes=["h_fT"])
            OP_B("dve", lambda e: e.tensor_scalar(out=fT[:], in0=fT[:], scalar1=-1.0, scalar2=1.0, op0=ALU.mult,
                                                   op1=ALU.add), reads=["h_fT"], writes=["h_fT"])
            for h in range(4):
                OP_B("pe", lambda e, h=h: e.matmul(ps_bT[:, h, :], lhsT=logf[:, h * 128:(h + 1) * 128], rhs=ublk[:],
                                                    start=True, stop=True), reads=["h_logf", "ublk"], writes=["hp_m1"])
            OP_B("act", lambda e: e.activation(out=ET[:], in_=ps_bT, func=AF.Exp), reads=["hp_m1"], writes=["h_ET"])
            OP_B("act", lambda e: e.activation(out=EiT[:], in_=ps_bT, func=AF.Exp, scale=-1.0),
                  reads=["hp_m1"], writes=["h_EiT"])
            OP_B("dve", lambda e, M_=M_: e.tensor_tensor(out=qtT[:], in0=M_[:, 0:4, :], in1=ET[:], op=ALU.mult),
                  reads=[mk, "h_ET"], writes=["h_qtT"])
            OP_B("dve", lambda e: e.tensor_tensor(out=ktT[:], in0=fT[:], in1=EiT[:], op=ALU.mult),
                  reads=["h_fT", "h_EiT"], writes=["h_ktT"])
            for h in range(4):
                OP_B("dve", lambda e, h=h: e.tensor_tensor(out=qtT4[h][:], in0=qtT[:, h, :].unsqueeze(1).broadcast_to([128, 4, 128]),
                                                            in1=colm[:], op=ALU.mult),
                      reads=["h_qtT", "h_colm"], writes=["h_qtT4%d" % h])
                OP_B("pe", lambda e, h=h: e.matmul(ps_sc[:, h, :], lhsT=ktT[:, h, :], rhs=qtT[:, h, :], start=True, stop=True),
                      reads=["h_ktT", "h_qtT"], writes=["hp_m2"])
            OP_B("dve", lambda e: e.tensor_tensor(out=scm[:], in0=ps_sc, in1=ublk[:].unsqueeze(1).broadcast_to([128, 4, 128]),
                                                   op=ALU.mult), reads=["hp_m2", "ublk"], writes=["h_scm"])
            for k_ in range(max(len(opsA), len(opsB))):
                if k_ < len(opsA):
                    opsA[k_]()
                if k_ < len(opsB):
                    opsB[k_]()
            for h in range(4):
                sc.op("pe", lambda e, h=h: e.matmul(ps_o[h][:, 0:128], lhsT=scm[:, h, :], rhs=vbf[:, h * 128:(h + 1) * 128],
                                                    start=True, stop=False), reads=["h_scm", "h_vbf"], writes=["hp_o%d" % h])
            for c4 in range(4):
                for h in range(4):
                    hs = slice(h * 128, (h + 1) * 128)
                    sc.op("pe", lambda e, h=h, c4=c4: e.matmul(ps_o[h][:, 0:128], lhsT=qtT4[h][:, c4, :], rhs=stbf[h][:],
                                                               start=False, stop=(c4 == 3)),
                          reads=["h_qtT4%d" % h, "h_sb%d" % h], writes=["hp_o%d" % h])
                    sc.op("pe", lambda e, c4=c4, hs=hs, h=h: e.matmul(ps_m[h][:, 0:128], lhsT=khat4[:, c4, hs], rhs=vbf[:, hs],
                                                                      start=True, stop=True),
                          reads=["h_khat4", "h_vbf"], writes=["hp_m%d" % h])
                    sc.op("dve", lambda e, h=h, c4=c4: e.scalar_tensor_tensor(
                        out=state[h][:], in0=state[h][:], scalar=ET[:, h, c4 * 32 + 31:c4 * 32 + 32], in1=ps_m[h][:, 0:128],
                        op0=ALU.mult, op1=ALU.add), reads=["h_st%d" % h, "h_ET", "hp_m%d" % h], writes=["h_st%d" % h])
                    sc.op("act", lambda e, h=h: e.copy(out=stbf[h][:], in_=state[h][:]),
                          reads=["h_st%d" % h], writes=["h_sb%d" % h])
            for h in range(4):
                sc.op("act", lambda e, h=h: e.activation(out=junk[:], in_=ps_o[h][:, 0:128], func=AF.Square, accum_out=ss[:, h:h + 1]),
                      reads=["hp_o%d" % h], writes=["h_junk", "h_ss"])
            sc.op("act", lambda e: e.activation(out=rstd[:], in_=ss[:], func=AF.Ln, scale=1.0 / 128, bias=epsc[:]),
                  reads=["h_ss"], writes=["h_rstd"])
            sc.op("act", lambda e: e.activation(out=rstd[:], in_=rstd[:], func=AF.Exp, scale=-0.5),
                  reads=["h_rstd"], writes=["h_rstd"])
            for h in range(4):
                hs = slice(h * 128, (h + 1) * 128)
                sc.op("dve", lambda e, h=h, b=b, hs=hs: e.scalar_tensor_tensor(
                    out=yt[b][:, hs], in0=ps_o[h][:, 0:128], scalar=rstd[:, h:h + 1], in1=hgw[:], op0=ALU.mult, op1=ALU.mult),
                    reads=["hp_o%d" % h, "h_rstd", "h_hgw"], writes=["h_yt%d" % b])
            sc.op("dve", lambda e, b=b: e.tensor_tensor(out=yt[b][:], in0=yt[b][:], in1=sgg[:], op=ALU.mult),
                  reads=["h_yt%d" % b, "h_sgg"], writes=["h_yt%d" % b])
            sc.dma("sp", lambda e, b=b, i=i: e.dma_start(out=Y_tm[i * 128:(i + 1) * 128, 0:512], in_=yt[b][:]),
                   reads=["h_yt%d" % b], writes=["Y_tm"])
        barrier(sc)


NSA_WARM = 1


def stage_nsa(nc, sc, NSEQ, P_tm, P_fm, Y_tm, positions, qnw, knw, pe_k, pe_v, w1k, w2k, w1v, w2v, onw, cst,
              ident_b, epsc, dbg=None):
    TINY = 1e-30
    with contextlib.ExitStack() as st:
        T = lambda name, shape, dt: st.enter_context(nc.sbuf_tensor(name, shape, dt))
        P = lambda name, shape, dt: st.enter_context(nc.psum_tensor(name, shape, dt))
        dmask = T("n_dmask", [128, 8, 512], BF16)
        cmask = T("n_cmask", [128, 4, 512], BF16)
        rm = T("n_rm", [64, 64], BF16)
        ones64 = T("n_ones", [64, 64], BF16)
        misc = T("n_misc", [128, 8], F32)
        valid = T("n_valid", [128, 16, 32], F32)
        addc = T("n_addc", [128, 16, 32], F32)
        wcol = T("n_wcol", [64, 4], F32)
        onwb = T("n_onwb", [128, 64], F32)
        w1 = [T("n_w1%d" % i, [64, 32, 256], BF16) for i in range(2)]
        w2 = [T("n_w2%d" % i, [128, 2, 64], BF16) for i in range(2)]
        peT = T("n_peT", [64, 2, 32], BF16)
        peTf = T("n_peTf", [64, 2, 32], F32)
        cbias = T("n_cbias", [128, 2, 2], F32)
        for name, t_, src in (("n_dmask", dmask, cst["c_dmask"]), ("n_cmask", cmask, cst["c_cmask"]),
                              ("n_rm", rm, cst["c_rm"])):
            sc.dma("pool", lambda e, t_=t_, src=src: e.dma_start(out=t_[:], in_=src), writes=[name])
        sc.dma("sp", lambda e: e.dma_start(out=misc[:], in_=cst["c_misc"]), writes=["n_misc"])
        sc.dma("sp", lambda e: e.dma_start(out=valid[:], in_=cst["c_valid"]), writes=["n_valid"])
        sc.dma("sp", lambda e: e.dma_start(out=addc[:], in_=cst["c_addc"]), writes=["n_addc"])
        sc.op("dve", lambda e: e.memset(ones64[:], 1.0), writes=["n_ones"])
        sc.dma("pool", lambda e: e.dma_start(out=ksT[64:96, :], in_=cst["c_emat"]), writes=["n_ksT"])
        sc.op("dve", lambda e: e.tensor_scalar(out=dm01[:], in0=dmask[:], scalar1=-1.0, scalar2=None, op0=ALU.is_ge),
              reads=["n_dmask"], writes=["n_dm01"])
        sc.op("dve", lambda e: e.memset(selb[:], 0.0), writes=["n_selb"])
        sc.op("dve", lambda e: e.memset(kwT[64:96, :], 0.0), writes=["n_kwT"])
        sc.op("dve", lambda e: e.memset(kcmpT[64:96, :], 0.0), writes=["n_kcmpT"])
        for r_ in range(4):
            sc.op("dve", lambda e, r_=r_: e.memset(qT[r_][64:96, :], 0.0), writes=["n_qT%d" % r_])
        sc.dma("sp", lambda e: e.dma_start(out=wcol[:, 0:1], in_=qnw.rearrange("o d -> d o"), allow_slow_non_contiguous=True),
               writes=["n_wcol"])
        sc.dma("sp", lambda e: e.dma_start(out=wcol[:, 1:4], in_=knw.rearrange("b d -> d b"), allow_slow_non_contiguous=True),
               writes=["n_wcol"])
        sc.op("dve", lambda e: e.tensor_scalar(out=wcol[:, 0:1], in0=wcol[:, 0:1], scalar1=0.125, scalar2=None, op0=ALU.mult),
              reads=["n_wcol"], writes=["n_wcol"])
        sc.dma("sp", lambda e: e.dma_start(out=onwb[:], in_=onw[0:1, :].partition_broadcast(128)), writes=["n_onwb"])
        for i, (w1_, w2_) in enumerate(((w1k, w2k), (w1v, w2v))):
            sc.dma("pool", lambda e, i=i, w1_=w1_: e.dma_start(out=w1[i][:], in_=w1_.rearrange("(l d) n -> d l n", d=64)),
                   writes=["n_w1%d" % i])
            sc.dma("pool", lambda e, i=i, w2_=w2_: e.dma_start(out=w2[i][:], in_=w2_.rearrange("(a p) n -> p a n", p=128)),
                   writes=["n_w2%d" % i])
        sc.dma("sp", lambda e: e.dma_start(out=peTf[:, 0, :], in_=pe_k.rearrange("l d -> d l"), allow_slow_non_contiguous=True),
               writes=["n_peTf"])
        sc.dma("sp", lambda e: e.dma_start(out=peTf[:, 1, :], in_=pe_v.rearrange("l d -> d l"), allow_slow_non_contiguous=True),
               writes=["n_peTf"])
        sc.op("dve", lambda e: e.tensor_copy(out=peT[:], in_=peTf[:]), reads=["n_peTf"], writes=["n_peT"])
        ki = T("n_ki", [128, 256], I32)
        ang = T("n_ang", [128, 256], F32)
        rr = T("n_rr", [128, 256], F32)
        tab2 = T("n_tab2", [128, 256], F32)
        kf = T("n_kf", [128, 256], F32)
        CT = T("n_CT", [64, 2048], F32)
        ST = T("n_ST", [64, 2048], F32)
        srcs = [T("n_src%d" % i, [64, 2048], F32) for i in range(2)]
        SRCc = [0]

        def nxt_src():
            i_ = SRCc[0] % 2
            SRCc[0] += 1
            return srcs[i_], "n_src%d" % i_
        sq = [T("n_sq%d" % i, [64, 512], BF16) for i in range(2)]
        rinv = [T("n_rinv%d" % i, [64, 512], F32) for i in range(2)]
        xnm = [T("n_xnm%d" % i, [64, 512], F32) for i in range(2)]
        xb = [T("n_xb%d" % i, [64, 512], BF16) for i in range(2)]
        t1 = [T("n_t1%d" % i, [64, 512], F32) for i in range(2)]
        t2 = [T("n_t2%d" % i, [64, 512], F32) for i in range(2)]
        qT = [T("n_qT%d" % r, [96, 2048], BF16) for r in range(4)]
        ksT = T("n_ksT", [96, 2048], BF16)
        dm01 = dmask
        kwT = T("n_kwT", [96, 2048], BF16)
        kcbs = [T("n_kcb%d" % i, [64, 2048], BF16) for i in range(2)]
        kcmpT = T("n_kcmpT", [96, 128], BF16)
        hx = T("n_hx", [128, 127], F32)
        hx2 = T("n_hx2", [128, 127], F32)
        hT = T("n_hT", [128, 2, 127], BF16)
        vcaug = T("n_vcaug", [128, 97], BF16)
        vaugf = T("n_vaugf", [128, 33], F32)
        vsa = T("n_vsa", [128, 16, 65], BF16)
        vwa = T("n_vwa", [128, 16, 65], BF16)
        vld = T("n_vld", [128, 16, 64], F32)
        gsig = T("n_gsig", [128, 16, 24], F32)
        PT = [T("n_PT%d" % i, [128, 512], BF16) for i in range(5)]
        oacc = T("n_oacc", [128, 16, 4, 64], F32)
        imp = T("n_imp", [128, 16, 32], F32)
        score = T("n_score", [128, 16, 32], F32)
        swk = T("n_swk", [128, 4, 32], F32)
        m8a = T("n_m8a", [128, 4, 8], F32)
        m8b = T("n_m8b", [128, 4, 8], F32)
        sel = T("n_sel", [128, 4, 32], F32)
        selb = T("n_selb", [128, 4, 96], BF16)
        nselT = T("n_nselT", [96, 2048], BF16)
        r1 = T("n_r1", [128, 1], F32)
        coef = T("n_coef", [128, 1], F32)
        ssq = T("n_ssq", [128, 64, 64], F32)
        ss64 = T("n_ss64", [128, 64], F32)
        ps_s = [P("np_s%d" % i, [128, 512], F32) for i in range(5)]
        ps_a = [P("np_a%d" % i, [128, 512], F32) for i in range(2)]
        ps_warm = P("np_w", [128, 512], F32)
        ps_x = [ps_s[2], ps_s[3]]
        sc.dma("sp", lambda e: e.dma_start(out=vaugf[:], in_=cst["c_vaug"]), writes=["n_vaugf"])
        sc.op("dve", lambda e: e.tensor_copy(out=vcaug[:, 64:97], in_=vaugf[:]), reads=["n_vaugf"], writes=["n_vcaug"])
        sc.op("dve", lambda e: e.memset(vcaug[:, 0:64], 0.0), writes=["n_vcaug"])
        sc.op("dve", lambda e: e.memset(vsa[:], 1.0), writes=["n_vsa"])
        sc.op("dve", lambda e: e.memset(vwa[:], 1.0), writes=["n_vwa"])
        sc.op("dve", lambda e: e.memset(CT[:], 1.0), writes=["n_CT"])
        sc.op("dve", lambda e: e.memset(ST[:], 0.0), writes=["n_ST"])
        for kv in range(2):
            for hh in range(2):
                for l in range(32):
                    sc.op("pe", lambda e, kv=kv, hh=hh, l=l: e.matmul(
                        ps_x[0][:, 0:1], lhsT=w1[kv][:, l, hh * 128:(hh + 1) * 128], rhs=peT[:, kv, l:l + 1],
                        start=(l == 0), stop=(l == 31)), reads=["n_w1%d" % kv, "n_peT"], writes=["np_s2"])
                sc.op("act", lambda e, kv=kv, hh=hh: e.copy(out=cbias[:, kv, hh:hh + 1], in_=ps_x[0][:, 0:1]),
                      reads=["np_s2"], writes=["n_cbias"])

        def norm_rope(calls):
            S = list(enumerate(calls))
            pa = [(ps_s[2], "np_s2", ps_s[3], "np_s3"), (ps_s[0], "np_s0", ps_s[1], "np_s1")]
            for k, (srcap, srckey, n, wc, Cap, Sap, outap, outkey) in S:
                sc.op("act", lambda e, k=k, n=n, srcap=srcap: e.activation(out=sq[k][:, 0:n], in_=srcap, func=AF.Square),
                      reads=[srckey], writes=["n_sq%d" % k])
            for k, (srcap, srckey, n, wc, Cap, Sap, outap, outkey) in S:
                sc.op("pe", lambda e, k=k, n=n: e.matmul(pa[k][0][0:64, 0:n], lhsT=ones64[:], rhs=sq[k][:, 0:n], start=True, stop=True),
                      reads=["n_sq%d" % k, "n_ones"], writes=[pa[k][1]])
            for k, (srcap, srckey, n, wc, Cap, Sap, outap, outkey) in S:
                sc.op("act", lambda e, k=k, n=n: e.activation(out=rinv[k][:, 0:n], in_=pa[k][0][0:64, 0:n], func=AF.Ln,
                                                              scale=1.0 / 64, bias=epsc[0:64, :]),
                      reads=[pa[k][1]], writes=["n_rinv%d" % k])
            for k, (srcap, srckey, n, wc, Cap, Sap, outap, outkey) in S:
                sc.op("act", lambda e, k=k, n=n: e.activation(out=rinv[k][:, 0:n], in_=rinv[k][:, 0:n], func=AF.Exp, scale=-0.5),
                      reads=["n_rinv%d" % k], writes=["n_rinv%d" % k])
            for k, (srcap, srckey, n, wc, Cap, Sap, outap, outkey) in S:
                sc.op("dve", lambda e, k=k, n=n, srcap=srcap, wc=wc: e.scalar_tensor_tensor(
                    out=xnm[k][:, 0:n], in0=srcap, scalar=wcol[:, wc:wc + 1], in1=rinv[k][:, 0:n], op0=ALU.mult, op1=ALU.mult),
                    reads=[srckey, "n_wcol", "n_rinv%d" % k], writes=["n_xnm%d" % k])
            for k, (srcap, srckey, n, wc, Cap, Sap, outap, outkey) in S:
                sc.op("act", lambda e, k=k, n=n: e.copy(out=xb[k][:, 0:n], in_=xnm[k][:, 0:n]),
                      reads=["n_xnm%d" % k], writes=["n_xb%d" % k])
            for k, (srcap, srckey, n, wc, Cap, Sap, outap, outkey) in S:
                sc.op("pe", lambda e, k=k, n=n: e.matmul(pa[k][2][0:64, 0:n], lhsT=rm[:], rhs=xb[k][:, 0:n], start=True, stop=True),
                      reads=["n_xb%d" % k, "n_rm"], writes=[pa[k][3]])
            for k, (srcap, srckey, n, wc, Cap, Sap, outap, outkey) in S:
                sc.op("dve", lambda e, k=k, n=n, Cap=Cap: e.tensor_tensor(out=t1[k][:, 0:n], in0=xnm[k][:, 0:n], in1=Cap, op=ALU.mult),
                      reads=["n_xnm%d" % k, "n_CT"], writes=["n_t1%d" % k])
            for k, (srcap, srckey, n, wc, Cap, Sap, outap, outkey) in S:
                sc.op("dve", lambda e, k=k, n=n, Sap=Sap: e.tensor_tensor(out=t2[k][:, 0:n], in0=pa[k][2][0:64, 0:n], in1=Sap, op=ALU.mult),
                      reads=[pa[k][3], "n_ST"], writes=["n_t2%d" % k])
            for k, (srcap, srckey, n, wc, Cap, Sap, outap, outkey) in S:
                sc.op("dve", lambda e, k=k, n=n, outap=outap: e.tensor_tensor(out=outap, in0=t1[k][:, 0:n], in1=t2[k][:, 0:n], op=ALU.add),
                      reads=["n_t1%d" % k, "n_t2%d" % k], writes=[outkey])

        P_fm_v = P_fm
        PTc = [0]
        ACc = [0, 0]
        accs = [T("n_accs%d" % i, [128, 4, 97], F32) for i in range(3)]
        r4 = T("n_r4", [128, 4], F32)
        c4t = T("n_c4", [128, 4], F32)
        o4 = T("n_o4", [128, 4, 64], F32)
        i4 = T("n_i4", [128, 4, 32], F32)

        def attn(kT, kkey, vfn, vkey, W, kts_fn, mask_fn, nk_fn, epi, krows=64, post_fn=None):
            SK = 4
            for r in range(4):
                units = [(qb, idx, kt, len(kts_fn(qb))) for qb in range(4) for idx, kt in enumerate(kts_fn(qb))]
                slot = {}

                def emit_qk(u):
                    qb, idx, kt, nkt = units[u]
                    nk = nk_fn(kt)
                    pb = PTc[0] % 5
                    PTc[0] += 1
                    slot[u] = pb
                    ps, psk = ps_s[pb], "np_s%d" % pb
                    mms = [(kT[0:krows, kt * 128:kt * 128 + nk], qT[r][0:krows, qb * 512:(qb + 1) * 512], [kkey, "n_qT%d" % r])]
                    mms += mask_fn(kt, qb)
                    for mi, (l_, r_, rk) in enumerate(mms):
                        sc.op("pe", lambda e, l_=l_, r_=r_, ps=ps, nk=nk, mi=mi, last=(mi == len(mms) - 1): e.matmul(
                            ps[0:nk, :], lhsT=l_, rhs=r_, start=(mi == 0), stop=last), reads=rk, writes=[psk])
                    sc.op("act", lambda e, ps=ps, nk=nk, pb=pb: e.activation(out=PT[pb][0:nk, :], in_=ps[0:nk, :], func=AF.Exp),
                          reads=[psk], writes=["n_PT%d" % pb])
                    for _w in range(NSA_WARM):
                        sc.op("pe", lambda e: e.matmul(ps_warm[:], lhsT=ident_b[:], rhs=dm01[:, 4, :], start=True, stop=True),
                              reads=["ident_b", "n_dm01"], writes=["np_w"])
                    m01 = post_fn(kt, qb) if post_fn else None
                    if m01 is not None:
                        sc.op("pool", lambda e, pb=pb, m01=m01: e.tensor_tensor(out=PT[pb][:], in0=PT[pb][:], in1=m01, op=ALU.mult),
                              reads=["n_PT%d" % pb, "n_dm01"], writes=["n_PT%d" % pb])

                def emit_pv(u):
                    qb, idx, kt, nkt = units[u]
                    nk = nk_fn(kt)
                    pb = slot[u]
                    ab = ACc[0] % 2
                    for ts in range(4):
                        sc.op("pe", lambda e, pb=pb, nk=nk, ts=ts, kt=kt, idx=idx, ab=ab, last=(idx == nkt - 1): e.matmul(
                            ps_a[ab][:, ts * W:(ts + 1) * W], lhsT=PT[pb][0:nk, ts * 128:(ts + 1) * 128], rhs=vfn(kt)[0:nk, :],
                            start=(idx == 0 and ts == 0), stop=last, skip_group_check=True),
                            reads=["n_PT%d" % pb, vkey], writes=["np_a%d" % ab])
                    if idx == nkt - 1:
                        ACc[0] += 1
                        sb3 = ACc[1] % 3
                        ACc[1] += 1
                        sc.op("act", lambda e, ab=ab, sb3=sb3: e.copy(
                            out=accs[sb3][:, :, 0:W], in_=ps_a[ab][:, 0:4 * W].rearrange("p (a b) -> p a b", b=W)),
                            reads=["np_a%d" % ab], writes=["n_accs%d" % sb3])
                        epi(r, qb, accs[sb3], "n_accs%d" % sb3)

                for j in range(len(units) + SK):
                    if j < len(units):
                        emit_qk(j)
                    if j - SK >= 0:
                        emit_pv(j - SK)

        def mk_epi(g, branch, first):
            def epi(r, qb, acc, acck):
                tsl = slice(qb * 4, qb * 4 + 4)
                gc = (g * 4 + r) * 3 + branch
                sc.op("dve", lambda e: e.tensor_scalar(out=r4[:], in0=acc[:, :, 64], scalar1=TINY, scalar2=None, op0=ALU.max),
                      reads=[acck], writes=["n_r4"])
                sc.op("dve", lambda e: e.reciprocal(out=r4[:], in_=r4[:]), reads=["n_r4"], writes=["n_r4"])
                sc.op("dve", lambda e: e.tensor_tensor(out=c4t[:], in0=r4[:], in1=gsig[:, tsl, gc], op=ALU.mult),
                      reads=["n_r4", "n_gsig"], writes=["n_c4"])
                if first:
                    sc.op("dve", lambda e: e.tensor_tensor(out=oacc[:, tsl, r, :], in0=acc[:, :, 0:64],
                                                           in1=c4t[:].unsqueeze(2).broadcast_to([128, 4, 64]), op=ALU.mult),
                          reads=[acck, "n_c4"], writes=["n_oacc"])
                    if r == 0:
                        sc.op("dve", lambda e: e.tensor_tensor(out=imp[:, tsl, :], in0=acc[:, :, 65:97],
                                                               in1=r4[:].unsqueeze(2).broadcast_to([128, 4, 32]), op=ALU.mult),
                              reads=[acck, "n_r4"], writes=["n_imp"])
                    else:
                        sc.op("dve", lambda e: e.tensor_tensor(out=i4[:], in0=acc[:, :, 65:97],
                                                               in1=r4[:].unsqueeze(2).broadcast_to([128, 4, 32]), op=ALU.mult),
                              reads=[acck, "n_r4"], writes=["n_i4"])
                        sc.op("dve", lambda e: e.tensor_tensor(out=imp[:, tsl, :], in0=imp[:, tsl, :], in1=i4[:], op=ALU.add),
                              reads=["n_i4", "n_imp"], writes=["n_imp"])
                else:
                    sc.op("dve", lambda e: e.tensor_tensor(out=o4[:], in0=acc[:, :, 0:64],
                                                           in1=c4t[:].unsqueeze(2).broadcast_to([128, 4, 64]), op=ALU.mult),
                          reads=[acck, "n_c4"], writes=["n_o4"])
                    sc.op("dve", lambda e: e.tensor_tensor(out=oacc[:, tsl, r, :], in0=oacc[:, tsl, r, :], in1=o4[:], op=ALU.add),
                          reads=["n_o4", "n_oacc"], writes=["n_oacc"])
            return epi

        TWO_PI = 6.283185307179586
        for s in range(NSEQ):
            sb = s * 2048
            for c in range(8):
                sc.dma("sp", lambda e, s=s, c=c: e.dma_start(
                    out=ki[c * 16:(c + 1) * 16, :], in_=positions[s:s + 1, c * 256:(c + 1) * 256].partition_broadcast(16)),
                    writes=["n_ki"])
            sc.op("dve", lambda e: e.tensor_copy(out=ang[:], in_=ki[:]), reads=["n_ki"], writes=["n_ang"])
            sc.op("dve", lambda e: e.tensor_scalar(out=ang[:], in0=ang[:], scalar1=misc[:, 1:2], scalar2=None, op0=ALU.mult),
                  reads=["n_ang", "n_misc"], writes=["n_ang"])
            for tab, tkey, shift in ((ST, "n_ST", 0.0), (CT, "n_CT", 1.5707963267948966)):
                sc.op("dve", lambda e, shift=shift: e.tensor_scalar(out=rr[:], in0=ang[:], scalar1=shift, scalar2=None, op0=ALU.add),
                      reads=["n_ang"], writes=["n_rr"])
                sc.op("dve", lambda e: e.tensor_scalar(out=kf[:], in0=rr[:], scalar1=1.0 / TWO_PI, scalar2=None, op0=ALU.mult),
                      reads=["n_rr"], writes=["n_kf"])
                sc.op("dve", lambda e: e.tensor_copy(out=ki[:], in_=kf[:]), reads=["n_kf"], writes=["n_ki"])
                sc.op("dve", lambda e: e.tensor_copy(out=kf[:], in_=ki[:]), reads=["n_ki"], writes=["n_kf"])
                sc.op("dve", lambda e: e.scalar_tensor_tensor(out=rr[:], in0=kf[:], scalar=-TWO_PI, in1=rr[:], op0=ALU.mult,
                                                              op1=ALU.add), reads=["n_kf", "n_rr"], writes=["n_rr"])
                sc.op("dve", lambda e: e.tensor_scalar(out=kf[:], in0=rr[:], scalar1=3.141592653589793, scalar2=-TWO_PI,
                                                       op0=ALU.is_gt, op1=ALU.mult), reads=["n_rr"], writes=["n_kf"])
                sc.op("dve", lambda e: e.tensor_tensor(out=rr[:], in0=rr[:], in1=kf[:], op=ALU.add), reads=["n_rr", "n_kf"], writes=["n_rr"])
                sc.op("dve", lambda e: e.tensor_scalar(out=kf[:], in0=rr[:], scalar1=-3.141592653589793, scalar2=TWO_PI,
                                                       op0=ALU.is_lt, op1=ALU.mult), reads=["n_rr"], writes=["n_kf"])
                sc.op("dve", lambda e: e.tensor_tensor(out=rr[:], in0=rr[:], in1=kf[:], op=ALU.add), reads=["n_rr", "n_kf"], writes=["n_rr"])
                sc.op("dve", lambda e: e.tensor_scalar(out=rr[:], in0=rr[:], scalar1=3.1415925, scalar2=-3.1415925,
                                                       op0=ALU.min, op1=ALU.max), reads=["n_rr"], writes=["n_rr"])
                sc.op("act", lambda e: e.activation(out=tab2[:], in_=rr[:], func=AF.Sin), reads=["n_rr"], writes=["n_tab2"])
                for c in range(8):
                    sc.dma("sp", lambda e, tab=tab, c=c: e.dma_start(out=tab[0:16, c * 256:(c + 1) * 256],
                                                                     in_=tab2[c * 16:(c + 1) * 16, :]),
                           reads=["n_tab2"], writes=[tkey])
            sc.dma("sp", lambda e, sb=sb: e.dma_start(out=gsig[:], in_=P_tm[sb:sb + 2048, 3328:3352].rearrange("(tt p) c -> p tt c", p=128)),
                   reads=["P_tm"], writes=["n_gsig"])
            sc.op("act", lambda e: e.activation(out=gsig[:], in_=gsig[:], func=AF.Sigmoid), reads=["n_gsig"], writes=["n_gsig"])
            for g in range(2):
                def load_fm(fmidx, half, dstap, dkey, eng="sp", sb=sb):
                    row0 = fmidx * 128 + half * 64
                    sc.dma(eng, lambda e, row0=row0, dstap=dstap, sb=sb: e.dma_start(out=dstap, in_=P_fm[row0:row0 + 64, sb:sb + 2048]),
                           reads=["P_fm"], writes=[dkey])
                for r in range(4):
                    hh = g * 4 + r
                    src, skey = nxt_src()
                    load_fm(8 + hh // 2, hh % 2, src[:], skey)
                    for q2 in range(2):
                        css = [slice(qb * 512, (qb + 1) * 512) for qb in (2 * q2, 2 * q2 + 1)]
                        norm_rope([(src[:, cs], skey, 512, 0, CT[:, cs], ST[:, cs], qT[r][0:64, cs], "n_qT%d" % r) for cs in css])
                for fmidx, wc, dst, dkey in ((14, 2, ksT, "n_ksT"), (15, 3, kwT, "n_kwT")):
                    src, skey = nxt_src()
                    load_fm(fmidx, g, src[:], skey)
                    for q2 in range(2):
                        css = [slice(qb * 512, (qb + 1) * 512) for qb in (2 * q2, 2 * q2 + 1)]
                        norm_rope([(src[:, cs], skey, 512, wc, CT[:, cs], ST[:, cs], dst[0:64, cs], dkey) for cs in css])
                for col0, va, vk in ((2944, vsa, "n_vsa"), (3200, vwa, "n_vwa")):
                    sc.dma("sp", lambda e, col0=col0, g=g, sb=sb: e.dma_start(
                        out=vld[:], in_=P_tm[sb:sb + 2048, col0 + g * 64:col0 + g * 64 + 64].rearrange("(tt p) c -> p tt c", p=128)),
                        reads=["P_tm"], writes=["n_vld"])
                    sc.op("act", lambda e, va=va: e.copy(out=va[:, :, 0:64], in_=vld[:]), reads=["n_vld"], writes=[vk])
                for kv in range(2):
                    kcb, kck = kcbs[kv], "n_kcb%d" % kv
                    load_fm(12 + kv, g, kcb[:], kck, eng="pool")
                    for hh in range(2):
                        for l in range(32):
                            sc.op("pe", lambda e, kv=kv, hh=hh, l=l, kcb=kcb: e.matmul(
                                ps_x[0][:, 0:127], lhsT=w1[kv][:, l, hh * 128:(hh + 1) * 128], rhs=kcb[:, l:l + 2017:16],
                                start=(l == 0), stop=(l == 31)), reads=["n_w1%d" % kv, kck], writes=["np_s2"])
                        sc.op("act", lambda e, kv=kv, hh=hh: e.activation(out=hx[:], in_=ps_x[0][:, 0:127], func=AF.Identity,
                                                                          bias=cbias[:, kv, hh:hh + 1]),
                              reads=["np_s2", "n_cbias"], writes=["n_hx"])
                        sc.op("act", lambda e: e.activation(out=hx2[:], in_=hx[:], func=AF.Square), reads=["n_hx"], writes=["n_hx2"])
                        sc.op("dve", lambda e: e.tensor_scalar(out=hx2[:], in0=hx2[:], scalar1=0.044715, scalar2=1.0, op0=ALU.mult,
                                                               op1=ALU.add), reads=["n_hx2"], writes=["n_hx2"])
                        sc.op("dve", lambda e: e.tensor_tensor(out=hx2[:], in0=hx2[:], in1=hx[:], op=ALU.mult),
                              reads=["n_hx2", "n_hx"], writes=["n_hx2"])
                        sc.op("act", lambda e: e.activation(out=hx2[:], in_=hx2[:], func=AF.Sigmoid, scale=1.5957691216057308),
                              reads=["n_hx2"], writes=["n_hx2"])
                        sc.op("dve", lambda e, hh=hh: e.tensor_tensor(out=hT[:, hh, :], in0=hx[:], in1=hx2[:], op=ALU.mult),
                              reads=["n_hx", "n_hx2"], writes=["n_hT"])
                    if kv == 0:
                        for hh in range(2):
                            sc.op("pe", lambda e, hh=hh: e.matmul(ps_x[1][0:64, 0:127], lhsT=w2[0][:, hh, :], rhs=hT[:, hh, :],
                                                                  start=(hh == 0), stop=(hh == 1)),
                                  reads=["n_w20", "n_hT"], writes=["np_s3"])
                        src, skey = nxt_src()
                        sc.op("act", lambda e, src=src: e.copy(out=src[:, 0:127], in_=ps_x[1][0:64, 0:127]), reads=["np_s3"], writes=[skey])
                        norm_rope([(src[:, 0:127], skey, 127, 1, CT[:, 31:2048:16], ST[:, 31:2048:16], kcmpT[0:64, 0:127], "n_kcmpT")])
                    else:
                        for hh in range(2):
                            sc.op("pe", lambda e, hh=hh: e.matmul(ps_x[1][0:127, 0:64], lhsT=hT[:, hh, :], rhs=w2[1][:, hh, :],
                                                                  start=(hh == 0), stop=(hh == 1)),
                                  reads=["n_w21", "n_hT"], writes=["np_s3"])
                        sc.op("act", lambda e: e.copy(out=vcaug[0:127, 0:64], in_=ps_x[1][0:127, 0:64]), reads=["np_s3"], writes=["n_vcaug"])
                attn(kcmpT, "n_kcmpT", lambda kt: vcaug, "n_vcaug", 97, lambda qb: [0],
                     lambda kt, qb: [(ident_b[0:127, 0:127], cmask[0:127, qb, :], ["ident_b", "n_cmask"])],
                     lambda kt: 127, mk_epi(g, 0, True), krows=96)
                sc.op("dve", lambda e: e.tensor_tensor(out=score[:], in0=imp[:], in1=valid[:], op=ALU.mult),
                      reads=["n_imp", "n_valid"], writes=["n_score"])
                sc.op("dve", lambda e: e.tensor_tensor(out=score[:], in0=score[:], in1=addc[:], op=ALU.add),
                      reads=["n_score", "n_addc"], writes=["n_score"])
                for g4 in range(4):
                    tts = [g4 * 4 + k for k in range(4)]
                    for k, tt in enumerate(tts):
                        sc.op("dve", lambda e, tt=tt, k=k: e.max(out=m8a[:, k, :], in_=score[:, tt, :]),
                              reads=["n_score"], writes=["n_m8a%d" % k])
                    for k, tt in enumerate(tts):
                        sc.op("dve", lambda e, tt=tt, k=k: e.match_replace(out=swk[:, k, :], in_to_replace=m8a[:, k, :],
                                                                           in_values=score[:, tt, :], imm_value=-3e38),
                              reads=["n_score", "n_m8a%d" % k], writes=["n_swk%d" % k])
                    for k, tt in enumerate(tts):
                        sc.op("dve", lambda e, k=k: e.max(out=m8b[:, k, :], in_=swk[:, k, :]),
                              reads=["n_swk%d" % k], writes=["n_m8b%d" % k])
                    for k, tt in enumerate(tts):
                        sc.op("dve", lambda e, tt=tt, k=k: e.tensor_scalar(out=sel[:, k, :], in0=score[:, tt, :], scalar1=m8b[:, k, 7:8],
                                                                           scalar2=None, op0=ALU.is_ge),
                              reads=["n_score", "n_m8b%d" % k], writes=["n_sel%d" % k])
                    sc.op("dve", lambda e: e.tensor_scalar(out=selb[:, :, 64:96], in0=sel[:], scalar1=-NEG, scalar2=NEG, op0=ALU.mult,
                                                           op1=ALU.add), reads=["n_sel%d" % k for k in range(4)], writes=["n_selb"])
                    pst = ps_x[0][:, 0:256].bitcast(BF16).rearrange("p (a b) -> p a b", b=128)
                    for k in range(4):
                        sc.op("pe", lambda e, pst=pst, k=k: e.transpose(out=pst[0:96, k, :], in_=selb[:, k, :], identity=ident_b[:]),
                              reads=["n_selb", "ident_b"], writes=["np_s2"])
                    sc.op("act", lambda e, g4=g4, pst=pst: e.copy(
                        out=nselT[64:96, g4 * 512:(g4 + 1) * 512].rearrange("p (a b) -> p a b", b=128), in_=pst[64:96, :, :]),
                        reads=["np_s2"], writes=["n_nselT"])
                for r in range(4):
                    sc.op("act" if r % 2 == 0 else "dve",
                          (lambda e, r=r: e.copy(out=qT[r][64:96, :], in_=nselT[64:96, :])) if r % 2 == 0 else
                          (lambda e, r=r: e.tensor_copy(out=qT[r][64:96, :], in_=nselT[64:96, :])),
                          reads=["n_nselT"], writes=["n_qT%d" % r])
                attn(ksT, "n_ksT", lambda kt: vsa[:, kt, :], "n_vsa", 65, lambda qb: list(range(0, 4 * qb + 4)),
                     lambda kt, qb: [], lambda kt: 128, mk_epi(g, 1, False), krows=96,
                     post_fn=lambda kt, qb: dm01[:, 4 + kt - 4 * qb, :] if kt >= 4 * qb else None)
                attn(kwT, "n_kwT", lambda kt: vwa[:, kt, :], "n_vwa", 65, lambda qb: list(range(max(0, 4 * qb - 4), 4 * qb + 4)),
                     lambda kt, qb: [], lambda kt: 128, mk_epi(g, 2, False), krows=96,
                     post_fn=lambda kt, qb: dm01[:, 4 + kt - 4 * qb, :])
                o3 = oacc[:].rearrange("p t r d -> p (t r) d")
                sc.op("dve", lambda e: e.tensor_tensor(out=ssq[:], in0=o3, in1=o3, op=ALU.mult), reads=["n_oacc"], writes=["n_ssq"])
                sc.op("dve", lambda e: e.tensor_reduce(out=ss64[:], in_=ssq[:], axis=AX.X, op=ALU.add), reads=["n_ssq"], writes=["n_ss64"])
                sc.op("act", lambda e: e.activation(out=ss64[:], in_=ss64[:], func=AF.Sqrt, scale=1.0 / 64, bias=epsc[:]),
                      reads=["n_ss64"], writes=["n_ss64"])
                sc.op("dve", lambda e: e.reciprocal(out=ss64[:], in_=ss64[:]), reads=["n_ss64"], writes=["n_ss64"])
                sc.op("dve", lambda e: e.tensor_tensor(out=ssq[:], in0=o3, in1=ss64[:].unsqueeze(2).broadcast_to([128, 64, 64]),
                                                       op=ALU.mult), reads=["n_oacc", "n_ss64"], writes=["n_ssq"])
                sc.op("dve", lambda e: e.tensor_tensor(out=ssq[:], in0=ssq[:], in1=onwb[:].unsqueeze(1).broadcast_to([128, 64, 64]),
                                                       op=ALU.mult), reads=["n_ssq", "n_onwb"], writes=["n_ssq"])
                for tt in range(16):
                    sc.dma("sp", lambda e, tt=tt, g=g, sb=sb: e.dma_start(
                        out=Y_tm[sb + tt * 128:sb + (tt + 1) * 128, 512 + g * 256:512 + (g + 1) * 256],
                        in_=ssq[:, tt * 4:(tt + 1) * 4, :].rearrange("p r d -> p (r d)")), reads=["n_ssq"], writes=["Y_tm"])
        barrier(sc)


def stage_uv(nc, sc, u_tab, v_tab, UV):
    with contextlib.ExitStack() as st:
        tmp = [st.enter_context(nc.sbuf_tensor("uv_tmp%d" % i, [128, 8, D], BF16)) for i in range(3)]
        UVv = UV.rearrange("(p r) d -> p r d", p=128)
        k = 0
        for half, tab in enumerate((u_tab, v_tab)):
            tv = tab.rearrange("(p r) d -> p r d", p=128)
            for ci in range(16):
                b = k % 3
                k += 1
                sc.dma("pool", lambda e, b=b, tv=tv, ci=ci: e.dma_start(out=tmp[b][:], in_=tv[:, ci * 8:(ci + 1) * 8, :]),
                       writes=["uv_tmp%d" % b])
                sc.dma("sp", lambda e, b=b, ci=ci, half=half: e.dma_start(
                    out=UVv[:, ci * 8:(ci + 1) * 8, half * D:(half + 1) * D], in_=tmp[b][:]),
                    reads=["uv_tmp%d" % b], writes=["UV"])
        barrier(sc)


def stage_out(nc, sc, stack, NT, x, Y_tm, w_out, out, peer):
    T = lambda name, shape, dt: stack.enter_context(nc.sbuf_tensor(name, shape, dt))
    wout = T("o_wout", [128, 8, D], BF16)
    yt = [T("o_yt%d" % i, [128, D], F32) for i in range(2)]
    xt = [T("o_xt%d" % i, [128, D], F32) for i in range(2)]
    ht = [T("o_ht%d" % i, [128, D], F32) for i in range(3)]
    ybf = T("o_ybf", [128, D], BF16)
    yT = T("o_yT", [128, 8, 128], BF16)
    sc.dma("pool", lambda e: e.dma_start(out=wout[:], in_=w_out.rearrange("(kc p) n -> p kc n", p=128)), writes=["o_wout"])

    def tile_ops(i):
        b = i % 2
        hb = i % 3
        ops = []
        add = lambda *a, **k: ops.append(lambda: sc.op(*a, **k))
        ops.append(lambda: sc.dma("sp", lambda e: e.dma_start(out=yt[b][:], in_=Y_tm[i * 128:(i + 1) * 128, :]),
                                  reads=["Y_tm"], writes=["o_yt%d" % b]))
        ops.append(lambda: sc.dma("sp", lambda e: e.dma_start(out=xt[b][:], in_=x[i * 128:(i + 1) * 128, :]),
                                  writes=["o_xt%d" % b]))
        add("act", lambda e: e.copy(out=ybf[:], in_=yt[b][:]), reads=["o_yt%d" % b], writes=["o_ybf"])
        for kc in range(8):
            add("pe", lambda e, kc=kc: e.transpose(out=peer.ps_t_b[:, kc, :], in_=ybf[:, kc * 128:(kc + 1) * 128],
                                                   identity=peer.ident_b[:]), reads=["o_ybf", "ident_b"], writes=["pp_t"])
        add("act", lambda e: e.copy(out=yT[:], in_=peer.ps_t_b), reads=["pp_t"], writes=["o_yT"])
        for hf in range(2):
            ps = peer.ps_a if hf == 0 else peer.ps_b
            pk = "pp_a" if hf == 0 else "pp_b"
            for kc in range(8):
                add("pe", lambda e, hf=hf, kc=kc, ps=ps: e.matmul(ps[:].rearrange("p a b -> p (a b)"), lhsT=yT[:, kc, :],
                                                                  rhs=wout[:, kc, hf * 512:(hf + 1) * 512],
                                                                  start=(kc == 0), stop=(kc == 7)),
                    reads=["o_yT", "o_wout"], writes=[pk])
            add("dve", lambda e, hf=hf, ps=ps: e.tensor_tensor(
                out=ht[hb][:, hf * 512:(hf + 1) * 512], in0=ps[:].rearrange("p a b -> p (a b)"),
                in1=xt[b][:, hf * 512:(hf + 1) * 512], op=ALU.add), reads=[pk, "o_xt%d" % b], writes=["o_ht%d" % hb])
        return ops + peer.pre_ops(i, ht[hb][:], "o_ht%d" % hb)

    for f in tile_ops(0):
        f()
    for i in range(NT):
        pending = tile_ops(i + 1) if i + 1 < NT else []
        peer.loop(i, ht[i % 3][:], "o_ht%d" % (i % 3), out[i * 128:(i + 1) * 128, :], "out", pending)


WNAMES = ["norm1_w", "w_in", "hg_lb_logits", "hg_out_norm_w", "nsa_q_norm_w", "nsa_k_norm_w", "cmp_pe_k", "cmp_pe_v",
          "cmp_w1_k", "cmp_w2_k", "cmp_w1_v", "cmp_w2_v", "nsa_out_norm_w", "w_out", "norm2_w", "peer_w_q",
          "peer_sub_keys", "peer_u", "peer_v"]
WSHAPES = {"norm1_w": [1, D], "w_in": [D, INW], "hg_lb_logits": [2, 512], "hg_out_norm_w": [1, 128],
           "nsa_q_norm_w": [1, 64], "nsa_k_norm_w": [3, 64], "cmp_pe_k": [32, 64], "cmp_pe_v": [32, 64],
           "cmp_w1_k": [2048, 256], "cmp_w2_k": [256, 64], "cmp_w1_v": [2048, 256], "cmp_w2_v": [256, 64],
           "nsa_out_norm_w": [1, 64], "w_out": [D, D], "norm2_w": [1, D], "peer_w_q": [D, 2048],
           "peer_sub_keys": [2, 128, 128], "peer_u": [16384, D], "peer_v": [16384, D]}


def build_program(NSEQ):
    NT = NSEQ * 16
    nc = bass.Bass("TRN2", target_bir_lowering=False)
    din = lambda name, shape, dt=F32: nc.dram_tensor(name, shape, dt, kind="ExternalInput").ap()
    x = din("x", [NT * 128, D])
    positions = din("positions", [NSEQ, 2048], I32)
    w = {k: din(k, v) for k, v in WSHAPES.items()}
    cst = {k: din(k, v) for k, v in CONST_SHAPES.items()}
    out = nc.dram_tensor("out", [NT * 128, D], F32, kind="ExternalOutput").ap()
    P_tm = nc.dram_tensor("P_tm", [NT * 128, INW], F32, kind="Internal").ap()
    P_fm = nc.dram_tensor("P_fm", [16 * 128, NT * 128], F32, kind="Internal").ap()
    Y_tm = nc.dram_tensor("Y_tm", [NT * 128, D], F32, kind="Internal").ap()
    UV = nc.dram_tensor("UV", [16384, 2 * D], BF16, kind="Internal").ap()
    with contextlib.ExitStack() as stack:
        sc = Sched(nc, stack)
        T = lambda name, shape, dt: stack.enter_context(nc.sbuf_tensor(name, shape, dt))
        ident_f = T("ident_f", [128, 128], F32)
        ident_b = T("ident_b", [128, 128], BF16)
        ublk = T("ublk", [128, 128], F32)
        wrev = T("wrev", [128, 128], F32)
        epsc = T("epsc", [128, 1], F32)
        pc = T("pc", [128, 64], F32)
        sc.dma("sp", lambda e: e.dma_start(out=ident_f[:], in_=cst["c_identf"]), writes=["ident_f"])
        sc.dma("pool", lambda e: e.dma_start(out=ident_b[:], in_=cst["c_identf"]), writes=["ident_b"])
        sc.dma("sp", lambda e: e.dma_start(out=ublk[:], in_=cst["c_ublk"]), writes=["ublk"])
        sc.dma("sp", lambda e: e.dma_start(out=wrev[:], in_=cst["c_wrev"]), writes=["wrev"])
        sc.dma("sp", lambda e: e.dma_start(out=pc[:], in_=cst["c_pc"]), writes=["pc"])
        sc.op("dve", lambda e: e.memset(epsc[:], EPS), writes=["epsc"])
        barrier(sc)
        stage_proj(nc, sc, NT, x, w["norm1_w"], w["w_in"], P_tm, P_fm, ident_b, epsc,
                   uv=(w["peer_u"], w["peer_v"], UV))
        stage_hgrn(nc, sc, NT, P_tm, P_fm, Y_tm, w["hg_lb_logits"], w["hg_out_norm_w"], ublk, wrev, epsc,
                   cst["c_rowm"], cst["c_colm"])
        stage_nsa(nc, sc, NSEQ, P_tm, P_fm, Y_tm, positions, w["nsa_q_norm_w"], w["nsa_k_norm_w"], w["cmp_pe_k"],
                  w["cmp_pe_v"], w["cmp_w1_k"], w["cmp_w2_k"], w["cmp_w1_v"], w["cmp_w2_v"], w["nsa_out_norm_w"],
                  cst, ident_b, epsc)
        with contextlib.ExitStack() as st2:
            peer = Peer(nc, sc, st2, w["norm2_w"], w["peer_w_q"], w["peer_sub_keys"], UV, ident_f, ident_b, pc, cst["c_wsel"])
            stage_out(nc, sc, st2, NT, x, Y_tm, w["w_out"], out, peer)
            sc.finish()
            with nc.Block() as block:
                sc.replay(block)
    return nc


def make_inputs(inputs, c0, c1):
    m = {"x": np.ascontiguousarray(inputs["x"][c0:c1]).reshape(-1, D).astype(np.float32, copy=False),
         "positions": np.ascontiguousarray(inputs["positions"][c0:c1]).astype(np.int32, copy=False)}
    for k in WNAMES:
        a = np.asarray(inputs[k])
        if k != "hg_lb_logits":
            a = a[0]
        m[k] = np.ascontiguousarray(a, dtype=np.float32).reshape(WSHAPES[k])
    return m


def kernel(**inputs):
    ncores = 8
    B = inputs["x"].shape[0]
    per = B // ncores
    nc = build_program(per)
    consts = host_consts()
    in_maps = []
    for c in range(ncores):
        m = make_inputs(inputs, c * per, (c + 1) * per)
        m.update(consts)
        in_maps.append(m)
    res = run_bass_kernel_spmd(nc, in_maps, core_ids=list(range(ncores)))
    outs = [np.asarray(r["out"]).reshape(per, S, D) for r in res.results]
    return np.concatenate(outs, axis=0).astype(np.float32, copy=False)
```
